# Optimizing a Trainium2 kernel written in Bass

```python
import math
import jax, jax.numpy as jnp
from jax import lax
import numpy as np

D_MODEL = 1024
BATCH = 2
SEQ = 8192
DEPTH = 1

HEAD_DIM = 64
N_DIFF_HEADS = D_MODEL // 256
DIFF_WIDTH = N_DIFF_HEADS * 2 * HEAD_DIM
N_NAT_HEADS = D_MODEL // 128
NAT_WIDTH = N_NAT_HEADS * HEAD_DIM
MIX_WIDTH = DIFF_WIDTH + NAT_WIDTH
GRID_W = 64
NAT_KH_MAX = 8
NAT_KW = 16
Q_BLOCK = 128
N_EXPERTS = 32
TOP_K = 4
D_FF_EXPERT = D_MODEL
SWIGLU_LIMIT = 7.0
SWIGLU_ALPHA = 1.702
MOE_BLOCK = 128
RMS_EPS = 1e-6

kernel_name = "hybrid_diffattn_natten_moe_encoder"


def rms_norm(x, g):
    xf = x.astype(jnp.float32)
    y = xf * lax.rsqrt(jnp.mean(xf * xf, axis=-1, keepdims=True) + RMS_EPS)
    return (y * g.astype(jnp.float32)).astype(x.dtype)


def alibi_slopes(n_heads):
    i = jnp.arange(1, n_heads + 1, dtype=jnp.float32)
    return jnp.exp2(-8.0 * i / n_heads)


def diff_attention(q, k, v, lam, lam_init, g_sub):
    B, L, H, _, Dh = q.shape
    nb = L // Q_BLOCK
    slopes = alibi_slopes(H)
    kpos = jnp.arange(L, dtype=jnp.float32)
    scale = Dh ** -0.5
    qb = q.reshape(B, nb, Q_BLOCK, H, 2, Dh).transpose(1, 0, 2, 3, 4, 5)

    def block(args):
        q_blk, i = args
        s = jnp.einsum('bqhmd,bkhmd->bhmqk', q_blk, k).astype(jnp.float32) * scale
        qpos = (i * Q_BLOCK + jnp.arange(Q_BLOCK)).astype(jnp.float32)
        dist = jnp.abs(qpos[:, None] - kpos[None, :])
        s = s - slopes[None, :, None, None, None] * dist[None, None, None]
        p = jax.nn.softmax(s, axis=-1)
        a = p[:, :, 0] - lam * p[:, :, 1]
        return jnp.einsum('bhqk,bkhe->bqhe', a.astype(v.dtype), v)

    o = lax.map(block, (qb, jnp.arange(nb)))
    o = o.transpose(1, 0, 2, 3, 4).reshape(B, L, H, 2 * Dh)
    o = rms_norm(o, g_sub) * (1.0 - lam_init)
    return o.reshape(B, L, H * 2 * Dh)


def neighbourhood_attention(q, k, v, rpb):
    B, L, H, Dh = q.shape
    rows = L // GRID_W
    kh = min(NAT_KH_MAX, rows)
    scale = Dh ** -0.5
    qg = q.reshape(B, rows, GRID_W, H, Dh)
    kg = k.reshape(B, rows, GRID_W, H, Dh)
    vg = v.reshape(B, rows, GRID_W, H, Dh)
    r = jnp.arange(rows)
    row_start = jnp.clip(r - kh // 2, 0, rows - kh)
    row_idx = row_start[:, None] + jnp.arange(kh)[None, :]
    k_rows = kg[:, row_idx]
    v_rows = vg[:, row_idx]
    c = jnp.arange(GRID_W)
    col_start = jnp.clip(c - NAT_KW // 2, 0, GRID_W - NAT_KW)
    col_in = (c[None, :] >= col_start[:, None]) & (c[None, :] < col_start[:, None] + NAT_KW)
    dr = row_idx - r[:, None] + (NAT_KH_MAX - 1)
    dc = jnp.clip(c[None, :] - c[:, None], -(NAT_KW - 1), NAT_KW - 1) + (NAT_KW - 1)
    bias = rpb.astype(jnp.float32)[:, dr[:, None, :, None], dc[None, :, None, :]]
    s = jnp.einsum('brqhd,brkwhd->bhrqkw', qg, k_rows).astype(jnp.float32) * scale + bias[None]
    s = jnp.where(col_in[:, None, :], s, -jnp.inf)
    p = jax.nn.softmax(s.reshape(B, H, rows, GRID_W, kh * GRID_W), axis=-1)
    p = p.reshape(B, H, rows, GRID_W, kh, GRID_W).astype(v.dtype)
    o = jnp.einsum('bhrqkw,brkwhd->brqhd', p, v_rows)
    return o.reshape(B, L, H * Dh)


def swiglu_clamped(hh):
    x_glu = jnp.minimum(hh[..., ::2], SWIGLU_LIMIT)
    x_lin = jnp.clip(hh[..., 1::2], -SWIGLU_LIMIT, SWIGLU_LIMIT)
    return x_glu * jax.nn.sigmoid(SWIGLU_ALPHA * x_glu) * (x_lin + 1.0)


def moe_ffn(h, w_router, b_router, w1, b1, w2, b2):
    B, L, D = h.shape
    T = B * L
    hf = h.reshape(T, D)
    logits = (hf @ w_router + b_router).astype(jnp.float32)
    top_val, top_idx = lax.top_k(logits, TOP_K)
    gates = jax.nn.softmax(top_val, axis=-1)
    n_assign = T * TOP_K
    flat_e = top_idx.reshape(-1).astype(jnp.int32)
    flat_tok = jnp.arange(n_assign, dtype=jnp.int32) // TOP_K
    order = jnp.argsort(flat_e, stable=True)
    sorted_e = flat_e[order]
    counts = jnp.bincount(flat_e, length=N_EXPERTS).astype(jnp.int32)
    padded = (counts + MOE_BLOCK - 1) // MOE_BLOCK * MOE_BLOCK
    pad_end = jnp.cumsum(padded)
    pad_start = pad_end - padded
    grp_start = jnp.cumsum(counts) - counts
    rank = jnp.arange(n_assign, dtype=jnp.int32) - grp_start[sorted_e]
    dest_sorted = (pad_start[sorted_e] + rank).astype(jnp.int32)
    cap = (n_assign + N_EXPERTS * (MOE_BLOCK - 1) + MOE_BLOCK - 1) // MOE_BLOCK * MOE_BLOCK
    n_blocks = cap // MOE_BLOCK
    tok_buf = jnp.zeros((cap,), jnp.int32).at[dest_sorted].set(flat_tok[order])
    blk_start = jnp.arange(n_blocks, dtype=jnp.int32) * MOE_BLOCK
    blk_expert = jnp.minimum(jnp.searchsorted(pad_end, blk_start, side='right'), N_EXPERTS - 1)
    x_buf = hf[tok_buf].reshape(n_blocks, MOE_BLOCK, D)

    def expert_block(args):
        xb, e = args
        hh = xb @ w1[e] + b1[e]
        return swiglu_clamped(hh) @ w2[e] + b2[e]

    y_buf = lax.map(expert_block, (x_buf, blk_expert)).reshape(cap, D)
    dest = jnp.zeros((n_assign,), jnp.int32).at[order].set(dest_sorted)
    y = y_buf[dest].reshape(T, TOP_K, D)
    out = jnp.einsum('tk,tkd->td', gates.astype(y.dtype), y)
    return out.reshape(B, L, D)


def setup_inputs(seed: int = 0) -> dict:
    key = jax.random.key(seed)
    ks = jax.random.split(key, 24)
    f32 = jnp.float32
    D, F = D_MODEL, D_FF_EXPERT
    n = lambda k, shape, s: jax.random.normal(k, shape, f32) * s
    return {
        "x": n(ks[0], (BATCH, SEQ, D), 1.0),
        "c": n(ks[1], (BATCH, D), 1.0),
        "w_ada": n(ks[2], (DEPTH, D, 6 * D), 0.5 * D ** -0.5),
        "b_ada": n(ks[3], (DEPTH, 6 * D), 0.02),
        "g_pre_mix": 1.0 + n(ks[4], (DEPTH, D), 0.02),
        "g_post_mix": 1.0 + n(ks[5], (DEPTH, D), 0.02),
        "w_in": n(ks[6], (DEPTH, D, 3 * MIX_WIDTH), D ** -0.5),
        "w_out": n(ks[7], (DEPTH, MIX_WIDTH, D), MIX_WIDTH ** -0.5),
        "lam_q1": n(ks[8], (DEPTH, HEAD_DIM), 0.1),
        "lam_k1": n(ks[9], (DEPTH, HEAD_DIM), 0.1),
        "lam_q2": n(ks[10], (DEPTH, HEAD_DIM), 0.1),
        "lam_k2": n(ks[11], (DEPTH, HEAD_DIM), 0.1),
        "g_subln": 1.0 + n(ks[12], (DEPTH, 2 * HEAD_DIM), 0.02),
        "nat_rpb": n(ks[13], (DEPTH, N_NAT_HEADS, 2 * NAT_KH_MAX - 1, 2 * NAT_KW - 1), 0.02),
        "g_pre_ffn": 1.0 + n(ks[14], (DEPTH, D), 0.02),
        "g_post_ffn": 1.0 + n(ks[15], (DEPTH, D), 0.02),
        "w_router": n(ks[16], (DEPTH, D, N_EXPERTS), D ** -0.5),
        "b_router": n(ks[17], (DEPTH, N_EXPERTS), 0.01),
        "w1": n(ks[18], (DEPTH, N_EXPERTS, D, 2 * F), D ** -0.5),
        "b1": n(ks[19], (DEPTH, N_EXPERTS, 2 * F), 0.01),
        "w2": n(ks[20], (DEPTH, N_EXPERTS, F, D), F ** -0.5),
        "b2": n(ks[21], (DEPTH, N_EXPERTS, D), 0.01),
    }


def reference(x, c, w_ada, b_ada, g_pre_mix, g_post_mix, w_in, w_out,
              lam_q1, lam_k1, lam_q2, lam_k2, g_subln, nat_rpb,
              g_pre_ffn, g_post_ffn, w_router, b_router, w1, b1, w2, b2):
    B, L, D = x.shape
    splits = [DIFF_WIDTH, 2 * DIFF_WIDTH, 3 * DIFF_WIDTH,
              3 * DIFF_WIDTH + NAT_WIDTH, 3 * DIFF_WIDTH + 2 * NAT_WIDTH]
    for l in range(DEPTH):
        lam_init = 0.8 - 0.6 * math.exp(-0.3 * l)
        mod = jax.nn.silu(c) @ w_ada[l] + b_ada[l]
        sh1, sc1, gt1, sh2, sc2, gt2 = jnp.split(mod[:, None, :], 6, axis=-1)

        h = rms_norm(x, g_pre_mix[l]) * (1.0 + sc1) + sh1
        proj = h @ w_in[l]
        dq, dk, dv, nq, nk, nv = jnp.split(proj, splits, axis=-1)
        dq = dq.reshape(B, L, N_DIFF_HEADS, 2, HEAD_DIM)
        dk = dk.reshape(B, L, N_DIFF_HEADS, 2, HEAD_DIM)
        dv = dv.reshape(B, L, N_DIFF_HEADS, 2 * HEAD_DIM)
        lam = (jnp.exp(jnp.sum(lam_q1[l].astype(jnp.float32) * lam_k1[l].astype(jnp.float32)))
               - jnp.exp(jnp.sum(lam_q2[l].astype(jnp.float32) * lam_k2[l].astype(jnp.float32)))
               + lam_init)
        o_diff = diff_attention(dq, dk, dv, lam, lam_init, g_subln[l])
        o_nat = neighbourhood_attention(nq.reshape(B, L, N_NAT_HEADS, HEAD_DIM),
                                        nk.reshape(B, L, N_NAT_HEADS, HEAD_DIM),
                                        nv.reshape(B, L, N_NAT_HEADS, HEAD_DIM),
                                        nat_rpb[l])
        mix = jnp.concatenate([o_diff, o_nat], axis=-1) @ w_out[l]
        x = x + gt1 * rms_norm(mix, g_post_mix[l])

        h2 = rms_norm(x, g_pre_ffn[l]) * (1.0 + sc2) + sh2
        f = moe_ffn(h2, w_router[l], b_router[l], w1[l], b1[l], w2[l], b2[l])
        x = x + gt2 * rms_norm(f, g_post_ffn[l])
    return x
```

```python
import contextlib
import os
import numpy as np
import ml_dtypes
import concourse.bass as bass
import concourse.mybir as mybir
from concourse.bass_utils import run_bass_kernel_spmd

F32 = mybir.dt.float32
BF16 = mybir.dt.bfloat16
I32 = mybir.dt.int32
U32 = mybir.dt.uint32
AF = mybir.ActivationFunctionType
ALU = mybir.AluOpType
AX = mybir.AxisListType

NB = 96
BIG = float(2 ** 30)


class Buf:
    __slots__ = ("name", "w", "rs", "dsem", "dcnt")

    def __init__(self, name):
        self.name = name
        self.w = []
        self.rs = []
        self.dsem = None
        self.dcnt = 0


class Em:
    ENG = ("pe", "act", "dve", "pool", "sp")

    def __init__(self, nc, stack):
        self.nc = nc
        self.stack = stack
        self.streams = {e: [] for e in self.ENG}
        self.cnt = {e: 0 for e in self.ENG}
        self.esem = {e: stack.enter_context(nc.semaphore("sem_" + e)) for e in self.ENG}
        self.waited = {e: {} for e in self.ENG}
        self.nbuf = 0
        self.dbufs = []

    def buf(self, name=None):
        self.nbuf += 1
        return Buf("%s_%d" % (name or "b", self.nbuf))

    def bufs(self, n, name=None):
        return [self.buf(name) for _ in range(n)]

    def _dsem(self, b):
        if b.dsem is None:
            b.dsem = self.stack.enter_context(self.nc.semaphore("d_" + b.name))
            self.dbufs.append(b)
        return b.dsem

    def _deps(self, eng, reads, writes):
        toks = {}

        def add(t):
            key, val, h = t
            if eng == "pe" and key == "pe":
                return
            if key not in toks or toks[key][1] < val:
                toks[key] = t
        for b in reads:
            for t in b.w:
                add(t)
        for b in writes:
            for t in b.w:
                add(t)
            for t in b.rs:
                add(t)
        return self._filter(eng, toks.values())

    def _filter(self, eng, toks):
        out = []
        wd = self.waited[eng]
        for key, val, h in toks:
            if wd.get(key, 0) >= val:
                continue
            wd[key] = val
            out.append((h, val))
        return out

    def _update(self, tok, reads, writes):
        for b in reads:
            b.rs.append(tok)
        for b in writes:
            b.w = [tok]
            b.rs = []

    def op(self, eng, fn, reads=(), writes=()):
        waits = self._deps(eng, reads, writes)
        self.cnt[eng] += 1
        sem = self.esem[eng]
        tok = (eng, self.cnt[eng], sem)

        def run(E, fn=fn, waits=waits, sem=sem):
            for h, v in waits:
                E.wait_ge(h, v)
            fn(E).then_inc(sem, 1)
        self.streams[eng].append(run)
        self._update(tok, reads, writes)

    def dma(self, q, fn, reads=(), writes=(), n=1, sembuf=None):
        waits = self._deps(q, reads, writes)
        sb = sembuf if sembuf is not None else (writes[0] if writes else reads[0])
        sem = self._dsem(sb)
        sb.dcnt += 16 * n
        tok = ("d_" + sb.name, sb.dcnt, sem)

        def run(E, fn=fn, waits=waits, sem=sem, n=n):
            for h, v in waits:
                E.wait_ge(h, v)
            lst = fn(E)
            assert len(lst) == n
            for ins in lst:
                ins.then_inc(sem, 16)
        self.streams[q].append(run)
        self._update(tok, reads, writes)

    def barrier(self):
        toks = [(e, self.cnt[e], self.esem[e]) for e in self.ENG if self.cnt[e] > 0]
        toks += [("d_" + b.name, b.dcnt, b.dsem) for b in self.dbufs]
        for eng in self.ENG:
            waits = self._filter(eng, [t for t in toks if t[0] != eng])

            def run(E, waits=waits):
                for h, v in waits:
                    E.wait_ge(h, v)
            self.streams[eng].append(run)

    def emit(self):
        nc = self.nc
        st = self.streams
        with nc.Block() as block:
            @block.sync
            def _(E):
                for f in st["sp"]:
                    f(E)

            @block.tensor
            def _(E):
                for f in st["pe"]:
                    f(E)

            @block.scalar
            def _(E):
                for f in st["act"]:
                    f(E)

            @block.vector
            def _(E):
                for f in st["dve"]:
                    f(E)

            @block.gpsimd
            def _(E):
                for f in st["pool"]:
                    f(E)
        self.streams = {e: [] for e in self.ENG}


def build_program(debug=()):
    nc = bass.Bass("TRN2", target_bir_lowering=False)

    def din(name, shape, dt=F32):
        return nc.dram_tensor(name, list(shape), dt, kind="ExternalInput").ap()

    def dscr(name, shape, dt):
        return nc.dram_tensor(name, list(shape), dt, kind="Internal").ap()

    xb = din("xb", [8192, 1024])
    xq = din("xq", [2048, 1024])
    xw = din("xw", [2816, 1024])
    cT = din("cT", [128, 8])
    w_ada = din("w_ada", [1024, 6144])
    b_ada = din("b_ada", [1, 6144])
    g_pre_mix = din("g_pre_mix", [1, 1024])
    g_post_mix = din("g_post_mix", [1, 1024])
    g_pre_ffn = din("g_pre_ffn", [1, 1024])
    g_post_ffn = din("g_post_ffn", [1, 1024])
    w_in = din("w_in", [1024, 3072])
    w_out = din("w_out", [1024, 1024])
    lamv = din("lamv", [1, 256])
    g_subln = din("g_subln", [1, 128])
    natb = din("natb", [5, 128, 8 * 7 * 128], BF16)
    w_router = din("w_router", [1024, 32])
    b_router = din("b_router", [1, 32])
    if os.environ.get("KSTOP", "") not in ("1", "2"):
        W1p = din("W1p", [8192, 8192])
        W2p = din("W2p", [8192, 4096])
        b1 = din("b1", [32, 2048])
        b2 = din("b2", [32, 1024])
    qaug = din("qaug", [4, 3, 2, 2048], BF16)
    kaug = din("kaug", [2, 8192], BF16)
    ktab = din("ktab", [128, 4 * 2 * 64])
    bdiag = din("bdiag", [128, 4 * 128], BF16)
    identb_d = din("identb", [128, 128], BF16)
    identf_d = din("identf", [128, 128])
    ltri_d = din("ltri", [128, 128], BF16)
    iota32_d = din("iota32", [128, 32])
    thr16_d = din("thr16", [128, 512])
    iota96_d = din("iota96", [128, NB])
    pidx_d = din("pidx", [128, 1])
    out = nc.dram_tensor("out", [2048, 1024], F32, kind="ExternalOutput").ap()

    Qs = dscr("Qs", [4, 128, 2048], BF16)
    Ks = dscr("Ks", [4, 128, 8192], BF16)
    Vs = dscr("Vs", [4, 128, 64, 130], BF16)
    NQs = dscr("NQs", [4, 128, 2048], BF16)
    NKs = dscr("NKs", [4, 128, 2816], BF16)
    NVs = dscr("NVs", [128, 22, 8 * 66], BF16)
    x1s = dscr("x1s", [2048, 1024], F32)
    Xs = dscr("Xs", [NB * 128, 1024], BF16)
    Os = dscr("Os", [NB * 128, 32], BF16)
    Ys = dscr("Ys", [NB * 128, 1024], F32)

    dbg = {}

    def dbgout(name, shape, dt=F32):
        if name in debug:
            dbg[name] = nc.dram_tensor("dbg_" + name, list(shape), dt, kind="ExternalOutput").ap()
            return dbg[name]
        return None

    with contextlib.ExitStack() as top:
        em = Em(nc, top)
        YB = em.buf("yout")

        def sbt(st, name, shape, dt=F32):
            return st.enter_context(nc.sbuf_tensor(name, list(shape), dt))

        def pst(st, name, shape, dt=F32):
            return st.enter_context(nc.psum_tensor(name, list(shape), dt))

        def dma(out_, in_, reads, writes, q="sp", sembuf=None):
            em.dma(q, lambda E: [E.dma_start(out=out_, in_=in_)], reads=reads, writes=writes, sembuf=sembuf)

        def act(out_, in_, func, reads, writes, **kw):
            em.op("act", lambda E: E.activation(out=out_, in_=in_, func=func, **kw), reads, writes)

        def pe(mms, reads, writes):
            def fn(E):
                ins = None
                for (o, l, r, s0, s1) in mms:
                    ins = E.matmul(o, lhsT=l, rhs=r, start=s0, stop=s1)
                return ins
            em.op("pe", fn, reads, writes)

        def pet(trs, reads, writes):
            def fn(E):
                ins = None
                for (o, i, idn) in trs:
                    ins = E.transpose(o, i, idn)
                return ins
            em.op("pe", fn, reads, writes)

        def dve(f, reads, writes, eng="dve"):
            em.op(eng, f, reads, writes)

        def ts(out_, in0, s1, s2, op0, op1=None, reads=(), writes=(), eng="dve"):
            if op1 is None:
                dve(lambda E: E.tensor_scalar(out=out_, in0=in0, scalar1=s1, scalar2=None, op0=op0), reads, writes, eng)
            else:
                dve(lambda E: E.tensor_scalar(out=out_, in0=in0, scalar1=s1, scalar2=s2, op0=op0, op1=op1), reads, writes, eng)

        def tt(out_, in0, in1, op, reads, writes, eng="dve"):
            dve(lambda E: E.tensor_tensor(out=out_, in0=in0, in1=in1, op=op), reads, writes, eng)

        def stt(out_, in0, scalar, in1, op0, op1, reads, writes):
            dve(lambda E: E.scalar_tensor_tensor(out=out_, in0=in0, scalar=scalar, in1=in1, op0=op0, op1=op1), reads, writes)

        def cp(out_, in_, reads, writes, eng="dve"):
            dve(lambda E: E.tensor_copy(out=out_, in_=in_), reads, writes, eng)

        def mset(ap, v, writes, eng="dve"):
            dve(lambda E: E.memset(ap, v), (), writes, eng)

        def dump(name, dst_shape_src):
            pass

        identb = sbt(top, "identb_s", [128, 128], BF16)
        identf = sbt(top, "identf_s", [128, 128])
        onesb = sbt(top, "onesb", [128, 512], BF16)
        onesf = sbt(top, "onesf", [128, 128])
        rows = sbt(top, "rows", [128, 4, 1024])
        o_all = sbt(top, "o_all", [128, 16, 1024], BF16)
        stat = sbt(top, "stat", [1, 16])
        negM = sbt(top, "negM", [128, 8])
        neglam = sbt(top, "neglam", [128, 1])
        gsub = sbt(top, "gsub", [128, 128])
        B_identb, B_identf, B_onesb, B_onesf, B_rows, B_stat, B_negM, B_neglam, B_gsub = em.bufs(9, "const")
        B_oall = em.bufs(16, "oall")
        dma(identb[:], identb_d, [], [B_identb])
        dma(identf[:], identf_d, [], [B_identf])
        mset(onesb[:], 1.0, [B_onesb])
        mset(onesf[:], 1.0, [B_onesf])
        mset(stat[:], 0.0, [B_stat])
        dma(gsub[:], g_subln.to_broadcast([128, 128]), [], [B_gsub])

        with contextlib.ExitStack() as st01:
            Wall = sbt(st01, "Wall", [128, 8, 3072], BF16)
            biasrow = sbt(st01, "biasrow", [1, 3072], BF16)
            B_Wall = em.bufs(8, "Wall")
            B_biasrow = em.buf("biasrow")
            with contextlib.ExitStack() as st:
                sil = sbt(st, "sil", [128, 8])
                silrep = sbt(st, "silrep", [128, 8, 128], BF16)
                wada = [sbt(st, "wada%d" % i, [128, 8, 512], BF16) for i in range(2)]
                bada = sbt(st, "bada", [1, 6144], BF16)
                modrow = sbt(st, "modrow", [128, 6144])
                grow = sbt(st, "grow", [128, 4, 1024])
                s1row = sbt(st, "s1row", [128, 1024])
                tmpd = sbt(st, "tmpd", [128, 128])
                s1T = sbt(st, "s1T", [128, 8])
                sh1T = sbt(st, "sh1T", [128, 8])
                wst = [sbt(st, "wst%d" % i, [128, 3072]) for i in range(2)]
                lam_t = sbt(st, "lam_t", [1, 256])
                lam_s = sbt(st, "lam_s", [1, 8])
                pmod = [pst(st, "pmod%d" % i, [128, 512]) for i in range(2)]
                pbias = pst(st, "pbias", [1, 3072])
                B_sil, B_silrep, B_bada, B_grow, B_s1row, B_tmpd, B_s1T, B_sh1T, B_lamt, B_lams, B_pbias, B_plam = em.bufs(12, "p0")
                B_wada = em.bufs(2, "wada")
                B_modrow = em.bufs(12, "modrow")
                B_wst = em.bufs(2, "wst")
                B_pmod = em.bufs(2, "pmod")

                dma(sil[:], cT, [], [B_sil])
                act(sil[:], sil[:], AF.Silu, [B_sil], [B_sil])
                for c in range(8):
                    cp(silrep[:, c, :], sil[:, c:c + 1].to_broadcast([128, 128]), [B_sil], [B_silrep])
                dma(bada[:], b_ada, [], [B_bada], q="pool")
                for i, g in enumerate([g_pre_mix, g_post_mix, g_pre_ffn, g_post_ffn]):
                    dma(grow[:, i, :], g.to_broadcast([128, 1024]), [], [B_grow])
                for j in range(12):
                    wb = wada[j % 2]
                    dma(wb[:], w_ada[:, j * 512:(j + 1) * 512].rearrange("(c p) f -> p c f", p=128), [], [B_wada[j % 2]], q="pool")
                    mms = [(pmod[j % 2][:], silrep[:, c, :], wb[:, c, :], c == 0, False) for c in range(8)]
                    mms.append((pmod[j % 2][:], onesb[0:1, 0:128], bada[0:1, j * 512:(j + 1) * 512], False, True))
                    pe(mms, [B_silrep, B_wada[j % 2], B_bada, B_onesb], [B_pmod[j % 2]])
                    cp(modrow[:, j * 512:(j + 1) * 512], pmod[j % 2][:], [B_pmod[j % 2]], [B_modrow[j]])
                MR = lambda i: [B_modrow[2 * i], B_modrow[2 * i + 1]]
                m = lambda i: modrow[:, i * 1024:(i + 1) * 1024]
                stt(s1row[:], m(1), 1.0, grow[:, 0, :], ALU.add, ALU.mult, MR(1) + [B_grow], [B_s1row])
                stt(rows[:, 0, :], m(4), 1.0, grow[:, 2, :], ALU.add, ALU.mult, MR(4) + [B_grow], [B_rows])
                cp(rows[:, 1, :], m(3), MR(3) + [B_rows], [B_rows])
                tt(rows[:, 2, :], m(2), grow[:, 1, :], ALU.mult, MR(2) + [B_grow, B_rows], [B_rows])
                tt(rows[:, 3, :], m(5), grow[:, 3, :], ALU.mult, MR(5) + [B_grow, B_rows], [B_rows])
                for c in range(8):
                    tt(tmpd[:], s1row[:, c * 128:(c + 1) * 128], identf[:], ALU.mult, [B_s1row, B_identf], [B_tmpd])
                    dve(lambda E, c=c: E.reduce_sum(out=s1T[:, c:c + 1], in_=tmpd[:], axis=AX.X), [B_tmpd], [B_s1T])
                    tt(tmpd[:], modrow[:, c * 128:(c + 1) * 128], identf[:], ALU.mult, MR(0) + [B_identf], [B_tmpd])
                    dve(lambda E, c=c: E.reduce_sum(out=sh1T[:, c:c + 1], in_=tmpd[:], axis=AX.X), [B_tmpd], [B_sh1T])
                for c in range(8):
                    wsb = wst[c % 2]
                    dma(wsb[:], w_in[c * 128:(c + 1) * 128, :], [], [B_wst[c % 2]])
                    mms = [(pbias[0:1, n * 512:(n + 1) * 512], sh1T[:, c:c + 1], wsb[:, n * 512:(n + 1) * 512], c == 0, c == 7) for n in range(6)]
                    pe(mms, [B_sh1T, B_wst[c % 2]], [B_pbias])
                    act(Wall[:, c, :], wsb[:], AF.Copy, [B_wst[c % 2], B_s1T], [B_Wall[c]], scale=s1T[:, c:c + 1])
                cp(biasrow[:], pbias[:], [B_pbias], [B_biasrow])
                dma(lam_t[:], lamv, [], [B_lamt])
                tt(lam_t[0:1, 0:64], lam_t[0:1, 0:64], lam_t[0:1, 64:128], ALU.mult, [B_lamt], [B_lamt])
                tt(lam_t[0:1, 128:192], lam_t[0:1, 128:192], lam_t[0:1, 192:256], ALU.mult, [B_lamt], [B_lamt])
                dve(lambda E: E.reduce_sum(out=lam_s[0:1, 0:1], in_=lam_t[0:1, 0:64], axis=AX.X), [B_lamt], [B_lams])
                dve(lambda E: E.reduce_sum(out=lam_s[0:1, 1:2], in_=lam_t[0:1, 128:192], axis=AX.X), [B_lamt], [B_lams])
                act(lam_s[0:1, 2:4], lam_s[0:1, 0:2], AF.Exp, [B_lams], [B_lams])
                stt(lam_s[0:1, 4:5], lam_s[0:1, 3:4], -0.2, lam_s[0:1, 2:3], ALU.add, ALU.subtract, [B_lams], [B_lams])
                pe([(pmod[0][:, 0:1], onesf[0:1, 0:128], lam_s[0:1, 4:5], True, True)], [B_onesf, B_lams], [B_pmod[0]])
                cp(neglam[:], pmod[0][:, 0:1], [B_pmod[0]], [B_neglam])
                d = dbgout("rows", [128, 4096])
                if d is not None:
                    dma(d, rows[:].rearrange("p a f -> p (a f)"), [B_rows], [YB], sembuf=B_rows)
                d = dbgout("neglam", [128, 1])
                if d is not None:
                    dma(d, neglam[:], [B_neglam], [YB], sembuf=B_neglam)
                em.barrier()
                em.emit()

            with contextlib.ExitStack() as st:
                xt = [sbt(st, "xt%d" % i, [128, 1024]) for i in range(2)]
                xn = [sbt(st, "xn%d" % i, [128, 1024], BF16) for i in range(2)]
                xnT = [sbt(st, "xnT%d" % i, [128, 8, 512], BF16) for i in range(2)]
                ssq = sbt(st, "ssq", [128, 4])
                junk = sbt(st, "junk", [128, 1024], BF16)
                ev = [sbt(st, "ev%d" % i, [128, 512], BF16) for i in range(3)]
                sq = [sbt(st, "sq%d" % i, [128, 512], BF16) for i in range(2)]
                vt = [sbt(st, "vt%d" % i, [128, 4, 130], BF16) for i in range(2)]
                nvt = [sbt(st, "nvt%d" % i, [128, 8, 66], BF16) for i in range(2)]
                mx = sbt(st, "mx", [1, 2])
                ptr = [pst(st, "ptr%d" % i, [128, 8, 128], BF16) for i in range(2)]
                pp = [pst(st, "pp%d" % i, [128, 512]) for i in range(3)]
                pn = pst(st, "pn", [1, 512])
                B_xt = em.bufs(2, "xt"); B_xn = em.bufs(2, "xn"); B_xnT = em.bufs(2, "xnT")
                B_ssq = em.buf("ssq"); B_junk = em.buf("junk"); B_ev = em.bufs(3, "ev"); B_sq = em.bufs(2, "sq")
                B_vt = em.bufs(2, "vt"); B_nvt = em.bufs(2, "nvt"); B_mx = em.buf("mx")
                B_ptr = em.bufs(2, "ptr"); B_pp = em.bufs(3, "pp"); B_pn = em.buf("pn")
                B_scr = em.buf("scr1")
                for i in range(2):
                    mset(vt[i][:, :, 128:129], 1.0, [B_vt[i]])
                    mset(vt[i][:, :, 129:130], 0.0, [B_vt[i]])
                    mset(nvt[i][:, :, 64:65], 1.0, [B_nvt[i]])
                    mset(nvt[i][:, :, 65:66], 0.0, [B_nvt[i]])
                cnt = {"tile": 0, "grp": 0, "pp": 0, "ev": 0, "sq": 0, "vt": 0, "nvt": 0}

                def norm_group(src, g):
                    gi = cnt["grp"] % 2
                    cnt["grp"] += 1
                    for t in range(4):
                        i = cnt["tile"] % 2
                        cnt["tile"] += 1
                        r0 = g * 512 + t * 128
                        dma(xt[i][:], src[r0:r0 + 128, :], [], [B_xt[i]])
                        act(junk[:], xt[i][:], AF.Square, [B_xt[i]], [B_junk, B_ssq], accum_out=ssq[:, 0:1])
                        ts(ssq[:, 1:2], ssq[:, 0:1], 1.0 / 1024, 1e-6, ALU.mult, ALU.add, [B_ssq], [B_ssq])
                        act(ssq[:, 2:3], ssq[:, 1:2], AF.Sqrt, [B_ssq], [B_ssq])
                        dve(lambda E: E.reciprocal(out=ssq[:, 3:4], in_=ssq[:, 2:3]), [B_ssq], [B_ssq])
                        act(xn[i][:], xt[i][:], AF.Copy, [B_xt[i], B_ssq], [B_xn[i]], scale=ssq[:, 3:4])
                        pet([(ptr[i][:, c, :], xn[i][:, c * 128:(c + 1) * 128], identb[:]) for c in range(8)],
                            [B_xn[i], B_identb], [B_ptr[i]])
                        cp(xnT[gi][:, :, t * 128:(t + 1) * 128], ptr[i][:], [B_ptr[i]], [B_xnT[gi]])
                    return gi

                def proj_T(gi, col0, scale, dst, stat_idx):
                    k = cnt["pp"] % 3; cnt["pp"] += 1
                    mms = [(pp[k][:], Wall[:, c, col0:col0 + 128], xnT[gi][:, c, :], c == 0, False) for c in range(8)]
                    mms.append((pp[k][:], biasrow[0:1, col0:col0 + 128], onesb[0:1, 0:512], False, True))
                    pe(mms, B_Wall + [B_xnT[gi], B_biasrow, B_onesb], [B_pp[k]])
                    e = cnt["ev"] % 3; cnt["ev"] += 1
                    act(ev[e][:], pp[k][:], AF.Copy, [B_pp[k]], [B_ev[e]], scale=scale)
                    dma(dst, ev[e][:], [B_ev[e]], [B_scr], sembuf=B_ev[e])
                    s = cnt["sq"] % 2; cnt["sq"] += 1
                    tt(sq[s][:], ev[e][:], ev[e][:], ALU.mult, [B_ev[e]], [B_sq[s]])
                    pe([(pn[:], onesb[:, 0:1], sq[s][:], True, True)], [B_onesb, B_sq[s]], [B_pn])
                    dve(lambda E: E.reduce_max(out=mx[0:1, 0:1], in_=pn[0:1, :], axis=AX.X), [B_pn], [B_mx])
                    tt(stat[0:1, stat_idx:stat_idx + 1], stat[0:1, stat_idx:stat_idx + 1], mx[0:1, 0:1], ALU.max, [B_mx, B_stat], [B_stat])

                def proj_tok(gi, t, col0):
                    k = cnt["pp"] % 3; cnt["pp"] += 1
                    mms = [(pp[k][:], xnT[gi][:, c, t * 128:(t + 1) * 128], Wall[:, c, col0:col0 + 512], c == 0, False) for c in range(8)]
                    mms.append((pp[k][:], onesb[0:1, 0:128], biasrow[0:1, col0:col0 + 512], False, True))
                    pe(mms, B_Wall + [B_xnT[gi], B_biasrow, B_onesb], [B_pp[k]])
                    return k

                def norm_part(src, g, ntl):
                    gi = cnt["grp"] % 2
                    cnt["grp"] += 1
                    for t in range(ntl):
                        i = cnt["tile"] % 2
                        cnt["tile"] += 1
                        r0 = g * 512 + t * 128
                        dma(xt[i][:], src[r0:r0 + 128, :], [], [B_xt[i]])
                        act(junk[:], xt[i][:], AF.Square, [B_xt[i]], [B_junk, B_ssq], accum_out=ssq[:, 0:1])
                        ts(ssq[:, 1:2], ssq[:, 0:1], 1.0 / 1024, 1e-6, ALU.mult, ALU.add, [B_ssq], [B_ssq])
                        act(ssq[:, 2:3], ssq[:, 1:2], AF.Sqrt, [B_ssq], [B_ssq])
                        dve(lambda E: E.reciprocal(out=ssq[:, 3:4], in_=ssq[:, 2:3]), [B_ssq], [B_ssq])
                        act(xn[i][:], xt[i][:], AF.Copy, [B_xt[i], B_ssq], [B_xn[i]], scale=ssq[:, 3:4])
                        pet([(ptr[i][:, c, :], xn[i][:, c * 128:(c + 1) * 128], identb[:]) for c in range(8)],
                            [B_xn[i], B_identb], [B_ptr[i]])
                        cp(xnT[gi][:, :, t * 128:(t + 1) * 128], ptr[i][:], [B_ptr[i]], [B_xnT[gi]])
                    return gi

                def projB(kind, g, gi):
                    if kind == "own":
                        for h in range(4):
                            proj_T(gi, h * 128, 0.125, Qs[h, :, g * 512:(g + 1) * 512], h)
                        for c4 in range(4):
                            proj_T(gi, 1536 + c4 * 128, 0.125, NQs[c4, :, g * 512:(g + 1) * 512], 8 + c4)
                    elif kind == "seq":
                        for h in range(4):
                            proj_T(gi, 512 + h * 128, 1.0, Ks[h, :, g * 512:(g + 1) * 512], 4 + h)
                        for t in range(4):
                            k = proj_tok(gi, t, 1024)
                            v = cnt["vt"] % 2; cnt["vt"] += 1
                            cp(vt[v][:, :, 0:128], pp[k][:].rearrange("p (h e) -> p h e", h=4), [B_pp[k]], [B_vt[v]])
                            dma(Vs[:, :, g * 4 + t, :].rearrange("h p e -> p h e"), vt[v][:], [B_vt[v]], [B_scr], sembuf=B_vt[v])
                    else:
                        ntl = 4 if g < 5 else 2
                        ncol = ntl * 128
                        for c4 in range(4):
                            k = cnt["pp"] % 3; cnt["pp"] += 1
                            col0 = 2048 + c4 * 128
                            mms = [(pp[k][:, 0:ncol], Wall[:, c, col0:col0 + 128], xnT[gi][:, c, 0:ncol], c == 0, False) for c in range(8)]
                            mms.append((pp[k][:, 0:ncol], biasrow[0:1, col0:col0 + 128], onesb[0:1, 0:ncol], False, True))
                            pe(mms, B_Wall + [B_xnT[gi], B_biasrow, B_onesb], [B_pp[k]])
                            e = cnt["ev"] % 3; cnt["ev"] += 1
                            act(ev[e][:, 0:ncol], pp[k][:, 0:ncol], AF.Copy, [B_pp[k]], [B_ev[e]])
                            dma(NKs[c4, :, g * 512:g * 512 + ncol], ev[e][:, 0:ncol], [B_ev[e]], [B_scr], sembuf=B_ev[e])
                            s_ = cnt["sq"] % 2; cnt["sq"] += 1
                            tt(sq[s_][:, 0:ncol], ev[e][:, 0:ncol], ev[e][:, 0:ncol], ALU.mult, [B_ev[e]], [B_sq[s_]])
                            pe([(pn[:, 0:ncol], onesb[:, 0:1], sq[s_][:, 0:ncol], True, True)], [B_onesb, B_sq[s_]], [B_pn])
                            dve(lambda E, ncol=ncol: E.reduce_max(out=mx[0:1, 0:1], in_=pn[0:1, 0:ncol], axis=AX.X), [B_pn], [B_mx])
                            tt(stat[0:1, 12 + c4:13 + c4], stat[0:1, 12 + c4:13 + c4], mx[0:1, 0:1], ALU.max, [B_mx, B_stat], [B_stat])
                        for t in range(ntl):
                            k = proj_tok(gi, t, 2560)
                            v = cnt["nvt"] % 2; cnt["nvt"] += 1
                            cp(nvt[v][:, :, 0:64], pp[k][:].rearrange("p (h e) -> p h e", h=8), [B_pp[k]], [B_nvt[v]])
                            dma(NVs[:, g * 4 + t, :], nvt[v][:].rearrange("p h e -> p (h e)"), [B_nvt[v]], [B_scr], sembuf=B_nvt[v])

                groups = [("own", g, xq, 4) for g in range(4)] + [("seq", g, xb, 4) for g in range(16)] \
                    + [("win", g, xw, 4 if g < 5 else 2) for g in range(6)]
                gis = [None] * len(groups)
                gis[0] = norm_part(groups[0][2], groups[0][1], groups[0][3])
                for n_, (kind, g, src, ntl) in enumerate(groups):
                    if n_ + 1 < len(groups):
                        kn, gn, sn, tn = groups[n_ + 1]
                        gis[n_ + 1] = norm_part(sn, gn, tn)
                    projB(kind, g, gis[n_])
                mm_ = sbt(st, "mm_", [1, 8])
                pM = pst(st, "pM", [128, 8])
                B_mm, B_pM = em.bufs(2, "mm")
                tt(mm_[0:1, 0:4], stat[0:1, 0:4], stat[0:1, 4:8], ALU.mult, [B_stat], [B_mm])
                tt(mm_[0:1, 4:8], stat[0:1, 8:12], stat[0:1, 12:16], ALU.mult, [B_stat, B_mm], [B_mm])
                act(mm_[:], mm_[:], AF.Sqrt, [B_mm], [B_mm])
                ts(mm_[:], mm_[:], -1.05, None, ALU.mult, None, [B_mm], [B_mm])
                pe([(pM[:], onesf[0:1, 0:128], mm_[0:1, :], True, True)], [B_onesf, B_mm], [B_pM])
                cp(negM[:], pM[:], [B_pM], [B_negM])
                d = dbgout("negM", [128, 8])
                if d is not None:
                    dma(d, negM[:], [B_negM], [YB], sembuf=B_negM)
                em.barrier()
                em.emit()
        STOP = os.environ.get("KSTOP", "")
        if STOP != "1":
            with contextlib.ExitStack() as st:
                KA = [sbt(st, "KA%d" % m, [66, 8192], BF16) for m in range(2)]
                QA = [[sbt(st, "QA%d_%d" % (m, v), [66, 2048], BF16) for v in range(3)] for m in range(2)]
                Vh = sbt(st, "Vh", [128, 64, 130], BF16)
                ktab_t = sbt(st, "ktab_s", [128, 4, 2, 64])
                kb = sbt(st, "kb", [128, 2, 64])
                bdg = sbt(st, "bdg_s", [128, 4, 128], BF16)
                PT = [sbt(st, "PT%d" % i, [128, 512], BF16) for i in range(3)]
                O1n = sbt(st, "O1n", [128, 4, 128])
                dtl = sbt(st, "dtl", [128, 128])
                sm = sbt(st, "sm", [128, 8])
                junk2 = sbt(st, "junk2", [128, 128])
                gs8 = sbt(st, "gs8", [128, 128])
                S = [pst(st, "S%d" % i, [128, 512]) for i in range(2)]
                O = [pst(st, "O%d" % i, [128, 512]) for i in range(4)]
                B_KA = em.bufs(2, "KA"); B_QA = em.bufs(2, "QA"); B_Vh = em.buf("Vh"); B_ktab = em.buf("ktab")
                B_kb = em.buf("kb"); B_bdg = em.buf("bdg"); B_PT = em.bufs(3, "PT"); B_O1n = em.buf("O1n")
                B_dtl = em.buf("dtl"); B_sm = em.buf("sm"); B_junk2 = em.buf("junk2"); B_gs8 = em.buf("gs8")
                B_S = em.bufs(2, "S"); B_O = em.bufs(4, "O")
                dma(ktab_t[:].rearrange("p a b c -> p (a b c)"), ktab, [], [B_ktab])
                dma(bdg[:].rearrange("p a b -> p (a b)"), bdiag, [], [B_bdg])
                ts(gs8[:], gsub[:], 0.8, None, ALU.mult, None, [B_gsub], [B_gs8])
                it = 0
                for h in range(4):
                    for m in range(2):
                        dma(KA[m][0:64, :], Ks[h, 64 * m:64 * m + 64, :], [], [B_KA[m]])
                        dma(KA[m][64:66, :], kaug, [], [B_KA[m]])
                        for v in range(3):
                            dma(QA[m][v][0:64, :], Qs[h, 64 * m:64 * m + 64, :], [], [B_QA[m]])
                            dma(QA[m][v][64:66, :], qaug[h, v], [], [B_QA[m]])
                    dma(Vh[:], Vs[h], [], [B_Vh])
                    ts(kb[:], ktab_t[:, h], negM[:, h:h + 1], None, ALU.add, None, [B_ktab, B_negM], [B_kb])
                    seq = [(qc, m, kt) for qc in range(4) for m in range(2) for kt in range(64)]

                    def segs_of(qc, kt):
                        if kt >= 16:
                            return [(0, 4, 0, kb[:, 0, kt:kt + 1])]
                        segs = []
                        for t in range(4):
                            qt = 4 * qc + t
                            if qt > kt:
                                cls = (0, kb[:, 0, kt:kt + 1])
                            elif qt == kt:
                                cls = (2, negM[:, h:h + 1])
                            else:
                                cls = (1, kb[:, 1, kt:kt + 1])
                            if segs and segs[-1][2] == cls[0]:
                                segs[-1] = (segs[-1][0], t + 1, cls[0], cls[1])
                            else:
                                segs.append((t, t + 1, cls[0], cls[1]))
                        return segs

                    def emit_S(n):
                        qc, m, kt = seq[n]
                        sb = (it + n) % 2
                        mms = []
                        for (t0, t1, v, col) in segs_of(qc, kt):
                            c0, c1 = t0 * 128, t1 * 128
                            q0 = qc * 512
                            mms.append((S[sb][:, c0:c1], KA[m][0:66, kt * 128:(kt + 1) * 128], QA[m][v][0:66, q0 + c0:q0 + c1], True, v != 2))
                            if v == 2:
                                mms.append((S[sb][:, c0:c1], identb[:], bdg[:, h, :], False, True))
                        pe(mms, [B_KA[m], B_QA[m], B_identb, B_bdg], [B_S[sb]])

                    def emit_rest(n):
                        qc, m, kt = seq[n]
                        sb = (it + n) % 2
                        pb = (it + n) % 3
                        for (t0, t1, v, col) in segs_of(qc, kt):
                            c0, c1 = t0 * 128, t1 * 128
                            act(PT[pb][:, c0:c1], S[sb][:, c0:c1], AF.Exp, [B_S[sb], B_kb, B_negM], [B_PT[pb]], bias=col, scale=1.0)
                        pe([(O[t][:, 0:130], PT[pb][:, t * 128:(t + 1) * 128], Vh[:, kt, :], kt == 0, kt == 63) for t in range(4)],
                           [B_PT[pb], B_Vh], B_O)
                        if kt != 63:
                            return
                        for t in range(4):
                            dve(lambda E, t=t: E.reciprocal(out=sm[:, 0:1], in_=O[t][:, 128:129]), [B_O[t]], [B_sm])
                            if m == 0:
                                ts(O1n[:, t, :], O[t][:, 0:128], sm[:, 0:1], None, ALU.mult, None, [B_O[t], B_sm], [B_O1n])
                            else:
                                ts(dtl[:], O[t][:, 0:128], sm[:, 0:1], None, ALU.mult, None, [B_O[t], B_sm], [B_dtl])
                                stt(dtl[:], dtl[:], neglam[:, 0:1], O1n[:, t, :], ALU.mult, ALU.add, [B_dtl, B_neglam, B_O1n], [B_dtl])
                                act(junk2[:], dtl[:], AF.Square, [B_dtl], [B_junk2, B_sm], accum_out=sm[:, 1:2])
                                ts(sm[:, 2:3], sm[:, 1:2], 1.0 / 128, 1e-6, ALU.mult, ALU.add, [B_sm], [B_sm])
                                act(sm[:, 3:4], sm[:, 2:3], AF.Sqrt, [B_sm], [B_sm])
                                dve(lambda E: E.reciprocal(out=sm[:, 4:5], in_=sm[:, 3:4]), [B_sm], [B_sm])
                                jt = 4 * qc + t
                                stt(o_all[:, jt, h * 128:(h + 1) * 128], dtl[:], sm[:, 4:5], gs8[:], ALU.mult, ALU.mult,
                                    [B_dtl, B_sm, B_gs8], [B_oall[jt]])

                    emit_S(0)
                    for n in range(len(seq)):
                        if n + 1 < len(seq):
                            emit_S(n + 1)
                        emit_rest(n)
                    it += len(seq)
                em.barrier()
                em.emit()

            with contextlib.ExitStack() as st:
                NQT = sbt(st, "NQT", [128, 4, 2048], BF16)
                NKT = sbt(st, "NKT", [128, 4, 2816], BF16)
                NV = sbt(st, "NV", [128, 22, 528], BF16)
                nbt = [sbt(st, "nbt%d" % i, [128, 8 * 7 * 128], BF16) for i in range(2)]
                PN = [sbt(st, "PN%d" % i, [128, 896], BF16) for i in range(2)]
                sn = sbt(st, "sn", [128, 2])
                SN = [pst(st, "SN%d" % i, [128, 1024]) for i in range(2)]
                NO = [pst(st, "NO%d" % i, [128, 512]) for i in range(2)]
                B_NQT, B_NKT, B_NV, B_sn = em.bufs(4, "nat")
                B_nbt = em.bufs(2, "nbt"); B_PN = em.bufs(2, "PN"); B_SN = em.bufs(2, "SN"); B_NO = em.bufs(2, "NO")
                dma(NQT[:], NQs.rearrange("c p t -> p c t"), [], [B_NQT])
                dma(NKT[:], NKs.rearrange("c p t -> p c t"), [], [B_NKT])
                dma(NV[:], NVs, [], [B_NV])
                items = [(j, h) for j in range(16) for h in range(8)]

                def nat_S(n):
                    j, h = items[n]
                    nb_ = nbt[j % 2]
                    if h == 0:
                        slot = {0: 1, 1: 2, 14: 3, 15: 4}.get(j, 0)
                        dma(nb_[:], natb[slot], [], [B_nbt[j % 2]])
                    c4, hp = h // 2, (h % 2) * 64
                    sb = n % 2
                    mms = []
                    for o in range(7):
                        oc = slice(o * 128, (o + 1) * 128)
                        mms.append((SN[sb][:, oc], NKT[hp:hp + 64, c4, (j + o) * 128:(j + o + 1) * 128],
                                    NQT[hp:hp + 64, c4, j * 128:(j + 1) * 128], True, False))
                        mms.append((SN[sb][:, oc], identb[:], nb_[:, (h * 7 + o) * 128:(h * 7 + o + 1) * 128], False, True))
                    pe(mms, [B_NKT, B_NQT, B_identb, B_nbt[j % 2]], [B_SN[sb]])

                def nat_rest(n):
                    j, h = items[n]
                    c4 = h // 2
                    sb = n % 2
                    act(PN[sb][:], SN[sb][:, 0:896], AF.Exp, [B_SN[sb], B_negM], [B_PN[sb]], bias=negM[:, 4 + c4:5 + c4], scale=1.0)
                    mms = [(NO[sb][:, 0:66], PN[sb][:, o * 128:(o + 1) * 128], NV[:, j + o, h * 66:h * 66 + 66], o == 0, o == 6) for o in range(7)]
                    pe(mms, [B_PN[sb], B_NV], [B_NO[sb]])
                    dve(lambda E, sb=sb: E.reciprocal(out=sn[:, 0:1], in_=NO[sb][:, 64:65]), [B_NO[sb]], [B_sn])
                    ts(o_all[:, j, 512 + h * 64:512 + (h + 1) * 64], NO[sb][:, 0:64], sn[:, 0:1], None, ALU.mult, None,
                       [B_NO[sb], B_sn], [B_oall[j]])

                nat_S(0)
                for n in range(len(items)):
                    if n + 1 < len(items):
                        nat_S(n + 1)
                    nat_rest(n)
                d = dbgout("o_all", [128, 16 * 1024], BF16)
                if d is not None:
                    dma(d, o_all[:].rearrange("p a f -> p (a f)"), B_oall, [YB], sembuf=B_oall[0])
                em.barrier()
                em.emit()

        if STOP not in ("1", "2"):
            with contextlib.ExitStack() as st:
                wr = sbt(st, "wr", [128, 8, 32])
                br = sbt(st, "br", [1, 32])
                mskb = sbt(st, "mskb", [128, 16, 32], BF16)
                gate4 = sbt(st, "gate4", [128, 16, 4])
                eidx = sbt(st, "eidx", [128, 16, 8])
                dsti = sbt(st, "dsti", [128, 64], I32)
                idxW = sbt(st, "idxW", [128, 4, NB], I32)
                ohTall = sbt(st, "ohTall", [32, NB])
                B_wr, B_br, B_mskb, B_gate4, B_eidx, B_dsti, B_idxW, B_ohTall = em.bufs(8, "p4")
                B_hrow = em.bufs(16, "hrow")
                B_x1s = em.bufs(16, "x1s")
                dma(wr[:], w_router.rearrange("(c p) f -> p c f", p=128), [], [B_wr])
                dma(br[:], b_router, [], [B_br])
                with contextlib.ExitStack() as s4:
                    Wout = sbt(s4, "Wout", [128, 8, 1024], BF16)
                    hrow = sbt(s4, "hrow", [128, 16, 1024], BF16)
                    zt = sbt(s4, "zt", [128, 2, 1024], BF16)
                    B_zt, B_Xz = em.bufs(2, "zx")
                    mset(zt[:], 0.0, [B_zt], eng="pool")
                    for cz in range(NB // 2):
                        dma(Xs[cz * 256:(cz + 1) * 256, :].rearrange("(r p) f -> p r f", p=128), zt[:], [B_zt], [B_Xz], sembuf=B_Xz)
                    B_Wout = em.buf("Wout")
                    dma(Wout[:], w_out.rearrange("(c p) f -> p c f", p=128), [], [B_Wout], q="pool")
                    ltri = sbt(s4, "ltri_s", [128, 128], BF16)
                    iota32 = sbt(s4, "iota32_s", [128, 32])
                    thr16 = sbt(s4, "thr16_s", [128, 32, 16])
                    iota96 = sbt(s4, "iota96_s", [128, NB])
                    pidx = sbt(s4, "pidx_s", [128, 1])
                    B_ltri, B_iota32, B_thr16, B_iota96, B_pidx = em.bufs(5, "cst")
                    dma(ltri[:], ltri_d, [], [B_ltri])
                    dma(iota32[:], iota32_d, [], [B_iota32])
                    dma(thr16[:].rearrange("p a b -> p (a b)"), thr16_d, [], [B_thr16])
                    dma(iota96[:], iota96_d, [], [B_iota96])
                    dma(pidx[:], pidx_d, [], [B_pidx])
                    oT = sbt(s4, "oT", [128, 8, 128], BF16)
                    xt4 = sbt(s4, "xt4", [128, 1024])
                    tmp4 = sbt(s4, "tmp4", [128, 1024])
                    x1t = sbt(s4, "x1t", [128, 1024])
                    h2t = sbt(s4, "h2t", [128, 1024])
                    h2Tf = sbt(s4, "h2Tf", [128, 8, 128])
                    junk4 = sbt(s4, "junk4", [128, 1024], BF16)
                    s4s = sbt(s4, "s4s", [128, 16])
                    lg = sbt(s4, "lg", [128, 32])
                    v8 = sbt(s4, "v8", [128, 8])
                    i8 = sbt(s4, "i8", [128, 8], U32)
                    msk = sbt(s4, "msk", [128, 32])
                    e4 = sbt(s4, "e4", [128, 4])
                    pto = pst(s4, "pto", [128, 8, 128], BF16)
                    pmix = pst(s4, "pmix", [128, 1024])
                    ptf = pst(s4, "ptf", [128, 8, 128])
                    plg = pst(s4, "plg", [128, 32])
                    (B_oT, B_xt4, B_tmp4, B_x1t, B_h2t, B_h2Tf, B_junk4, B_s4s, B_lg, B_v8, B_i8, B_msk, B_e4,
                     B_pto, B_pmix, B_ptf, B_plg) = em.bufs(17, "s4")
                    for j in range(16):
                        pet([(pto[:, c, :], o_all[:, j, c * 128:(c + 1) * 128], identb[:]) for c in range(8)], [B_oall[j], B_identb], [B_pto])
                        cp(oT[:], pto[:], [B_pto], [B_oT])
                        mms = []
                        for n in range(2):
                            for c in range(8):
                                mms.append((pmix[:, n * 512:(n + 1) * 512], oT[:, c, :], Wout[:, c, n * 512:(n + 1) * 512], c == 0, c == 7))
                        pe(mms, [B_oT, B_Wout], [B_pmix])
                        dma(xt4[:], xq[j * 128:(j + 1) * 128, :], [], [B_xt4])
                        act(junk4[:], pmix[:], AF.Square, [B_pmix], [B_junk4, B_s4s], accum_out=s4s[:, 0:1])
                        ts(s4s[:, 1:2], s4s[:, 0:1], 1.0 / 1024, 1e-6, ALU.mult, ALU.add, [B_s4s], [B_s4s])
                        act(s4s[:, 2:3], s4s[:, 1:2], AF.Sqrt, [B_s4s], [B_s4s])
                        dve(lambda E: E.reciprocal(out=s4s[:, 3:4], in_=s4s[:, 2:3]), [B_s4s], [B_s4s])
                        stt(tmp4[:], pmix[:], s4s[:, 3:4], rows[:, 2, :], ALU.mult, ALU.mult, [B_pmix, B_s4s, B_rows], [B_tmp4])
                        tt(x1t[:], tmp4[:], xt4[:], ALU.add, [B_tmp4, B_xt4], [B_x1t])
                        dma(x1s[j * 128:(j + 1) * 128, :], x1t[:], [B_x1t], [B_x1s[j]], sembuf=B_x1s[j])
                        act(junk4[:], x1t[:], AF.Square, [B_x1t], [B_junk4, B_s4s], accum_out=s4s[:, 4:5])
                        ts(s4s[:, 5:6], s4s[:, 4:5], 1.0 / 1024, 1e-6, ALU.mult, ALU.add, [B_s4s], [B_s4s])
                        act(s4s[:, 6:7], s4s[:, 5:6], AF.Sqrt, [B_s4s], [B_s4s])
                        dve(lambda E: E.reciprocal(out=s4s[:, 7:8], in_=s4s[:, 6:7]), [B_s4s], [B_s4s])
                        stt(tmp4[:], x1t[:], s4s[:, 7:8], rows[:, 0, :], ALU.mult, ALU.mult, [B_x1t, B_s4s, B_rows], [B_tmp4])
                        tt(h2t[:], tmp4[:], rows[:, 1, :], ALU.add, [B_tmp4, B_rows], [B_h2t])
                        act(hrow[:, j, :], h2t[:], AF.Copy, [B_h2t], [B_hrow[j]])
                        pet([(ptf[:, c, :], h2t[:, c * 128:(c + 1) * 128], identf[:]) for c in range(8)], [B_h2t, B_identf], [B_ptf])
                        cp(h2Tf[:], ptf[:], [B_ptf], [B_h2Tf])
                        mms = [(plg[:], h2Tf[:, c, :], wr[:, c, :], c == 0, False) for c in range(8)]
                        mms.append((plg[:], onesf[0:1, 0:128], br[0:1, :], False, True))
                        pe(mms, [B_h2Tf, B_wr, B_br, B_onesf], [B_plg])
                        cp(lg[:], plg[:], [B_plg], [B_lg])
                        dve(lambda E: E.max(out=v8[:], in_=lg[:]), [B_lg], [B_v8])
                        dve(lambda E: E.max_index(out=i8[:], in_max=v8[:], in_values=lg[:]), [B_lg, B_v8], [B_i8])
                        cp(eidx[:, j, :], i8[:], [B_i8], [B_eidx])
                        ts(msk[:], lg[:], v8[:, 3:4], None, ALU.is_ge, None, [B_lg, B_v8], [B_msk])
                        cp(mskb[:, j, :], msk[:], [B_msk], [B_mskb])
                        ts(s4s[:, 8:9], v8[:, 0:1], -1.0, None, ALU.mult, None, [B_v8, B_s4s], [B_s4s])
                        act(e4[:], v8[:, 0:4], AF.Exp, [B_v8, B_s4s], [B_e4], bias=s4s[:, 8:9], scale=1.0)
                        dve(lambda E: E.reduce_sum(out=s4s[:, 9:10], in_=e4[:], axis=AX.X), [B_e4, B_s4s], [B_s4s])
                        dve(lambda E: E.reciprocal(out=s4s[:, 10:11], in_=s4s[:, 9:10]), [B_s4s], [B_s4s])
                        ts(gate4[:, j, :], e4[:], s4s[:, 10:11], None, ALU.mult, None, [B_e4, B_s4s], [B_gate4])
                    cnt_t = sbt(s4, "cnt_t", [128, 32])
                    cmp1 = sbt(s4, "cmp1", [128, 32, 16])
                    nbk = sbt(s4, "nbk", [128, 32])
                    ones32 = sbt(s4, "ones32", [128, 32])
                    cum = sbt(s4, "cum", [128, 32])
                    pstart = sbt(s4, "pstart", [128, 32])
                    cmp2 = sbt(s4, "cmp2", [128, NB, 32])
                    blk = sbt(s4, "blk", [128, NB])
                    chg = sbt(s4, "chg", [128, NB])
                    idxf = sbt(s4, "idxf", [128, 5, NB])
                    chg2 = sbt(s4, "chg2", [128, NB])
                    B_chg2 = em.buf("chg2")
                    destf = sbt(s4, "destf", [128, 16, 32])
                    ohf = sbt(s4, "ohf", [128, 32])
                    dstf = sbt(s4, "dstf", [128, 64])
                    (B_cnt, B_cmp1, B_nbk, B_ones32, B_cum, B_pstart, B_cmp2, B_blk, B_chg, B_idxf, B_destf, B_ohf, B_dstf) = em.bufs(13, "rt")
                    mms = [(plg[:], onesb[:, 0:128], mskb[:, j, :], j == 0, j == 15) for j in range(16)]
                    pe(mms, [B_onesb, B_mskb], [B_plg])
                    cp(cnt_t[:], plg[:], [B_plg], [B_cnt])
                    tt(cmp1[:], cnt_t[:].unsqueeze(2).to_broadcast([128, 32, 16]), thr16[:], ALU.is_gt, [B_cnt, B_thr16], [B_cmp1])
                    dve(lambda E: E.reduce_sum(out=nbk[:], in_=cmp1[:], axis=AX.X), [B_cmp1], [B_nbk])
                    mset(ones32[:], 1.0, [B_ones32])
                    dve(lambda E: E.tensor_tensor_scan(out=cum[:], data0=ones32[:], data1=nbk[:], initial=0.0, op0=ALU.mult, op1=ALU.add),
                        [B_ones32, B_nbk], [B_cum])
                    tt(pstart[:], cum[:], nbk[:], ALU.subtract, [B_cum, B_nbk], [B_pstart])
                    ts(pstart[:], pstart[:], 128.0, None, ALU.mult, None, [B_pstart], [B_pstart])
                    tt(cmp2[:], cum[:].unsqueeze(1).to_broadcast([128, NB, 32]), iota96[:].unsqueeze(2).to_broadcast([128, NB, 32]), ALU.is_le,
                       [B_cum, B_iota96], [B_cmp2])
                    dve(lambda E: E.reduce_sum(out=blk[:], in_=cmp2[:], axis=AX.X), [B_cmp2], [B_blk])
                    ts(blk[:], blk[:], 31.0, None, ALU.min, None, [B_blk], [B_blk])
                    ts(ohTall[:], blk[0:32, :], pidx[0:32, 0:1], None, ALU.is_equal, None, [B_blk, B_pidx], [B_ohTall])
                    mset(chg[:, 0:1], 1.0, [B_chg])
                    tt(chg[:, 1:NB], blk[:, 1:NB], blk[:, 0:NB - 1], ALU.not_equal, [B_blk, B_chg], [B_chg])
                    mset(chg2[:, 0:2], 1.0, [B_chg2])
                    tt(chg2[:, 2:NB], blk[:, 2:NB], blk[:, 0:NB - 2], ALU.not_equal, [B_blk, B_chg2], [B_chg2])
                    ts(chg[:], chg[:], -float(2 ** 27), float(2 ** 27), ALU.mult, ALU.add, [B_chg], [B_chg])
                    ts(chg2[:], chg2[:], -float(2 ** 27), float(2 ** 27), ALU.mult, ALU.add, [B_chg2], [B_chg2])
                    ts(blk[:], blk[:], 128.0, pidx[:, 0:1], ALU.mult, ALU.add, [B_blk, B_pidx], [B_blk])
                    tt(idxf[:, 4, :], blk[:], chg[:], ALU.add, [B_blk, B_chg], [B_idxf])
                    ts(idxf[:, 0, :], idxf[:, 4, :], 2.0, None, ALU.mult, None, [B_idxf], [B_idxf])
                    ts(idxf[:, 1, :], idxf[:, 4, :], 2.0, 1.0, ALU.mult, ALU.add, [B_idxf], [B_idxf])
                    tt(idxf[:, 4, :], blk[:], chg2[:], ALU.add, [B_blk, B_chg2, B_idxf], [B_idxf])
                    ts(idxf[:, 2, :], idxf[:, 4, :], 2.0, None, ALU.mult, None, [B_idxf], [B_idxf])
                    ts(idxf[:, 3, :], idxf[:, 4, :], 2.0, 1.0, ALU.mult, ALU.add, [B_idxf], [B_idxf])
                    cp(idxW[:], idxf[:, 0:4, :], [B_idxf], [B_idxW])
                    for j in range(16):
                        mms = [(plg[:], onesb[:, 0:128], mskb[:, jj, :], jj == 0, False) for jj in range(j)]
                        mms.append((plg[:], ltri[:], mskb[:, j, :], j == 0, True))
                        pe(mms, [B_onesb, B_ltri, B_mskb], [B_plg])
                        tt(destf[:, j, :], plg[:], pstart[:], ALU.add, [B_plg, B_pstart], [B_destf])
                    for j in range(16):
                        for k in range(4):
                            ts(ohf[:], iota32[:], eidx[:, j, k:k + 1], None, ALU.is_equal, None, [B_iota32, B_eidx], [B_ohf])
                            tt(ohf[:], ohf[:], destf[:, j, :], ALU.mult, [B_ohf, B_destf], [B_ohf])
                            dve(lambda E, j=j, k=k: E.reduce_sum(out=dstf[:, 4 * j + k:4 * j + k + 1], in_=ohf[:], axis=AX.X), [B_ohf], [B_dstf])
                    cp(dsti[:], dstf[:], [B_dstf], [B_dsti])
                    B_XO = em.buf("XO")
                    for j in range(16):
                        for k in range(4):
                            col = dsti[:, 4 * j + k:4 * j + k + 1]
                            bsc = em.buf("sc")
                            em.dma("pool", lambda E, j=j, col=col: [E.indirect_dma_start(
                                out=Xs, out_offset=bass.IndirectOffsetOnAxis(ap=col, axis=0), in_=hrow[:, j, :], in_offset=None)],
                                reads=[B_hrow[j], B_dsti, B_Xz], writes=[bsc], sembuf=B_XO)
                    for nm, srct, bb in (("dsti", dsti, B_dsti), ("idxW", idxW, B_idxW)):
                        d = dbgout(nm, [128, srct.shape[1] * (srct.shape[2] if len(srct.shape) > 2 else 1)], I32)
                        if d is not None:
                            dma(d, srct[:] if len(srct.shape) == 2 else srct[:].rearrange("p a b -> p (a b)"), [bb], [YB], sembuf=bb)
                    d = dbgout("gate4", [128, 64])
                    if d is not None:
                        dma(d, gate4[:].rearrange("p a b -> p (a b)"), [B_gate4], [YB], sembuf=B_gate4)
                    em.barrier()
                    em.emit()
                with contextlib.ExitStack() as s5:
                    wb1s = [sbt(s5, "wb1_%d" % i, [128, 8, 2048], BF16) for i in range(2)]
                    B_wb1 = [em.bufs(2, "wb1p%d" % i) for i in range(2)]
                    wb2 = sbt(s5, "wb2", [128, 9, 1024], BF16)
                    b1all = sbt(s5, "b1all", [32, 2048], BF16)
                    b2all = sbt(s5, "b2all", [32, 1024], BF16)
                    xbk = [sbt(s5, "xbk%d" % i, [128, 1024], BF16) for i in range(2)]
                    xT = [sbt(s5, "xT%d" % i, [128, 8, 128], BF16) for i in range(2)]
                    ohT = [sbt(s5, "ohT%d" % i, [32, 128], BF16) for i in range(2)]
                    glu = sbt(s5, "glu", [128, 1024])
                    lin = sbt(s5, "lin", [128, 1024])
                    sig = sbt(s5, "sig", [128, 1024], BF16)
                    ab = sbt(s5, "ab", [128, 1024], BF16)
                    aT = sbt(s5, "aT", [128, 8, 128], BF16)
                    yb = [sbt(s5, "yb%d" % i, [128, 1024]) for i in range(2)]
                    TX = pst(s5, "TX", [128, 8, 128], BF16)
                    TA = pst(s5, "TA", [128, 8, 128], BF16)
                    H = pst(s5, "H", [128, 2048])
                    Y = pst(s5, "Y", [128, 1024])
                    Ybf = Y.bitcast(BF16)
                    B_wb1a, B_wb1b, B_wb2, B_b1all, B_b2all, B_glu, B_lin, B_sig, B_ab, B_aT, B_TX, B_TA, B_H, B_Y, B_Ys = em.bufs(15, "s5")
                    B_xbk = em.bufs(2, "xbk"); B_obk = em.bufs(2, "obk"); B_xT = em.bufs(2, "xT"); B_ohT = em.bufs(2, "ohT"); B_yb = em.bufs(2, "yb")
                    bcreg = s5.enter_context(nc.gpsimd.register("bcreg"))
                    em.streams["pool"].append(lambda E: E.reg_mov(bcreg, 8191))
                    dma(b1all[:], b1, [], [B_b1all], q="pool")
                    dma(b2all[:], b2, [], [B_b2all], q="pool")
                    ab2 = [ab, sbt(s5, "ab_b", [128, 1024], BF16)]
                    B_ab2 = [B_ab, em.buf("ab_b")]

                    def loads(i):
                        p = i % 2
                        dma(xbk[p][:], Xs[i * 128:(i + 1) * 128, :], [], [B_xbk[p]])

                    def gathers(i):
                        for hh in range(2):
                            em.dma("pool", lambda E, i=i, hh=hh: [E.indirect_dma_start(
                                out=wb1s[i % 2][:, 4 * hh:4 * hh + 4, :].rearrange("p c f -> p (c f)"), out_offset=None, in_=W1p,
                                in_offset=bass.IndirectOffsetOnAxis(ap=idxW[:, 2 + hh, i:i + 1], axis=0), bounds_check=bcreg, oob_is_err=False)],
                                reads=[B_idxW], writes=[B_wb1[i % 2][hh]])

                    def gathers2(i):
                        for hh in range(2):
                            em.dma("pool", lambda E, i=i, hh=hh: [E.indirect_dma_start(
                                out=wb2[:, 4 * hh:4 * hh + 4, :].rearrange("p c f -> p (c f)"), out_offset=None, in_=W2p,
                                in_offset=bass.IndirectOffsetOnAxis(ap=idxW[:, hh, i:i + 1], axis=0), bounds_check=bcreg, oob_is_err=False)],
                                reads=[B_idxW], writes=[B_wb2])

                    def stageA(i):
                        p = i % 2
                        if i + 1 < NB:
                            loads(i + 1)
                        pet([(TX[:, c, :], xbk[p][:, c * 128:(c + 1) * 128], identb[:]) for c in range(8)], [B_xbk[p], B_identb], [B_TX])
                        cp(xT[p][:], TX[:], [B_TX], [B_xT[p]])
                        cp(ohT[p][:], ohTall[:, i:i + 1].to_broadcast([32, 128]), [B_ohTall], [B_ohT[p]])
                        mms = []
                        for n in range(4):
                            for c in range(8):
                                mms.append((H[:, n * 512:(n + 1) * 512], xT[p][:, c, :], wb1s[p][:, c, n * 512:(n + 1) * 512], c == 0, False))
                            mms.append((H[:, n * 512:(n + 1) * 512], ohT[p][:], b1all[:, n * 512:(n + 1) * 512], False, True))
                        pe(mms, [B_xT[p], B_ohT[p], B_wb1[p][0], B_wb1[p][1], B_b1all], [B_H])
                        if i + 2 < NB:
                            gathers(i + 2)
                        ts(glu[:], H[:, 0:2048:2], 7.0, None, ALU.min, None, [B_H], [B_glu])
                        ts(lin[:], H[:, 1:2048:2], -7.0, 7.0, ALU.max, ALU.min, [B_H], [B_lin])
                        act(sig[:], glu[:], AF.Sigmoid, [B_glu], [B_sig], scale=1.702)
                        stt(lin[:], lin[:], 1.0, glu[:], ALU.add, ALU.mult, [B_lin, B_glu], [B_lin])
                        tt(ab2[p][:], lin[:], sig[:], ALU.mult, [B_lin, B_sig], [B_ab2[p]])

                    def stageB(i):
                        p = i % 2
                        pet([(TA[:, c, :], ab2[p][:, c * 128:(c + 1) * 128], identb[:]) for c in range(8)], [B_ab2[p], B_identb], [B_TA])
                        act(aT[:], TA[:], AF.Copy, [B_TA], [B_aT])
                        mms = []
                        for n in range(2):
                            for c in range(8):
                                mms.append((Y[:, n * 512:(n + 1) * 512], aT[:, c, :], wb2[:, c, n * 512:(n + 1) * 512], c == 0, False))
                            mms.append((Y[:, n * 512:(n + 1) * 512], ohT[p][:], b2all[:, n * 512:(n + 1) * 512], False, True))
                        pe(mms, [B_aT, B_ohT[p], B_wb2, B_b2all], [B_Y])
                        if i + 1 < NB:
                            gathers2(i + 1)
                        act(yb[p][:], Y[:], AF.Copy, [B_Y], [B_yb[p]])
                        dma(Ys[i * 128:(i + 1) * 128, :], yb[p][:], [B_yb[p]], [B_Ys], sembuf=B_yb[p])

                    loads(0)
                    gathers(0)
                    gathers(1)
                    gathers2(0)
                    stageA(0)
                    for i in range(NB):
                        if i + 1 < NB:
                            stageA(i + 1)
                        stageB(i)
                    em.barrier()
                    em.emit()
                with contextlib.ExitStack() as s6:
                    x1r = [sbt(s6, "x1r%d" % i, [128, 1024]) for i in range(2)]
                    ot = [sbt(s6, "ot%d" % i, [128, 1024]) for i in range(2)]
                    yk = [sbt(s6, "yk%d" % i, [128, 4, 1024]) for i in range(2)]
                    ft = sbt(s6, "ft", [128, 1024])
                    junk6 = sbt(s6, "junk6", [128, 1024], BF16)
                    s6s = sbt(s6, "s6s", [128, 4])
                    B_x1r = em.bufs(2, "x1r"); B_ot = em.bufs(2, "ot"); B_yk = em.bufs(2, "yk")
                    B_ft, B_junk6, B_s6s = em.bufs(3, "s6")
                    for j in range(16):
                        i = j % 2
                        dma(x1r[i][:], x1s[j * 128:(j + 1) * 128, :], [B_x1s[j]], [B_x1r[i]])
                        for k in range(4):
                            em.dma("pool", lambda E, i=i, j=j, k=k: [E.indirect_dma_start(
                                out=yk[i][:, k, :], out_offset=None, in_=Ys,
                                in_offset=bass.IndirectOffsetOnAxis(ap=dsti[:, 4 * j + k:4 * j + k + 1], axis=0))],
                                reads=[B_dsti], writes=[B_yk[i]])
                        ts(ft[:], yk[i][:, 0, :], gate4[:, j, 0:1], None, ALU.mult, None, [B_yk[i], B_gate4], [B_ft])
                        for k in range(1, 4):
                            stt(ft[:], yk[i][:, k, :], gate4[:, j, k:k + 1], ft[:], ALU.mult, ALU.add, [B_yk[i], B_gate4, B_ft], [B_ft])
                        act(junk6[:], ft[:], AF.Square, [B_ft], [B_junk6, B_s6s], accum_out=s6s[:, 0:1])
                        ts(s6s[:, 1:2], s6s[:, 0:1], 1.0 / 1024, 1e-6, ALU.mult, ALU.add, [B_s6s], [B_s6s])
                        act(s6s[:, 2:3], s6s[:, 1:2], AF.Sqrt, [B_s6s], [B_s6s])
                        dve(lambda E: E.reciprocal(out=s6s[:, 3:4], in_=s6s[:, 2:3]), [B_s6s], [B_s6s])
                        stt(ot[i][:], ft[:], s6s[:, 3:4], rows[:, 3, :], ALU.mult, ALU.mult, [B_ft, B_s6s, B_rows], [B_ot[i]])
                        tt(ot[i][:], ot[i][:], x1r[i][:], ALU.add, [B_ot[i], B_x1r[i]], [B_ot[i]])
                        dma(out[j * 128:(j + 1) * 128, :], ot[i][:], [B_ot[i]], [YB], sembuf=B_ot[i])
                    em.barrier()
                    em.emit()
        else:
            mz = sbt(top, "mz", [128, 1024])
            B_mz = em.buf("mz")
            mset(mz[:], 0.0, [B_mz])
            for j in range(16):
                dma(out[j * 128:(j + 1) * 128, :], mz[:], [B_mz], [YB], sembuf=B_mz)
            for nm, src in (("Qs", Qs), ("Ks", Ks), ("NQs", NQs), ("NKs", NKs), ("Vs", Vs), ("NVs", NVs)):
                d = dbgout(nm, src.shape, BF16)
                if d is not None:
                    dma(d, src, [], [YB], sembuf=YB)
            em.barrier()
            em.emit()
    return nc, dbg


SLOPES = [2.0 ** (-2 * (h + 1)) for h in range(4)]
_CACHE = {}


def _bf(a):
    return np.ascontiguousarray(a.astype(ml_dtypes.bfloat16))


def _nat_tables(rpb, qr):
    R0 = 32 * qr
    def table(r0):
        t = np.full((128, 8, 7, 128), -30000.0, np.float32)
        qrow = np.repeat(np.array([r0, r0 + 1]), 64)
        qcol = np.tile(np.arange(64), 2)
        qstart = np.clip(qrow - 4, 0, 120)
        qcs = np.clip(qcol - 8, 0, 48)
        for o in range(7):
            krow = np.repeat(np.array([r0 - 6 + 2 * o, r0 - 5 + 2 * o]), 64)
            kcol = np.tile(np.arange(64), 2)
            valid = ((krow[:, None] >= qstart[None, :]) & (krow[:, None] < qstart[None, :] + 8)
                     & (krow[:, None] >= 0) & (krow[:, None] < 128)
                     & (kcol[:, None] >= qcs[None, :]) & (kcol[:, None] < qcs[None, :] + 16))
            dr = np.clip(krow[:, None] - qrow[None, :] + 7, 0, 14)
            dc = np.clip(kcol[:, None] - qcol[None, :], -15, 15) + 15
            for h in range(8):
                vals = rpb[h][dr, dc]
                t[:, h, o, :] = np.where(valid, vals, -30000.0)
        return t
    slots = [table(R0 + 2 * 6)] + [table(R0 + 2 * j) for j in (0, 1, 14, 15)]
    return _bf(np.stack(slots).reshape(5, 128, 8 * 7 * 128))


def _prep(inputs):
    f32 = np.float32
    x = np.asarray(inputs["x"], f32)
    c = np.asarray(inputs["c"], f32)
    shared = dict(
        w_ada=np.ascontiguousarray(inputs["w_ada"][0], f32), b_ada=np.ascontiguousarray(inputs["b_ada"], f32).reshape(1, 6144),
        g_pre_mix=np.asarray(inputs["g_pre_mix"], f32).reshape(1, 1024), g_post_mix=np.asarray(inputs["g_post_mix"], f32).reshape(1, 1024),
        g_pre_ffn=np.asarray(inputs["g_pre_ffn"], f32).reshape(1, 1024), g_post_ffn=np.asarray(inputs["g_post_ffn"], f32).reshape(1, 1024),
        w_in=np.ascontiguousarray(inputs["w_in"][0], f32), w_out=np.ascontiguousarray(inputs["w_out"][0], f32),
        lamv=np.concatenate([np.asarray(inputs[k], f32).reshape(-1) for k in ("lam_q1", "lam_k1", "lam_q2", "lam_k2")]).reshape(1, 256),
        g_subln=np.asarray(inputs["g_subln"], f32).reshape(1, 128),
        w_router=np.ascontiguousarray(inputs["w_router"][0], f32), b_router=np.asarray(inputs["b_router"], f32).reshape(1, 32),
        identb=_bf(np.eye(128, dtype=f32)), identf=np.eye(128, dtype=f32),
        ltri=_bf(np.triu(np.ones((128, 128), f32), 1)),
        iota32=np.tile(np.arange(32, dtype=f32), (128, 1)),
        thr16=np.tile((128.0 * np.arange(16, dtype=f32))[None, None, :], (128, 32, 1)).reshape(128, 512),
        iota96=np.tile(np.arange(NB, dtype=f32), (128, 1)),
        pidx=np.arange(128, dtype=f32).reshape(128, 1),
    )
    if os.environ.get("KSTOP", "") not in ("1", "2"):
        shared.update(
            W1p=np.ascontiguousarray(np.asarray(inputs["w1"][0], f32).reshape(32, 8, 128, 2048).transpose(0, 2, 1, 3)).reshape(8192, 8192),
            W2p=np.ascontiguousarray(np.asarray(inputs["w2"][0], f32).reshape(32, 8, 128, 1024).transpose(0, 2, 1, 3)).reshape(8192, 4096),
            b1=np.ascontiguousarray(inputs["b1"][0], f32), b2=np.ascontiguousarray(inputs["b2"][0], f32))
    kl = np.arange(128)
    bd = np.stack([-SLOPES[h] * np.abs(kl[:, None] - kl[None, :]) for h in range(4)], axis=1)
    shared["bdiag"] = _bf(bd.reshape(128, 512).astype(f32))
    rpb = np.asarray(inputs["nat_rpb"], f32)[0]
    in_maps = []
    for core in range(8):
        b, qr = core // 4, core % 4
        own = np.arange(qr * 2048, (qr + 1) * 2048)
        rest = np.concatenate([np.arange(0, qr * 2048), np.arange((qr + 1) * 2048, 8192)])
        perm = np.concatenate([own, rest])
        R0 = 32 * qr
        tok0 = (R0 - 6) * 64
        xw = np.zeros((2816, 1024), f32)
        lo, hi = max(tok0, 0), min(tok0 + 2816, 8192)
        xw[lo - tok0:hi - tok0] = x[b, lo:hi]
        ql = np.arange(2048)
        q_lo = (ql % 128).astype(f32)
        qt_abs = (qr * 16 + ql // 128).astype(f32)
        qa = np.zeros((4, 3, 2, 2048), f32)
        for h in range(4):
            qa[h, 0, 0] = -SLOPES[h] * q_lo
            qa[h, 0, 1] = -SLOPES[h] * 128.0 * qt_abs
            qa[h, 1] = -qa[h, 0]
        kabs = perm.astype(f32)
        ktile_abs = perm[::128] // 128
        sig = np.ones(64, f32)
        sig[16:] = np.where(ktile_abs[16:] < qr * 16, 1.0, -1.0)
        ka = np.ones((2, 8192), f32) * np.repeat(sig, 128)[None, :]
        kt_tab = np.zeros((128, 4, 2, 64), f32)
        kpos = kabs.reshape(64, 128).T
        for h in range(4):
            kt_tab[:, h, 0, :] = SLOPES[h] * kpos * sig[None, :]
            kt_tab[:, h, 1, :] = -SLOPES[h] * kpos
        m = dict(shared)
        m.update(
            xb=np.ascontiguousarray(x[b][perm]), xq=np.ascontiguousarray(x[b, own]), xw=xw,
            cT=np.ascontiguousarray(c[b].reshape(8, 128).T),
            natb=_nat_tables(rpb, qr), qaug=_bf(qa), kaug=_bf(ka), ktab=kt_tab.reshape(128, 512),
        )
        in_maps.append(m)
    return in_maps


def kernel(**inputs):
    debug = tuple(os.environ.get("KDEBUG", "").split(",")) if os.environ.get("KDEBUG") else ()
    key = (debug, os.environ.get("KSTOP", ""))
    if key not in _CACHE:
        _CACHE[key] = build_program(debug)
    nc, dbg = _CACHE[key]
    in_maps = _prep(inputs)
    res = run_bass_kernel_spmd(nc, in_maps, core_ids=list(range(8)))
    outs = [np.asarray(r["out"], np.float32) for r in res.results]
    full = np.stack([np.concatenate(outs[0:4], axis=0), np.concatenate(outs[4:8], axis=0)], axis=0)
    if debug:
        kernel.last_debug = [{k: np.asarray(r["dbg_" + k]) for k in dbg} for r in res.results]
    return full
```

```python
import contextlib
import os
import numpy as np
import ml_dtypes
import concourse.bass as bass
import concourse.mybir as mybir
from concourse.bass_utils import run_bass_kernel_spmd

F32 = mybir.dt.float32
BF16 = mybir.dt.bfloat16
I32 = mybir.dt.int32
U32 = mybir.dt.uint32
AF = mybir.ActivationFunctionType
ALU = mybir.AluOpType
AX = mybir.AxisListType

NB = 96
BIG = float(2 ** 30)


class Buf:
    __slots__ = ("name", "w", "rs", "dsem", "dcnt")

    def __init__(self, name):
        self.name = name
        self.w = []
        self.rs = []
        self.dsem = None
        self.dcnt = 0


class Em:
    ENG = ("pe", "act", "dve", "pool", "sp")

    def __init__(self, nc, stack):
        self.nc = nc
        self.stack = stack
        self.streams = {e: [] for e in self.ENG}
        self.cnt = {e: 0 for e in self.ENG}
        self.esem = {e: stack.enter_context(nc.semaphore("sem_" + e)) for e in self.ENG}
        self.waited = {e: {} for e in self.ENG}
        self.nbuf = 0
        self.dbufs = []

    def buf(self, name=None):
        self.nbuf += 1
        return Buf("%s_%d" % (name or "b", self.nbuf))

    def bufs(self, n, name=None):
        return [self.buf(name) for _ in range(n)]

    def _dsem(self, b):
        if b.dsem is None:
            b.dsem = self.stack.enter_context(self.nc.semaphore("d_" + b.name))
            self.dbufs.append(b)
        return b.dsem

    def _deps(self, eng, reads, writes):
        toks = {}

        def add(t):
            key, val, h = t
            if eng == "pe" and key == "pe":
                return
            if key not in toks or toks[key][1] < val:
                toks[key] = t
        for b in reads:
            for t in b.w:
                add(t)
        for b in writes:
            for t in b.w:
                add(t)
            for t in b.rs:
                add(t)
        return self._filter(eng, toks.values())

    def _filter(self, eng, toks):
        out = []
        wd = self.waited[eng]
        for key, val, h in toks:
            if wd.get(key, 0) >= val:
                continue
            wd[key] = val
            out.append((h, val))
        return out

    def _update(self, tok, reads, writes):
        for b in reads:
            b.rs.append(tok)
        for b in writes:
            b.w = [tok]
            b.rs = []

    def op(self, eng, fn, reads=(), writes=()):
        waits = self._deps(eng, reads, writes)
        self.cnt[eng] += 1
        sem = self.esem[eng]
        tok = (eng, self.cnt[eng], sem)

        def run(E, fn=fn, waits=waits, sem=sem):
            for h, v in waits:
                E.wait_ge(h, v)
            fn(E).then_inc(sem, 1)
        self.streams[eng].append(run)
        self._update(tok, reads, writes)

    def dma(self, q, fn, reads=(), writes=(), n=1, sembuf=None):
        waits = self._deps(q, reads, writes)
        sb = sembuf if sembuf is not None else (writes[0] if writes else reads[0])
        sem = self._dsem(sb)
        sb.dcnt += 16 * n
        tok = ("d_" + sb.name, sb.dcnt, sem)

        def run(E, fn=fn, waits=waits, sem=sem, n=n):
            for h, v in waits:
                E.wait_ge(h, v)
            lst = fn(E)
            assert len(lst) == n
            for ins in lst:
                ins.then_inc(sem, 16)
        self.streams[q].append(run)
        self._update(tok, reads, writes)

    def barrier(self):
        toks = [(e, self.cnt[e], self.esem[e]) for e in self.ENG if self.cnt[e] > 0]
        toks += [("d_" + b.name, b.dcnt, b.dsem) for b in self.dbufs]
        for eng in self.ENG:
            waits = self._filter(eng, [t for t in toks if t[0] != eng])

            def run(E, waits=waits):
                for h, v in waits:
                    E.wait_ge(h, v)
            self.streams[eng].append(run)

    def emit(self):
        nc = self.nc
        st = self.streams
        with nc.Block() as block:
            @block.sync
            def _(E):
                for f in st["sp"]:
                    f(E)

            @block.tensor
            def _(E):
                for f in st["pe"]:
                    f(E)

            @block.scalar
            def _(E):
                for f in st["act"]:
                    f(E)

            @block.vector
            def _(E):
                for f in st["dve"]:
                    f(E)

            @block.gpsimd
            def _(E):
                for f in st["pool"]:
                    f(E)
        self.streams = {e: [] for e in self.ENG}


def build_program(debug=()):
    nc = bass.Bass("TRN2", target_bir_lowering=False)

    def din(name, shape, dt=F32):
        return nc.dram_tensor(name, list(shape), dt, kind="ExternalInput").ap()

    def dscr(name, shape, dt):
        return nc.dram_tensor(name, list(shape), dt, kind="Internal").ap()

    xb = din("xb", [8192, 1024])
    xq = din("xq", [2048, 1024])
    xw = din("xw", [2816, 1024])
    cT = din("cT", [128, 8])
    w_ada = din("w_ada", [1024, 6144])
    b_ada = din("b_ada", [1, 6144])
    g_pre_mix = din("g_pre_mix", [1, 1024])
    g_post_mix = din("g_post_mix", [1, 1024])
    g_pre_ffn = din("g_pre_ffn", [1, 1024])
    g_post_ffn = din("g_post_ffn", [1, 1024])
    w_in = din("w_in", [1024, 3072])
    w_out = din("w_out", [1024, 1024])
    lamv = din("lamv", [1, 256])
    g_subln = din("g_subln", [1, 128])
    natb = din("natb", [5, 128, 8 * 7 * 128], BF16)
    w_router = din("w_router", [1024, 32])
    b_router = din("b_router", [1, 32])
    if os.environ.get("KSTOP", "") not in ("1", "2"):
        W1p = din("W1p", [8192, 8192])
        W2p = din("W2p", [8192, 4096])
        b1 = din("b1", [32, 2048])
        b2 = din("b2", [32, 1024])
    qaug = din("qaug", [4, 3, 2, 2048], BF16)
    kaug = din("kaug", [2, 8192], BF16)
    ktab = din("ktab", [128, 4 * 2 * 64])
    bdiag = din("bdiag", [128, 4 * 128], BF16)
    identb_d = din("identb", [128, 128], BF16)
    identf_d = din("identf", [128, 128])
    ltri_d = din("ltri", [128, 128], BF16)
    iota32_d = din("iota32", [128, 32])
    thr16_d = din("thr16", [128, 512])
    iota96_d = din("iota96", [128, NB])
    pidx_d = din("pidx", [128, 1])
    out = nc.dram_tensor("out", [2048, 1024], F32, kind="ExternalOutput").ap()

    Qs = dscr("Qs", [4, 128, 2048], BF16)
    Ks = dscr("Ks", [4, 128, 8192], BF16)
    Vs = dscr("Vs", [4, 128, 64, 130], BF16)
    NQs = dscr("NQs", [4, 128, 2048], BF16)
    NKs = dscr("NKs", [4, 128, 2816], BF16)
    NVs = dscr("NVs", [128, 22, 8 * 66], BF16)
    x1s = dscr("x1s", [2048, 1024], F32)
    Xs = dscr("Xs", [NB * 128, 1024], BF16)
    Os = dscr("Os", [NB * 128, 32], BF16)
    Ys = dscr("Ys", [NB * 128, 1024], F32)

    dbg = {}

    def dbgout(name, shape, dt=F32):
        if name in debug:
            dbg[name] = nc.dram_tensor("dbg_" + name, list(shape), dt, kind="ExternalOutput").ap()
            return dbg[name]
        return None

    with contextlib.ExitStack() as top:
        em = Em(nc, top)
        YB = em.buf("yout")

        def sbt(st, name, shape, dt=F32):
            return st.enter_context(nc.sbuf_tensor(name, list(shape), dt))

        def pst(st, name, shape, dt=F32):
            return st.enter_context(nc.psum_tensor(name, list(shape), dt))

        def dma(out_, in_, reads, writes, q="sp", sembuf=None):
            em.dma(q, lambda E: [E.dma_start(out=out_, in_=in_)], reads=reads, writes=writes, sembuf=sembuf)

        def act(out_, in_, func, reads, writes, **kw):
            em.op("act", lambda E: E.activation(out=out_, in_=in_, func=func, **kw), reads, writes)

        def pe(mms, reads, writes):
            def fn(E):
                ins = None
                for (o, l, r, s0, s1) in mms:
                    ins = E.matmul(o, lhsT=l, rhs=r, start=s0, stop=s1)
                return ins
            em.op("pe", fn, reads, writes)

        def pet(trs, reads, writes):
            def fn(E):
                ins = None
                for (o, i, idn) in trs:
                    ins = E.transpose(o, i, idn)
                return ins
            em.op("pe", fn, reads, writes)

        def dve(f, reads, writes, eng="dve"):
            em.op(eng, f, reads, writes)

        def ts(out_, in0, s1, s2, op0, op1=None, reads=(), writes=(), eng="dve"):
            if op1 is None:
                dve(lambda E: E.tensor_scalar(out=out_, in0=in0, scalar1=s1, scalar2=None, op0=op0), reads, writes, eng)
            else:
                dve(lambda E: E.tensor_scalar(out=out_, in0=in0, scalar1=s1, scalar2=s2, op0=op0, op1=op1), reads, writes, eng)

        def tt(out_, in0, in1, op, reads, writes, eng="dve"):
            dve(lambda E: E.tensor_tensor(out=out_, in0=in0, in1=in1, op=op), reads, writes, eng)

        def stt(out_, in0, scalar, in1, op0, op1, reads, writes):
            dve(lambda E: E.scalar_tensor_tensor(out=out_, in0=in0, scalar=scalar, in1=in1, op0=op0, op1=op1), reads, writes)

        def cp(out_, in_, reads, writes, eng="dve"):
            dve(lambda E: E.tensor_copy(out=out_, in_=in_), reads, writes, eng)

        def mset(ap, v, writes, eng="dve"):
            dve(lambda E: E.memset(ap, v), (), writes, eng)

        def dump(name, dst_shape_src):
            pass

        identb = sbt(top, "identb_s", [128, 128], BF16)
        identf = sbt(top, "identf_s", [128, 128])
        onesb = sbt(top, "onesb", [128, 512], BF16)
        onesf = sbt(top, "onesf", [128, 128])
        rows = sbt(top, "rows", [128, 4, 1024])
        o_all = sbt(top, "o_all", [128, 16, 1024], BF16)
        stat = sbt(top, "stat", [1, 16])
        negM = sbt(top, "negM", [128, 8])
        neglam = sbt(top, "neglam", [128, 1])
        gsub = sbt(top, "gsub", [128, 128])
        B_identb, B_identf, B_onesb, B_onesf, B_rows, B_stat, B_negM, B_neglam, B_gsub = em.bufs(9, "const")
        B_oall = em.bufs(16, "oall")
        dma(identb[:], identb_d, [], [B_identb])
        dma(identf[:], identf_d, [], [B_identf])
        mset(onesb[:], 1.0, [B_onesb])
        mset(onesf[:], 1.0, [B_onesf])
        mset(stat[:], 0.0, [B_stat])
        dma(gsub[:], g_subln.to_broadcast([128, 128]), [], [B_gsub])

        with contextlib.ExitStack() as st01:
            Wall = sbt(st01, "Wall", [128, 8, 3072], BF16)
            biasrow = sbt(st01, "biasrow", [1, 3072], BF16)
            B_Wall = em.bufs(8, "Wall")
            B_biasrow = em.buf("biasrow")
            with contextlib.ExitStack() as st:
                sil = sbt(st, "sil", [128, 8])
                silrep = sbt(st, "silrep", [128, 8, 128], BF16)
                wada = [sbt(st, "wada%d" % i, [128, 8, 512], BF16) for i in range(2)]
                bada = sbt(st, "bada", [1, 6144], BF16)
                modrow = sbt(st, "modrow", [128, 6144])
                grow = sbt(st, "grow", [128, 4, 1024])
                s1row = sbt(st, "s1row", [128, 1024])
                tmpd = sbt(st, "tmpd", [128, 128])
                s1T = sbt(st, "s1T", [128, 8])
                sh1T = sbt(st, "sh1T", [128, 8])
                wst = [sbt(st, "wst%d" % i, [128, 3072]) for i in range(2)]
                lam_t = sbt(st, "lam_t", [1, 256])
                lam_s = sbt(st, "lam_s", [1, 8])
                pmod = [pst(st, "pmod%d" % i, [128, 512]) for i in range(2)]
                pbias = pst(st, "pbias", [1, 3072])
                B_sil, B_silrep, B_bada, B_grow, B_s1row, B_tmpd, B_s1T, B_sh1T, B_lamt, B_lams, B_pbias, B_plam = em.bufs(12, "p0")
                B_wada = em.bufs(2, "wada")
                B_modrow = em.bufs(12, "modrow")
                B_wst = em.bufs(2, "wst")
                B_pmod = em.bufs(2, "pmod")

                dma(sil[:], cT, [], [B_sil])
                act(sil[:], sil[:], AF.Silu, [B_sil], [B_sil])
                for c in range(8):
                    cp(silrep[:, c, :], sil[:, c:c + 1].to_broadcast([128, 128]), [B_sil], [B_silrep])
                dma(bada[:], b_ada, [], [B_bada], q="pool")
                for i, g in enumerate([g_pre_mix, g_post_mix, g_pre_ffn, g_post_ffn]):
                    dma(grow[:, i, :], g.to_broadcast([128, 1024]), [], [B_grow])
                for j in range(12):
                    wb = wada[j % 2]
                    dma(wb[:], w_ada[:, j * 512:(j + 1) * 512].rearrange("(c p) f -> p c f", p=128), [], [B_wada[j % 2]], q="pool")
                    mms = [(pmod[j % 2][:], silrep[:, c, :], wb[:, c, :], c == 0, False) for c in range(8)]
                    mms.append((pmod[j % 2][:], onesb[0:1, 0:128], bada[0:1, j * 512:(j + 1) * 512], False, True))
                    pe(mms, [B_silrep, B_wada[j % 2], B_bada, B_onesb], [B_pmod[j % 2]])
                    cp(modrow[:, j * 512:(j + 1) * 512], pmod[j % 2][:], [B_pmod[j % 2]], [B_modrow[j]])
                MR = lambda i: [B_modrow[2 * i], B_modrow[2 * i + 1]]
                m = lambda i: modrow[:, i * 1024:(i + 1) * 1024]
                stt(s1row[:], m(1), 1.0, grow[:, 0, :], ALU.add, ALU.mult, MR(1) + [B_grow], [B_s1row])
                stt(rows[:, 0, :], m(4), 1.0, grow[:, 2, :], ALU.add, ALU.mult, MR(4) + [B_grow], [B_rows])
                cp(rows[:, 1, :], m(3), MR(3) + [B_rows], [B_rows])
                tt(rows[:, 2, :], m(2), grow[:, 1, :], ALU.mult, MR(2) + [B_grow, B_rows], [B_rows])
                tt(rows[:, 3, :], m(5), grow[:, 3, :], ALU.mult, MR(5) + [B_grow, B_rows], [B_rows])
                for c in range(8):
                    tt(tmpd[:], s1row[:, c * 128:(c + 1) * 128], identf[:], ALU.mult, [B_s1row, B_identf], [B_tmpd])
                    dve(lambda E, c=c: E.reduce_sum(out=s1T[:, c:c + 1], in_=tmpd[:], axis=AX.X), [B_tmpd], [B_s1T])
                    tt(tmpd[:], modrow[:, c * 128:(c + 1) * 128], identf[:], ALU.mult, MR(0) + [B_identf], [B_tmpd])
                    dve(lambda E, c=c: E.reduce_sum(out=sh1T[:, c:c + 1], in_=tmpd[:], axis=AX.X), [B_tmpd], [B_sh1T])
                for c in range(8):
                    wsb = wst[c % 2]
                    dma(wsb[:], w_in[c * 128:(c + 1) * 128, :], [], [B_wst[c % 2]])
                    mms = [(pbias[0:1, n * 512:(n + 1) * 512], sh1T[:, c:c + 1], wsb[:, n * 512:(n + 1) * 512], c == 0, c == 7) for n in range(6)]
                    pe(mms, [B_sh1T, B_wst[c % 2]], [B_pbias])
                    act(Wall[:, c, :], wsb[:], AF.Copy, [B_wst[c % 2], B_s1T], [B_Wall[c]], scale=s1T[:, c:c + 1])
                cp(biasrow[:], pbias[:], [B_pbias], [B_biasrow])
                dma(lam_t[:], lamv, [], [B_lamt])
                tt(lam_t[0:1, 0:64], lam_t[0:1, 0:64], lam_t[0:1, 64:128], ALU.mult, [B_lamt], [B_lamt])
                tt(lam_t[0:1, 128:192], lam_t[0:1, 128:192], lam_t[0:1, 192:256], ALU.mult, [B_lamt], [B_lamt])
                dve(lambda E: E.reduce_sum(out=lam_s[0:1, 0:1], in_=lam_t[0:1, 0:64], axis=AX.X), [B_lamt], [B_lams])
                dve(lambda E: E.reduce_sum(out=lam_s[0:1, 1:2], in_=lam_t[0:1, 128:192], axis=AX.X), [B_lamt], [B_lams])
                act(lam_s[0:1, 2:4], lam_s[0:1, 0:2], AF.Exp, [B_lams], [B_lams])
                stt(lam_s[0:1, 4:5], lam_s[0:1, 3:4], -0.2, lam_s[0:1, 2:3], ALU.add, ALU.subtract, [B_lams], [B_lams])
                pe([(pmod[0][:, 0:1], onesf[0:1, 0:128], lam_s[0:1, 4:5], True, True)], [B_onesf, B_lams], [B_pmod[0]])
                cp(neglam[:], pmod[0][:, 0:1], [B_pmod[0]], [B_neglam])
                d = dbgout("rows", [128, 4096])
                if d is not None:
                    dma(d, rows[:].rearrange("p a f -> p (a f)"), [B_rows], [YB], sembuf=B_rows)
                d = dbgout("neglam", [128, 1])
                if d is not None:
                    dma(d, neglam[:], [B_neglam], [YB], sembuf=B_neglam)
                em.barrier()
                em.emit()

            with contextlib.ExitStack() as st:
                xt = [sbt(st, "xt%d" % i, [128, 1024]) for i in range(2)]
                xn = [sbt(st, "xn%d" % i, [128, 1024], BF16) for i in range(2)]
                xnT = [sbt(st, "xnT%d" % i, [128, 8, 512], BF16) for i in range(2)]
                ssq = sbt(st, "ssq", [128, 4])
                junk = sbt(st, "junk", [128, 1024], BF16)
                ev = [sbt(st, "ev%d" % i, [128, 512], BF16) for i in range(3)]
                sq = [sbt(st, "sq%d" % i, [128, 512], BF16) for i in range(2)]
                vt = [sbt(st, "vt%d" % i, [128, 4, 130], BF16) for i in range(2)]
                nvt = [sbt(st, "nvt%d" % i, [128, 8, 66], BF16) for i in range(2)]
                mx = sbt(st, "mx", [1, 2])
                ptr = [pst(st, "ptr%d" % i, [128, 8, 128], BF16) for i in range(2)]
                pp = [pst(st, "pp%d" % i, [128, 512]) for i in range(3)]
                pn = pst(st, "pn", [1, 512])
                B_xt = em.bufs(2, "xt"); B_xn = em.bufs(2, "xn"); B_xnT = em.bufs(2, "xnT")
                B_ssq = em.buf("ssq"); B_junk = em.buf("junk"); B_ev = em.bufs(3, "ev"); B_sq = em.bufs(2, "sq")
                B_vt = em.bufs(2, "vt"); B_nvt = em.bufs(2, "nvt"); B_mx = em.buf("mx")
                B_ptr = em.bufs(2, "ptr"); B_pp = em.bufs(3, "pp"); B_pn = em.buf("pn")
                B_scr = em.buf("scr1")
                for i in range(2):
                    mset(vt[i][:, :, 128:129], 1.0, [B_vt[i]])
                    mset(vt[i][:, :, 129:130], 0.0, [B_vt[i]])
                    mset(nvt[i][:, :, 64:65], 1.0, [B_nvt[i]])
                    mset(nvt[i][:, :, 65:66], 0.0, [B_nvt[i]])
                cnt = {"tile": 0, "grp": 0, "pp": 0, "ev": 0, "sq": 0, "vt": 0, "nvt": 0}

                def norm_group(src, g):
                    gi = cnt["grp"] % 2
                    cnt["grp"] += 1
                    for t in range(4):
                        i = cnt["tile"] % 2
                        cnt["tile"] += 1
                        r0 = g * 512 + t * 128
                        dma(xt[i][:], src[r0:r0 + 128, :], [], [B_xt[i]])
                        act(junk[:], xt[i][:], AF.Square, [B_xt[i]], [B_junk, B_ssq], accum_out=ssq[:, 0:1])
                        ts(ssq[:, 1:2], ssq[:, 0:1], 1.0 / 1024, 1e-6, ALU.mult, ALU.add, [B_ssq], [B_ssq])
                        act(ssq[:, 2:3], ssq[:, 1:2], AF.Sqrt, [B_ssq], [B_ssq])
                        dve(lambda E: E.reciprocal(out=ssq[:, 3:4], in_=ssq[:, 2:3]), [B_ssq], [B_ssq])
                        act(xn[i][:], xt[i][:], AF.Copy, [B_xt[i], B_ssq], [B_xn[i]], scale=ssq[:, 3:4])
                        pet([(ptr[i][:, c, :], xn[i][:, c * 128:(c + 1) * 128], identb[:]) for c in range(8)],
                            [B_xn[i], B_identb], [B_ptr[i]])
                        cp(xnT[gi][:, :, t * 128:(t + 1) * 128], ptr[i][:], [B_ptr[i]], [B_xnT[gi]])
                    return gi

                def proj_T(gi, col0, scale, dst, stat_idx):
                    k = cnt["pp"] % 3; cnt["pp"] += 1
                    mms = [(pp[k][:], Wall[:, c, col0:col0 + 128], xnT[gi][:, c, :], c == 0, False) for c in range(8)]
                    mms.append((pp[k][:], biasrow[0:1, col0:col0 + 128], onesb[0:1, 0:512], False, True))
                    pe(mms, B_Wall + [B_xnT[gi], B_biasrow, B_onesb], [B_pp[k]])
                    e = cnt["ev"] % 3; cnt["ev"] += 1
                    act(ev[e][:], pp[k][:], AF.Copy, [B_pp[k]], [B_ev[e]], scale=scale)
                    dma(dst, ev[e][:], [B_ev[e]], [B_scr], sembuf=B_ev[e])
                    s = cnt["sq"] % 2; cnt["sq"] += 1
                    tt(sq[s][:], ev[e][:], ev[e][:], ALU.mult, [B_ev[e]], [B_sq[s]])
                    pe([(pn[:], onesb[:, 0:1], sq[s][:], True, True)], [B_onesb, B_sq[s]], [B_pn])
                    dve(lambda E: E.reduce_max(out=mx[0:1, 0:1], in_=pn[0:1, :], axis=AX.X), [B_pn], [B_mx])
                    tt(stat[0:1, stat_idx:stat_idx + 1], stat[0:1, stat_idx:stat_idx + 1], mx[0:1, 0:1], ALU.max, [B_mx, B_stat], [B_stat])

                def proj_tok(gi, t, col0):
                    k = cnt["pp"] % 3; cnt["pp"] += 1
                    mms = [(pp[k][:], xnT[gi][:, c, t * 128:(t + 1) * 128], Wall[:, c, col0:col0 + 512], c == 0, False) for c in range(8)]
                    mms.append((pp[k][:], onesb[0:1, 0:128], biasrow[0:1, col0:col0 + 512], False, True))
                    pe(mms, B_Wall + [B_xnT[gi], B_biasrow, B_onesb], [B_pp[k]])
                    return k

                def norm_part(src, g, ntl):
                    gi = cnt["grp"] % 2
                    cnt["grp"] += 1
                    for t in range(ntl):
                        i = cnt["tile"] % 2
                        cnt["tile"] += 1
                        r0 = g * 512 + t * 128
                        dma(xt[i][:], src[r0:r0 + 128, :], [], [B_xt[i]])
                        act(junk[:], xt[i][:], AF.Square, [B_xt[i]], [B_junk, B_ssq], accum_out=ssq[:, 0:1])
                        ts(ssq[:, 1:2], ssq[:, 0:1], 1.0 / 1024, 1e-6, ALU.mult, ALU.add, [B_ssq], [B_ssq])
                        act(ssq[:, 2:3], ssq[:, 1:2], AF.Sqrt, [B_ssq], [B_ssq])
                        dve(lambda E: E.reciprocal(out=ssq[:, 3:4], in_=ssq[:, 2:3]), [B_ssq], [B_ssq])
                        act(xn[i][:], xt[i][:], AF.Copy, [B_xt[i], B_ssq], [B_xn[i]], scale=ssq[:, 3:4])
                        pet([(ptr[i][:, c, :], xn[i][:, c * 128:(c + 1) * 128], identb[:]) for c in range(8)],
                            [B_xn[i], B_identb], [B_ptr[i]])
                        cp(xnT[gi][:, :, t * 128:(t + 1) * 128], ptr[i][:], [B_ptr[i]], [B_xnT[gi]])
                    return gi

                def projB(kind, g, gi):
                    if kind == "own":
                        for h in range(4):
                            proj_T(gi, h * 128, 0.125, Qs[h, :, g * 512:(g + 1) * 512], h)
                        for c4 in range(4):
                            proj_T(gi, 1536 + c4 * 128, 0.125, NQs[c4, :, g * 512:(g + 1) * 512], 8 + c4)
                    elif kind == "seq":
                        for h in range(4):
                            proj_T(gi, 512 + h * 128, 1.0, Ks[h, :, g * 512:(g + 1) * 512], 4 + h)
                        for t in range(4):
                            k = proj_tok(gi, t, 1024)
                            v = cnt["vt"] % 2; cnt["vt"] += 1
                            cp(vt[v][:, :, 0:128], pp[k][:].rearrange("p (h e) -> p h e", h=4), [B_pp[k]], [B_vt[v]])
                            dma(Vs[:, :, g * 4 + t, :].rearrange("h p e -> p h e"), vt[v][:], [B_vt[v]], [B_scr], sembuf=B_vt[v])
                    else:
                        ntl = 4 if g < 5 else 2
                        ncol = ntl * 128
                        for c4 in range(4):
                            k = cnt["pp"] % 3; cnt["pp"] += 1
                            col0 = 2048 + c4 * 128
                            mms = [(pp[k][:, 0:ncol], Wall[:, c, col0:col0 + 128], xnT[gi][:, c, 0:ncol], c == 0, False) for c in range(8)]
                            mms.append((pp[k][:, 0:ncol], biasrow[0:1, col0:col0 + 128], onesb[0:1, 0:ncol], False, True))
                            pe(mms, B_Wall + [B_xnT[gi], B_biasrow, B_onesb], [B_pp[k]])
                            e = cnt["ev"] % 3; cnt["ev"] += 1
                            act(ev[e][:, 0:ncol], pp[k][:, 0:ncol], AF.Copy, [B_pp[k]], [B_ev[e]])
                            dma(NKs[c4, :, g * 512:g * 512 + ncol], ev[e][:, 0:ncol], [B_ev[e]], [B_scr], sembuf=B_ev[e])
                            s_ = cnt["sq"] % 2; cnt["sq"] += 1
                            tt(sq[s_][:, 0:ncol], ev[e][:, 0:ncol], ev[e][:, 0:ncol], ALU.mult, [B_ev[e]], [B_sq[s_]])
                            pe([(pn[:, 0:ncol], onesb[:, 0:1], sq[s_][:, 0:ncol], True, True)], [B_onesb, B_sq[s_]], [B_pn])
                            dve(lambda E, ncol=ncol: E.reduce_max(out=mx[0:1, 0:1], in_=pn[0:1, 0:ncol], axis=AX.X), [B_pn], [B_mx])
                            tt(stat[0:1, 12 + c4:13 + c4], stat[0:1, 12 + c4:13 + c4], mx[0:1, 0:1], ALU.max, [B_mx, B_stat], [B_stat])
                        for t in range(ntl):
                            k = proj_tok(gi, t, 2560)
                            v = cnt["nvt"] % 2; cnt["nvt"] += 1
                            cp(nvt[v][:, :, 0:64], pp[k][:].rearrange("p (h e) -> p h e", h=8), [B_pp[k]], [B_nvt[v]])
                            dma(NVs[:, g * 4 + t, :], nvt[v][:].rearrange("p h e -> p (h e)"), [B_nvt[v]], [B_scr], sembuf=B_nvt[v])

                groups = [("own", g, xq, 4) for g in range(4)] + [("seq", g, xb, 4) for g in range(16)] \
                    + [("win", g, xw, 4 if g < 5 else 2) for g in range(6)]
                gis = [None] * len(groups)
                gis[0] = norm_part(groups[0][2], groups[0][1], groups[0][3])
                for n_, (kind, g, src, ntl) in enumerate(groups):
                    if n_ + 1 < len(groups):
                        kn, gn, sn, tn = groups[n_ + 1]
                        gis[n_ + 1] = norm_part(sn, gn, tn)
                    projB(kind, g, gis[n_])
                mm_ = sbt(st, "mm_", [1, 8])
                pM = pst(st, "pM", [128, 8])
                B_mm, B_pM = em.bufs(2, "mm")
                tt(mm_[0:1, 0:4], stat[0:1, 0:4], stat[0:1, 4:8], ALU.mult, [B_stat], [B_mm])
                tt(mm_[0:1, 4:8], stat[0:1, 8:12], stat[0:1, 12:16], ALU.mult, [B_stat, B_mm], [B_mm])
                act(mm_[:], mm_[:], AF.Sqrt, [B_mm], [B_mm])
                ts(mm_[:], mm_[:], -1.05, None, ALU.mult, None, [B_mm], [B_mm])
                pe([(pM[:], onesf[0:1, 0:128], mm_[0:1, :], True, True)], [B_onesf, B_mm], [B_pM])
                cp(negM[:], pM[:], [B_pM], [B_negM])
                d = dbgout("negM", [128, 8])
                if d is not None:
                    dma(d, negM[:], [B_negM], [YB], sembuf=B_negM)
                em.barrier()
                em.emit()
        STOP = os.environ.get("KSTOP", "")
        if STOP != "1":
            with contextlib.ExitStack() as st:
                KA = [sbt(st, "KA%d" % m, [66, 8192], BF16) for m in range(2)]
                QA = [[sbt(st, "QA%d_%d" % (m, v), [66, 2048], BF16) for v in range(3)] for m in range(2)]
                Vh = sbt(st, "Vh", [128, 64, 130], BF16)
                ktab_t = sbt(st, "ktab_s", [128, 4, 2, 64])
                kb = sbt(st, "kb", [128, 2, 64])
                bdg = sbt(st, "bdg_s", [128, 4, 128], BF16)
                PT = [sbt(st, "PT%d" % i, [128, 512], BF16) for i in range(3)]
                O1n = sbt(st, "O1n", [128, 4, 128])
                dtl = sbt(st, "dtl", [128, 128])
                sm = sbt(st, "sm", [128, 8])
                junk2 = sbt(st, "junk2", [128, 128])
                gs8 = sbt(st, "gs8", [128, 128])
                S = [pst(st, "S%d" % i, [128, 512]) for i in range(2)]
                O = [pst(st, "O%d" % i, [128, 512]) for i in range(4)]
                B_KA = em.bufs(2, "KA"); B_QA = em.bufs(2, "QA"); B_Vh = em.buf("Vh"); B_ktab = em.buf("ktab")
                B_kb = em.buf("kb"); B_bdg = em.buf("bdg"); B_PT = em.bufs(3, "PT"); B_O1n = em.buf("O1n")
                B_dtl = em.buf("dtl"); B_sm = em.buf("sm"); B_junk2 = em.buf("junk2"); B_gs8 = em.buf("gs8")
                B_S = em.bufs(2, "S"); B_O = em.bufs(4, "O")
                dma(ktab_t[:].rearrange("p a b c -> p (a b c)"), ktab, [], [B_ktab])
                dma(bdg[:].rearrange("p a b -> p (a b)"), bdiag, [], [B_bdg])
                ts(gs8[:], gsub[:], 0.8, None, ALU.mult, None, [B_gsub], [B_gs8])
                it = 0
                for h in range(4):
                    for m in range(2):
                        dma(KA[m][0:64, :], Ks[h, 64 * m:64 * m + 64, :], [], [B_KA[m]])
                        dma(KA[m][64:66, :], kaug, [], [B_KA[m]])
                        for v in range(3):
                            dma(QA[m][v][0:64, :], Qs[h, 64 * m:64 * m + 64, :], [], [B_QA[m]])
                            dma(QA[m][v][64:66, :], qaug[h, v], [], [B_QA[m]])
                    dma(Vh[:], Vs[h], [], [B_Vh])
                    ts(kb[:], ktab_t[:, h], negM[:, h:h + 1], None, ALU.add, None, [B_ktab, B_negM], [B_kb])
                    seq = [(qc, m, kt) for qc in range(4) for m in range(2) for kt in range(64)]

                    def segs_of(qc, kt):
                        if kt >= 16:
                            return [(0, 4, 0, kb[:, 0, kt:kt + 1])]
                        segs = []
                        for t in range(4):
                            qt = 4 * qc + t
                            if qt > kt:
                                cls = (0, kb[:, 0, kt:kt + 1])
                            elif qt == kt:
                                cls = (2, negM[:, h:h + 1])
                            else:
                                cls = (1, kb[:, 1, kt:kt + 1])
                            if segs and segs[-1][2] == cls[0]:
                                segs[-1] = (segs[-1][0], t + 1, cls[0], cls[1])
                            else:
                                segs.append((t, t + 1, cls[0], cls[1]))
                        return segs

                    def emit_S(n):
                        qc, m, kt = seq[n]
                        sb = (it + n) % 2
                        mms = []
                        for (t0, t1, v, col) in segs_of(qc, kt):
                            c0, c1 = t0 * 128, t1 * 128
                            q0 = qc * 512
                            mms.append((S[sb][:, c0:c1], KA[m][0:66, kt * 128:(kt + 1) * 128], QA[m][v][0:66, q0 + c0:q0 + c1], True, v != 2))
                            if v == 2:
                                mms.append((S[sb][:, c0:c1], identb[:], bdg[:, h, :], False, True))
                        pe(mms, [B_KA[m], B_QA[m], B_identb, B_bdg], [B_S[sb]])

                    def emit_rest(n):
                        qc, m, kt = seq[n]
                        sb = (it + n) % 2
                        pb = (it + n) % 3
                        for (t0, t1, v, col) in segs_of(qc, kt):
                            c0, c1 = t0 * 128, t1 * 128
                            act(PT[pb][:, c0:c1], S[sb][:, c0:c1], AF.Exp, [B_S[sb], B_kb, B_negM], [B_PT[pb]], bias=col, scale=1.0)
                        pe([(O[t][:, 0:130], PT[pb][:, t * 128:(t + 1) * 128], Vh[:, kt, :], kt == 0, kt == 63) for t in range(4)],
                           [B_PT[pb], B_Vh], B_O)
                        if kt != 63:
                            return
                        for t in range(4):
                            dve(lambda E, t=t: E.reciprocal(out=sm[:, 0:1], in_=O[t][:, 128:129]), [B_O[t]], [B_sm])
                            if m == 0:
                                ts(O1n[:, t, :], O[t][:, 0:128], sm[:, 0:1], None, ALU.mult, None, [B_O[t], B_sm], [B_O1n])
                            else:
                                ts(dtl[:], O[t][:, 0:128], sm[:, 0:1], None, ALU.mult, None, [B_O[t], B_sm], [B_dtl])
                                stt(dtl[:], dtl[:], neglam[:, 0:1], O1n[:, t, :], ALU.mult, ALU.add, [B_dtl, B_neglam, B_O1n], [B_dtl])
                                act(junk2[:], dtl[:], AF.Square, [B_dtl], [B_junk2, B_sm], accum_out=sm[:, 1:2])
                                ts(sm[:, 2:3], sm[:, 1:2], 1.0 / 128, 1e-6, ALU.mult, ALU.add, [B_sm], [B_sm])
                                act(sm[:, 3:4], sm[:, 2:3], AF.Sqrt, [B_sm], [B_sm])
                                dve(lambda E: E.reciprocal(out=sm[:, 4:5], in_=sm[:, 3:4]), [B_sm], [B_sm])
                                jt = 4 * qc + t
                                stt(o_all[:, jt, h * 128:(h + 1) * 128], dtl[:], sm[:, 4:5], gs8[:], ALU.mult, ALU.mult,
                                    [B_dtl, B_sm, B_gs8], [B_oall[jt]])

                    emit_S(0)
                    for n in range(len(seq)):
                        if n + 1 < len(seq):
                            emit_S(n + 1)
                        emit_rest(n)
                    it += len(seq)
                em.barrier()
                em.emit()

            with contextlib.ExitStack() as st:
                NQT = sbt(st, "NQT", [128, 4, 2048], BF16)
                NKT = sbt(st, "NKT", [128, 4, 2816], BF16)
                NV = sbt(st, "NV", [128, 22, 528], BF16)
                nbt = [sbt(st, "nbt%d" % i, [128, 8 * 7 * 128], BF16) for i in range(2)]
                PN = [sbt(st, "PN%d" % i, [128, 896], BF16) for i in range(2)]
                sn = sbt(st, "sn", [128, 2])
                SN = [pst(st, "SN%d" % i, [128, 1024]) for i in range(2)]
                NO = [pst(st, "NO%d" % i, [128, 512]) for i in range(2)]
                B_NQT, B_NKT, B_NV, B_sn = em.bufs(4, "nat")
                B_nbt = em.bufs(2, "nbt"); B_PN = em.bufs(2, "PN"); B_SN = em.bufs(2, "SN"); B_NO = em.bufs(2, "NO")
                dma(NQT[:], NQs.rearrange("c p t -> p c t"), [], [B_NQT])
                dma(NKT[:], NKs.rearrange("c p t -> p c t"), [], [B_NKT])
                dma(NV[:], NVs, [], [B_NV])
                items = [(j, h) for j in range(16) for h in range(8)]

                def nat_S(n):
                    j, h = items[n]
                    nb_ = nbt[j % 2]
                    if h == 0:
                        slot = {0: 1, 1: 2, 14: 3, 15: 4}.get(j, 0)
                        dma(nb_[:], natb[slot], [], [B_nbt[j % 2]])
                    c4, hp = h // 2, (h % 2) * 64
                    sb = n % 2
                    mms = []
                    for o in range(7):
                        oc = slice(o * 128, (o + 1) * 128)
                        mms.append((SN[sb][:, oc], NKT[hp:hp + 64, c4, (j + o) * 128:(j + o + 1) * 128],
                                    NQT[hp:hp + 64, c4, j * 128:(j + 1) * 128], True, False))
                        mms.append((SN[sb][:, oc], identb[:], nb_[:, (h * 7 + o) * 128:(h * 7 + o + 1) * 128], False, True))
                    pe(mms, [B_NKT, B_NQT, B_identb, B_nbt[j % 2]], [B_SN[sb]])

                def nat_rest(n):
                    j, h = items[n]
                    c4 = h // 2
                    sb = n % 2
                    act(PN[sb][:], SN[sb][:, 0:896], AF.Exp, [B_SN[sb], B_negM], [B_PN[sb]], bias=negM[:, 4 + c4:5 + c4], scale=1.0)
                    mms = [(NO[sb][:, 0:66], PN[sb][:, o * 128:(o + 1) * 128], NV[:, j + o, h * 66:h * 66 + 66], o == 0, o == 6) for o in range(7)]
                    pe(mms, [B_PN[sb], B_NV], [B_NO[sb]])
                    dve(lambda E, sb=sb: E.reciprocal(out=sn[:, 0:1], in_=NO[sb][:, 64:65]), [B_NO[sb]], [B_sn])
                    ts(o_all[:, j, 512 + h * 64:512 + (h + 1) * 64], NO[sb][:, 0:64], sn[:, 0:1], None, ALU.mult, None,
                       [B_NO[sb], B_sn], [B_oall[j]])

                nat_S(0)
                for n in range(len(items)):
                    if n + 1 < len(items):
                        nat_S(n + 1)
                    nat_rest(n)
                d = dbgout("o_all", [128, 16 * 1024], BF16)
                if d is not None:
                    dma(d, o_all[:].rearrange("p a f -> p (a f)"), B_oall, [YB], sembuf=B_oall[0])
                em.barrier()
                em.emit()

        if STOP not in ("1", "2"):
            with contextlib.ExitStack() as st:
                wr = sbt(st, "wr", [128, 8, 32])
                br = sbt(st, "br", [1, 32])
                mskb = sbt(st, "mskb", [128, 16, 32], BF16)
                gate4 = sbt(st, "gate4", [128, 16, 4])
                eidx = sbt(st, "eidx", [128, 16, 8])
                dsti = sbt(st, "dsti", [128, 64], I32)
                idxW = sbt(st, "idxW", [128, 4, NB], I32)
                ohTall = sbt(st, "ohTall", [32, NB])
                B_wr, B_br, B_mskb, B_gate4, B_eidx, B_dsti, B_idxW, B_ohTall = em.bufs(8, "p4")
                B_hrow = em.bufs(16, "hrow")
                B_x1s = em.bufs(16, "x1s")
                dma(wr[:], w_router.rearrange("(c p) f -> p c f", p=128), [], [B_wr])
                dma(br[:], b_router, [], [B_br])
                with contextlib.ExitStack() as s4:
                    Wout = sbt(s4, "Wout", [128, 8, 1024], BF16)
                    hrow = sbt(s4, "hrow", [128, 16, 1024], BF16)
                    zt = sbt(s4, "zt", [128, 2, 1024], BF16)
                    B_zt, B_Xz = em.bufs(2, "zx")
                    mset(zt[:], 0.0, [B_zt], eng="pool")
                    for cz in range(NB // 2):
                        dma(Xs[cz * 256:(cz + 1) * 256, :].rearrange("(r p) f -> p r f", p=128), zt[:], [B_zt], [B_Xz], sembuf=B_Xz)
                    B_Wout = em.buf("Wout")
                    dma(Wout[:], w_out.rearrange("(c p) f -> p c f", p=128), [], [B_Wout], q="pool")
                    ltri = sbt(s4, "ltri_s", [128, 128], BF16)
                    iota32 = sbt(s4, "iota32_s", [128, 32])
                    thr16 = sbt(s4, "thr16_s", [128, 32, 16])
                    iota96 = sbt(s4, "iota96_s", [128, NB])
                    pidx = sbt(s4, "pidx_s", [128, 1])
                    B_ltri, B_iota32, B_thr16, B_iota96, B_pidx = em.bufs(5, "cst")
                    dma(ltri[:], ltri_d, [], [B_ltri])
                    dma(iota32[:], iota32_d, [], [B_iota32])
                    dma(thr16[:].rearrange("p a b -> p (a b)"), thr16_d, [], [B_thr16])
                    dma(iota96[:], iota96_d, [], [B_iota96])
                    dma(pidx[:], pidx_d, [], [B_pidx])
                    def dbl(fn):
                        return [fn(0), fn(1)]
                    oT_ = dbl(lambda i: sbt(s4, "oT%d" % i, [128, 8, 128], BF16))
                    xt4_ = dbl(lambda i: sbt(s4, "xt4_%d" % i, [128, 1024]))
                    tmp4_ = dbl(lambda i: sbt(s4, "tmp4_%d" % i, [128, 1024]))
                    x1t_ = dbl(lambda i: sbt(s4, "x1t_%d" % i, [128, 1024]))
                    h2t_ = dbl(lambda i: sbt(s4, "h2t_%d" % i, [128, 1024]))
                    h2Tf_ = dbl(lambda i: sbt(s4, "h2Tf_%d" % i, [128, 8, 128]))
                    junk4_ = dbl(lambda i: sbt(s4, "junk4_%d" % i, [128, 1024], BF16))
                    s4s_ = dbl(lambda i: sbt(s4, "s4s_%d" % i, [128, 16]))
                    lg_ = dbl(lambda i: sbt(s4, "lg_%d" % i, [128, 32]))
                    v8_ = dbl(lambda i: sbt(s4, "v8_%d" % i, [128, 8]))
                    i8_ = dbl(lambda i: sbt(s4, "i8_%d" % i, [128, 8], U32))
                    msk_ = dbl(lambda i: sbt(s4, "msk_%d" % i, [128, 32]))
                    e4_ = dbl(lambda i: sbt(s4, "e4_%d" % i, [128, 4]))
                    pto_ = dbl(lambda i: pst(s4, "pto%d" % i, [128, 8, 128], BF16))
                    pmix = pst(s4, "pmix", [128, 1024])
                    ptf = pst(s4, "ptf", [128, 8, 128])
                    plg = pst(s4, "plg", [128, 32])
                    BB = {n: em.bufs(2, "s4" + n) for n in ["oT", "xt4", "tmp4", "x1t", "h2t", "h2Tf", "junk4", "s4s", "lg", "v8", "i8", "msk", "e4", "pto"]}
                    B_pmix, B_ptf, B_plg = em.bufs(3, "s4p")
                    for j in range(16):
                        q2 = j % 2
                        oT, xt4, tmp4, x1t, h2t, h2Tf, junk4, s4s, lg, v8, i8, msk, e4, pto = (
                            oT_[q2], xt4_[q2], tmp4_[q2], x1t_[q2], h2t_[q2], h2Tf_[q2], junk4_[q2], s4s_[q2], lg_[q2], v8_[q2], i8_[q2], msk_[q2], e4_[q2], pto_[q2])
                        (B_oT, B_xt4, B_tmp4, B_x1t, B_h2t, B_h2Tf, B_junk4, B_s4s, B_lg, B_v8, B_i8, B_msk, B_e4, B_pto) = (
                            BB[n][q2] for n in ["oT", "xt4", "tmp4", "x1t", "h2t", "h2Tf", "junk4", "s4s", "lg", "v8", "i8", "msk", "e4", "pto"])
                        pet([(pto[:, c, :], o_all[:, j, c * 128:(c + 1) * 128], identb[:]) for c in range(8)], [B_oall[j], B_identb], [B_pto])
                        cp(oT[:], pto[:], [B_pto], [B_oT])
                        mms = []
                        for n in range(2):
                            for c in range(8):
                                mms.append((pmix[:, n * 512:(n + 1) * 512], oT[:, c, :], Wout[:, c, n * 512:(n + 1) * 512], c == 0, c == 7))
                        pe(mms, [B_oT, B_Wout], [B_pmix])
                        dma(xt4[:], xq[j * 128:(j + 1) * 128, :], [], [B_xt4])
                        act(junk4[:], pmix[:], AF.Square, [B_pmix], [B_junk4, B_s4s], accum_out=s4s[:, 0:1])
                        ts(s4s[:, 1:2], s4s[:, 0:1], 1.0 / 1024, 1e-6, ALU.mult, ALU.add, [B_s4s], [B_s4s])
                        act(s4s[:, 2:3], s4s[:, 1:2], AF.Sqrt, [B_s4s], [B_s4s])
                        dve(lambda E, s4s=s4s, v8=v8, lg=lg, i8=i8, e4=e4: E.reciprocal(out=s4s[:, 3:4], in_=s4s[:, 2:3]), [B_s4s], [B_s4s])
                        stt(tmp4[:], pmix[:], s4s[:, 3:4], rows[:, 2, :], ALU.mult, ALU.mult, [B_pmix, B_s4s, B_rows], [B_tmp4])
                        tt(x1t[:], tmp4[:], xt4[:], ALU.add, [B_tmp4, B_xt4], [B_x1t])
                        dma(x1s[j * 128:(j + 1) * 128, :], x1t[:], [B_x1t], [B_x1s[j]], sembuf=B_x1s[j])
                        act(junk4[:], x1t[:], AF.Square, [B_x1t], [B_junk4, B_s4s], accum_out=s4s[:, 4:5])
                        ts(s4s[:, 5:6], s4s[:, 4:5], 1.0 / 1024, 1e-6, ALU.mult, ALU.add, [B_s4s], [B_s4s])
                        act(s4s[:, 6:7], s4s[:, 5:6], AF.Sqrt, [B_s4s], [B_s4s])
                        dve(lambda E, s4s=s4s, v8=v8, lg=lg, i8=i8, e4=e4: E.reciprocal(out=s4s[:, 7:8], in_=s4s[:, 6:7]), [B_s4s], [B_s4s])
                        stt(tmp4[:], x1t[:], s4s[:, 7:8], rows[:, 0, :], ALU.mult, ALU.mult, [B_x1t, B_s4s, B_rows], [B_tmp4])
                        tt(h2t[:], tmp4[:], rows[:, 1, :], ALU.add, [B_tmp4, B_rows], [B_h2t])
                        act(hrow[:, j, :], h2t[:], AF.Copy, [B_h2t], [B_hrow[j]])
                        pet([(ptf[:, c, :], h2t[:, c * 128:(c + 1) * 128], identf[:]) for c in range(8)], [B_h2t, B_identf], [B_ptf])
                        cp(h2Tf[:], ptf[:], [B_ptf], [B_h2Tf])
                        mms = [(plg[:], h2Tf[:, c, :], wr[:, c, :], c == 0, False) for c in range(8)]
                        mms.append((plg[:], onesf[0:1, 0:128], br[0:1, :], False, True))
                        pe(mms, [B_h2Tf, B_wr, B_br, B_onesf], [B_plg])
                        cp(lg[:], plg[:], [B_plg], [B_lg])
                        dve(lambda E, s4s=s4s, v8=v8, lg=lg, i8=i8, e4=e4: E.max(out=v8[:], in_=lg[:]), [B_lg], [B_v8])
                        dve(lambda E, s4s=s4s, v8=v8, lg=lg, i8=i8, e4=e4: E.max_index(out=i8[:], in_max=v8[:], in_values=lg[:]), [B_lg, B_v8], [B_i8])
                        cp(eidx[:, j, :], i8[:], [B_i8], [B_eidx])
                        ts(msk[:], lg[:], v8[:, 3:4], None, ALU.is_ge, None, [B_lg, B_v8], [B_msk])
                        cp(mskb[:, j, :], msk[:], [B_msk], [B_mskb])
                        ts(s4s[:, 8:9], v8[:, 0:1], -1.0, None, ALU.mult, None, [B_v8, B_s4s], [B_s4s])
                        act(e4[:], v8[:, 0:4], AF.Exp, [B_v8, B_s4s], [B_e4], bias=s4s[:, 8:9], scale=1.0)
                        dve(lambda E, s4s=s4s, v8=v8, lg=lg, i8=i8, e4=e4: E.reduce_sum(out=s4s[:, 9:10], in_=e4[:], axis=AX.X), [B_e4, B_s4s], [B_s4s])
                        dve(lambda E, s4s=s4s, v8=v8, lg=lg, i8=i8, e4=e4: E.reciprocal(out=s4s[:, 10:11], in_=s4s[:, 9:10]), [B_s4s], [B_s4s])
                        ts(gate4[:, j, :], e4[:], s4s[:, 10:11], None, ALU.mult, None, [B_e4, B_s4s], [B_gate4])
                    cnt_t = sbt(s4, "cnt_t", [128, 32])
                    cmp1 = sbt(s4, "cmp1", [128, 32, 16])
                    nbk = sbt(s4, "nbk", [128, 32])
                    ones32 = sbt(s4, "ones32", [128, 32])
                    cum = sbt(s4, "cum", [128, 32])
                    pstart = sbt(s4, "pstart", [128, 32])
                    cmp2 = sbt(s4, "cmp2", [128, NB, 32])
                    blk = sbt(s4, "blk", [128, NB])
                    chg = sbt(s4, "chg", [128, NB])
                    idxf = sbt(s4, "idxf", [128, 5, NB])
                    chg2 = sbt(s4, "chg2", [128, NB])
                    B_chg2 = em.buf("chg2")
                    destf = sbt(s4, "destf", [128, 16, 32])
                    ohf = sbt(s4, "ohf", [128, 32])
                    dstf = sbt(s4, "dstf", [128, 64])
                    (B_cnt, B_cmp1, B_nbk, B_ones32, B_cum, B_pstart, B_cmp2, B_blk, B_chg, B_idxf, B_destf, B_ohf, B_dstf) = em.bufs(13, "rt")
                    mms = [(plg[:], onesb[:, 0:128], mskb[:, j, :], j == 0, j == 15) for j in range(16)]
                    pe(mms, [B_onesb, B_mskb], [B_plg])
                    cp(cnt_t[:], plg[:], [B_plg], [B_cnt])
                    tt(cmp1[:], cnt_t[:].unsqueeze(2).to_broadcast([128, 32, 16]), thr16[:], ALU.is_gt, [B_cnt, B_thr16], [B_cmp1])
                    dve(lambda E: E.reduce_sum(out=nbk[:], in_=cmp1[:], axis=AX.X), [B_cmp1], [B_nbk])
                    mset(ones32[:], 1.0, [B_ones32])
                    dve(lambda E: E.tensor_tensor_scan(out=cum[:], data0=ones32[:], data1=nbk[:], initial=0.0, op0=ALU.mult, op1=ALU.add),
                        [B_ones32, B_nbk], [B_cum])
                    tt(pstart[:], cum[:], nbk[:], ALU.subtract, [B_cum, B_nbk], [B_pstart])
                    ts(pstart[:], pstart[:], 128.0, None, ALU.mult, None, [B_pstart], [B_pstart])
                    tt(cmp2[:], cum[:].unsqueeze(1).to_broadcast([128, NB, 32]), iota96[:].unsqueeze(2).to_broadcast([128, NB, 32]), ALU.is_le,
                       [B_cum, B_iota96], [B_cmp2])
                    dve(lambda E: E.reduce_sum(out=blk[:], in_=cmp2[:], axis=AX.X), [B_cmp2], [B_blk])
                    ts(blk[:], blk[:], 31.0, None, ALU.min, None, [B_blk], [B_blk])
                    ts(ohTall[:], blk[0:32, :], pidx[0:32, 0:1], None, ALU.is_equal, None, [B_blk, B_pidx], [B_ohTall])
                    mset(chg[:, 0:1], 1.0, [B_chg])
                    tt(chg[:, 1:NB], blk[:, 1:NB], blk[:, 0:NB - 1], ALU.not_equal, [B_blk, B_chg], [B_chg])
                    mset(chg2[:, 0:2], 1.0, [B_chg2])
                    tt(chg2[:, 2:NB], blk[:, 2:NB], blk[:, 0:NB - 2], ALU.not_equal, [B_blk, B_chg2], [B_chg2])
                    ts(chg[:], chg[:], -float(2 ** 27), float(2 ** 27), ALU.mult, ALU.add, [B_chg], [B_chg])
                    ts(chg2[:], chg2[:], -float(2 ** 27), float(2 ** 27), ALU.mult, ALU.add, [B_chg2], [B_chg2])
                    ts(blk[:], blk[:], 128.0, pidx[:, 0:1], ALU.mult, ALU.add, [B_blk, B_pidx], [B_blk])
                    tt(idxf[:, 4, :], blk[:], chg[:], ALU.add, [B_blk, B_chg], [B_idxf])
                    ts(idxf[:, 0, :], idxf[:, 4, :], 2.0, None, ALU.mult, None, [B_idxf], [B_idxf])
                    ts(idxf[:, 1, :], idxf[:, 4, :], 2.0, 1.0, ALU.mult, ALU.add, [B_idxf], [B_idxf])
                    tt(idxf[:, 4, :], blk[:], chg2[:], ALU.add, [B_blk, B_chg2, B_idxf], [B_idxf])
                    ts(idxf[:, 2, :], idxf[:, 4, :], 2.0, None, ALU.mult, None, [B_idxf], [B_idxf])
                    ts(idxf[:, 3, :], idxf[:, 4, :], 2.0, 1.0, ALU.mult, ALU.add, [B_idxf], [B_idxf])
                    cp(idxW[:], idxf[:, 0:4, :], [B_idxf], [B_idxW])
                    for j in range(16):
                        mms = [(plg[:], onesb[:, 0:128], mskb[:, jj, :], jj == 0, False) for jj in range(j)]
                        mms.append((plg[:], ltri[:], mskb[:, j, :], j == 0, True))
                        pe(mms, [B_onesb, B_ltri, B_mskb], [B_plg])
                        tt(destf[:, j, :], plg[:], pstart[:], ALU.add, [B_plg, B_pstart], [B_destf])
                    for j in range(16):
                        for k in range(4):
                            ts(ohf[:], iota32[:], eidx[:, j, k:k + 1], None, ALU.is_equal, None, [B_iota32, B_eidx], [B_ohf])
                            tt(ohf[:], ohf[:], destf[:, j, :], ALU.mult, [B_ohf, B_destf], [B_ohf])
                            dve(lambda E, j=j, k=k: E.reduce_sum(out=dstf[:, 4 * j + k:4 * j + k + 1], in_=ohf[:], axis=AX.X), [B_ohf], [B_dstf])
                    cp(dsti[:], dstf[:], [B_dstf], [B_dsti])
                    B_XO = em.buf("XO")
                    for j in range(16):
                        for k in range(4):
                            col = dsti[:, 4 * j + k:4 * j + k + 1]
                            bsc = em.buf("sc")
                            em.dma("pool", lambda E, j=j, col=col: [E.indirect_dma_start(
                                out=Xs, out_offset=bass.IndirectOffsetOnAxis(ap=col, axis=0), in_=hrow[:, j, :], in_offset=None)],
                                reads=[B_hrow[j], B_dsti, B_Xz], writes=[bsc], sembuf=B_XO)
                    for nm, srct, bb in (("dsti", dsti, B_dsti), ("idxW", idxW, B_idxW)):
                        d = dbgout(nm, [128, srct.shape[1] * (srct.shape[2] if len(srct.shape) > 2 else 1)], I32)
                        if d is not None:
                            dma(d, srct[:] if len(srct.shape) == 2 else srct[:].rearrange("p a b -> p (a b)"), [bb], [YB], sembuf=bb)
                    d = dbgout("gate4", [128, 64])
                    if d is not None:
                        dma(d, gate4[:].rearrange("p a b -> p (a b)"), [B_gate4], [YB], sembuf=B_gate4)
                    em.barrier()
                    em.emit()
                with contextlib.ExitStack() as s5:
                    wb1s = [sbt(s5, "wb1_%d" % i, [128, 8, 2048], BF16) for i in range(2)]
                    B_wb1 = [em.bufs(2, "wb1p%d" % i) for i in range(2)]
                    wb2 = sbt(s5, "wb2", [128, 9, 1024], BF16)
                    b1all = sbt(s5, "b1all", [32, 2048], BF16)
                    b2all = sbt(s5, "b2all", [32, 1024], BF16)
                    xbk = [sbt(s5, "xbk%d" % i, [128, 1024], BF16) for i in range(2)]
                    xT = [sbt(s5, "xT%d" % i, [128, 8, 128], BF16) for i in range(2)]
                    ohT = [sbt(s5, "ohT%d" % i, [32, 128], BF16) for i in range(2)]
                    glu = sbt(s5, "glu", [128, 1024])
                    lin = sbt(s5, "lin", [128, 1024])
                    sig = sbt(s5, "sig", [128, 1024], BF16)
                    ab = sbt(s5, "ab", [128, 1024], BF16)
                    aT = sbt(s5, "aT", [128, 8, 128], BF16)
                    yb = [sbt(s5, "yb%d" % i, [128, 1024]) for i in range(2)]
                    TX = pst(s5, "TX", [128, 8, 128], BF16)
                    TA = pst(s5, "TA", [128, 8, 128], BF16)
                    H = pst(s5, "H", [128, 2048])
                    Y = pst(s5, "Y", [128, 1024])
                    Ybf = Y.bitcast(BF16)
                    B_wb1a, B_wb1b, B_wb2, B_b1all, B_b2all, B_glu, B_lin, B_sig, B_ab, B_aT, B_TX, B_TA, B_H, B_Y, B_Ys = em.bufs(15, "s5")
                    B_xbk = em.bufs(2, "xbk"); B_obk = em.bufs(2, "obk"); B_xT = em.bufs(2, "xT"); B_ohT = em.bufs(2, "ohT"); B_yb = em.bufs(2, "yb")
                    bcreg = s5.enter_context(nc.gpsimd.register("bcreg"))
                    em.streams["pool"].append(lambda E: E.reg_mov(bcreg, 8191))
                    dma(b1all[:], b1, [], [B_b1all], q="pool")
                    dma(b2all[:], b2, [], [B_b2all], q="pool")
                    ab2 = [ab, sbt(s5, "ab_b", [128, 1024], BF16)]
                    B_ab2 = [B_ab, em.buf("ab_b")]

                    def loads(i):
                        p = i % 2
                        dma(xbk[p][:], Xs[i * 128:(i + 1) * 128, :], [], [B_xbk[p]])

                    def gathers(i):
                        for hh in range(2):
                            em.dma("pool", lambda E, i=i, hh=hh: [E.indirect_dma_start(
                                out=wb1s[i % 2][:, 4 * hh:4 * hh + 4, :].rearrange("p c f -> p (c f)"), out_offset=None, in_=W1p,
                                in_offset=bass.IndirectOffsetOnAxis(ap=idxW[:, 2 + hh, i:i + 1], axis=0), bounds_check=bcreg, oob_is_err=False)],
                                reads=[B_idxW], writes=[B_wb1[i % 2][hh]])

                    def gathers2(i):
                        for hh in range(2):
                            em.dma("pool", lambda E, i=i, hh=hh: [E.indirect_dma_start(
                                out=wb2[:, 4 * hh:4 * hh + 4, :].rearrange("p c f -> p (c f)"), out_offset=None, in_=W2p,
                                in_offset=bass.IndirectOffsetOnAxis(ap=idxW[:, hh, i:i + 1], axis=0), bounds_check=bcreg, oob_is_err=False)],
                                reads=[B_idxW], writes=[B_wb2])

                    def stageA(i):
                        p = i % 2
                        if i + 1 < NB:
                            loads(i + 1)
                        pet([(TX[:, c, :], xbk[p][:, c * 128:(c + 1) * 128], identb[:]) for c in range(8)], [B_xbk[p], B_identb], [B_TX])
                        cp(xT[p][:], TX[:], [B_TX], [B_xT[p]])
                        cp(ohT[p][:], ohTall[:, i:i + 1].to_broadcast([32, 128]), [B_ohTall], [B_ohT[p]])
                        mms = []
                        for n in range(4):
                            for c in range(8):
                                mms.append((H[:, n * 512:(n + 1) * 512], xT[p][:, c, :], wb1s[p][:, c, n * 512:(n + 1) * 512], c == 0, False))
                            mms.append((H[:, n * 512:(n + 1) * 512], ohT[p][:], b1all[:, n * 512:(n + 1) * 512], False, True))
                        pe(mms, [B_xT[p], B_ohT[p], B_wb1[p][0], B_wb1[p][1], B_b1all], [B_H])
                        if i + 2 < NB:
                            gathers(i + 2)
                        ts(glu[:], H[:, 0:2048:2], 7.0, None, ALU.min, None, [B_H], [B_glu])
                        ts(lin[:], H[:, 1:2048:2], -7.0, 7.0, ALU.max, ALU.min, [B_H], [B_lin])
                        act(sig[:], glu[:], AF.Sigmoid, [B_glu], [B_sig], scale=1.702)
                        stt(lin[:], lin[:], 1.0, glu[:], ALU.add, ALU.mult, [B_lin, B_glu], [B_lin])
                        tt(ab2[p][:], lin[:], sig[:], ALU.mult, [B_lin, B_sig], [B_ab2[p]])

                    def stageB(i):
                        p = i % 2
                        pet([(TA[:, c, :], ab2[p][:, c * 128:(c + 1) * 128], identb[:]) for c in range(8)], [B_ab2[p], B_identb], [B_TA])
                        act(aT[:], TA[:], AF.Copy, [B_TA], [B_aT])
                        mms = []
                        for n in range(2):
                            for c in range(8):
                                mms.append((Y[:, n * 512:(n + 1) * 512], aT[:, c, :], wb2[:, c, n * 512:(n + 1) * 512], c == 0, False))
                            mms.append((Y[:, n * 512:(n + 1) * 512], ohT[p][:], b2all[:, n * 512:(n + 1) * 512], False, True))
                        pe(mms, [B_aT, B_ohT[p], B_wb2, B_b2all], [B_Y])
                        if i + 1 < NB:
                            gathers2(i + 1)
                        act(yb[p][:], Y[:], AF.Copy, [B_Y], [B_yb[p]])
                        dma(Ys[i * 128:(i + 1) * 128, :], yb[p][:], [B_yb[p]], [B_Ys], sembuf=B_yb[p])

                    loads(0)
                    gathers(0)
                    gathers(1)
                    gathers2(0)
                    stageA(0)
                    for i in range(NB):
                        if i + 1 < NB:
                            stageA(i + 1)
                        stageB(i)
                    em.barrier()
                    em.emit()
                with contextlib.ExitStack() as s6:
                    x1r = [sbt(s6, "x1r%d" % i, [128, 1024]) for i in range(2)]
                    ot = [sbt(s6, "ot%d" % i, [128, 1024]) for i in range(2)]
                    yk = [sbt(s6, "yk%d" % i, [128, 4, 1024]) for i in range(2)]
                    ft = sbt(s6, "ft", [128, 1024])
                    junk6 = sbt(s6, "junk6", [128, 1024], BF16)
                    s6s = sbt(s6, "s6s", [128, 4])
                    B_x1r = em.bufs(2, "x1r"); B_ot = em.bufs(2, "ot"); B_yk = [em.bufs(4, "yk%d" % i) for i in range(2)]
                    B_ft, B_junk6, B_s6s = em.bufs(3, "s6")
                    for j in range(16):
                        i = j % 2
                        dma(x1r[i][:], x1s[j * 128:(j + 1) * 128, :], [B_x1s[j]], [B_x1r[i]])
                        for k in range(4):
                            em.dma("pool", lambda E, i=i, j=j, k=k: [E.indirect_dma_start(
                                out=yk[i][:, k, :], out_offset=None, in_=Ys,
                                in_offset=bass.IndirectOffsetOnAxis(ap=dsti[:, 4 * j + k:4 * j + k + 1], axis=0))],
                                reads=[B_dsti], writes=[B_yk[i][k]])
                        ts(ft[:], yk[i][:, 0, :], gate4[:, j, 0:1], None, ALU.mult, None, [B_yk[i][0], B_gate4], [B_ft])
                        for k in range(1, 4):
                            stt(ft[:], yk[i][:, k, :], gate4[:, j, k:k + 1], ft[:], ALU.mult, ALU.add, [B_yk[i][k], B_gate4, B_ft], [B_ft])
                        act(junk6[:], ft[:], AF.Square, [B_ft], [B_junk6, B_s6s], accum_out=s6s[:, 0:1])
                        ts(s6s[:, 1:2], s6s[:, 0:1], 1.0 / 1024, 1e-6, ALU.mult, ALU.add, [B_s6s], [B_s6s])
                        act(s6s[:, 2:3], s6s[:, 1:2], AF.Sqrt, [B_s6s], [B_s6s])
                        dve(lambda E: E.reciprocal(out=s6s[:, 3:4], in_=s6s[:, 2:3]), [B_s6s], [B_s6s])
                        stt(ot[i][:], ft[:], s6s[:, 3:4], rows[:, 3, :], ALU.mult, ALU.mult, [B_ft, B_s6s, B_rows], [B_ot[i]])
                        tt(ot[i][:], ot[i][:], x1r[i][:], ALU.add, [B_ot[i], B_x1r[i]], [B_ot[i]])
                        dma(out[j * 128:(j + 1) * 128, :], ot[i][:], [B_ot[i]], [YB], sembuf=B_ot[i])
                    em.barrier()
                    em.emit()
        else:
            mz = sbt(top, "mz", [128, 1024])
            B_mz = em.buf("mz")
            mset(mz[:], 0.0, [B_mz])
            for j in range(16):
                dma(out[j * 128:(j + 1) * 128, :], mz[:], [B_mz], [YB], sembuf=B_mz)
            for nm, src in (("Qs", Qs), ("Ks", Ks), ("NQs", NQs), ("NKs", NKs), ("Vs", Vs), ("NVs", NVs)):
                d = dbgout(nm, src.shape, BF16)
                if d is not None:
                    dma(d, src, [], [YB], sembuf=YB)
            em.barrier()
            em.emit()
    return nc, dbg


SLOPES = [2.0 ** (-2 * (h + 1)) for h in range(4)]
_CACHE = {}


def _bf(a):
    return np.ascontiguousarray(a.astype(ml_dtypes.bfloat16))


def _nat_tables(rpb, qr):
    R0 = 32 * qr
    def table(r0):
        t = np.full((128, 8, 7, 128), -30000.0, np.float32)
        qrow = np.repeat(np.array([r0, r0 + 1]), 64)
        qcol = np.tile(np.arange(64), 2)
        qstart = np.clip(qrow - 4, 0, 120)
        qcs = np.clip(qcol - 8, 0, 48)
        for o in range(7):
            krow = np.repeat(np.array([r0 - 6 + 2 * o, r0 - 5 + 2 * o]), 64)
            kcol = np.tile(np.arange(64), 2)
            valid = ((krow[:, None] >= qstart[None, :]) & (krow[:, None] < qstart[None, :] + 8)
                     & (krow[:, None] >= 0) & (krow[:, None] < 128)
                     & (kcol[:, None] >= qcs[None, :]) & (kcol[:, None] < qcs[None, :] + 16))
            dr = np.clip(krow[:, None] - qrow[None, :] + 7, 0, 14)
            dc = np.clip(kcol[:, None] - qcol[None, :], -15, 15) + 15
            for h in range(8):
                vals = rpb[h][dr, dc]
                t[:, h, o, :] = np.where(valid, vals, -30000.0)
        return t
    slots = [table(R0 + 2 * 6)] + [table(R0 + 2 * j) for j in (0, 1, 14, 15)]
    return _bf(np.stack(slots).reshape(5, 128, 8 * 7 * 128))


def _prep(inputs):
    f32 = np.float32
    x = np.asarray(inputs["x"], f32)
    c = np.asarray(inputs["c"], f32)
    shared = dict(
        w_ada=np.ascontiguousarray(inputs["w_ada"][0], f32), b_ada=np.ascontiguousarray(inputs["b_ada"], f32).reshape(1, 6144),
        g_pre_mix=np.asarray(inputs["g_pre_mix"], f32).reshape(1, 1024), g_post_mix=np.asarray(inputs["g_post_mix"], f32).reshape(1, 1024),
        g_pre_ffn=np.asarray(inputs["g_pre_ffn"], f32).reshape(1, 1024), g_post_ffn=np.asarray(inputs["g_post_ffn"], f32).reshape(1, 1024),
        w_in=np.ascontiguousarray(inputs["w_in"][0], f32), w_out=np.ascontiguousarray(inputs["w_out"][0], f32),
        lamv=np.concatenate([np.asarray(inputs[k], f32).reshape(-1) for k in ("lam_q1", "lam_k1", "lam_q2", "lam_k2")]).reshape(1, 256),
        g_subln=np.asarray(inputs["g_subln"], f32).reshape(1, 128),
        w_router=np.ascontiguousarray(inputs["w_router"][0], f32), b_router=np.asarray(inputs["b_router"], f32).reshape(1, 32),
        identb=_bf(np.eye(128, dtype=f32)), identf=np.eye(128, dtype=f32),
        ltri=_bf(np.triu(np.ones((128, 128), f32), 1)),
        iota32=np.tile(np.arange(32, dtype=f32), (128, 1)),
        thr16=np.tile((128.0 * np.arange(16, dtype=f32))[None, None, :], (128, 32, 1)).reshape(128, 512),
        iota96=np.tile(np.arange(NB, dtype=f32), (128, 1)),
        pidx=np.arange(128, dtype=f32).reshape(128, 1),
    )
    if os.environ.get("KSTOP", "") not in ("1", "2"):
        shared.update(
            W1p=np.ascontiguousarray(np.asarray(inputs["w1"][0], f32).reshape(32, 8, 128, 2048).transpose(0, 2, 1, 3)).reshape(8192, 8192),
            W2p=np.ascontiguousarray(np.asarray(inputs["w2"][0], f32).reshape(32, 8, 128, 1024).transpose(0, 2, 1, 3)).reshape(8192, 4096),
            b1=np.ascontiguousarray(inputs["b1"][0], f32), b2=np.ascontiguousarray(inputs["b2"][0], f32))
    kl = np.arange(128)
    bd = np.stack([-SLOPES[h] * np.abs(kl[:, None] - kl[None, :]) for h in range(4)], axis=1)
    shared["bdiag"] = _bf(bd.reshape(128, 512).astype(f32))
    rpb = np.asarray(inputs["nat_rpb"], f32)[0]
    in_maps = []
    for core in range(8):
        b, qr = core // 4, core % 4
        own = np.arange(qr * 2048, (qr + 1) * 2048)
        rest = np.concatenate([np.arange(0, qr * 2048), np.arange((qr + 1) * 2048, 8192)])
        perm = np.concatenate([own, rest])
        R0 = 32 * qr
        tok0 = (R0 - 6) * 64
        xw = np.zeros((2816, 1024), f32)
        lo, hi = max(tok0, 0), min(tok0 + 2816, 8192)
        xw[lo - tok0:hi - tok0] = x[b, lo:hi]
        ql = np.arange(2048)
        q_lo = (ql % 128).astype(f32)
        qt_abs = (qr * 16 + ql // 128).astype(f32)
        qa = np.zeros((4, 3, 2, 2048), f32)
        for h in range(4):
            qa[h, 0, 0] = -SLOPES[h] * q_lo
            qa[h, 0, 1] = -SLOPES[h] * 128.0 * qt_abs
            qa[h, 1] = -qa[h, 0]
        kabs = perm.astype(f32)
        ktile_abs = perm[::128] // 128
        sig = np.ones(64, f32)
        sig[16:] = np.where(ktile_abs[16:] < qr * 16, 1.0, -1.0)
        ka = np.ones((2, 8192), f32) * np.repeat(sig, 128)[None, :]
        kt_tab = np.zeros((128, 4, 2, 64), f32)
        kpos = kabs.reshape(64, 128).T
        for h in range(4):
            kt_tab[:, h, 0, :] = SLOPES[h] * kpos * sig[None, :]
            kt_tab[:, h, 1, :] = -SLOPES[h] * kpos
        m = dict(shared)
        m.update(
            xb=np.ascontiguousarray(x[b][perm]), xq=np.ascontiguousarray(x[b, own]), xw=xw,
            cT=np.ascontiguousarray(c[b].reshape(8, 128).T),
            natb=_nat_tables(rpb, qr), qaug=_bf(qa), kaug=_bf(ka), ktab=kt_tab.reshape(128, 512),
        )
        in_maps.append(m)
    return in_maps


def kernel(**inputs):
    debug = tuple(os.environ.get("KDEBUG", "").split(",")) if os.environ.get("KDEBUG") else ()
    key = (debug, os.environ.get("KSTOP", ""))
    if key not in _CACHE:
        _CACHE[key] = build_program(debug)
    nc, dbg = _CACHE[key]
    in_maps = _prep(inputs)
    res = run_bass_kernel_spmd(nc, in_maps, core_ids=list(range(8)))
    outs = [np.asarray(r["out"], np.float32) for r in res.results]
    full = np.stack([np.concatenate(outs[0:4], axis=0), np.concatenate(outs[4:8], axis=0)], axis=0)
    if debug:
        kernel.last_debug = [{k: np.asarray(r["dbg_" + k]) for k in dbg} for r in res.results]
    return full
```

```python
import contextlib
import os
import numpy as np
import ml_dtypes
import concourse.bass as bass
import concourse.mybir as mybir
from concourse.bass_utils import run_bass_kernel_spmd

F32 = mybir.dt.float32
BF16 = mybir.dt.bfloat16
I32 = mybir.dt.int32
U32 = mybir.dt.uint32
AF = mybir.ActivationFunctionType
ALU = mybir.AluOpType
AX = mybir.AxisListType

NB = 96
BIG = float(2 ** 30)


class Buf:
    __slots__ = ("name", "w", "rs", "dsem", "dcnt")

    def __init__(self, name):
        self.name = name
        self.w = []
        self.rs = []
        self.dsem = None
        self.dcnt = 0


class Em:
    ENG = ("pe", "act", "dve", "pool", "sp")

    def __init__(self, nc, stack):
        self.nc = nc
        self.stack = stack
        self.streams = {e: [] for e in self.ENG}
        self.cnt = {e: 0 for e in self.ENG}
        self.esem = {e: stack.enter_context(nc.semaphore("sem_" + e)) for e in self.ENG}
        self.waited = {e: {} for e in self.ENG}
        self.nbuf = 0
        self.dbufs = []

    def buf(self, name=None):
        self.nbuf += 1
        return Buf("%s_%d" % (name or "b", self.nbuf))

    def bufs(self, n, name=None):
        return [self.buf(name) for _ in range(n)]

    def _dsem(self, b):
        if b.dsem is None:
            b.dsem = self.stack.enter_context(self.nc.semaphore("d_" + b.name))
            self.dbufs.append(b)
        return b.dsem

    def _deps(self, eng, reads, writes):
        toks = {}

        def add(t):
            key, val, h = t
            if eng == "pe" and key == "pe":
                return
            if key not in toks or toks[key][1] < val:
                toks[key] = t
        for b in reads:
            for t in b.w:
                add(t)
        for b in writes:
            for t in b.w:
                add(t)
            for t in b.rs:
                add(t)
        return self._filter(eng, toks.values())

    def _filter(self, eng, toks):
        out = []
        wd = self.waited[eng]
        for key, val, h in toks:
            if wd.get(key, 0) >= val:
                continue
            wd[key] = val
            out.append((h, val))
        return out

    def _update(self, tok, reads, writes):
        for b in reads:
            b.rs.append(tok)
        for b in writes:
            b.w = [tok]
            b.rs = []

    def op(self, eng, fn, reads=(), writes=()):
        waits = self._deps(eng, reads, writes)
        self.cnt[eng] += 1
        sem = self.esem[eng]
        tok = (eng, self.cnt[eng], sem)

        def run(E, fn=fn, waits=waits, sem=sem):
            for h, v in waits:
                E.wait_ge(h, v)
            fn(E).then_inc(sem, 1)
        self.streams[eng].append(run)
        self._update(tok, reads, writes)

    def dma(self, q, fn, reads=(), writes=(), n=1, sembuf=None):
        waits = self._deps(q, reads, writes)
        sb = sembuf if sembuf is not None else (writes[0] if writes else reads[0])
        sem = self._dsem(sb)
        sb.dcnt += 16 * n
        tok = ("d_" + sb.name, sb.dcnt, sem)

        def run(E, fn=fn, waits=waits, sem=sem, n=n):
            for h, v in waits:
                E.wait_ge(h, v)
            lst = fn(E)
            assert len(lst) == n
            for ins in lst:
                ins.then_inc(sem, 16)
        self.streams[q].append(run)
        self._update(tok, reads, writes)

    def barrier(self):
        toks = [(e, self.cnt[e], self.esem[e]) for e in self.ENG if self.cnt[e] > 0]
        toks += [("d_" + b.name, b.dcnt, b.dsem) for b in self.dbufs]
        for eng in self.ENG:
            waits = self._filter(eng, [t for t in toks if t[0] != eng])

            def run(E, waits=waits):
                for h, v in waits:
                    E.wait_ge(h, v)
            self.streams[eng].append(run)

    def emit(self):
        nc = self.nc
        st = self.streams
        with nc.Block() as block:
            @block.sync
            def _(E):
                for f in st["sp"]:
                    f(E)

            @block.tensor
            def _(E):
                for f in st["pe"]:
                    f(E)

            @block.scalar
            def _(E):
                for f in st["act"]:
                    f(E)

            @block.vector
            def _(E):
                for f in st["dve"]:
                    f(E)

            @block.gpsimd
            def _(E):
                for f in st["pool"]:
                    f(E)
        self.streams = {e: [] for e in self.ENG}


def build_program(debug=()):
    nc = bass.Bass("TRN2", target_bir_lowering=False)

    def din(name, shape, dt=F32):
        return nc.dram_tensor(name, list(shape), dt, kind="ExternalInput").ap()

    def dscr(name, shape, dt):
        return nc.dram_tensor(name, list(shape), dt, kind="Internal").ap()

    xb = din("xb", [8192, 1024])
    xq = din("xq", [2048, 1024])
    xw = din("xw", [2816, 1024])
    cT = din("cT", [128, 8])
    w_ada = din("w_ada", [1024, 6144])
    b_ada = din("b_ada", [1, 6144])
    g_pre_mix = din("g_pre_mix", [1, 1024])
    g_post_mix = din("g_post_mix", [1, 1024])
    g_pre_ffn = din("g_pre_ffn", [1, 1024])
    g_post_ffn = din("g_post_ffn", [1, 1024])
    w_in = din("w_in", [1024, 3072])
    w_out = din("w_out", [1024, 1024])
    lamv = din("lamv", [1, 256])
    g_subln = din("g_subln", [1, 128])
    natb = din("natb", [5, 128, 8 * 7 * 128], BF16)
    w_router = din("w_router", [1024, 32])
    b_router = din("b_router", [1, 32])
    if os.environ.get("KSTOP", "") not in ("1", "2"):
        W1p = din("W1p", [8192, 8192])
        W2p = din("W2p", [8192, 4096])
        b1 = din("b1", [32, 2048])
        b2 = din("b2", [32, 1024])
    qaug = din("qaug", [4, 3, 2, 2048], BF16)
    kaug = din("kaug", [2, 8192], BF16)
    ktab = din("ktab", [128, 4 * 2 * 64])
    bdiag = din("bdiag", [128, 4 * 128], BF16)
    identb_d = din("identb", [128, 128], BF16)
    identf_d = din("identf", [128, 128])
    ltri_d = din("ltri", [128, 128], BF16)
    iota32_d = din("iota32", [128, 32])
    thr16_d = din("thr16", [128, 512])
    iota96_d = din("iota96", [128, NB])
    pidx_d = din("pidx", [128, 1])
    out = nc.dram_tensor("out", [2048, 1024], F32, kind="ExternalOutput").ap()

    Qs = dscr("Qs", [4, 128, 2048], BF16)
    Ks = dscr("Ks", [4, 128, 8192], BF16)
    Vs = dscr("Vs", [4, 128, 64, 130], BF16)
    NQs = dscr("NQs", [4, 128, 2048], BF16)
    NKs = dscr("NKs", [4, 128, 2816], BF16)
    NVs = dscr("NVs", [128, 22, 8 * 66], BF16)
    x1s = dscr("x1s", [2048, 1024], F32)
    Xs = dscr("Xs", [NB * 128, 1024], BF16)
    Os = dscr("Os", [NB * 128, 32], BF16)
    Ys = dscr("Ys", [NB * 128, 1024], F32)

    dbg = {}

    def dbgout(name, shape, dt=F32):
        if name in debug:
            dbg[name] = nc.dram_tensor("dbg_" + name, list(shape), dt, kind="ExternalOutput").ap()
            return dbg[name]
        return None

    with contextlib.ExitStack() as top:
        em = Em(nc, top)
        YB = em.buf("yout")

        def sbt(st, name, shape, dt=F32):
            return st.enter_context(nc.sbuf_tensor(name, list(shape), dt))

        def pst(st, name, shape, dt=F32):
            return st.enter_context(nc.psum_tensor(name, list(shape), dt))

        def dma(out_, in_, reads, writes, q="sp", sembuf=None):
            em.dma(q, lambda E: [E.dma_start(out=out_, in_=in_)], reads=reads, writes=writes, sembuf=sembuf)

        def act(out_, in_, func, reads, writes, **kw):
            em.op("act", lambda E: E.activation(out=out_, in_=in_, func=func, **kw), reads, writes)

        def pe(mms, reads, writes):
            def fn(E):
                ins = None
                for (o, l, r, s0, s1) in mms:
                    ins = E.matmul(o, lhsT=l, rhs=r, start=s0, stop=s1)
                return ins
            em.op("pe", fn, reads, writes)

        def pet(trs, reads, writes):
            def fn(E):
                ins = None
                for (o, i, idn) in trs:
                    ins = E.transpose(o, i, idn)
                return ins
            em.op("pe", fn, reads, writes)

        def dve(f, reads, writes, eng="dve"):
            em.op(eng, f, reads, writes)

        def ts(out_, in0, s1, s2, op0, op1=None, reads=(), writes=(), eng="dve"):
            if op1 is None:
                dve(lambda E: E.tensor_scalar(out=out_, in0=in0, scalar1=s1, scalar2=None, op0=op0), reads, writes, eng)
            else:
                dve(lambda E: E.tensor_scalar(out=out_, in0=in0, scalar1=s1, scalar2=s2, op0=op0, op1=op1), reads, writes, eng)

        def tt(out_, in0, in1, op, reads, writes, eng="dve"):
            dve(lambda E: E.tensor_tensor(out=out_, in0=in0, in1=in1, op=op), reads, writes, eng)

        def stt(out_, in0, scalar, in1, op0, op1, reads, writes):
            dve(lambda E: E.scalar_tensor_tensor(out=out_, in0=in0, scalar=scalar, in1=in1, op0=op0, op1=op1), reads, writes)

        def cp(out_, in_, reads, writes, eng="dve"):
            dve(lambda E: E.tensor_copy(out=out_, in_=in_), reads, writes, eng)

        def mset(ap, v, writes, eng="dve"):
            dve(lambda E: E.memset(ap, v), (), writes, eng)

        def dump(name, dst_shape_src):
            pass

        identb = sbt(top, "identb_s", [128, 128], BF16)
        identf = sbt(top, "identf_s", [128, 128])
        onesb = sbt(top, "onesb", [128, 512], BF16)
        onesf = sbt(top, "onesf", [128, 128])
        rows = sbt(top, "rows", [128, 4, 1024])
        o_all = sbt(top, "o_all", [128, 16, 512], BF16)
        stat = sbt(top, "stat", [1, 16])
        negM = sbt(top, "negM", [128, 8])
        neglam = sbt(top, "neglam", [128, 1])
        gsub = sbt(top, "gsub", [128, 128])
        B_identb, B_identf, B_onesb, B_onesf, B_rows, B_stat, B_negM, B_neglam, B_gsub = em.bufs(9, "const")
        B_oall = em.bufs(16, "oall")
        oTd = sbt(top, "oTd", [128, 4, 2048], BF16)
        B_oTd = [em.bufs(4, "oTd%d" % h) for h in range(4)]
        g8col = sbt(top, "g8col", [128, 1])
        B_g8col = em.buf("g8col")
        dma(g8col[:], g_subln.rearrange("o e -> e o"), [], [B_g8col])
        ts(g8col[:], g8col[:], 0.8, None, ALU.mult, None, [B_g8col], [B_g8col])
        dma(identb[:], identb_d, [], [B_identb])
        dma(identf[:], identf_d, [], [B_identf])
        mset(onesb[:], 1.0, [B_onesb])
        mset(onesf[:], 1.0, [B_onesf])
        mset(stat[:], 0.0, [B_stat])
        dma(gsub[:], g_subln.to_broadcast([128, 128]), [], [B_gsub])

        with contextlib.ExitStack() as st01:
            Wall = sbt(st01, "Wall", [128, 8, 3072], BF16)
            biasrow = sbt(st01, "biasrow", [1, 3072], BF16)
            B_Wall = em.bufs(8, "Wall")
            B_biasrow = em.buf("biasrow")
            with contextlib.ExitStack() as st:
                sil = sbt(st, "sil", [128, 8])
                silrep = sbt(st, "silrep", [128, 8, 128], BF16)
                wada = [sbt(st, "wada%d" % i, [128, 8, 512], BF16) for i in range(2)]
                bada = sbt(st, "bada", [1, 6144], BF16)
                modrow = sbt(st, "modrow", [128, 6144])
                grow = sbt(st, "grow", [128, 4, 1024])
                s1row = sbt(st, "s1row", [128, 1024])
                tmpd = sbt(st, "tmpd", [128, 128])
                s1T = sbt(st, "s1T", [128, 8])
                sh1T = sbt(st, "sh1T", [128, 8])
                wst = [sbt(st, "wst0", [128, 3072])] * 2
                lam_t = sbt(st, "lam_t", [1, 256])
                lam_s = sbt(st, "lam_s", [1, 8])
                pmod = [pst(st, "pmod%d" % i, [128, 512]) for i in range(2)]
                pbias = pst(st, "pbias", [1, 3072])
                B_sil, B_silrep, B_bada, B_grow, B_s1row, B_tmpd, B_s1T, B_sh1T, B_lamt, B_lams, B_pbias, B_plam = em.bufs(12, "p0")
                B_wada = em.bufs(2, "wada")
                B_modrow = em.bufs(12, "modrow")
                B_wst = [em.buf("wst")] * 2
                B_pmod = em.bufs(2, "pmod")

                dma(sil[:], cT, [], [B_sil])
                act(sil[:], sil[:], AF.Silu, [B_sil], [B_sil])
                for c in range(8):
                    cp(silrep[:, c, :], sil[:, c:c + 1].to_broadcast([128, 128]), [B_sil], [B_silrep])
                dma(bada[:], b_ada, [], [B_bada], q="pool")
                for i, g in enumerate([g_pre_mix, g_post_mix, g_pre_ffn, g_post_ffn]):
                    dma(grow[:, i, :], g.to_broadcast([128, 1024]), [], [B_grow])
                for j in range(12):
                    wb = wada[j % 2]
                    dma(wb[:], w_ada[:, j * 512:(j + 1) * 512].rearrange("(c p) f -> p c f", p=128), [], [B_wada[j % 2]], q="pool")
                    mms = [(pmod[j % 2][:], silrep[:, c, :], wb[:, c, :], c == 0, False) for c in range(8)]
                    mms.append((pmod[j % 2][:], onesb[0:1, 0:128], bada[0:1, j * 512:(j + 1) * 512], False, True))
                    pe(mms, [B_silrep, B_wada[j % 2], B_bada, B_onesb], [B_pmod[j % 2]])
                    cp(modrow[:, j * 512:(j + 1) * 512], pmod[j % 2][:], [B_pmod[j % 2]], [B_modrow[j]])
                MR = lambda i: [B_modrow[2 * i], B_modrow[2 * i + 1]]
                m = lambda i: modrow[:, i * 1024:(i + 1) * 1024]
                stt(s1row[:], m(1), 1.0, grow[:, 0, :], ALU.add, ALU.mult, MR(1) + [B_grow], [B_s1row])
                stt(rows[:, 0, :], m(4), 1.0, grow[:, 2, :], ALU.add, ALU.mult, MR(4) + [B_grow], [B_rows])
                cp(rows[:, 1, :], m(3), MR(3) + [B_rows], [B_rows])
                tt(rows[:, 2, :], m(2), grow[:, 1, :], ALU.mult, MR(2) + [B_grow, B_rows], [B_rows])
                tt(rows[:, 3, :], m(5), grow[:, 3, :], ALU.mult, MR(5) + [B_grow, B_rows], [B_rows])
                for c in range(8):
                    tt(tmpd[:], s1row[:, c * 128:(c + 1) * 128], identf[:], ALU.mult, [B_s1row, B_identf], [B_tmpd])
                    dve(lambda E, c=c: E.reduce_sum(out=s1T[:, c:c + 1], in_=tmpd[:], axis=AX.X), [B_tmpd], [B_s1T])
                    tt(tmpd[:], modrow[:, c * 128:(c + 1) * 128], identf[:], ALU.mult, MR(0) + [B_identf], [B_tmpd])
                    dve(lambda E, c=c: E.reduce_sum(out=sh1T[:, c:c + 1], in_=tmpd[:], axis=AX.X), [B_tmpd], [B_sh1T])
                for c in range(8):
                    wsb = wst[c % 2]
                    dma(wsb[:], w_in[c * 128:(c + 1) * 128, :], [], [B_wst[c % 2]])
                    mms = [(pbias[0:1, n * 512:(n + 1) * 512], sh1T[:, c:c + 1], wsb[:, n * 512:(n + 1) * 512], c == 0, c == 7) for n in range(6)]
                    pe(mms, [B_sh1T, B_wst[c % 2]], [B_pbias])
                    act(Wall[:, c, :], wsb[:], AF.Copy, [B_wst[c % 2], B_s1T], [B_Wall[c]], scale=s1T[:, c:c + 1])
                cp(biasrow[:], pbias[:], [B_pbias], [B_biasrow])
                dma(lam_t[:], lamv, [], [B_lamt])
                tt(lam_t[0:1, 0:64], lam_t[0:1, 0:64], lam_t[0:1, 64:128], ALU.mult, [B_lamt], [B_lamt])
                tt(lam_t[0:1, 128:192], lam_t[0:1, 128:192], lam_t[0:1, 192:256], ALU.mult, [B_lamt], [B_lamt])
                dve(lambda E: E.reduce_sum(out=lam_s[0:1, 0:1], in_=lam_t[0:1, 0:64], axis=AX.X), [B_lamt], [B_lams])
                dve(lambda E: E.reduce_sum(out=lam_s[0:1, 1:2], in_=lam_t[0:1, 128:192], axis=AX.X), [B_lamt], [B_lams])
                act(lam_s[0:1, 2:4], lam_s[0:1, 0:2], AF.Exp, [B_lams], [B_lams])
                stt(lam_s[0:1, 4:5], lam_s[0:1, 3:4], -0.2, lam_s[0:1, 2:3], ALU.add, ALU.subtract, [B_lams], [B_lams])
                pe([(pmod[0][:, 0:1], onesf[0:1, 0:128], lam_s[0:1, 4:5], True, True)], [B_onesf, B_lams], [B_pmod[0]])
                cp(neglam[:], pmod[0][:, 0:1], [B_pmod[0]], [B_neglam])
                d = dbgout("rows", [128, 4096])
                if d is not None:
                    dma(d, rows[:].rearrange("p a f -> p (a f)"), [B_rows], [YB], sembuf=B_rows)
                d = dbgout("neglam", [128, 1])
                if d is not None:
                    dma(d, neglam[:], [B_neglam], [YB], sembuf=B_neglam)
                em.barrier()
                em.emit()

            with contextlib.ExitStack() as st:
                xt = [sbt(st, "xt%d" % i, [128, 1024]) for i in range(2)]
                xn = [sbt(st, "xn%d" % i, [128, 1024], BF16) for i in range(2)]
                xnT = [sbt(st, "xnT%d" % i, [128, 8, 512], BF16) for i in range(2)]
                ssq = sbt(st, "ssq", [128, 4])
                junk = sbt(st, "junk", [128, 1024], BF16)
                ev = [sbt(st, "ev%d" % i, [128, 512], BF16) for i in range(3)]
                sq = [sbt(st, "sq%d" % i, [128, 512], BF16) for i in range(2)]
                vt = [sbt(st, "vt%d" % i, [128, 4, 130], BF16) for i in range(2)]
                nvt = [sbt(st, "nvt%d" % i, [128, 8, 66], BF16) for i in range(2)]
                mx = sbt(st, "mx", [1, 2])
                ptr = [pst(st, "ptr%d" % i, [128, 8, 128], BF16) for i in range(2)]
                pp = [pst(st, "pp%d" % i, [128, 512]) for i in range(3)]
                pn = pst(st, "pn", [1, 512])
                B_xt = em.bufs(2, "xt"); B_xn = em.bufs(2, "xn"); B_xnT = em.bufs(2, "xnT")
                B_ssq = em.buf("ssq"); B_junk = em.buf("junk"); B_ev = em.bufs(3, "ev"); B_sq = em.bufs(2, "sq")
                B_vt = em.bufs(2, "vt"); B_nvt = em.bufs(2, "nvt"); B_mx = em.buf("mx")
                B_ptr = em.bufs(2, "ptr"); B_pp = em.bufs(3, "pp"); B_pn = em.buf("pn")
                B_scr = em.buf("scr1")
                for i in range(2):
                    mset(vt[i][:, :, 128:129], 1.0, [B_vt[i]])
                    mset(vt[i][:, :, 129:130], 0.0, [B_vt[i]])
                    mset(nvt[i][:, :, 64:65], 1.0, [B_nvt[i]])
                    mset(nvt[i][:, :, 65:66], 0.0, [B_nvt[i]])
                cnt = {"tile": 0, "grp": 0, "pp": 0, "ev": 0, "sq": 0, "vt": 0, "nvt": 0}

                def norm_group(src, g):
                    gi = cnt["grp"] % 2
                    cnt["grp"] += 1
                    for t in range(4):
                        i = cnt["tile"] % 2
                        cnt["tile"] += 1
                        r0 = g * 512 + t * 128
                        dma(xt[i][:], src[r0:r0 + 128, :], [], [B_xt[i]])
                        act(junk[:], xt[i][:], AF.Square, [B_xt[i]], [B_junk, B_ssq], accum_out=ssq[:, 0:1])
                        ts(ssq[:, 1:2], ssq[:, 0:1], 1.0 / 1024, 1e-6, ALU.mult, ALU.add, [B_ssq], [B_ssq])
                        act(ssq[:, 2:3], ssq[:, 1:2], AF.Sqrt, [B_ssq], [B_ssq])
                        dve(lambda E: E.reciprocal(out=ssq[:, 3:4], in_=ssq[:, 2:3]), [B_ssq], [B_ssq])
                        act(xn[i][:], xt[i][:], AF.Copy, [B_xt[i], B_ssq], [B_xn[i]], scale=ssq[:, 3:4])
                        pet([(ptr[i][:, c, :], xn[i][:, c * 128:(c + 1) * 128], identb[:]) for c in range(8)],
                            [B_xn[i], B_identb], [B_ptr[i]])
                        cp(xnT[gi][:, :, t * 128:(t + 1) * 128], ptr[i][:], [B_ptr[i]], [B_xnT[gi]])
                    return gi

                def proj_T(gi, col0, scale, dst, stat_idx):
                    k = cnt["pp"] % 3; cnt["pp"] += 1
                    mms = [(pp[k][:], Wall[:, c, col0:col0 + 128], xnT[gi][:, c, :], c == 0, False) for c in range(8)]
                    mms.append((pp[k][:], biasrow[0:1, col0:col0 + 128], onesb[0:1, 0:512], False, True))
                    pe(mms, B_Wall + [B_xnT[gi], B_biasrow, B_onesb], [B_pp[k]])
                    e = cnt["ev"] % 3; cnt["ev"] += 1
                    act(ev[e][:], pp[k][:], AF.Copy, [B_pp[k]], [B_ev[e]], scale=scale)
                    dma(dst, ev[e][:], [B_ev[e]], [B_scr], sembuf=B_ev[e])
                    s = cnt["sq"] % 2; cnt["sq"] += 1
                    tt(sq[s][:], ev[e][:], ev[e][:], ALU.mult, [B_ev[e]], [B_sq[s]])
                    pe([(pn[:], onesb[:, 0:1], sq[s][:], True, True)], [B_onesb, B_sq[s]], [B_pn])
                    dve(lambda E: E.reduce_max(out=mx[0:1, 0:1], in_=pn[0:1, :], axis=AX.X), [B_pn], [B_mx])
                    tt(stat[0:1, stat_idx:stat_idx + 1], stat[0:1, stat_idx:stat_idx + 1], mx[0:1, 0:1], ALU.max, [B_mx, B_stat], [B_stat])

                def proj_tok(gi, t, col0):
                    k = cnt["pp"] % 3; cnt["pp"] += 1
                    mms = [(pp[k][:], xnT[gi][:, c, t * 128:(t + 1) * 128], Wall[:, c, col0:col0 + 512], c == 0, False) for c in range(8)]
                    mms.append((pp[k][:], onesb[0:1, 0:128], biasrow[0:1, col0:col0 + 512], False, True))
                    pe(mms, B_Wall + [B_xnT[gi], B_biasrow, B_onesb], [B_pp[k]])
                    return k

                def norm_part(src, g, ntl):
                    gi = cnt["grp"] % 2
                    cnt["grp"] += 1
                    for t in range(ntl):
                        i = cnt["tile"] % 2
                        cnt["tile"] += 1
                        r0 = g * 512 + t * 128
                        dma(xt[i][:], src[r0:r0 + 128, :], [], [B_xt[i]])
                        act(junk[:], xt[i][:], AF.Square, [B_xt[i]], [B_junk, B_ssq], accum_out=ssq[:, 0:1])
                        ts(ssq[:, 1:2], ssq[:, 0:1], 1.0 / 1024, 1e-6, ALU.mult, ALU.add, [B_ssq], [B_ssq])
                        act(ssq[:, 2:3], ssq[:, 1:2], AF.Sqrt, [B_ssq], [B_ssq])
                        dve(lambda E: E.reciprocal(out=ssq[:, 3:4], in_=ssq[:, 2:3]), [B_ssq], [B_ssq])
                        act(xn[i][:], xt[i][:], AF.Copy, [B_xt[i], B_ssq], [B_xn[i]], scale=ssq[:, 3:4])
                        pet([(ptr[i][:, c, :], xn[i][:, c * 128:(c + 1) * 128], identb[:]) for c in range(8)],
                            [B_xn[i], B_identb], [B_ptr[i]])
                        cp(xnT[gi][:, :, t * 128:(t + 1) * 128], ptr[i][:], [B_ptr[i]], [B_xnT[gi]])
                    return gi

                def projB(kind, g, gi):
                    if kind == "own":
                        for h in range(4):
                            proj_T(gi, h * 128, 0.125, Qs[h, :, g * 512:(g + 1) * 512], h)
                        for c4 in range(4):
                            proj_T(gi, 1536 + c4 * 128, 0.125, NQs[c4, :, g * 512:(g + 1) * 512], 8 + c4)
                    elif kind == "seq":
                        for h in range(4):
                            proj_T(gi, 512 + h * 128, 1.0, Ks[h, :, g * 512:(g + 1) * 512], 4 + h)
                        for t in range(4):
                            k = proj_tok(gi, t, 1024)
                            v = cnt["vt"] % 2; cnt["vt"] += 1
                            cp(vt[v][:, :, 0:128], pp[k][:].rearrange("p (h e) -> p h e", h=4), [B_pp[k]], [B_vt[v]])
                            dma(Vs[:, :, g * 4 + t, :].rearrange("h p e -> p h e"), vt[v][:], [B_vt[v]], [B_scr], sembuf=B_vt[v])
                    else:
                        ntl = 4 if g < 5 else 2
                        ncol = ntl * 128
                        for c4 in range(4):
                            k = cnt["pp"] % 3; cnt["pp"] += 1
                            col0 = 2048 + c4 * 128
                            mms = [(pp[k][:, 0:ncol], Wall[:, c, col0:col0 + 128], xnT[gi][:, c, 0:ncol], c == 0, False) for c in range(8)]
                            mms.append((pp[k][:, 0:ncol], biasrow[0:1, col0:col0 + 128], onesb[0:1, 0:ncol], False, True))
                            pe(mms, B_Wall + [B_xnT[gi], B_biasrow, B_onesb], [B_pp[k]])
                            e = cnt["ev"] % 3; cnt["ev"] += 1
                            act(ev[e][:, 0:ncol], pp[k][:, 0:ncol], AF.Copy, [B_pp[k]], [B_ev[e]])
                            dma(NKs[c4, :, g * 512:g * 512 + ncol], ev[e][:, 0:ncol], [B_ev[e]], [B_scr], sembuf=B_ev[e])
                            s_ = cnt["sq"] % 2; cnt["sq"] += 1
                            tt(sq[s_][:, 0:ncol], ev[e][:, 0:ncol], ev[e][:, 0:ncol], ALU.mult, [B_ev[e]], [B_sq[s_]])
                            pe([(pn[:, 0:ncol], onesb[:, 0:1], sq[s_][:, 0:ncol], True, True)], [B_onesb, B_sq[s_]], [B_pn])
                            dve(lambda E, ncol=ncol: E.reduce_max(out=mx[0:1, 0:1], in_=pn[0:1, 0:ncol], axis=AX.X), [B_pn], [B_mx])
                            tt(stat[0:1, 12 + c4:13 + c4], stat[0:1, 12 + c4:13 + c4], mx[0:1, 0:1], ALU.max, [B_mx, B_stat], [B_stat])
                        for t in range(ntl):
                            k = proj_tok(gi, t, 2560)
                            v = cnt["nvt"] % 2; cnt["nvt"] += 1
                            cp(nvt[v][:, :, 0:64], pp[k][:].rearrange("p (h e) -> p h e", h=8), [B_pp[k]], [B_nvt[v]])
                            dma(NVs[:, g * 4 + t, :], nvt[v][:].rearrange("p h e -> p (h e)"), [B_nvt[v]], [B_scr], sembuf=B_nvt[v])

                groups = [("own", g, xq, 4) for g in range(4)] + [("seq", g, xb, 4) for g in range(16)] \
                    + [("win", g, xw, 4 if g < 5 else 2) for g in range(6)]
                gis = [None] * len(groups)
                gis[0] = norm_part(groups[0][2], groups[0][1], groups[0][3])
                for n_, (kind, g, src, ntl) in enumerate(groups):
                    if n_ + 1 < len(groups):
                        kn, gn, sn, tn = groups[n_ + 1]
                        gis[n_ + 1] = norm_part(sn, gn, tn)
                    projB(kind, g, gis[n_])
                mm_ = sbt(st, "mm_", [1, 8])
                pM = pst(st, "pM", [128, 8])
                B_mm, B_pM = em.bufs(2, "mm")
                tt(mm_[0:1, 0:4], stat[0:1, 0:4], stat[0:1, 4:8], ALU.mult, [B_stat], [B_mm])
                tt(mm_[0:1, 4:8], stat[0:1, 8:12], stat[0:1, 12:16], ALU.mult, [B_stat, B_mm], [B_mm])
                act(mm_[:], mm_[:], AF.Sqrt, [B_mm], [B_mm])
                ts(mm_[:], mm_[:], -1.05, None, ALU.mult, None, [B_mm], [B_mm])
                pe([(pM[:], onesf[0:1, 0:128], mm_[0:1, :], True, True)], [B_onesf, B_mm], [B_pM])
                cp(negM[:], pM[:], [B_pM], [B_negM])
                d = dbgout("negM", [128, 8])
                if d is not None:
                    dma(d, negM[:], [B_negM], [YB], sembuf=B_negM)
                em.barrier()
                em.emit()
        STOP = os.environ.get("KSTOP", "")
        if STOP != "1":
            with contextlib.ExitStack() as st:
                KA = [sbt(st, "KA%d" % m, [66, 8192], BF16) for m in range(2)]
                QA = [[sbt(st, "QA%d_%d" % (m, v), [66, 2048], BF16) for v in range(3)] for m in range(2)]
                Vh = sbt(st, "Vh", [128, 64, 130], BF16)
                ktab_t = sbt(st, "ktab_s", [128, 4, 2, 64])
                kb = sbt(st, "kb", [128, 2, 64])
                bdg = sbt(st, "bdg_s", [128, 4, 128], BF16)
                PT = [sbt(st, "PT%d" % i, [128, 512], BF16) for i in range(3)]
                rz = sbt(st, "rz", [128, 512])
                O1T = sbt(st, "O1T", [128, 512])
                dT = sbt(st, "dT", [128, 512])
                sqb = sbt(st, "sqb", [128, 512], BF16)
                rs = sbt(st, "rs", [128, 512])
                gs8 = sbt(st, "gs8", [128, 128])
                S = [pst(st, "S%d" % i, [128, 512]) for i in range(2)]
                OT = [pst(st, "OT%d" % i, [128, 512]) for i in range(2)]
                ZB = [pst(st, "ZB%d" % i, [128, 512]) for i in range(2)]
                B_OT = em.bufs(2, "OT"); B_ZB = em.bufs(2, "ZB")
                B_rz, B_O1T, B_dT, B_sqb, B_rs = em.bufs(5, "ep")
                B_KA = em.bufs(2, "KA"); B_QA = em.bufs(2, "QA"); B_Vh = em.buf("Vh"); B_ktab = em.buf("ktab")
                B_kb = em.buf("kb"); B_bdg = em.buf("bdg"); B_PT = em.bufs(3, "PT")
                B_gs8 = em.buf("gs8")
                B_S = em.bufs(2, "S")
                dma(ktab_t[:].rearrange("p a b c -> p (a b c)"), ktab, [], [B_ktab])
                dma(bdg[:].rearrange("p a b -> p (a b)"), bdiag, [], [B_bdg])
                ts(gs8[:], gsub[:], 0.8, None, ALU.mult, None, [B_gsub], [B_gs8])
                it = 0
                for h in range(4):
                    for m in range(2):
                        dma(KA[m][0:64, :], Ks[h, 64 * m:64 * m + 64, :], [], [B_KA[m]])
                        dma(KA[m][64:66, :], kaug, [], [B_KA[m]])
                        for v in range(3):
                            dma(QA[m][v][0:64, :], Qs[h, 64 * m:64 * m + 64, :], [], [B_QA[m]])
                            dma(QA[m][v][64:66, :], qaug[h, v], [], [B_QA[m]])
                    dma(Vh[:], Vs[h], [], [B_Vh])
                    ts(kb[:], ktab_t[:, h], negM[:, h:h + 1], None, ALU.add, None, [B_ktab, B_negM], [B_kb])
                    seq = [(qc, m, kt) for qc in range(4) for m in range(2) for kt in range(64)]

                    def segs_of(qc, kt):
                        if kt >= 16:
                            return [(0, 4, 0, kb[:, 0, kt:kt + 1])]
                        segs = []
                        for t in range(4):
                            qt = 4 * qc + t
                            if qt > kt:
                                cls = (0, kb[:, 0, kt:kt + 1])
                            elif qt == kt:
                                cls = (2, negM[:, h:h + 1])
                            else:
                                cls = (1, kb[:, 1, kt:kt + 1])
                            if segs and segs[-1][2] == cls[0]:
                                segs[-1] = (segs[-1][0], t + 1, cls[0], cls[1])
                            else:
                                segs.append((t, t + 1, cls[0], cls[1]))
                        return segs

                    def emit_S(n):
                        qc, m, kt = seq[n]
                        sb = (it + n) % 2
                        mms = []
                        for (t0, t1, v, col) in segs_of(qc, kt):
                            c0, c1 = t0 * 128, t1 * 128
                            q0 = qc * 512
                            mms.append((S[sb][:, c0:c1], KA[m][0:66, kt * 128:(kt + 1) * 128], QA[m][v][0:66, q0 + c0:q0 + c1], True, v != 2))
                            if v == 2:
                                mms.append((S[sb][:, c0:c1], identb[:], bdg[:, h, :], False, True))
                        pe(mms, [B_KA[m], B_QA[m], B_identb, B_bdg], [B_S[sb]])

                    def emit_rest(n):
                        qc, m, kt = seq[n]
                        sb = (it + n) % 2
                        pb = (it + n) % 3
                        for (t0, t1, v, col) in segs_of(qc, kt):
                            c0, c1 = t0 * 128, t1 * 128
                            act(PT[pb][:, c0:c1], S[sb][:, c0:c1], AF.Exp, [B_S[sb], B_kb, B_negM], [B_PT[pb]], bias=col, scale=1.0)
                        ob = ((it + n) // 64) % 2
                        pe([(OT[ob][:], Vh[:, kt, 0:128], PT[pb][:], kt == 0, kt == 63),
                            (ZB[ob][:], onesb[:, 0:128], PT[pb][:], kt == 0, kt == 63)],
                           [B_PT[pb], B_Vh, B_onesb], [B_OT[ob], B_ZB[ob]])
                        if kt != 63:
                            return
                        dve(lambda E, ob=ob: E.reciprocal(out=rz[:], in_=ZB[ob][:]), [B_ZB[ob]], [B_rz])
                        if m == 0:
                            tt(O1T[:], OT[ob][:], rz[:], ALU.mult, [B_OT[ob], B_rz], [B_O1T])
                        else:
                            tt(dT[:], OT[ob][:], rz[:], ALU.mult, [B_OT[ob], B_rz], [B_dT])
                            stt(dT[:], dT[:], neglam[:, 0:1], O1T[:], ALU.mult, ALU.add, [B_dT, B_neglam, B_O1T], [B_dT])
                            tt(sqb[:], dT[:], dT[:], ALU.mult, [B_dT], [B_sqb])
                            pe([(ZB[ob][:], onesb[:, 0:128], sqb[:], True, True)], [B_sqb, B_onesb], [B_ZB[ob]])
                            ts(rs[:], ZB[ob][:], 1.0 / 128, 1e-6, ALU.mult, ALU.add, [B_ZB[ob]], [B_rs])
                            act(rs[:], rs[:], AF.Sqrt, [B_rs], [B_rs])
                            dve(lambda E: E.reciprocal(out=rs[:], in_=rs[:]), [B_rs], [B_rs])
                            tt(dT[:], dT[:], rs[:], ALU.mult, [B_dT, B_rs], [B_dT])
                            ts(oTd[:, h, qc * 512:(qc + 1) * 512], dT[:], g8col[:, 0:1], None, ALU.mult, None,
                               [B_dT, B_g8col], [B_oTd[h][qc]])

                    emit_S(0)
                    for n in range(len(seq)):
                        if n + 1 < len(seq):
                            emit_S(n + 1)
                        emit_rest(n)
                    it += len(seq)
                em.barrier()
                em.emit()

            with contextlib.ExitStack() as st:
                NQT = sbt(st, "NQT", [128, 4, 2048], BF16)
                NKT = sbt(st, "NKT", [128, 4, 2816], BF16)
                NV = sbt(st, "NV", [128, 22, 528], BF16)
                nbt = [sbt(st, "nbt%d" % i, [128, 8 * 7 * 128], BF16) for i in range(2)]
                PN = [sbt(st, "PN%d" % i, [128, 896], BF16) for i in range(2)]
                sn = sbt(st, "sn", [128, 2])
                SN = [pst(st, "SN%d" % i, [128, 1024]) for i in range(2)]
                NO = [pst(st, "NO%d" % i, [128, 512]) for i in range(2)]
                B_NQT, B_NKT, B_NV, B_sn = em.bufs(4, "nat")
                B_nbt = em.bufs(2, "nbt"); B_PN = em.bufs(2, "PN"); B_SN = em.bufs(2, "SN"); B_NO = em.bufs(2, "NO")
                dma(NQT[:], NQs.rearrange("c p t -> p c t"), [], [B_NQT])
                dma(NKT[:], NKs.rearrange("c p t -> p c t"), [], [B_NKT])
                dma(NV[:], NVs, [], [B_NV])
                items = [(j, h) for j in range(16) for h in range(8)]

                def nat_S(n):
                    j, h = items[n]
                    nb_ = nbt[j % 2]
                    if h == 0:
                        slot = {0: 1, 1: 2, 14: 3, 15: 4}.get(j, 0)
                        dma(nb_[:], natb[slot], [], [B_nbt[j % 2]])
                    c4, hp = h // 2, (h % 2) * 64
                    sb = n % 2
                    mms = []
                    for o in range(7):
                        oc = slice(o * 128, (o + 1) * 128)
                        mms.append((SN[sb][:, oc], NKT[hp:hp + 64, c4, (j + o) * 128:(j + o + 1) * 128],
                                    NQT[hp:hp + 64, c4, j * 128:(j + 1) * 128], True, False))
                        mms.append((SN[sb][:, oc], identb[:], nb_[:, (h * 7 + o) * 128:(h * 7 + o + 1) * 128], False, True))
                    pe(mms, [B_NKT, B_NQT, B_identb, B_nbt[j % 2]], [B_SN[sb]])

                def nat_rest(n):
                    j, h = items[n]
                    c4 = h // 2
                    sb = n % 2
                    act(PN[sb][:], SN[sb][:, 0:896], AF.Exp, [B_SN[sb], B_negM], [B_PN[sb]], bias=negM[:, 4 + c4:5 + c4], scale=1.0)
                    mms = [(NO[sb][:, 0:66], PN[sb][:, o * 128:(o + 1) * 128], NV[:, j + o, h * 66:h * 66 + 66], o == 0, o == 6) for o in range(7)]
                    pe(mms, [B_PN[sb], B_NV], [B_NO[sb]])
                    dve(lambda E, sb=sb: E.reciprocal(out=sn[:, 0:1], in_=NO[sb][:, 64:65]), [B_NO[sb]], [B_sn])
                    ts(o_all[:, j, h * 64:(h + 1) * 64], NO[sb][:, 0:64], sn[:, 0:1], None, ALU.mult, None,
                       [B_NO[sb], B_sn], [B_oall[j]])

                nat_S(0)
                for n in range(len(items)):
                    if n + 1 < len(items):
                        nat_S(n + 1)
                    nat_rest(n)
                d = dbgout("o_all", [128, 16 * 512], BF16)
                if d is not None:
                    dma(d, o_all[:].rearrange("p a f -> p (a f)"), B_oall, [YB], sembuf=B_oall[0])
                em.barrier()
                em.emit()

        if STOP not in ("1", "2"):
            with contextlib.ExitStack() as st:
                wr = sbt(st, "wr", [128, 8, 32])
                br = sbt(st, "br", [1, 32])
                mskb = sbt(st, "mskb", [128, 16, 32], BF16)
                gate4 = sbt(st, "gate4", [128, 16, 4])
                eidx = sbt(st, "eidx", [128, 16, 8])
                dsti = sbt(st, "dsti", [128, 64], I32)
                idxW = sbt(st, "idxW", [128, 4, NB], I32)
                ohTall = sbt(st, "ohTall", [32, NB])
                B_wr, B_br, B_mskb, B_gate4, B_eidx, B_dsti, B_idxW, B_ohTall = em.bufs(8, "p4")
                B_hrow = em.bufs(16, "hrow")
                B_x1s = em.bufs(16, "x1s")
                dma(wr[:], w_router.rearrange("(c p) f -> p c f", p=128), [], [B_wr])
                dma(br[:], b_router, [], [B_br])
                with contextlib.ExitStack() as s4:
                    Wout = sbt(s4, "Wout", [128, 8, 1024], BF16)
                    hrow = sbt(s4, "hrow", [128, 16, 1024], BF16)
                    zt = sbt(s4, "zt", [128, 2, 1024], BF16)
                    B_zt, B_Xz = em.bufs(2, "zx")
                    mset(zt[:], 0.0, [B_zt], eng="pool")
                    for cz in range(NB // 2):
                        dma(Xs[cz * 256:(cz + 1) * 256, :].rearrange("(r p) f -> p r f", p=128), zt[:], [B_zt], [B_Xz], sembuf=B_Xz)
                    B_Wout = em.buf("Wout")
                    dma(Wout[:], w_out.rearrange("(c p) f -> p c f", p=128), [], [B_Wout], q="pool")
                    ltri = sbt(s4, "ltri_s", [128, 128], BF16)
                    iota32 = sbt(s4, "iota32_s", [128, 32])
                    thr16 = sbt(s4, "thr16_s", [128, 32, 16])
                    iota96 = sbt(s4, "iota96_s", [128, NB])
                    pidx = sbt(s4, "pidx_s", [128, 1])
                    B_ltri, B_iota32, B_thr16, B_iota96, B_pidx = em.bufs(5, "cst")
                    dma(ltri[:], ltri_d, [], [B_ltri])
                    dma(iota32[:], iota32_d, [], [B_iota32])
                    dma(thr16[:].rearrange("p a b -> p (a b)"), thr16_d, [], [B_thr16])
                    dma(iota96[:], iota96_d, [], [B_iota96])
                    dma(pidx[:], pidx_d, [], [B_pidx])
                    def dbl(fn):
                        return [fn(0), fn(1)]
                    oT_ = dbl(lambda i: sbt(s4, "oT%d" % i, [128, 8, 128], BF16))
                    xt4_ = dbl(lambda i: sbt(s4, "xt4_%d" % i, [128, 1024]))
                    tmp4_ = dbl(lambda i: sbt(s4, "tmp4_%d" % i, [128, 1024]))
                    x1t_ = dbl(lambda i: sbt(s4, "x1t_%d" % i, [128, 1024]))
                    h2t_ = dbl(lambda i: sbt(s4, "h2t_%d" % i, [128, 1024]))
                    h2Tf_ = dbl(lambda i: sbt(s4, "h2Tf_%d" % i, [128, 8, 128]))
                    junk4_ = dbl(lambda i: sbt(s4, "junk4_%d" % i, [128, 1024], BF16))
                    s4s_ = dbl(lambda i: sbt(s4, "s4s_%d" % i, [128, 16]))
                    lg_ = dbl(lambda i: sbt(s4, "lg_%d" % i, [128, 32]))
                    v8_ = dbl(lambda i: sbt(s4, "v8_%d" % i, [128, 8]))
                    i8_ = dbl(lambda i: sbt(s4, "i8_%d" % i, [128, 8], U32))
                    msk_ = dbl(lambda i: sbt(s4, "msk_%d" % i, [128, 32]))
                    e4_ = dbl(lambda i: sbt(s4, "e4_%d" % i, [128, 4]))
                    pto_ = dbl(lambda i: pst(s4, "pto%d" % i, [128, 8, 128], BF16))
                    pmix = pst(s4, "pmix", [128, 1024])
                    ptf = pst(s4, "ptf", [128, 8, 128])
                    plg = pst(s4, "plg", [128, 32])
                    BB = {n: em.bufs(2, "s4" + n) for n in ["oT", "xt4", "tmp4", "x1t", "h2t", "h2Tf", "junk4", "s4s", "lg", "v8", "i8", "msk", "e4", "pto"]}
                    B_pmix, B_ptf, B_plg = em.bufs(3, "s4p")
                    for j in range(16):
                        q2 = j % 2
                        oT, xt4, tmp4, x1t, h2t, h2Tf, junk4, s4s, lg, v8, i8, msk, e4, pto = (
                            oT_[q2], xt4_[q2], tmp4_[q2], x1t_[q2], h2t_[q2], h2Tf_[q2], junk4_[q2], s4s_[q2], lg_[q2], v8_[q2], i8_[q2], msk_[q2], e4_[q2], pto_[q2])
                        (B_oT, B_xt4, B_tmp4, B_x1t, B_h2t, B_h2Tf, B_junk4, B_s4s, B_lg, B_v8, B_i8, B_msk, B_e4, B_pto) = (
                            BB[n][q2] for n in ["oT", "xt4", "tmp4", "x1t", "h2t", "h2Tf", "junk4", "s4s", "lg", "v8", "i8", "msk", "e4", "pto"])
                        pet([(pto[:, c, :], o_all[:, j, c * 128:(c + 1) * 128], identb[:]) for c in range(4)], [B_oall[j], B_identb], [B_pto])
                        cp(oT[:, 0:4, :], pto[:, 0:4, :], [B_pto], [B_oT])
                        mms = []
                        for n in range(2):
                            for c in range(4):
                                mms.append((pmix[:, n * 512:(n + 1) * 512], oTd[:, c, j * 128:(j + 1) * 128], Wout[:, c, n * 512:(n + 1) * 512], c == 0, False))
                            for c in range(4):
                                mms.append((pmix[:, n * 512:(n + 1) * 512], oT[:, c, :], Wout[:, 4 + c, n * 512:(n + 1) * 512], False, c == 3))
                        pe(mms, [B_oT, B_Wout] + [B_oTd[c][j // 4] for c in range(4)], [B_pmix])
                        dma(xt4[:], xq[j * 128:(j + 1) * 128, :], [], [B_xt4])
                        act(junk4[:], pmix[:], AF.Square, [B_pmix], [B_junk4, B_s4s], accum_out=s4s[:, 0:1])
                        ts(s4s[:, 1:2], s4s[:, 0:1], 1.0 / 1024, 1e-6, ALU.mult, ALU.add, [B_s4s], [B_s4s])
                        act(s4s[:, 2:3], s4s[:, 1:2], AF.Sqrt, [B_s4s], [B_s4s])
                        dve(lambda E, s4s=s4s, v8=v8, lg=lg, i8=i8, e4=e4: E.reciprocal(out=s4s[:, 3:4], in_=s4s[:, 2:3]), [B_s4s], [B_s4s])
                        stt(tmp4[:], pmix[:], s4s[:, 3:4], rows[:, 2, :], ALU.mult, ALU.mult, [B_pmix, B_s4s, B_rows], [B_tmp4])
                        tt(x1t[:], tmp4[:], xt4[:], ALU.add, [B_tmp4, B_xt4], [B_x1t])
                        dma(x1s[j * 128:(j + 1) * 128, :], x1t[:], [B_x1t], [B_x1s[j]], sembuf=B_x1s[j])
                        act(junk4[:], x1t[:], AF.Square, [B_x1t], [B_junk4, B_s4s], accum_out=s4s[:, 4:5])
                        ts(s4s[:, 5:6], s4s[:, 4:5], 1.0 / 1024, 1e-6, ALU.mult, ALU.add, [B_s4s], [B_s4s])
                        act(s4s[:, 6:7], s4s[:, 5:6], AF.Sqrt, [B_s4s], [B_s4s])
                        dve(lambda E, s4s=s4s, v8=v8, lg=lg, i8=i8, e4=e4: E.reciprocal(out=s4s[:, 7:8], in_=s4s[:, 6:7]), [B_s4s], [B_s4s])
                        stt(tmp4[:], x1t[:], s4s[:, 7:8], rows[:, 0, :], ALU.mult, ALU.mult, [B_x1t, B_s4s, B_rows], [B_tmp4])
                        tt(h2t[:], tmp4[:], rows[:, 1, :], ALU.add, [B_tmp4, B_rows], [B_h2t])
                        act(hrow[:, j, :], h2t[:], AF.Copy, [B_h2t], [B_hrow[j]])
                        pet([(ptf[:, c, :], h2t[:, c * 128:(c + 1) * 128], identf[:]) for c in range(8)], [B_h2t, B_identf], [B_ptf])
                        cp(h2Tf[:], ptf[:], [B_ptf], [B_h2Tf])
                        mms = [(plg[:], h2Tf[:, c, :], wr[:, c, :], c == 0, False) for c in range(8)]
                        mms.append((plg[:], onesf[0:1, 0:128], br[0:1, :], False, True))
                        pe(mms, [B_h2Tf, B_wr, B_br, B_onesf], [B_plg])
                        cp(lg[:], plg[:], [B_plg], [B_lg])
                        dve(lambda E, s4s=s4s, v8=v8, lg=lg, i8=i8, e4=e4: E.max(out=v8[:], in_=lg[:]), [B_lg], [B_v8])
                        dve(lambda E, s4s=s4s, v8=v8, lg=lg, i8=i8, e4=e4: E.max_index(out=i8[:], in_max=v8[:], in_values=lg[:]), [B_lg, B_v8], [B_i8])
                        cp(eidx[:, j, :], i8[:], [B_i8], [B_eidx])
                        ts(msk[:], lg[:], v8[:, 3:4], None, ALU.is_ge, None, [B_lg, B_v8], [B_msk])
                        cp(mskb[:, j, :], msk[:], [B_msk], [B_mskb])
                        ts(s4s[:, 8:9], v8[:, 0:1], -1.0, None, ALU.mult, None, [B_v8, B_s4s], [B_s4s])
                        act(e4[:], v8[:, 0:4], AF.Exp, [B_v8, B_s4s], [B_e4], bias=s4s[:, 8:9], scale=1.0)
                        dve(lambda E, s4s=s4s, v8=v8, lg=lg, i8=i8, e4=e4: E.reduce_sum(out=s4s[:, 9:10], in_=e4[:], axis=AX.X), [B_e4, B_s4s], [B_s4s])
                        dve(lambda E, s4s=s4s, v8=v8, lg=lg, i8=i8, e4=e4: E.reciprocal(out=s4s[:, 10:11], in_=s4s[:, 9:10]), [B_s4s], [B_s4s])
                        ts(gate4[:, j, :], e4[:], s4s[:, 10:11], None, ALU.mult, None, [B_e4, B_s4s], [B_gate4])
                    cnt_t = sbt(s4, "cnt_t", [128, 32])
                    cmp1 = sbt(s4, "cmp1", [128, 32, 16])
                    nbk = sbt(s4, "nbk", [128, 32])
                    ones32 = sbt(s4, "ones32", [128, 32])
                    cum = sbt(s4, "cum", [128, 32])
                    pstart = sbt(s4, "pstart", [128, 32])
                    cmp2 = sbt(s4, "cmp2", [128, NB, 32])
                    blk = sbt(s4, "blk", [128, NB])
                    chg = sbt(s4, "chg", [128, NB])
                    idxf = sbt(s4, "idxf", [128, 5, NB])
                    chg2 = sbt(s4, "chg2", [128, NB])
                    B_chg2 = em.buf("chg2")
                    destf = sbt(s4, "destf", [128, 16, 32])
                    ohf = sbt(s4, "ohf", [128, 32])
                    dstf = sbt(s4, "dstf", [128, 64])
                    (B_cnt, B_cmp1, B_nbk, B_ones32, B_cum, B_pstart, B_cmp2, B_blk, B_chg, B_idxf, B_destf, B_ohf, B_dstf) = em.bufs(13, "rt")
                    mms = [(plg[:], onesb[:, 0:128], mskb[:, j, :], j == 0, j == 15) for j in range(16)]
                    pe(mms, [B_onesb, B_mskb], [B_plg])
                    cp(cnt_t[:], plg[:], [B_plg], [B_cnt])
                    tt(cmp1[:], cnt_t[:].unsqueeze(2).to_broadcast([128, 32, 16]), thr16[:], ALU.is_gt, [B_cnt, B_thr16], [B_cmp1])
                    dve(lambda E: E.reduce_sum(out=nbk[:], in_=cmp1[:], axis=AX.X), [B_cmp1], [B_nbk])
                    mset(ones32[:], 1.0, [B_ones32])
                    dve(lambda E: E.tensor_tensor_scan(out=cum[:], data0=ones32[:], data1=nbk[:], initial=0.0, op0=ALU.mult, op1=ALU.add),
                        [B_ones32, B_nbk], [B_cum])
                    tt(pstart[:], cum[:], nbk[:], ALU.subtract, [B_cum, B_nbk], [B_pstart])
                    ts(pstart[:], pstart[:], 128.0, None, ALU.mult, None, [B_pstart], [B_pstart])
                    tt(cmp2[:], cum[:].unsqueeze(1).to_broadcast([128, NB, 32]), iota96[:].unsqueeze(2).to_broadcast([128, NB, 32]), ALU.is_le,
                       [B_cum, B_iota96], [B_cmp2])
                    dve(lambda E: E.reduce_sum(out=blk[:], in_=cmp2[:], axis=AX.X), [B_cmp2], [B_blk])
                    ts(blk[:], blk[:], 31.0, None, ALU.min, None, [B_blk], [B_blk])
                    ts(ohTall[:], blk[0:32, :], pidx[0:32, 0:1], None, ALU.is_equal, None, [B_blk, B_pidx], [B_ohTall])
                    mset(chg[:, 0:1], 1.0, [B_chg])
                    tt(chg[:, 1:NB], blk[:, 1:NB], blk[:, 0:NB - 1], ALU.not_equal, [B_blk, B_chg], [B_chg])
                    mset(chg2[:, 0:2], 1.0, [B_chg2])
                    tt(chg2[:, 2:NB], blk[:, 2:NB], blk[:, 0:NB - 2], ALU.not_equal, [B_blk, B_chg2], [B_chg2])
                    ts(chg[:], chg[:], -float(2 ** 27), float(2 ** 27), ALU.mult, ALU.add, [B_chg], [B_chg])
                    ts(chg2[:], chg2[:], -float(2 ** 27), float(2 ** 27), ALU.mult, ALU.add, [B_chg2], [B_chg2])
                    ts(blk[:], blk[:], 128.0, pidx[:, 0:1], ALU.mult, ALU.add, [B_blk, B_pidx], [B_blk])
                    tt(idxf[:, 4, :], blk[:], chg[:], ALU.add, [B_blk, B_chg], [B_idxf])
                    ts(idxf[:, 0, :], idxf[:, 4, :], 2.0, None, ALU.mult, None, [B_idxf], [B_idxf])
                    ts(idxf[:, 1, :], idxf[:, 4, :], 2.0, 1.0, ALU.mult, ALU.add, [B_idxf], [B_idxf])
                    tt(idxf[:, 4, :], blk[:], chg2[:], ALU.add, [B_blk, B_chg2, B_idxf], [B_idxf])
                    ts(idxf[:, 2, :], idxf[:, 4, :], 2.0, None, ALU.mult, None, [B_idxf], [B_idxf])
                    ts(idxf[:, 3, :], idxf[:, 4, :], 2.0, 1.0, ALU.mult, ALU.add, [B_idxf], [B_idxf])
                    cp(idxW[:], idxf[:, 0:4, :], [B_idxf], [B_idxW])
                    for j in range(16):
                        mms = [(plg[:], onesb[:, 0:128], mskb[:, jj, :], jj == 0, False) for jj in range(j)]
                        mms.append((plg[:], ltri[:], mskb[:, j, :], j == 0, True))
                        pe(mms, [B_onesb, B_ltri, B_mskb], [B_plg])
                        tt(destf[:, j, :], plg[:], pstart[:], ALU.add, [B_plg, B_pstart], [B_destf])
                    for j in range(16):
                        for k in range(4):
                            ts(ohf[:], iota32[:], eidx[:, j, k:k + 1], None, ALU.is_equal, None, [B_iota32, B_eidx], [B_ohf])
                            tt(ohf[:], ohf[:], destf[:, j, :], ALU.mult, [B_ohf, B_destf], [B_ohf])
                            dve(lambda E, j=j, k=k: E.reduce_sum(out=dstf[:, 4 * j + k:4 * j + k + 1], in_=ohf[:], axis=AX.X), [B_ohf], [B_dstf])
                    cp(dsti[:], dstf[:], [B_dstf], [B_dsti])
                    B_XO = em.buf("XO")
                    for j in range(16):
                        for k in range(4):
                            col = dsti[:, 4 * j + k:4 * j + k + 1]
                            bsc = em.buf("sc")
                            em.dma("pool", lambda E, j=j, col=col: [E.indirect_dma_start(
                                out=Xs, out_offset=bass.IndirectOffsetOnAxis(ap=col, axis=0), in_=hrow[:, j, :], in_offset=None)],
                                reads=[B_hrow[j], B_dsti, B_Xz], writes=[bsc], sembuf=B_XO)
                    for nm, srct, bb in (("dsti", dsti, B_dsti), ("idxW", idxW, B_idxW)):
                        d = dbgout(nm, [128, srct.shape[1] * (srct.shape[2] if len(srct.shape) > 2 else 1)], I32)
                        if d is not None:
                            dma(d, srct[:] if len(srct.shape) == 2 else srct[:].rearrange("p a b -> p (a b)"), [bb], [YB], sembuf=bb)
                    d = dbgout("gate4", [128, 64])
                    if d is not None:
                        dma(d, gate4[:].rearrange("p a b -> p (a b)"), [B_gate4], [YB], sembuf=B_gate4)
                    em.barrier()
                    em.emit()
                with contextlib.ExitStack() as s5:
                    wb1s = [sbt(s5, "wb1_%d" % i, [128, 8, 2048], BF16) for i in range(2)]
                    B_wb1 = [em.bufs(2, "wb1p%d" % i) for i in range(2)]
                    wb2 = sbt(s5, "wb2", [128, 9, 1024], BF16)
                    b1all = sbt(s5, "b1all", [32, 2048], BF16)
                    b2all = sbt(s5, "b2all", [32, 1024], BF16)
                    xbk = [sbt(s5, "xbk%d" % i, [128, 1024], BF16) for i in range(2)]
                    xT = [sbt(s5, "xT%d" % i, [128, 8, 128], BF16) for i in range(2)]
                    ohT = [sbt(s5, "ohT%d" % i, [32, 128], BF16) for i in range(2)]
                    glu = sbt(s5, "glu", [128, 1024])
                    lin = sbt(s5, "lin", [128, 1024])
                    sig = sbt(s5, "sig", [128, 1024], BF16)
                    ab = sbt(s5, "ab", [128, 1024], BF16)
                    aT = sbt(s5, "aT", [128, 8, 128], BF16)
                    yb = [sbt(s5, "yb%d" % i, [128, 1024]) for i in range(2)]
                    TX = pst(s5, "TX", [128, 8, 128], BF16)
                    TA = pst(s5, "TA", [128, 8, 128], BF16)
                    H = pst(s5, "H", [128, 2048])
                    Y = pst(s5, "Y", [128, 1024])
                    Ybf = Y.bitcast(BF16)
                    B_wb1a, B_wb1b, B_wb2, B_b1all, B_b2all, B_glu, B_lin, B_sig, B_ab, B_aT, B_TX, B_TA, B_H, B_Y, B_Ys = em.bufs(15, "s5")
                    B_xbk = em.bufs(2, "xbk"); B_obk = em.bufs(2, "obk"); B_xT = em.bufs(2, "xT"); B_ohT = em.bufs(2, "ohT"); B_yb = em.bufs(2, "yb")
                    bcreg = s5.enter_context(nc.gpsimd.register("bcreg"))
                    em.streams["pool"].append(lambda E: E.reg_mov(bcreg, 8191))
                    dma(b1all[:], b1, [], [B_b1all], q="pool")
                    dma(b2all[:], b2, [], [B_b2all], q="pool")
                    ab2 = [ab, sbt(s5, "ab_b", [128, 1024], BF16)]
                    B_ab2 = [B_ab, em.buf("ab_b")]

                    def loads(i):
                        p = i % 2
                        dma(xbk[p][:], Xs[i * 128:(i + 1) * 128, :], [], [B_xbk[p]])

                    def gathers(i):
                        for hh in range(2):
                            em.dma("pool", lambda E, i=i, hh=hh: [E.indirect_dma_start(
                                out=wb1s[i % 2][:, 4 * hh:4 * hh + 4, :].rearrange("p c f -> p (c f)"), out_offset=None, in_=W1p,
                                in_offset=bass.IndirectOffsetOnAxis(ap=idxW[:, 2 + hh, i:i + 1], axis=0), bounds_check=bcreg, oob_is_err=False)],
                                reads=[B_idxW], writes=[B_wb1[i % 2][hh]])

                    def gathers2(i):
                        for hh in range(2):
                            em.dma("pool", lambda E, i=i, hh=hh: [E.indirect_dma_start(
                                out=wb2[:, 4 * hh:4 * hh + 4, :].rearrange("p c f -> p (c f)"), out_offset=None, in_=W2p,
                                in_offset=bass.IndirectOffsetOnAxis(ap=idxW[:, hh, i:i + 1], axis=0), bounds_check=bcreg, oob_is_err=False)],
                                reads=[B_idxW], writes=[B_wb2])

                    def stageA(i):
                        p = i % 2
                        if i + 1 < NB:
                            loads(i + 1)
                        pet([(TX[:, c, :], xbk[p][:, c * 128:(c + 1) * 128], identb[:]) for c in range(8)], [B_xbk[p], B_identb], [B_TX])
                        cp(xT[p][:], TX[:], [B_TX], [B_xT[p]])
                        cp(ohT[p][:], ohTall[:, i:i + 1].to_broadcast([32, 128]), [B_ohTall], [B_ohT[p]])
                        mms = []
                        for n in range(4):
                            for c in range(8):
                                mms.append((H[:, n * 512:(n + 1) * 512], xT[p][:, c, :], wb1s[p][:, c, n * 512:(n + 1) * 512], c == 0, False))
                            mms.append((H[:, n * 512:(n + 1) * 512], ohT[p][:], b1all[:, n * 512:(n + 1) * 512], False, True))
                        pe(mms, [B_xT[p], B_ohT[p], B_wb1[p][0], B_wb1[p][1], B_b1all], [B_H])
                        if i + 2 < NB:
                            gathers(i + 2)
                        ts(glu[:], H[:, 0:2048:2], 7.0, None, ALU.min, None, [B_H], [B_glu])
                        ts(lin[:], H[:, 1:2048:2], -7.0, 7.0, ALU.max, ALU.min, [B_H], [B_lin])
                        act(sig[:], glu[:], AF.Sigmoid, [B_glu], [B_sig], scale=1.702)
                        stt(lin[:], lin[:], 1.0, glu[:], ALU.add, ALU.mult, [B_lin, B_glu], [B_lin])
                        tt(ab2[p][:], lin[:], sig[:], ALU.mult, [B_lin, B_sig], [B_ab2[p]])

                    def stageB(i):
                        p = i % 2
                        pet([(TA[:, c, :], ab2[p][:, c * 128:(c + 1) * 128], identb[:]) for c in range(8)], [B_ab2[p], B_identb], [B_TA])
                        act(aT[:], TA[:], AF.Copy, [B_TA], [B_aT])
                        mms = []
                        for n in range(2):
                            for c in range(8):
                                mms.append((Y[:, n * 512:(n + 1) * 512], aT[:, c, :], wb2[:, c, n * 512:(n + 1) * 512], c == 0, False))
                            mms.append((Y[:, n * 512:(n + 1) * 512], ohT[p][:], b2all[:, n * 512:(n + 1) * 512], False, True))
                        pe(mms, [B_aT, B_ohT[p], B_wb2, B_b2all], [B_Y])
                        if i + 1 < NB:
                            gathers2(i + 1)
                        act(yb[p][:], Y[:], AF.Copy, [B_Y], [B_yb[p]])
                        dma(Ys[i * 128:(i + 1) * 128, :], yb[p][:], [B_yb[p]], [B_Ys], sembuf=B_yb[p])

                    loads(0)
                    gathers(0)
                    gathers(1)
                    gathers2(0)
                    stageA(0)
                    for i in range(NB):
                        if i + 1 < NB:
                            stageA(i + 1)
                        stageB(i)
                    em.barrier()
                    em.emit()
                with contextlib.ExitStack() as s6:
                    x1r = [sbt(s6, "x1r%d" % i, [128, 1024]) for i in range(2)]
                    ot = [sbt(s6, "ot%d" % i, [128, 1024]) for i in range(2)]
                    yk = [sbt(s6, "yk%d" % i, [128, 4, 1024]) for i in range(2)]
                    ft = sbt(s6, "ft", [128, 1024])
                    junk6 = sbt(s6, "junk6", [128, 1024], BF16)
                    s6s = sbt(s6, "s6s", [128, 4])
                    B_x1r = em.bufs(2, "x1r"); B_ot = em.bufs(2, "ot"); B_yk = [em.bufs(4, "yk%d" % i) for i in range(2)]
                    B_ft, B_junk6, B_s6s = em.bufs(3, "s6")
                    for j in range(16):
                        i = j % 2
                        dma(x1r[i][:], x1s[j * 128:(j + 1) * 128, :], [B_x1s[j]], [B_x1r[i]])
                        for k in range(4):
                            em.dma("pool", lambda E, i=i, j=j, k=k: [E.indirect_dma_start(
                                out=yk[i][:, k, :], out_offset=None, in_=Ys,
                                in_offset=bass.IndirectOffsetOnAxis(ap=dsti[:, 4 * j + k:4 * j + k + 1], axis=0))],
                                reads=[B_dsti], writes=[B_yk[i][k]])
                        ts(ft[:], yk[i][:, 0, :], gate4[:, j, 0:1], None, ALU.mult, None, [B_yk[i][0], B_gate4], [B_ft])
                        for k in range(1, 4):
                            stt(ft[:], yk[i][:, k, :], gate4[:, j, k:k + 1], ft[:], ALU.mult, ALU.add, [B_yk[i][k], B_gate4, B_ft], [B_ft])
                        act(junk6[:], ft[:], AF.Square, [B_ft], [B_junk6, B_s6s], accum_out=s6s[:, 0:1])
                        ts(s6s[:, 1:2], s6s[:, 0:1], 1.0 / 1024, 1e-6, ALU.mult, ALU.add, [B_s6s], [B_s6s])
                        act(s6s[:, 2:3], s6s[:, 1:2], AF.Sqrt, [B_s6s], [B_s6s])
                        dve(lambda E: E.reciprocal(out=s6s[:, 3:4], in_=s6s[:, 2:3]), [B_s6s], [B_s6s])
                        stt(ot[i][:], ft[:], s6s[:, 3:4], rows[:, 3, :], ALU.mult, ALU.mult, [B_ft, B_s6s, B_rows], [B_ot[i]])
                        tt(ot[i][:], ot[i][:], x1r[i][:], ALU.add, [B_ot[i], B_x1r[i]], [B_ot[i]])
                        dma(out[j * 128:(j + 1) * 128, :], ot[i][:], [B_ot[i]], [YB], sembuf=B_ot[i])
                    em.barrier()
                    em.emit()
        else:
            mz = sbt(top, "mz", [128, 1024])
            B_mz = em.buf("mz")
            mset(mz[:], 0.0, [B_mz])
            for j in range(16):
                dma(out[j * 128:(j + 1) * 128, :], mz[:], [B_mz], [YB], sembuf=B_mz)
            for nm, src in (("Qs", Qs), ("Ks", Ks), ("NQs", NQs), ("NKs", NKs), ("Vs", Vs), ("NVs", NVs)):
                d = dbgout(nm, src.shape, BF16)
                if d is not None:
                    dma(d, src, [], [YB], sembuf=YB)
            em.barrier()
            em.emit()
    return nc, dbg


SLOPES = [2.0 ** (-2 * (h + 1)) for h in range(4)]
_CACHE = {}


def _bf(a):
    return np.ascontiguousarray(a.astype(ml_dtypes.bfloat16))


def _nat_tables(rpb, qr):
    R0 = 32 * qr
    def table(r0):
        t = np.full((128, 8, 7, 128), -30000.0, np.float32)
        qrow = np.repeat(np.array([r0, r0 + 1]), 64)
        qcol = np.tile(np.arange(64), 2)
        qstart = np.clip(qrow - 4, 0, 120)
        qcs = np.clip(qcol - 8, 0, 48)
        for o in range(7):
            krow = np.repeat(np.array([r0 - 6 + 2 * o, r0 - 5 + 2 * o]), 64)
            kcol = np.tile(np.arange(64), 2)
            valid = ((krow[:, None] >= qstart[None, :]) & (krow[:, None] < qstart[None, :] + 8)
                     & (krow[:, None] >= 0) & (krow[:, None] < 128)
                     & (kcol[:, None] >= qcs[None, :]) & (kcol[:, None] < qcs[None, :] + 16))
            dr = np.clip(krow[:, None] - qrow[None, :] + 7, 0, 14)
            dc = np.clip(kcol[:, None] - qcol[None, :], -15, 15) + 15
            for h in range(8):
                vals = rpb[h][dr, dc]
                t[:, h, o, :] = np.where(valid, vals, -30000.0)
        return t
    slots = [table(R0 + 2 * 6)] + [table(R0 + 2 * j) for j in (0, 1, 14, 15)]
    return _bf(np.stack(slots).reshape(5, 128, 8 * 7 * 128))


def _prep(inputs):
    f32 = np.float32
    x = np.asarray(inputs["x"], f32)
    c = np.asarray(inputs["c"], f32)
    shared = dict(
        w_ada=np.ascontiguousarray(inputs["w_ada"][0], f32), b_ada=np.ascontiguousarray(inputs["b_ada"], f32).reshape(1, 6144),
        g_pre_mix=np.asarray(inputs["g_pre_mix"], f32).reshape(1, 1024), g_post_mix=np.asarray(inputs["g_post_mix"], f32).reshape(1, 1024),
        g_pre_ffn=np.asarray(inputs["g_pre_ffn"], f32).reshape(1, 1024), g_post_ffn=np.asarray(inputs["g_post_ffn"], f32).reshape(1, 1024),
        w_in=np.ascontiguousarray(inputs["w_in"][0], f32), w_out=np.ascontiguousarray(inputs["w_out"][0], f32),
        lamv=np.concatenate([np.asarray(inputs[k], f32).reshape(-1) for k in ("lam_q1", "lam_k1", "lam_q2", "lam_k2")]).reshape(1, 256),
        g_subln=np.asarray(inputs["g_subln"], f32).reshape(1, 128),
        w_router=np.ascontiguousarray(inputs["w_router"][0], f32), b_router=np.asarray(inputs["b_router"], f32).reshape(1, 32),
        identb=_bf(np.eye(128, dtype=f32)), identf=np.eye(128, dtype=f32),
        ltri=_bf(np.triu(np.ones((128, 128), f32), 1)),
        iota32=np.tile(np.arange(32, dtype=f32), (128, 1)),
        thr16=np.tile((128.0 * np.arange(16, dtype=f32))[None, None, :], (128, 32, 1)).reshape(128, 512),
        iota96=np.tile(np.arange(NB, dtype=f32), (128, 1)),
        pidx=np.arange(128, dtype=f32).reshape(128, 1),
    )
    if os.environ.get("KSTOP", "") not in ("1", "2"):
        shared.update(
            W1p=np.ascontiguousarray(np.asarray(inputs["w1"][0], f32).reshape(32, 8, 128, 2048).transpose(0, 2, 1, 3)).reshape(8192, 8192),
            W2p=np.ascontiguousarray(np.asarray(inputs["w2"][0], f32).reshape(32, 8, 128, 1024).transpose(0, 2, 1, 3)).reshape(8192, 4096),
            b1=np.ascontiguousarray(inputs["b1"][0], f32), b2=np.ascontiguousarray(inputs["b2"][0], f32))
    kl = np.arange(128)
    bd = np.stack([-SLOPES[h] * np.abs(kl[:, None] - kl[None, :]) for h in range(4)], axis=1)
    shared["bdiag"] = _bf(bd.reshape(128, 512).astype(f32))
    rpb = np.asarray(inputs["nat_rpb"], f32)[0]
    in_maps = []
    for core in range(8):
        b, qr = core // 4, core % 4
        own = np.arange(qr * 2048, (qr + 1) * 2048)
        rest = np.concatenate([np.arange(0, qr * 2048), np.arange((qr + 1) * 2048, 8192)])
        perm = np.concatenate([own, rest])
        R0 = 32 * qr
        tok0 = (R0 - 6) * 64
        xw = np.zeros((2816, 1024), f32)
        lo, hi = max(tok0, 0), min(tok0 + 2816, 8192)
        xw[lo - tok0:hi - tok0] = x[b, lo:hi]
        ql = np.arange(2048)
        q_lo = (ql % 128).astype(f32)
        qt_abs = (qr * 16 + ql // 128).astype(f32)
        qa = np.zeros((4, 3, 2, 2048), f32)
        for h in range(4):
            qa[h, 0, 0] = -SLOPES[h] * q_lo
            qa[h, 0, 1] = -SLOPES[h] * 128.0 * qt_abs
            qa[h, 1] = -qa[h, 0]
        kabs = perm.astype(f32)
        ktile_abs = perm[::128] // 128
        sig = np.ones(64, f32)
        sig[16:] = np.where(ktile_abs[16:] < qr * 16, 1.0, -1.0)
        ka = np.ones((2, 8192), f32) * np.repeat(sig, 128)[None, :]
        kt_tab = np.zeros((128, 4, 2, 64), f32)
        kpos = kabs.reshape(64, 128).T
        for h in range(4):
            kt_tab[:, h, 0, :] = SLOPES[h] * kpos * sig[None, :]
            kt_tab[:, h, 1, :] = -SLOPES[h] * kpos
        m = dict(shared)
        m.update(
            xb=np.ascontiguousarray(x[b][perm]), xq=np.ascontiguousarray(x[b, own]), xw=xw,
            cT=np.ascontiguousarray(c[b].reshape(8, 128).T),
            natb=_nat_tables(rpb, qr), qaug=_bf(qa), kaug=_bf(ka), ktab=kt_tab.reshape(128, 512),
        )
        in_maps.append(m)
    return in_maps


def kernel(**inputs):
    debug = tuple(os.environ.get("KDEBUG", "").split(",")) if os.environ.get("KDEBUG") else ()
    key = (debug, os.environ.get("KSTOP", ""))
    if key not in _CACHE:
        _CACHE[key] = build_program(debug)
    nc, dbg = _CACHE[key]
    in_maps = _prep(inputs)
    res = run_bass_kernel_spmd(nc, in_maps, core_ids=list(range(8)))
    outs = [np.asarray(r["out"], np.float32) for r in res.results]
    full = np.stack([np.concatenate(outs[0:4], axis=0), np.concatenate(outs[4:8], axis=0)], axis=0)
    if debug:
        kernel.last_debug = [{k: np.asarray(r["dbg_" + k]) for k in dbg} for r in res.results]
    return full
```

```python
import contextlib
import os
import numpy as np
import ml_dtypes
import concourse.bass as bass
import concourse.mybir as mybir
from concourse.bass_utils import run_bass_kernel_spmd

F32 = mybir.dt.float32
BF16 = mybir.dt.bfloat16
I32 = mybir.dt.int32
U32 = mybir.dt.uint32
AF = mybir.ActivationFunctionType
ALU = mybir.AluOpType
AX = mybir.AxisListType

NB = 96
BIG = float(2 ** 30)


class Buf:
    __slots__ = ("name", "w", "rs", "dsem", "dcnt")

    def __init__(self, name):
        self.name = name
        self.w = []
        self.rs = []
        self.dsem = None
        self.dcnt = 0


class Em:
    ENG = ("pe", "act", "dve", "pool", "sp")

    def __init__(self, nc, stack):
        self.nc = nc
        self.stack = stack
        self.streams = {e: [] for e in self.ENG}
        self.cnt = {e: 0 for e in self.ENG}
        self.esem = {e: stack.enter_context(nc.semaphore("sem_" + e)) for e in self.ENG}
        self.waited = {e: {} for e in self.ENG}
        self.nbuf = 0
        self.dbufs = []

    def buf(self, name=None):
        self.nbuf += 1
        return Buf("%s_%d" % (name or "b", self.nbuf))

    def bufs(self, n, name=None):
        return [self.buf(name) for _ in range(n)]

    def _dsem(self, b):
        if b.dsem is None:
            b.dsem = self.stack.enter_context(self.nc.semaphore("d_" + b.name))
            self.dbufs.append(b)
        return b.dsem

    def _deps(self, eng, reads, writes):
        toks = {}

        def add(t):
            key, val, h = t
            if eng == "pe" and key == "pe":
                return
            if key not in toks or toks[key][1] < val:
                toks[key] = t
        for b in reads:
            for t in b.w:
                add(t)
        for b in writes:
            for t in b.w:
                add(t)
            for t in b.rs:
                add(t)
        return self._filter(eng, toks.values())

    def _filter(self, eng, toks):
        out = []
        wd = self.waited[eng]
        for key, val, h in toks:
            if wd.get(key, 0) >= val:
                continue
            wd[key] = val
            out.append((h, val))
        return out

    def _update(self, tok, reads, writes):
        for b in reads:
            b.rs.append(tok)
        for b in writes:
            b.w = [tok]
            b.rs = []

    def op(self, eng, fn, reads=(), writes=()):
        waits = self._deps(eng, reads, writes)
        self.cnt[eng] += 1
        sem = self.esem[eng]
        tok = (eng, self.cnt[eng], sem)

        def run(E, fn=fn, waits=waits, sem=sem):
            for h, v in waits:
                E.wait_ge(h, v)
            fn(E).then_inc(sem, 1)
        self.streams[eng].append(run)
        self._update(tok, reads, writes)

    def dma(self, q, fn, reads=(), writes=(), n=1, sembuf=None):
        waits = self._deps(q, reads, writes)
        sb = sembuf if sembuf is not None else (writes[0] if writes else reads[0])
        sem = self._dsem(sb)
        sb.dcnt += 16 * n
        tok = ("d_" + sb.name, sb.dcnt, sem)

        def run(E, fn=fn, waits=waits, sem=sem, n=n):
            for h, v in waits:
                E.wait_ge(h, v)
            lst = fn(E)
            assert len(lst) == n
            for ins in lst:
                ins.then_inc(sem, 16)
        self.streams[q].append(run)
        self._update(tok, reads, writes)

    def barrier(self):
        toks = [(e, self.cnt[e], self.esem[e]) for e in self.ENG if self.cnt[e] > 0]
        toks += [("d_" + b.name, b.dcnt, b.dsem) for b in self.dbufs]
        for eng in self.ENG:
            waits = self._filter(eng, [t for t in toks if t[0] != eng])

            def run(E, waits=waits):
                for h, v in waits:
                    E.wait_ge(h, v)
            self.streams[eng].append(run)

    def emit(self):
        nc = self.nc
        st = self.streams
        with nc.Block() as block:
            @block.sync
            def _(E):
                for f in st["sp"]:
                    f(E)

            @block.tensor
            def _(E):
                for f in st["pe"]:
                    f(E)

            @block.scalar
            def _(E):
                for f in st["act"]:
                    f(E)

            @block.vector
            def _(E):
                for f in st["dve"]:
                    f(E)

            @block.gpsimd
            def _(E):
                for f in st["pool"]:
                    f(E)
        self.streams = {e: [] for e in self.ENG}


def build_program(debug=()):
    nc = bass.Bass("TRN2", target_bir_lowering=False)

    def din(name, shape, dt=F32):
        return nc.dram_tensor(name, list(shape), dt, kind="ExternalInput").ap()

    def dscr(name, shape, dt):
        return nc.dram_tensor(name, list(shape), dt, kind="Internal").ap()

    xb = din("xb", [8192, 1024])
    xq = din("xq", [2048, 1024])
    xw = din("xw", [2816, 1024])
    cT = din("cT", [128, 8])
    w_ada = din("w_ada", [1024, 6144])
    b_ada = din("b_ada", [1, 6144])
    g_pre_mix = din("g_pre_mix", [1, 1024])
    g_post_mix = din("g_post_mix", [1, 1024])
    g_pre_ffn = din("g_pre_ffn", [1, 1024])
    g_post_ffn = din("g_post_ffn", [1, 1024])
    w_in = din("w_in", [1024, 3072])
    w_out = din("w_out", [1024, 1024])
    lamv = din("lamv", [1, 256])
    g_subln = din("g_subln", [1, 128])
    natb = din("natb", [5, 128, 8 * 7 * 128], BF16)
    w_router = din("w_router", [1024, 32])
    b_router = din("b_router", [1, 32])
    if os.environ.get("KSTOP", "") not in ("1", "2"):
        W1p = din("W1p", [8192, 8192])
        W2p = din("W2p", [8192, 4096])
        b1 = din("b1", [32, 2048])
        b2 = din("b2", [32, 1024])
    qaug = din("qaug", [4, 3, 2, 2048], BF16)
    kaug = din("kaug", [2, 8192], BF16)
    ktab = din("ktab", [128, 4 * 2 * 64])
    bdiag = din("bdiag", [128, 4 * 128], BF16)
    identb_d = din("identb", [128, 128], BF16)
    identf_d = din("identf", [128, 128])
    ltri_d = din("ltri", [128, 128], BF16)
    iota32_d = din("iota32", [128, 32])
    thr16_d = din("thr16", [128, 512])
    iota96_d = din("iota96", [128, NB])
    pidx_d = din("pidx", [128, 1])
    out = nc.dram_tensor("out", [2048, 1024], F32, kind="ExternalOutput").ap()

    Qs = dscr("Qs", [4, 128, 2048], BF16)
    Ks = dscr("Ks", [4, 128, 8192], BF16)
    Vs = dscr("Vs", [4, 128, 64, 130], BF16)
    NQs = dscr("NQs", [4, 128, 2048], BF16)
    NKs = dscr("NKs", [4, 128, 2816], BF16)
    NVs = dscr("NVs", [128, 22, 8 * 66], BF16)
    x1s = dscr("x1s", [2048, 1024], F32)
    Xs = dscr("Xs", [NB * 128, 1024], BF16)
    Os = dscr("Os", [NB * 128, 32], BF16)
    Ys = dscr("Ys", [NB * 128, 1024], F32)

    dbg = {}

    def dbgout(name, shape, dt=F32):
        if name in debug:
            dbg[name] = nc.dram_tensor("dbg_" + name, list(shape), dt, kind="ExternalOutput").ap()
            return dbg[name]
        return None

    with contextlib.ExitStack() as top:
        em = Em(nc, top)
        YB = em.buf("yout")

        def sbt(st, name, shape, dt=F32):
            return st.enter_context(nc.sbuf_tensor(name, list(shape), dt))

        def pst(st, name, shape, dt=F32):
            return st.enter_context(nc.psum_tensor(name, list(shape), dt))

        def dma(out_, in_, reads, writes, q="sp", sembuf=None):
            em.dma(q, lambda E: [E.dma_start(out=out_, in_=in_)], reads=reads, writes=writes, sembuf=sembuf)

        def act(out_, in_, func, reads, writes, **kw):
            em.op("act", lambda E: E.activation(out=out_, in_=in_, func=func, **kw), reads, writes)

        def pe(mms, reads, writes):
            def fn(E):
                ins = None
                for (o, l, r, s0, s1) in mms:
                    ins = E.matmul(o, lhsT=l, rhs=r, start=s0, stop=s1)
                return ins
            em.op("pe", fn, reads, writes)

        def pet(trs, reads, writes):
            def fn(E):
                ins = None
                for (o, i, idn) in trs:
                    ins = E.transpose(o, i, idn)
                return ins
            em.op("pe", fn, reads, writes)

        def dve(f, reads, writes, eng="dve"):
            em.op(eng, f, reads, writes)

        def ts(out_, in0, s1, s2, op0, op1=None, reads=(), writes=(), eng="dve"):
            if op1 is None:
                dve(lambda E: E.tensor_scalar(out=out_, in0=in0, scalar1=s1, scalar2=None, op0=op0), reads, writes, eng)
            else:
                dve(lambda E: E.tensor_scalar(out=out_, in0=in0, scalar1=s1, scalar2=s2, op0=op0, op1=op1), reads, writes, eng)

        def tt(out_, in0, in1, op, reads, writes, eng="dve"):
            dve(lambda E: E.tensor_tensor(out=out_, in0=in0, in1=in1, op=op), reads, writes, eng)

        def stt(out_, in0, scalar, in1, op0, op1, reads, writes):
            dve(lambda E: E.scalar_tensor_tensor(out=out_, in0=in0, scalar=scalar, in1=in1, op0=op0, op1=op1), reads, writes)

        def cp(out_, in_, reads, writes, eng="dve"):
            dve(lambda E: E.tensor_copy(out=out_, in_=in_), reads, writes, eng)

        def mset(ap, v, writes, eng="dve"):
            dve(lambda E: E.memset(ap, v), (), writes, eng)

        def dump(name, dst_shape_src):
            pass

        identb = sbt(top, "identb_s", [128, 128], BF16)
        identf = sbt(top, "identf_s", [128, 128])
        onesb = sbt(top, "onesb", [128, 512], BF16)
        onesf = sbt(top, "onesf", [128, 128])
        rows = sbt(top, "rows", [128, 4, 1024])
        o_all = sbt(top, "o_all", [128, 16, 512], BF16)
        stat = sbt(top, "stat", [1, 16])
        negM = sbt(top, "negM", [128, 8])
        neglam = sbt(top, "neglam", [128, 1])
        gsub = sbt(top, "gsub", [128, 128])
        B_identb, B_identf, B_onesb, B_onesf, B_rows, B_stat, B_negM, B_neglam, B_gsub = em.bufs(9, "const")
        B_oall = em.bufs(16, "oall")
        oTd = sbt(top, "oTd", [128, 4, 2048], BF16)
        B_oTd = [em.bufs(4, "oTd%d" % h) for h in range(4)]
        g8col = sbt(top, "g8col", [128, 1])
        B_g8col = em.buf("g8col")
        dma(g8col[:], g_subln.rearrange("o e -> e o"), [], [B_g8col])
        ts(g8col[:], g8col[:], 0.8, None, ALU.mult, None, [B_g8col], [B_g8col])
        dma(identb[:], identb_d, [], [B_identb])
        dma(identf[:], identf_d, [], [B_identf])
        mset(onesb[:], 1.0, [B_onesb])
        mset(onesf[:], 1.0, [B_onesf])
        mset(stat[:], 0.0, [B_stat])
        dma(gsub[:], g_subln.to_broadcast([128, 128]), [], [B_gsub])

        with contextlib.ExitStack() as st01:
            Wall = sbt(st01, "Wall", [128, 8, 3072], BF16)
            biasrow = sbt(st01, "biasrow", [1, 3072], BF16)
            B_Wall = em.bufs(8, "Wall")
            B_biasrow = em.buf("biasrow")
            with contextlib.ExitStack() as st:
                sil = sbt(st, "sil", [128, 8])
                silrep = sbt(st, "silrep", [128, 8, 128], BF16)
                wada = [sbt(st, "wada%d" % i, [128, 8, 512], BF16) for i in range(2)]
                bada = sbt(st, "bada", [1, 6144], BF16)
                modrow = sbt(st, "modrow", [128, 6144])
                grow = sbt(st, "grow", [128, 4, 1024])
                s1row = sbt(st, "s1row", [128, 1024])
                tmpd = sbt(st, "tmpd", [128, 128])
                s1T = sbt(st, "s1T", [128, 8])
                sh1T = sbt(st, "sh1T", [128, 8])
                wst = [sbt(st, "wst0", [128, 3072])] * 2
                lam_t = sbt(st, "lam_t", [1, 256])
                lam_s = sbt(st, "lam_s", [1, 8])
                pmod = [pst(st, "pmod%d" % i, [128, 512]) for i in range(2)]
                pbias = pst(st, "pbias", [1, 3072])
                B_sil, B_silrep, B_bada, B_grow, B_s1row, B_tmpd, B_s1T, B_sh1T, B_lamt, B_lams, B_pbias, B_plam = em.bufs(12, "p0")
                B_wada = em.bufs(2, "wada")
                B_modrow = em.bufs(12, "modrow")
                B_wst = [em.buf("wst")] * 2
                B_pmod = em.bufs(2, "pmod")

                dma(sil[:], cT, [], [B_sil])
                act(sil[:], sil[:], AF.Silu, [B_sil], [B_sil])
                for c in range(8):
                    cp(silrep[:, c, :], sil[:, c:c + 1].to_broadcast([128, 128]), [B_sil], [B_silrep])
                dma(bada[:], b_ada, [], [B_bada], q="pool")
                for i, g in enumerate([g_pre_mix, g_post_mix, g_pre_ffn, g_post_ffn]):
                    dma(grow[:, i, :], g.to_broadcast([128, 1024]), [], [B_grow])
                for j in range(12):
                    wb = wada[j % 2]
                    dma(wb[:], w_ada[:, j * 512:(j + 1) * 512].rearrange("(c p) f -> p c f", p=128), [], [B_wada[j % 2]], q="pool")
                    mms = [(pmod[j % 2][:], silrep[:, c, :], wb[:, c, :], c == 0, False) for c in range(8)]
                    mms.append((pmod[j % 2][:], onesb[0:1, 0:128], bada[0:1, j * 512:(j + 1) * 512], False, True))
                    pe(mms, [B_silrep, B_wada[j % 2], B_bada, B_onesb], [B_pmod[j % 2]])
                    cp(modrow[:, j * 512:(j + 1) * 512], pmod[j % 2][:], [B_pmod[j % 2]], [B_modrow[j]])
                MR = lambda i: [B_modrow[2 * i], B_modrow[2 * i + 1]]
                m = lambda i: modrow[:, i * 1024:(i + 1) * 1024]
                stt(s1row[:], m(1), 1.0, grow[:, 0, :], ALU.add, ALU.mult, MR(1) + [B_grow], [B_s1row])
                stt(rows[:, 0, :], m(4), 1.0, grow[:, 2, :], ALU.add, ALU.mult, MR(4) + [B_grow], [B_rows])
                cp(rows[:, 1, :], m(3), MR(3) + [B_rows], [B_rows])
                tt(rows[:, 2, :], m(2), grow[:, 1, :], ALU.mult, MR(2) + [B_grow, B_rows], [B_rows])
                tt(rows[:, 3, :], m(5), grow[:, 3, :], ALU.mult, MR(5) + [B_grow, B_rows], [B_rows])
                for c in range(8):
                    tt(tmpd[:], s1row[:, c * 128:(c + 1) * 128], identf[:], ALU.mult, [B_s1row, B_identf], [B_tmpd])
                    dve(lambda E, c=c: E.reduce_sum(out=s1T[:, c:c + 1], in_=tmpd[:], axis=AX.X), [B_tmpd], [B_s1T])
                    tt(tmpd[:], modrow[:, c * 128:(c + 1) * 128], identf[:], ALU.mult, MR(0) + [B_identf], [B_tmpd])
                    dve(lambda E, c=c: E.reduce_sum(out=sh1T[:, c:c + 1], in_=tmpd[:], axis=AX.X), [B_tmpd], [B_sh1T])
                for c in range(8):
                    wsb = wst[c % 2]
                    dma(wsb[:], w_in[c * 128:(c + 1) * 128, :], [], [B_wst[c % 2]])
                    mms = [(pbias[0:1, n * 512:(n + 1) * 512], sh1T[:, c:c + 1], wsb[:, n * 512:(n + 1) * 512], c == 0, c == 7) for n in range(6)]
                    pe(mms, [B_sh1T, B_wst[c % 2]], [B_pbias])
                    act(Wall[:, c, :], wsb[:], AF.Copy, [B_wst[c % 2], B_s1T], [B_Wall[c]], scale=s1T[:, c:c + 1])
                cp(biasrow[:], pbias[:], [B_pbias], [B_biasrow])
                dma(lam_t[:], lamv, [], [B_lamt])
                tt(lam_t[0:1, 0:64], lam_t[0:1, 0:64], lam_t[0:1, 64:128], ALU.mult, [B_lamt], [B_lamt])
                tt(lam_t[0:1, 128:192], lam_t[0:1, 128:192], lam_t[0:1, 192:256], ALU.mult, [B_lamt], [B_lamt])
                dve(lambda E: E.reduce_sum(out=lam_s[0:1, 0:1], in_=lam_t[0:1, 0:64], axis=AX.X), [B_lamt], [B_lams])
                dve(lambda E: E.reduce_sum(out=lam_s[0:1, 1:2], in_=lam_t[0:1, 128:192], axis=AX.X), [B_lamt], [B_lams])
                act(lam_s[0:1, 2:4], lam_s[0:1, 0:2], AF.Exp, [B_lams], [B_lams])
                stt(lam_s[0:1, 4:5], lam_s[0:1, 3:4], -0.2, lam_s[0:1, 2:3], ALU.add, ALU.subtract, [B_lams], [B_lams])
                pe([(pmod[0][:, 0:1], onesf[0:1, 0:128], lam_s[0:1, 4:5], True, True)], [B_onesf, B_lams], [B_pmod[0]])
                cp(neglam[:], pmod[0][:, 0:1], [B_pmod[0]], [B_neglam])
                d = dbgout("rows", [128, 4096])
                if d is not None:
                    dma(d, rows[:].rearrange("p a f -> p (a f)"), [B_rows], [YB], sembuf=B_rows)
                d = dbgout("neglam", [128, 1])
                if d is not None:
                    dma(d, neglam[:], [B_neglam], [YB], sembuf=B_neglam)
                em.barrier()
                em.emit()

            with contextlib.ExitStack() as st:
                xt = [sbt(st, "xt%d" % i, [128, 1024]) for i in range(2)]
                xn = [sbt(st, "xn%d" % i, [128, 1024], BF16) for i in range(2)]
                xnT = [sbt(st, "xnT%d" % i, [128, 8, 512], BF16) for i in range(2)]
                ssq = sbt(st, "ssq", [128, 4])
                junk = sbt(st, "junk", [128, 1024], BF16)
                ev = [sbt(st, "ev%d" % i, [128, 512], BF16) for i in range(3)]
                sq = [sbt(st, "sq%d" % i, [128, 512], BF16) for i in range(2)]
                vt = [sbt(st, "vt%d" % i, [128, 4, 130], BF16) for i in range(2)]
                nvt = [sbt(st, "nvt%d" % i, [128, 8, 66], BF16) for i in range(2)]
                mx = sbt(st, "mx", [1, 2])
                ptr = [pst(st, "ptr%d" % i, [128, 8, 128], BF16) for i in range(2)]
                pp = [pst(st, "pp%d" % i, [128, 512]) for i in range(3)]
                pn = pst(st, "pn", [1, 512])
                B_xt = em.bufs(2, "xt"); B_xn = em.bufs(2, "xn"); B_xnT = em.bufs(2, "xnT")
                B_ssq = em.buf("ssq"); B_junk = em.buf("junk"); B_ev = em.bufs(3, "ev"); B_sq = em.bufs(2, "sq")
                B_vt = em.bufs(2, "vt"); B_nvt = em.bufs(2, "nvt"); B_mx = em.buf("mx")
                B_ptr = em.bufs(2, "ptr"); B_pp = em.bufs(3, "pp"); B_pn = em.buf("pn")
                B_scr = em.buf("scr1")
                for i in range(2):
                    mset(vt[i][:, :, 128:129], 1.0, [B_vt[i]])
                    mset(vt[i][:, :, 129:130], 0.0, [B_vt[i]])
                    mset(nvt[i][:, :, 64:65], 1.0, [B_nvt[i]])
                    mset(nvt[i][:, :, 65:66], 0.0, [B_nvt[i]])
                cnt = {"tile": 0, "grp": 0, "pp": 0, "ev": 0, "sq": 0, "vt": 0, "nvt": 0}

                def norm_group(src, g):
                    gi = cnt["grp"] % 2
                    cnt["grp"] += 1
                    for t in range(4):
                        i = cnt["tile"] % 2
                        cnt["tile"] += 1
                        r0 = g * 512 + t * 128
                        dma(xt[i][:], src[r0:r0 + 128, :], [], [B_xt[i]])
                        act(junk[:], xt[i][:], AF.Square, [B_xt[i]], [B_junk, B_ssq], accum_out=ssq[:, 0:1])
                        ts(ssq[:, 1:2], ssq[:, 0:1], 1.0 / 1024, 1e-6, ALU.mult, ALU.add, [B_ssq], [B_ssq])
                        act(ssq[:, 2:3], ssq[:, 1:2], AF.Sqrt, [B_ssq], [B_ssq])
                        dve(lambda E: E.reciprocal(out=ssq[:, 3:4], in_=ssq[:, 2:3]), [B_ssq], [B_ssq])
                        act(xn[i][:], xt[i][:], AF.Copy, [B_xt[i], B_ssq], [B_xn[i]], scale=ssq[:, 3:4])
                        pet([(ptr[i][:, c, :], xn[i][:, c * 128:(c + 1) * 128], identb[:]) for c in range(8)],
                            [B_xn[i], B_identb], [B_ptr[i]])
                        cp(xnT[gi][:, :, t * 128:(t + 1) * 128], ptr[i][:], [B_ptr[i]], [B_xnT[gi]])
                    return gi

                def proj_T(gi, col0, scale, dst, stat_idx):
                    k = cnt["pp"] % 3; cnt["pp"] += 1
                    mms = [(pp[k][:], Wall[:, c, col0:col0 + 128], xnT[gi][:, c, :], c == 0, False) for c in range(8)]
                    mms.append((pp[k][:], biasrow[0:1, col0:col0 + 128], onesb[0:1, 0:512], False, True))
                    pe(mms, B_Wall + [B_xnT[gi], B_biasrow, B_onesb], [B_pp[k]])
                    e = cnt["ev"] % 3; cnt["ev"] += 1
                    act(ev[e][:], pp[k][:], AF.Copy, [B_pp[k]], [B_ev[e]], scale=scale)
                    dma(dst, ev[e][:], [B_ev[e]], [B_scr], sembuf=B_ev[e])
                    s = cnt["sq"] % 2; cnt["sq"] += 1
                    tt(sq[s][:], ev[e][:], ev[e][:], ALU.mult, [B_ev[e]], [B_sq[s]])
                    pe([(pn[:], onesb[:, 0:1], sq[s][:], True, True)], [B_onesb, B_sq[s]], [B_pn])
                    dve(lambda E: E.reduce_max(out=mx[0:1, 0:1], in_=pn[0:1, :], axis=AX.X), [B_pn], [B_mx])
                    tt(stat[0:1, stat_idx:stat_idx + 1], stat[0:1, stat_idx:stat_idx + 1], mx[0:1, 0:1], ALU.max, [B_mx, B_stat], [B_stat])

                def proj_tok(gi, t, col0):
                    k = cnt["pp"] % 3; cnt["pp"] += 1
                    mms = [(pp[k][:], xnT[gi][:, c, t * 128:(t + 1) * 128], Wall[:, c, col0:col0 + 512], c == 0, False) for c in range(8)]
                    mms.append((pp[k][:], onesb[0:1, 0:128], biasrow[0:1, col0:col0 + 512], False, True))
                    pe(mms, B_Wall + [B_xnT[gi], B_biasrow, B_onesb], [B_pp[k]])
                    return k

                def norm_part(src, g, ntl):
                    gi = cnt["grp"] % 2
                    cnt["grp"] += 1
                    for t in range(ntl):
                        i = cnt["tile"] % 2
                        cnt["tile"] += 1
                        r0 = g * 512 + t * 128
                        dma(xt[i][:], src[r0:r0 + 128, :], [], [B_xt[i]])
                        act(junk[:], xt[i][:], AF.Square, [B_xt[i]], [B_junk, B_ssq], accum_out=ssq[:, 0:1])
                        ts(ssq[:, 1:2], ssq[:, 0:1], 1.0 / 1024, 1e-6, ALU.mult, ALU.add, [B_ssq], [B_ssq])
                        act(ssq[:, 2:3], ssq[:, 1:2], AF.Sqrt, [B_ssq], [B_ssq])
                        dve(lambda E: E.reciprocal(out=ssq[:, 3:4], in_=ssq[:, 2:3]), [B_ssq], [B_ssq])
                        act(xn[i][:], xt[i][:], AF.Copy, [B_xt[i], B_ssq], [B_xn[i]], scale=ssq[:, 3:4])
                        pet([(ptr[i][:, c, :], xn[i][:, c * 128:(c + 1) * 128], identb[:]) for c in range(8)],
                            [B_xn[i], B_identb], [B_ptr[i]])
                        cp(xnT[gi][:, :, t * 128:(t + 1) * 128], ptr[i][:], [B_ptr[i]], [B_xnT[gi]])
                    return gi

                def projB(kind, g, gi):
                    if kind == "own":
                        for h in range(4):
                            proj_T(gi, h * 128, 0.125, Qs[h, :, g * 512:(g + 1) * 512], h)
                        for c4 in range(4):
                            proj_T(gi, 1536 + c4 * 128, 0.125, NQs[c4, :, g * 512:(g + 1) * 512], 8 + c4)
                    elif kind == "seq":
                        for h in range(4):
                            proj_T(gi, 512 + h * 128, 1.0, Ks[h, :, g * 512:(g + 1) * 512], 4 + h)
                        for t in range(4):
                            k = proj_tok(gi, t, 1024)
                            v = cnt["vt"] % 2; cnt["vt"] += 1
                            cp(vt[v][:, :, 0:128], pp[k][:].rearrange("p (h e) -> p h e", h=4), [B_pp[k]], [B_vt[v]])
                            dma(Vs[:, :, g * 4 + t, :].rearrange("h p e -> p h e"), vt[v][:], [B_vt[v]], [B_scr], sembuf=B_vt[v])
                    else:
                        ntl = 4 if g < 5 else 2
                        ncol = ntl * 128
                        for c4 in range(4):
                            k = cnt["pp"] % 3; cnt["pp"] += 1
                            col0 = 2048 + c4 * 128
                            mms = [(pp[k][:, 0:ncol], Wall[:, c, col0:col0 + 128], xnT[gi][:, c, 0:ncol], c == 0, False) for c in range(8)]
                            mms.append((pp[k][:, 0:ncol], biasrow[0:1, col0:col0 + 128], onesb[0:1, 0:ncol], False, True))
                            pe(mms, B_Wall + [B_xnT[gi], B_biasrow, B_onesb], [B_pp[k]])
                            e = cnt["ev"] % 3; cnt["ev"] += 1
                            act(ev[e][:, 0:ncol], pp[k][:, 0:ncol], AF.Copy, [B_pp[k]], [B_ev[e]])
                            dma(NKs[c4, :, g * 512:g * 512 + ncol], ev[e][:, 0:ncol], [B_ev[e]], [B_scr], sembuf=B_ev[e])
                            s_ = cnt["sq"] % 2; cnt["sq"] += 1
                            tt(sq[s_][:, 0:ncol], ev[e][:, 0:ncol], ev[e][:, 0:ncol], ALU.mult, [B_ev[e]], [B_sq[s_]])
                            pe([(pn[:, 0:ncol], onesb[:, 0:1], sq[s_][:, 0:ncol], True, True)], [B_onesb, B_sq[s_]], [B_pn])
                            dve(lambda E, ncol=ncol: E.reduce_max(out=mx[0:1, 0:1], in_=pn[0:1, 0:ncol], axis=AX.X), [B_pn], [B_mx])
                            tt(stat[0:1, 12 + c4:13 + c4], stat[0:1, 12 + c4:13 + c4], mx[0:1, 0:1], ALU.max, [B_mx, B_stat], [B_stat])
                        for t in range(ntl):
                            k = proj_tok(gi, t, 2560)
                            v = cnt["nvt"] % 2; cnt["nvt"] += 1
                            cp(nvt[v][:, :, 0:64], pp[k][:].rearrange("p (h e) -> p h e", h=8), [B_pp[k]], [B_nvt[v]])
                            dma(NVs[:, g * 4 + t, :], nvt[v][:].rearrange("p h e -> p (h e)"), [B_nvt[v]], [B_scr], sembuf=B_nvt[v])

                groups = [("own", g, xq, 4) for g in range(4)] + [("seq", g, xb, 4) for g in range(16)] \
                    + [("win", g, xw, 4 if g < 5 else 2) for g in range(6)]
                gis = [None] * len(groups)
                gis[0] = norm_part(groups[0][2], groups[0][1], groups[0][3])
                for n_, (kind, g, src, ntl) in enumerate(groups):
                    if n_ + 1 < len(groups):
                        kn, gn, sn, tn = groups[n_ + 1]
                        gis[n_ + 1] = norm_part(sn, gn, tn)
                    projB(kind, g, gis[n_])
                mm_ = sbt(st, "mm_", [1, 8])
                pM = pst(st, "pM", [128, 8])
                B_mm, B_pM = em.bufs(2, "mm")
                tt(mm_[0:1, 0:4], stat[0:1, 0:4], stat[0:1, 4:8], ALU.mult, [B_stat], [B_mm])
                tt(mm_[0:1, 4:8], stat[0:1, 8:12], stat[0:1, 12:16], ALU.mult, [B_stat, B_mm], [B_mm])
                act(mm_[:], mm_[:], AF.Sqrt, [B_mm], [B_mm])
                ts(mm_[:], mm_[:], -1.05, None, ALU.mult, None, [B_mm], [B_mm])
                pe([(pM[:], onesf[0:1, 0:128], mm_[0:1, :], True, True)], [B_onesf, B_mm], [B_pM])
                cp(negM[:], pM[:], [B_pM], [B_negM])
                d = dbgout("negM", [128, 8])
                if d is not None:
                    dma(d, negM[:], [B_negM], [YB], sembuf=B_negM)
                em.barrier()
                em.emit()
        STOP = os.environ.get("KSTOP", "")
        if STOP != "1":
            with contextlib.ExitStack() as st:
                KA = [sbt(st, "KA%d" % m, [66, 8192], BF16) for m in range(2)]
                QA = [[sbt(st, "QA%d_%d" % (m, v), [66, 2048], BF16) for v in range(3)] for m in range(2)]
                Vh = sbt(st, "Vh", [128, 64, 130], BF16)
                ktab_t = sbt(st, "ktab_s", [128, 4, 2, 64])
                kb = sbt(st, "kb", [128, 2, 64])
                bdg = sbt(st, "bdg_s", [128, 4, 128], BF16)
                PT = [sbt(st, "PT%d" % i, [128, 512], BF16) for i in range(3)]
                rz = sbt(st, "rz", [128, 512])
                O1T = sbt(st, "O1T", [128, 512])
                dT = sbt(st, "dT", [128, 512])
                sqb = sbt(st, "sqb", [128, 512], BF16)
                rs = sbt(st, "rs", [128, 512])
                gs8 = sbt(st, "gs8", [128, 128])
                S = [pst(st, "S%d" % i, [128, 512]) for i in range(3)]
                OT = [pst(st, "OT%d" % i, [128, 512]) for i in range(2)]
                ZB = [pst(st, "ZB%d" % i, [128, 512]) for i in range(2)]
                B_OT = em.bufs(2, "OT"); B_ZB = em.bufs(2, "ZB")
                B_rz, B_O1T, B_dT, B_sqb, B_rs = em.bufs(5, "ep")
                B_KA = em.bufs(2, "KA"); B_QA = em.bufs(2, "QA"); B_Vh = em.buf("Vh"); B_ktab = em.buf("ktab")
                B_kb = em.buf("kb"); B_bdg = em.buf("bdg"); B_PT = em.bufs(3, "PT")
                B_gs8 = em.buf("gs8")
                B_S = em.bufs(3, "S")
                dma(ktab_t[:].rearrange("p a b c -> p (a b c)"), ktab, [], [B_ktab])
                dma(bdg[:].rearrange("p a b -> p (a b)"), bdiag, [], [B_bdg])
                ts(gs8[:], gsub[:], 0.8, None, ALU.mult, None, [B_gsub], [B_gs8])
                it = 0
                for h in range(4):
                    for m in range(2):
                        dma(KA[m][0:64, :], Ks[h, 64 * m:64 * m + 64, :], [], [B_KA[m]])
                        dma(KA[m][64:66, :], kaug, [], [B_KA[m]])
                        for v in range(3):
                            dma(QA[m][v][0:64, :], Qs[h, 64 * m:64 * m + 64, :], [], [B_QA[m]])
                            dma(QA[m][v][64:66, :], qaug[h, v], [], [B_QA[m]])
                    dma(Vh[:], Vs[h], [], [B_Vh])
                    ts(kb[:], ktab_t[:, h], negM[:, h:h + 1], None, ALU.add, None, [B_ktab, B_negM], [B_kb])
                    seq = [(qc, m, kt) for qc in range(4) for m in range(2) for kt in range(64)]

                    def segs_of(qc, kt):
                        if kt >= 16:
                            return [(0, 4, 0, kb[:, 0, kt:kt + 1])]
                        segs = []
                        for t in range(4):
                            qt = 4 * qc + t
                            if qt > kt:
                                cls = (0, kb[:, 0, kt:kt + 1])
                            elif qt == kt:
                                cls = (2, negM[:, h:h + 1])
                            else:
                                cls = (1, kb[:, 1, kt:kt + 1])
                            if segs and segs[-1][2] == cls[0]:
                                segs[-1] = (segs[-1][0], t + 1, cls[0], cls[1])
                            else:
                                segs.append((t, t + 1, cls[0], cls[1]))
                        return segs

                    def emit_S(n):
                        qc, m, kt = seq[n]
                        sb = (it + n) % 3
                        mms = []
                        for (t0, t1, v, col) in segs_of(qc, kt):
                            c0, c1 = t0 * 128, t1 * 128
                            q0 = qc * 512
                            mms.append((S[sb][:, c0:c1], KA[m][0:66, kt * 128:(kt + 1) * 128], QA[m][v][0:66, q0 + c0:q0 + c1], True, v != 2))
                            if v == 2:
                                mms.append((S[sb][:, c0:c1], identb[:], bdg[:, h, :], False, True))
                        pe(mms, [B_KA[m], B_QA[m], B_identb, B_bdg], [B_S[sb]])

                    def emit_rest(n):
                        qc, m, kt = seq[n]
                        sb = (it + n) % 3
                        pb = (it + n) % 3
                        for (t0, t1, v, col) in segs_of(qc, kt):
                            c0, c1 = t0 * 128, t1 * 128
                            act(PT[pb][:, c0:c1], S[sb][:, c0:c1], AF.Exp, [B_S[sb], B_kb, B_negM], [B_PT[pb]], bias=col, scale=1.0)
                        ob = ((it + n) // 64) % 2
                        pe([(OT[ob][:], Vh[:, kt, 0:128], PT[pb][:], kt == 0, kt == 63),
                            (ZB[ob][:], onesb[:, 0:128], PT[pb][:], kt == 0, kt == 63)],
                           [B_PT[pb], B_Vh, B_onesb], [B_OT[ob], B_ZB[ob]])
                        if kt != 63:
                            return
                        dve(lambda E, ob=ob: E.reciprocal(out=rz[:], in_=ZB[ob][:]), [B_ZB[ob]], [B_rz])
                        if m == 0:
                            tt(O1T[:], OT[ob][:], rz[:], ALU.mult, [B_OT[ob], B_rz], [B_O1T])
                        else:
                            tt(dT[:], OT[ob][:], rz[:], ALU.mult, [B_OT[ob], B_rz], [B_dT])
                            stt(dT[:], dT[:], neglam[:, 0:1], O1T[:], ALU.mult, ALU.add, [B_dT, B_neglam, B_O1T], [B_dT])
                            tt(sqb[:], dT[:], dT[:], ALU.mult, [B_dT], [B_sqb])
                            pe([(ZB[ob][:], onesb[:, 0:128], sqb[:], True, True)], [B_sqb, B_onesb], [B_ZB[ob]])
                            ts(rs[:], ZB[ob][:], 1.0 / 128, 1e-6, ALU.mult, ALU.add, [B_ZB[ob]], [B_rs])
                            act(rs[:], rs[:], AF.Sqrt, [B_rs], [B_rs])
                            dve(lambda E: E.reciprocal(out=rs[:], in_=rs[:]), [B_rs], [B_rs])
                            tt(dT[:], dT[:], rs[:], ALU.mult, [B_dT, B_rs], [B_dT])
                            ts(oTd[:, h, qc * 512:(qc + 1) * 512], dT[:], g8col[:, 0:1], None, ALU.mult, None,
                               [B_dT, B_g8col], [B_oTd[h][qc]])

                    emit_S(0)
                    emit_S(1)
                    for n in range(len(seq)):
                        if n + 2 < len(seq):
                            emit_S(n + 2)
                        emit_rest(n)
                    it += len(seq)
                em.barrier()
                em.emit()

            with contextlib.ExitStack() as st:
                NQT = sbt(st, "NQT", [128, 4, 2048], BF16)
                NKT = sbt(st, "NKT", [128, 4, 2816], BF16)
                NV = sbt(st, "NV", [128, 22, 528], BF16)
                nbt = [sbt(st, "nbt%d" % i, [128, 8 * 7 * 128], BF16) for i in range(2)]
                PN = [sbt(st, "PN%d" % i, [128, 896], BF16) for i in range(2)]
                sn = sbt(st, "sn", [128, 2])
                SN = [pst(st, "SN%d" % i, [128, 1024]) for i in range(2)]
                NO = [pst(st, "NO%d" % i, [128, 512]) for i in range(2)]
                B_NQT, B_NKT, B_NV, B_sn = em.bufs(4, "nat")
                B_nbt = em.bufs(2, "nbt"); B_PN = em.bufs(2, "PN"); B_SN = em.bufs(2, "SN"); B_NO = em.bufs(2, "NO")
                dma(NQT[:], NQs.rearrange("c p t -> p c t"), [], [B_NQT])
                dma(NKT[:], NKs.rearrange("c p t -> p c t"), [], [B_NKT])
                dma(NV[:], NVs, [], [B_NV])
                items = [(j, h) for j in range(16) for h in range(8)]

                def nat_S(n):
                    j, h = items[n]
                    nb_ = nbt[j % 2]
                    if h == 0:
                        slot = {0: 1, 1: 2, 14: 3, 15: 4}.get(j, 0)
                        dma(nb_[:], natb[slot], [], [B_nbt[j % 2]])
                    c4, hp = h // 2, (h % 2) * 64
                    sb = n % 2
                    mms = []
                    for o in range(7):
                        oc = slice(o * 128, (o + 1) * 128)
                        mms.append((SN[sb][:, oc], NKT[hp:hp + 64, c4, (j + o) * 128:(j + o + 1) * 128],
                                    NQT[hp:hp + 64, c4, j * 128:(j + 1) * 128], True, False))
                        mms.append((SN[sb][:, oc], identb[:], nb_[:, (h * 7 + o) * 128:(h * 7 + o + 1) * 128], False, True))
                    pe(mms, [B_NKT, B_NQT, B_identb, B_nbt[j % 2]], [B_SN[sb]])

                def nat_rest(n):
                    j, h = items[n]
                    c4 = h // 2
                    sb = n % 2
                    act(PN[sb][:], SN[sb][:, 0:896], AF.Exp, [B_SN[sb], B_negM], [B_PN[sb]], bias=negM[:, 4 + c4:5 + c4], scale=1.0)
                    mms = [(NO[sb][:, 0:66], PN[sb][:, o * 128:(o + 1) * 128], NV[:, j + o, h * 66:h * 66 + 66], o == 0, o == 6) for o in range(7)]
                    pe(mms, [B_PN[sb], B_NV], [B_NO[sb]])
                    dve(lambda E, sb=sb: E.reciprocal(out=sn[:, 0:1], in_=NO[sb][:, 64:65]), [B_NO[sb]], [B_sn])
                    ts(o_all[:, j, h * 64:(h + 1) * 64], NO[sb][:, 0:64], sn[:, 0:1], None, ALU.mult, None,
                       [B_NO[sb], B_sn], [B_oall[j]])

                nat_S(0)
                for n in range(len(items)):
                    if n + 1 < len(items):
                        nat_S(n + 1)
                    nat_rest(n)
                d = dbgout("o_all", [128, 16 * 512], BF16)
                if d is not None:
                    dma(d, o_all[:].rearrange("p a f -> p (a f)"), B_oall, [YB], sembuf=B_oall[0])
                em.barrier()
                em.emit()

        if STOP not in ("1", "2"):
            with contextlib.ExitStack() as st:
                wr = sbt(st, "wr", [128, 8, 32])
                br = sbt(st, "br", [1, 32])
                mskb = sbt(st, "mskb", [128, 16, 32], BF16)
                gate4 = sbt(st, "gate4", [128, 16, 4])
                eidx = sbt(st, "eidx", [128, 16, 8])
                dsti = sbt(st, "dsti", [128, 64], I32)
                idxW = sbt(st, "idxW", [128, 4, NB], I32)
                ohTall = sbt(st, "ohTall", [32, NB])
                B_wr, B_br, B_mskb, B_gate4, B_eidx, B_dsti, B_idxW, B_ohTall = em.bufs(8, "p4")
                B_hrow = em.bufs(16, "hrow")
                B_x1s = em.bufs(16, "x1s")
                dma(wr[:], w_router.rearrange("(c p) f -> p c f", p=128), [], [B_wr])
                dma(br[:], b_router, [], [B_br])
                with contextlib.ExitStack() as s4:
                    Wout = sbt(s4, "Wout", [128, 8, 1024], BF16)
                    hrow = sbt(s4, "hrow", [128, 16, 1024], BF16)
                    zt = sbt(s4, "zt", [128, 2, 1024], BF16)
                    B_zt, B_Xz = em.bufs(2, "zx")
                    mset(zt[:], 0.0, [B_zt], eng="pool")
                    for cz in range(NB // 2):
                        dma(Xs[cz * 256:(cz + 1) * 256, :].rearrange("(r p) f -> p r f", p=128), zt[:], [B_zt], [B_Xz], sembuf=B_Xz)
                    B_Wout = em.buf("Wout")
                    dma(Wout[:], w_out.rearrange("(c p) f -> p c f", p=128), [], [B_Wout], q="pool")
                    ltri = sbt(s4, "ltri_s", [128, 128], BF16)
                    iota32 = sbt(s4, "iota32_s", [128, 32])
                    thr16 = sbt(s4, "thr16_s", [128, 32, 16])
                    iota96 = sbt(s4, "iota96_s", [128, NB])
                    pidx = sbt(s4, "pidx_s", [128, 1])
                    B_ltri, B_iota32, B_thr16, B_iota96, B_pidx = em.bufs(5, "cst")
                    dma(ltri[:], ltri_d, [], [B_ltri])
                    dma(iota32[:], iota32_d, [], [B_iota32])
                    dma(thr16[:].rearrange("p a b -> p (a b)"), thr16_d, [], [B_thr16])
                    dma(iota96[:], iota96_d, [], [B_iota96])
                    dma(pidx[:], pidx_d, [], [B_pidx])
                    def dbl(fn):
                        return [fn(0), fn(1)]
                    oT_ = dbl(lambda i: sbt(s4, "oT%d" % i, [128, 8, 128], BF16))
                    xt4_ = dbl(lambda i: sbt(s4, "xt4_%d" % i, [128, 1024]))
                    tmp4_ = dbl(lambda i: sbt(s4, "tmp4_%d" % i, [128, 1024]))
                    x1t_ = dbl(lambda i: sbt(s4, "x1t_%d" % i, [128, 1024]))
                    h2t_ = dbl(lambda i: sbt(s4, "h2t_%d" % i, [128, 1024]))
                    h2Tf_ = dbl(lambda i: sbt(s4, "h2Tf_%d" % i, [128, 8, 128]))
                    junk4_ = dbl(lambda i: sbt(s4, "junk4_%d" % i, [128, 1024], BF16))
                    s4s_ = dbl(lambda i: sbt(s4, "s4s_%d" % i, [128, 16]))
                    lg_ = dbl(lambda i: sbt(s4, "lg_%d" % i, [128, 32]))
                    v8_ = dbl(lambda i: sbt(s4, "v8_%d" % i, [128, 8]))
                    i8_ = dbl(lambda i: sbt(s4, "i8_%d" % i, [128, 8], U32))
                    msk_ = dbl(lambda i: sbt(s4, "msk_%d" % i, [128, 32]))
                    e4_ = dbl(lambda i: sbt(s4, "e4_%d" % i, [128, 4]))
                    pto_ = dbl(lambda i: pst(s4, "pto%d" % i, [128, 8, 128], BF16))
                    pmix = pst(s4, "pmix", [128, 1024])
                    ptf = pst(s4, "ptf", [128, 8, 128])
                    plg = pst(s4, "plg", [128, 32])
                    BB = {n: em.bufs(2, "s4" + n) for n in ["oT", "xt4", "tmp4", "x1t", "h2t", "h2Tf", "junk4", "s4s", "lg", "v8", "i8", "msk", "e4", "pto"]}
                    B_pmix, B_ptf, B_plg = em.bufs(3, "s4p")
                    for j in range(16):
                        q2 = j % 2
                        oT, xt4, tmp4, x1t, h2t, h2Tf, junk4, s4s, lg, v8, i8, msk, e4, pto = (
                            oT_[q2], xt4_[q2], tmp4_[q2], x1t_[q2], h2t_[q2], h2Tf_[q2], junk4_[q2], s4s_[q2], lg_[q2], v8_[q2], i8_[q2], msk_[q2], e4_[q2], pto_[q2])
                        (B_oT, B_xt4, B_tmp4, B_x1t, B_h2t, B_h2Tf, B_junk4, B_s4s, B_lg, B_v8, B_i8, B_msk, B_e4, B_pto) = (
                            BB[n][q2] for n in ["oT", "xt4", "tmp4", "x1t", "h2t", "h2Tf", "junk4", "s4s", "lg", "v8", "i8", "msk", "e4", "pto"])
                        pet([(pto[:, c, :], o_all[:, j, c * 128:(c + 1) * 128], identb[:]) for c in range(4)], [B_oall[j], B_identb], [B_pto])
                        cp(oT[:, 0:4, :], pto[:, 0:4, :], [B_pto], [B_oT])
                        mms = []
                        for n in range(2):
                            for c in range(4):
                                mms.append((pmix[:, n * 512:(n + 1) * 512], oTd[:, c, j * 128:(j + 1) * 128], Wout[:, c, n * 512:(n + 1) * 512], c == 0, False))
                            for c in range(4):
                                mms.append((pmix[:, n * 512:(n + 1) * 512], oT[:, c, :], Wout[:, 4 + c, n * 512:(n + 1) * 512], False, c == 3))
                        pe(mms, [B_oT, B_Wout] + [B_oTd[c][j // 4] for c in range(4)], [B_pmix])
                        dma(xt4[:], xq[j * 128:(j + 1) * 128, :], [], [B_xt4])
                        act(junk4[:], pmix[:], AF.Square, [B_pmix], [B_junk4, B_s4s], accum_out=s4s[:, 0:1])
                        ts(s4s[:, 1:2], s4s[:, 0:1], 1.0 / 1024, 1e-6, ALU.mult, ALU.add, [B_s4s], [B_s4s])
                        act(s4s[:, 2:3], s4s[:, 1:2], AF.Sqrt, [B_s4s], [B_s4s])
                        dve(lambda E, s4s=s4s, v8=v8, lg=lg, i8=i8, e4=e4: E.reciprocal(out=s4s[:, 3:4], in_=s4s[:, 2:3]), [B_s4s], [B_s4s])
                        stt(tmp4[:], pmix[:], s4s[:, 3:4], rows[:, 2, :], ALU.mult, ALU.mult, [B_pmix, B_s4s, B_rows], [B_tmp4])
                        tt(x1t[:], tmp4[:], xt4[:], ALU.add, [B_tmp4, B_xt4], [B_x1t])
                        dma(x1s[j * 128:(j + 1) * 128, :], x1t[:], [B_x1t], [B_x1s[j]], sembuf=B_x1s[j])
                        act(junk4[:], x1t[:], AF.Square, [B_x1t], [B_junk4, B_s4s], accum_out=s4s[:, 4:5])
                        ts(s4s[:, 5:6], s4s[:, 4:5], 1.0 / 1024, 1e-6, ALU.mult, ALU.add, [B_s4s], [B_s4s])
                        act(s4s[:, 6:7], s4s[:, 5:6], AF.Sqrt, [B_s4s], [B_s4s])
                        dve(lambda E, s4s=s4s, v8=v8, lg=lg, i8=i8, e4=e4: E.reciprocal(out=s4s[:, 7:8], in_=s4s[:, 6:7]), [B_s4s], [B_s4s])
                        stt(tmp4[:], x1t[:], s4s[:, 7:8], rows[:, 0, :], ALU.mult, ALU.mult, [B_x1t, B_s4s, B_rows], [B_tmp4])
                        tt(h2t[:], tmp4[:], rows[:, 1, :], ALU.add, [B_tmp4, B_rows], [B_h2t])
                        act(hrow[:, j, :], h2t[:], AF.Copy, [B_h2t], [B_hrow[j]])
                        pet([(ptf[:, c, :], h2t[:, c * 128:(c + 1) * 128], identf[:]) for c in range(8)], [B_h2t, B_identf], [B_ptf])
                        cp(h2Tf[:], ptf[:], [B_ptf], [B_h2Tf])
                        mms = [(plg[:], h2Tf[:, c, :], wr[:, c, :], c == 0, False) for c in range(8)]
                        mms.append((plg[:], onesf[0:1, 0:128], br[0:1, :], False, True))
                        pe(mms, [B_h2Tf, B_wr, B_br, B_onesf], [B_plg])
                        cp(lg[:], plg[:], [B_plg], [B_lg])
                        dve(lambda E, s4s=s4s, v8=v8, lg=lg, i8=i8, e4=e4: E.max(out=v8[:], in_=lg[:]), [B_lg], [B_v8])
                        dve(lambda E, s4s=s4s, v8=v8, lg=lg, i8=i8, e4=e4: E.max_index(out=i8[:], in_max=v8[:], in_values=lg[:]), [B_lg, B_v8], [B_i8])
                        cp(eidx[:, j, :], i8[:], [B_i8], [B_eidx])
                        ts(msk[:], lg[:], v8[:, 3:4], None, ALU.is_ge, None, [B_lg, B_v8], [B_msk])
                        cp(mskb[:, j, :], msk[:], [B_msk], [B_mskb])
                        ts(s4s[:, 8:9], v8[:, 0:1], -1.0, None, ALU.mult, None, [B_v8, B_s4s], [B_s4s])
                        act(e4[:], v8[:, 0:4], AF.Exp, [B_v8, B_s4s], [B_e4], bias=s4s[:, 8:9], scale=1.0)
                        dve(lambda E, s4s=s4s, v8=v8, lg=lg, i8=i8, e4=e4: E.reduce_sum(out=s4s[:, 9:10], in_=e4[:], axis=AX.X), [B_e4, B_s4s], [B_s4s])
                        dve(lambda E, s4s=s4s, v8=v8, lg=lg, i8=i8, e4=e4: E.reciprocal(out=s4s[:, 10:11], in_=s4s[:, 9:10]), [B_s4s], [B_s4s])
                        ts(gate4[:, j, :], e4[:], s4s[:, 10:11], None, ALU.mult, None, [B_e4, B_s4s], [B_gate4])
                    cnt_t = sbt(s4, "cnt_t", [128, 32])
                    cmp1 = sbt(s4, "cmp1", [128, 32, 16])
                    nbk = sbt(s4, "nbk", [128, 32])
                    ones32 = sbt(s4, "ones32", [128, 32])
                    cum = sbt(s4, "cum", [128, 32])
                    pstart = sbt(s4, "pstart", [128, 32])
                    cmp2 = sbt(s4, "cmp2", [128, NB, 32])
                    blk = sbt(s4, "blk", [128, NB])
                    chg = sbt(s4, "chg", [128, NB])
                    idxf = sbt(s4, "idxf", [128, 5, NB])
                    chg2 = sbt(s4, "chg2", [128, NB])
                    B_chg2 = em.buf("chg2")
                    destf = sbt(s4, "destf", [128, 16, 32])
                    ohf = sbt(s4, "ohf", [128, 32])
                    dstf = sbt(s4, "dstf", [128, 64])
                    (B_cnt, B_cmp1, B_nbk, B_ones32, B_cum, B_pstart, B_cmp2, B_blk, B_chg, B_idxf, B_destf, B_ohf, B_dstf) = em.bufs(13, "rt")
                    mms = [(plg[:], onesb[:, 0:128], mskb[:, j, :], j == 0, j == 15) for j in range(16)]
                    pe(mms, [B_onesb, B_mskb], [B_plg])
                    cp(cnt_t[:], plg[:], [B_plg], [B_cnt])
                    tt(cmp1[:], cnt_t[:].unsqueeze(2).to_broadcast([128, 32, 16]), thr16[:], ALU.is_gt, [B_cnt, B_thr16], [B_cmp1])
                    dve(lambda E: E.reduce_sum(out=nbk[:], in_=cmp1[:], axis=AX.X), [B_cmp1], [B_nbk])
                    mset(ones32[:], 1.0, [B_ones32])
                    dve(lambda E: E.tensor_tensor_scan(out=cum[:], data0=ones32[:], data1=nbk[:], initial=0.0, op0=ALU.mult, op1=ALU.add),
                        [B_ones32, B_nbk], [B_cum])
                    tt(pstart[:], cum[:], nbk[:], ALU.subtract, [B_cum, B_nbk], [B_pstart])
                    ts(pstart[:], pstart[:], 128.0, None, ALU.mult, None, [B_pstart], [B_pstart])
                    tt(cmp2[:], cum[:].unsqueeze(1).to_broadcast([128, NB, 32]), iota96[:].unsqueeze(2).to_broadcast([128, NB, 32]), ALU.is_le,
                       [B_cum, B_iota96], [B_cmp2])
                    dve(lambda E: E.reduce_sum(out=blk[:], in_=cmp2[:], axis=AX.X), [B_cmp2], [B_blk])
                    ts(blk[:], blk[:], 31.0, None, ALU.min, None, [B_blk], [B_blk])
                    ts(ohTall[:], blk[0:32, :], pidx[0:32, 0:1], None, ALU.is_equal, None, [B_blk, B_pidx], [B_ohTall])
                    mset(chg[:, 0:1], 1.0, [B_chg])
                    tt(chg[:, 1:NB], blk[:, 1:NB], blk[:, 0:NB - 1], ALU.not_equal, [B_blk, B_chg], [B_chg])
                    mset(chg2[:, 0:2], 1.0, [B_chg2])
                    tt(chg2[:, 2:NB], blk[:, 2:NB], blk[:, 0:NB - 2], ALU.not_equal, [B_blk, B_chg2], [B_chg2])
                    ts(chg[:], chg[:], -float(2 ** 27), float(2 ** 27), ALU.mult, ALU.add, [B_chg], [B_chg])
                    ts(chg2[:], chg2[:], -float(2 ** 27), float(2 ** 27), ALU.mult, ALU.add, [B_chg2], [B_chg2])
                    ts(blk[:], blk[:], 128.0, pidx[:, 0:1], ALU.mult, ALU.add, [B_blk, B_pidx], [B_blk])
                    tt(idxf[:, 4, :], blk[:], chg[:], ALU.add, [B_blk, B_chg], [B_idxf])
                    ts(idxf[:, 0, :], idxf[:, 4, :], 2.0, None, ALU.mult, None, [B_idxf], [B_idxf])
                    ts(idxf[:, 1, :], idxf[:, 4, :], 2.0, 1.0, ALU.mult, ALU.add, [B_idxf], [B_idxf])
                    tt(idxf[:, 4, :], blk[:], chg2[:], ALU.add, [B_blk, B_chg2, B_idxf], [B_idxf])
                    ts(idxf[:, 2, :], idxf[:, 4, :], 2.0, None, ALU.mult, None, [B_idxf], [B_idxf])
                    ts(idxf[:, 3, :], idxf[:, 4, :], 2.0, 1.0, ALU.mult, ALU.add, [B_idxf], [B_idxf])
                    cp(idxW[:], idxf[:, 0:4, :], [B_idxf], [B_idxW])
                    for j in range(16):
                        mms = [(plg[:], onesb[:, 0:128], mskb[:, jj, :], jj == 0, False) for jj in range(j)]
                        mms.append((plg[:], ltri[:], mskb[:, j, :], j == 0, True))
                        pe(mms, [B_onesb, B_ltri, B_mskb], [B_plg])
                        tt(destf[:, j, :], plg[:], pstart[:], ALU.add, [B_plg, B_pstart], [B_destf])
                    for j in range(16):
                        for k in range(4):
                            ts(ohf[:], iota32[:], eidx[:, j, k:k + 1], None, ALU.is_equal, None, [B_iota32, B_eidx], [B_ohf])
                            tt(ohf[:], ohf[:], destf[:, j, :], ALU.mult, [B_ohf, B_destf], [B_ohf])
                            dve(lambda E, j=j, k=k: E.reduce_sum(out=dstf[:, 4 * j + k:4 * j + k + 1], in_=ohf[:], axis=AX.X), [B_ohf], [B_dstf])
                    cp(dsti[:], dstf[:], [B_dstf], [B_dsti])
                    B_XO = em.buf("XO")
                    for j in range(16):
                        for k in range(4):
                            col = dsti[:, 4 * j + k:4 * j + k + 1]
                            bsc = em.buf("sc")
                            em.dma("pool", lambda E, j=j, col=col: [E.indirect_dma_start(
                                out=Xs, out_offset=bass.IndirectOffsetOnAxis(ap=col, axis=0), in_=hrow[:, j, :], in_offset=None)],
                                reads=[B_hrow[j], B_dsti, B_Xz], writes=[bsc], sembuf=B_XO)
                    for nm, srct, bb in (("dsti", dsti, B_dsti), ("idxW", idxW, B_idxW)):
                        d = dbgout(nm, [128, srct.shape[1] * (srct.shape[2] if len(srct.shape) > 2 else 1)], I32)
                        if d is not None:
                            dma(d, srct[:] if len(srct.shape) == 2 else srct[:].rearrange("p a b -> p (a b)"), [bb], [YB], sembuf=bb)
                    d = dbgout("gate4", [128, 64])
                    if d is not None:
                        dma(d, gate4[:].rearrange("p a b -> p (a b)"), [B_gate4], [YB], sembuf=B_gate4)
                    em.barrier()
                    em.emit()
                with contextlib.ExitStack() as s5:
                    wb1s = [sbt(s5, "wb1_%d" % i, [128, 8, 2048], BF16) for i in range(2)]
                    B_wb1 = [em.bufs(2, "wb1p%d" % i) for i in range(2)]
                    wb2 = sbt(s5, "wb2", [128, 9, 1024], BF16)
                    b1all = sbt(s5, "b1all", [32, 2048], BF16)
                    b2all = sbt(s5, "b2all", [32, 1024], BF16)
                    xbk = [sbt(s5, "xbk%d" % i, [128, 1024], BF16) for i in range(2)]
                    xT = [sbt(s5, "xT%d" % i, [128, 8, 128], BF16) for i in range(2)]
                    ohT = [sbt(s5, "ohT%d" % i, [32, 128], BF16) for i in range(2)]
                    glu = sbt(s5, "glu", [128, 1024])
                    lin = sbt(s5, "lin", [128, 1024])
                    sig = sbt(s5, "sig", [128, 1024], BF16)
                    ab = sbt(s5, "ab", [128, 1024], BF16)
                    aT = sbt(s5, "aT", [128, 8, 128], BF16)
                    yb = [sbt(s5, "yb%d" % i, [128, 1024]) for i in range(2)]
                    TX = pst(s5, "TX", [128, 8, 128], BF16)
                    TA = pst(s5, "TA", [128, 8, 128], BF16)
                    H = pst(s5, "H", [128, 2048])
                    Y = pst(s5, "Y", [128, 1024])
                    Ybf = Y.bitcast(BF16)
                    B_wb1a, B_wb1b, B_wb2, B_b1all, B_b2all, B_glu, B_lin, B_sig, B_ab, B_aT, B_TX, B_TA, B_H, B_Y, B_Ys = em.bufs(15, "s5")
                    B_xbk = em.bufs(2, "xbk"); B_obk = em.bufs(2, "obk"); B_xT = em.bufs(2, "xT"); B_ohT = em.bufs(2, "ohT"); B_yb = em.bufs(2, "yb")
                    bcreg = s5.enter_context(nc.gpsimd.register("bcreg"))
                    em.streams["pool"].append(lambda E: E.reg_mov(bcreg, 8191))
                    dma(b1all[:], b1, [], [B_b1all], q="pool")
                    dma(b2all[:], b2, [], [B_b2all], q="pool")
                    ab2 = [ab, sbt(s5, "ab_b", [128, 1024], BF16)]
                    B_ab2 = [B_ab, em.buf("ab_b")]

                    def loads(i):
                        p = i % 2
                        dma(xbk[p][:], Xs[i * 128:(i + 1) * 128, :], [], [B_xbk[p]])

                    def gathers(i):
                        for hh in range(2):
                            em.dma("pool", lambda E, i=i, hh=hh: [E.indirect_dma_start(
                                out=wb1s[i % 2][:, 4 * hh:4 * hh + 4, :].rearrange("p c f -> p (c f)"), out_offset=None, in_=W1p,
                                in_offset=bass.IndirectOffsetOnAxis(ap=idxW[:, 2 + hh, i:i + 1], axis=0), bounds_check=bcreg, oob_is_err=False)],
                                reads=[B_idxW], writes=[B_wb1[i % 2][hh]])

                    def gathers2(i):
                        for hh in range(2):
                            em.dma("pool", lambda E, i=i, hh=hh: [E.indirect_dma_start(
                                out=wb2[:, 4 * hh:4 * hh + 4, :].rearrange("p c f -> p (c f)"), out_offset=None, in_=W2p,
                                in_offset=bass.IndirectOffsetOnAxis(ap=idxW[:, hh, i:i + 1], axis=0), bounds_check=bcreg, oob_is_err=False)],
                                reads=[B_idxW], writes=[B_wb2])

                    def stageA(i):
                        p = i % 2
                        if i + 1 < NB:
                            loads(i + 1)
                        pet([(TX[:, c, :], xbk[p][:, c * 128:(c + 1) * 128], identb[:]) for c in range(8)], [B_xbk[p], B_identb], [B_TX])
                        cp(xT[p][:], TX[:], [B_TX], [B_xT[p]])
                        cp(ohT[p][:], ohTall[:, i:i + 1].to_broadcast([32, 128]), [B_ohTall], [B_ohT[p]])
                        mms = []
                        for n in range(4):
                            for c in range(8):
                                mms.append((H[:, n * 512:(n + 1) * 512], xT[p][:, c, :], wb1s[p][:, c, n * 512:(n + 1) * 512], c == 0, False))
                            mms.append((H[:, n * 512:(n + 1) * 512], ohT[p][:], b1all[:, n * 512:(n + 1) * 512], False, True))
                        pe(mms, [B_xT[p], B_ohT[p], B_wb1[p][0], B_wb1[p][1], B_b1all], [B_H])
                        if i + 2 < NB:
                            gathers(i + 2)
                        ts(glu[:], H[:, 0:2048:2], 7.0, None, ALU.min, None, [B_H], [B_glu])
                        ts(lin[:], H[:, 1:2048:2], -7.0, 7.0, ALU.max, ALU.min, [B_H], [B_lin])
                        act(sig[:], glu[:], AF.Sigmoid, [B_glu], [B_sig], scale=1.702)
                        stt(lin[:], lin[:], 1.0, glu[:], ALU.add, ALU.mult, [B_lin, B_glu], [B_lin])
                        tt(ab2[p][:], lin[:], sig[:], ALU.mult, [B_lin, B_sig], [B_ab2[p]])

                    def stageB(i):
                        p = i % 2
                        pet([(TA[:, c, :], ab2[p][:, c * 128:(c + 1) * 128], identb[:]) for c in range(8)], [B_ab2[p], B_identb], [B_TA])
                        act(aT[:], TA[:], AF.Copy, [B_TA], [B_aT])
                        mms = []
                        for n in range(2):
                            for c in range(8):
                                mms.append((Y[:, n * 512:(n + 1) * 512], aT[:, c, :], wb2[:, c, n * 512:(n + 1) * 512], c == 0, False))
                            mms.append((Y[:, n * 512:(n + 1) * 512], ohT[p][:], b2all[:, n * 512:(n + 1) * 512], False, True))
                        pe(mms, [B_aT, B_ohT[p], B_wb2, B_b2all], [B_Y])
                        if i + 1 < NB:
                            gathers2(i + 1)
                        act(yb[p][:], Y[:], AF.Copy, [B_Y], [B_yb[p]])
                        dma(Ys[i * 128:(i + 1) * 128, :], yb[p][:], [B_yb[p]], [B_Ys], sembuf=B_yb[p])

                    loads(0)
                    gathers(0)
                    gathers(1)
                    gathers2(0)
                    stageA(0)
                    for i in range(NB):
                        if i + 1 < NB:
                            stageA(i + 1)
                        stageB(i)
                    em.barrier()
                    em.emit()
                with contextlib.ExitStack() as s6:
                    x1r = [sbt(s6, "x1r%d" % i, [128, 1024]) for i in range(2)]
                    ot = [sbt(s6, "ot%d" % i, [128, 1024]) for i in range(2)]
                    yk = [sbt(s6, "yk%d" % i, [128, 4, 1024]) for i in range(2)]
                    ft = sbt(s6, "ft", [128, 1024])
                    junk6 = sbt(s6, "junk6", [128, 1024], BF16)
                    s6s = sbt(s6, "s6s", [128, 4])
                    B_x1r = em.bufs(2, "x1r"); B_ot = em.bufs(2, "ot"); B_yk = [em.bufs(4, "yk%d" % i) for i in range(2)]
                    B_ft, B_junk6, B_s6s = em.bufs(3, "s6")
                    for j in range(16):
                        i = j % 2
                        dma(x1r[i][:], x1s[j * 128:(j + 1) * 128, :], [B_x1s[j]], [B_x1r[i]])
                        for k in range(4):
                            em.dma("pool", lambda E, i=i, j=j, k=k: [E.indirect_dma_start(
                                out=yk[i][:, k, :], out_offset=None, in_=Ys,
                                in_offset=bass.IndirectOffsetOnAxis(ap=dsti[:, 4 * j + k:4 * j + k + 1], axis=0))],
                                reads=[B_dsti], writes=[B_yk[i][k]])
                        ts(ft[:], yk[i][:, 0, :], gate4[:, j, 0:1], None, ALU.mult, None, [B_yk[i][0], B_gate4], [B_ft])
                        for k in range(1, 4):
                            stt(ft[:], yk[i][:, k, :], gate4[:, j, k:k + 1], ft[:], ALU.mult, ALU.add, [B_yk[i][k], B_gate4, B_ft], [B_ft])
                        act(junk6[:], ft[:], AF.Square, [B_ft], [B_junk6, B_s6s], accum_out=s6s[:, 0:1])
                        ts(s6s[:, 1:2], s6s[:, 0:1], 1.0 / 1024, 1e-6, ALU.mult, ALU.add, [B_s6s], [B_s6s])
                        act(s6s[:, 2:3], s6s[:, 1:2], AF.Sqrt, [B_s6s], [B_s6s])
                        dve(lambda E: E.reciprocal(out=s6s[:, 3:4], in_=s6s[:, 2:3]), [B_s6s], [B_s6s])
                        stt(ot[i][:], ft[:], s6s[:, 3:4], rows[:, 3, :], ALU.mult, ALU.mult, [B_ft, B_s6s, B_rows], [B_ot[i]])
                        tt(ot[i][:], ot[i][:], x1r[i][:], ALU.add, [B_ot[i], B_x1r[i]], [B_ot[i]])
                        dma(out[j * 128:(j + 1) * 128, :], ot[i][:], [B_ot[i]], [YB], sembuf=B_ot[i])
                    em.barrier()
                    em.emit()
        else:
            mz = sbt(top, "mz", [128, 1024])
            B_mz = em.buf("mz")
            mset(mz[:], 0.0, [B_mz])
            for j in range(16):
                dma(out[j * 128:(j + 1) * 128, :], mz[:], [B_mz], [YB], sembuf=B_mz)
            for nm, src in (("Qs", Qs), ("Ks", Ks), ("NQs", NQs), ("NKs", NKs), ("Vs", Vs), ("NVs", NVs)):
                d = dbgout(nm, src.shape, BF16)
                if d is not None:
                    dma(d, src, [], [YB], sembuf=YB)
            em.barrier()
            em.emit()
    return nc, dbg


SLOPES = [2.0 ** (-2 * (h + 1)) for h in range(4)]
_CACHE = {}


def _bf(a):
    return np.ascontiguousarray(a.astype(ml_dtypes.bfloat16))


def _nat_tables(rpb, qr):
    R0 = 32 * qr
    def table(r0):
        t = np.full((128, 8, 7, 128), -30000.0, np.float32)
        qrow = np.repeat(np.array([r0, r0 + 1]), 64)
        qcol = np.tile(np.arange(64), 2)
        qstart = np.clip(qrow - 4, 0, 120)
        qcs = np.clip(qcol - 8, 0, 48)
        for o in range(7):
            krow = np.repeat(np.array([r0 - 6 + 2 * o, r0 - 5 + 2 * o]), 64)
            kcol = np.tile(np.arange(64), 2)
            valid = ((krow[:, None] >= qstart[None, :]) & (krow[:, None] < qstart[None, :] + 8)
                     & (krow[:, None] >= 0) & (krow[:, None] < 128)
                     & (kcol[:, None] >= qcs[None, :]) & (kcol[:, None] < qcs[None, :] + 16))
            dr = np.clip(krow[:, None] - qrow[None, :] + 7, 0, 14)
            dc = np.clip(kcol[:, None] - qcol[None, :], -15, 15) + 15
            for h in range(8):
                vals = rpb[h][dr, dc]
                t[:, h, o, :] = np.where(valid, vals, -30000.0)
        return t
    slots = [table(R0 + 2 * 6)] + [table(R0 + 2 * j) for j in (0, 1, 14, 15)]
    return _bf(np.stack(slots).reshape(5, 128, 8 * 7 * 128))


def _prep(inputs):
    f32 = np.float32
    x = np.asarray(inputs["x"], f32)
    c = np.asarray(inputs["c"], f32)
    shared = dict(
        w_ada=np.ascontiguousarray(inputs["w_ada"][0], f32), b_ada=np.ascontiguousarray(inputs["b_ada"], f32).reshape(1, 6144),
        g_pre_mix=np.asarray(inputs["g_pre_mix"], f32).reshape(1, 1024), g_post_mix=np.asarray(inputs["g_post_mix"], f32).reshape(1, 1024),
        g_pre_ffn=np.asarray(inputs["g_pre_ffn"], f32).reshape(1, 1024), g_post_ffn=np.asarray(inputs["g_post_ffn"], f32).reshape(1, 1024),
        w_in=np.ascontiguousarray(inputs["w_in"][0], f32), w_out=np.ascontiguousarray(inputs["w_out"][0], f32),
        lamv=np.concatenate([np.asarray(inputs[k], f32).reshape(-1) for k in ("lam_q1", "lam_k1", "lam_q2", "lam_k2")]).reshape(1, 256),
        g_subln=np.asarray(inputs["g_subln"], f32).reshape(1, 128),
        w_router=np.ascontiguousarray(inputs["w_router"][0], f32), b_router=np.asarray(inputs["b_router"], f32).reshape(1, 32),
        identb=_bf(np.eye(128, dtype=f32)), identf=np.eye(128, dtype=f32),
        ltri=_bf(np.triu(np.ones((128, 128), f32), 1)),
        iota32=np.tile(np.arange(32, dtype=f32), (128, 1)),
        thr16=np.tile((128.0 * np.arange(16, dtype=f32))[None, None, :], (128, 32, 1)).reshape(128, 512),
        iota96=np.tile(np.arange(NB, dtype=f32), (128, 1)),
        pidx=np.arange(128, dtype=f32).reshape(128, 1),
    )
    if os.environ.get("KSTOP", "") not in ("1", "2"):
        shared.update(
            W1p=np.ascontiguousarray(np.asarray(inputs["w1"][0], f32).reshape(32, 8, 128, 2048).transpose(0, 2, 1, 3)).reshape(8192, 8192),
            W2p=np.ascontiguousarray(np.asarray(inputs["w2"][0], f32).reshape(32, 8, 128, 1024).transpose(0, 2, 1, 3)).reshape(8192, 4096),
            b1=np.ascontiguousarray(inputs["b1"][0], f32), b2=np.ascontiguousarray(inputs["b2"][0], f32))
    kl = np.arange(128)
    bd = np.stack([-SLOPES[h] * np.abs(kl[:, None] - kl[None, :]) for h in range(4)], axis=1)
    shared["bdiag"] = _bf(bd.reshape(128, 512).astype(f32))
    rpb = np.asarray(inputs["nat_rpb"], f32)[0]
    in_maps = []
    for core in range(8):
        b, qr = core // 4, core % 4
        own = np.arange(qr * 2048, (qr + 1) * 2048)
        rest = np.concatenate([np.arange(0, qr * 2048), np.arange((qr + 1) * 2048, 8192)])
        perm = np.concatenate([own, rest])
        R0 = 32 * qr
        tok0 = (R0 - 6) * 64
        xw = np.zeros((2816, 1024), f32)
        lo, hi = max(tok0, 0), min(tok0 + 2816, 8192)
        xw[lo - tok0:hi - tok0] = x[b, lo:hi]
        ql = np.arange(2048)
        q_lo = (ql % 128).astype(f32)
        qt_abs = (qr * 16 + ql // 128).astype(f32)
        qa = np.zeros((4, 3, 2, 2048), f32)
        for h in range(4):
            qa[h, 0, 0] = -SLOPES[h] * q_lo
            qa[h, 0, 1] = -SLOPES[h] * 128.0 * qt_abs
            qa[h, 1] = -qa[h, 0]
        kabs = perm.astype(f32)
        ktile_abs = perm[::128] // 128
        sig = np.ones(64, f32)
        sig[16:] = np.where(ktile_abs[16:] < qr * 16, 1.0, -1.0)
        ka = np.ones((2, 8192), f32) * np.repeat(sig, 128)[None, :]
        kt_tab = np.zeros((128, 4, 2, 64), f32)
        kpos = kabs.reshape(64, 128).T
        for h in range(4):
            kt_tab[:, h, 0, :] = SLOPES[h] * kpos * sig[None, :]
            kt_tab[:, h, 1, :] = -SLOPES[h] * kpos
        m = dict(shared)
        m.update(
            xb=np.ascontiguousarray(x[b][perm]), xq=np.ascontiguousarray(x[b, own]), xw=xw,
            cT=np.ascontiguousarray(c[b].reshape(8, 128).T),
            natb=_nat_tables(rpb, qr), qaug=_bf(qa), kaug=_bf(ka), ktab=kt_tab.reshape(128, 512),
        )
        in_maps.append(m)
    return in_maps


def kernel(**inputs):
    debug = tuple(os.environ.get("KDEBUG", "").split(",")) if os.environ.get("KDEBUG") else ()
    key = (debug, os.environ.get("KSTOP", ""))
    if key not in _CACHE:
        _CACHE[key] = build_program(debug)
    nc, dbg = _CACHE[key]
    in_maps = _prep(inputs)
    res = run_bass_kernel_spmd(nc, in_maps, core_ids=list(range(8)))
    outs = [np.asarray(r["out"], np.float32) for r in res.results]
    full = np.stack([np.concatenate(outs[0:4], axis=0), np.concatenate(outs[4:8], axis=0)], axis=0)
    if debug:
        kernel.last_debug = [{k: np.asarray(r["dbg_" + k]) for k in dbg} for r in res.results]
    return full
```

```python
import contextlib
import os
import numpy as np
import ml_dtypes
import concourse.bass as bass
import concourse.mybir as mybir
from concourse.bass_utils import run_bass_kernel_spmd

F32 = mybir.dt.float32
BF16 = mybir.dt.bfloat16
I32 = mybir.dt.int32
U32 = mybir.dt.uint32
AF = mybir.ActivationFunctionType
ALU = mybir.AluOpType
AX = mybir.AxisListType

NB = 96
BIG = float(2 ** 30)


class Buf:
    __slots__ = ("name", "w", "rs", "dsem", "dcnt")

    def __init__(self, name):
        self.name = name
        self.w = []
        self.rs = []
        self.dsem = None
        self.dcnt = 0


class Em:
    ENG = ("pe", "act", "dve", "pool", "sp")

    def __init__(self, nc, stack):
        self.nc = nc
        self.stack = stack
        self.streams = {e: [] for e in self.ENG}
        self.cnt = {e: 0 for e in self.ENG}
        self.esem = {e: stack.enter_context(nc.semaphore("sem_" + e)) for e in self.ENG}
        self.waited = {e: {} for e in self.ENG}
        self.nbuf = 0
        self.dbufs = []

    def buf(self, name=None):
        self.nbuf += 1
        return Buf("%s_%d" % (name or "b", self.nbuf))

    def bufs(self, n, name=None):
        return [self.buf(name) for _ in range(n)]

    def _dsem(self, b):
        if b.dsem is None:
            b.dsem = self.stack.enter_context(self.nc.semaphore("d_" + b.name))
            self.dbufs.append(b)
        return b.dsem

    def _deps(self, eng, reads, writes):
        toks = {}

        def add(t):
            key, val, h = t
            if eng == "pe" and key == "pe":
                return
            if key not in toks or toks[key][1] < val:
                toks[key] = t
        for b in reads:
            for t in b.w:
                add(t)
        for b in writes:
            for t in b.w:
                add(t)
            for t in b.rs:
                add(t)
        return self._filter(eng, toks.values())

    def _filter(self, eng, toks):
        out = []
        wd = self.waited[eng]
        for key, val, h in toks:
            if wd.get(key, 0) >= val:
                continue
            wd[key] = val
            out.append((h, val))
        return out

    def _update(self, tok, reads, writes):
        for b in reads:
            b.rs.append(tok)
        for b in writes:
            b.w = [tok]
            b.rs = []

    def op(self, eng, fn, reads=(), writes=()):
        waits = self._deps(eng, reads, writes)
        self.cnt[eng] += 1
        sem = self.esem[eng]
        tok = (eng, self.cnt[eng], sem)

        def run(E, fn=fn, waits=waits, sem=sem):
            for h, v in waits:
                E.wait_ge(h, v)
            fn(E).then_inc(sem, 1)
        self.streams[eng].append(run)
        self._update(tok, reads, writes)

    def dma(self, q, fn, reads=(), writes=(), n=1, sembuf=None):
        waits = self._deps(q, reads, writes)
        sb = sembuf if sembuf is not None else (writes[0] if writes else reads[0])
        sem = self._dsem(sb)
        sb.dcnt += 16 * n
        tok = ("d_" + sb.name, sb.dcnt, sem)

        def run(E, fn=fn, waits=waits, sem=sem, n=n):
            for h, v in waits:
                E.wait_ge(h, v)
            lst = fn(E)
            assert len(lst) == n
            for ins in lst:
                ins.then_inc(sem, 16)
        self.streams[q].append(run)
        self._update(tok, reads, writes)

    def barrier(self):
        toks = [(e, self.cnt[e], self.esem[e]) for e in self.ENG if self.cnt[e] > 0]
        toks += [("d_" + b.name, b.dcnt, b.dsem) for b in self.dbufs]
        for eng in self.ENG:
            waits = self._filter(eng, [t for t in toks if t[0] != eng])

            def run(E, waits=waits):
                for h, v in waits:
                    E.wait_ge(h, v)
            self.streams[eng].append(run)

    def emit(self):
        nc = self.nc
        st = self.streams
        with nc.Block() as block:
            @block.sync
            def _(E):
                for f in st["sp"]:
                    f(E)

            @block.tensor
            def _(E):
                for f in st["pe"]:
                    f(E)

            @block.scalar
            def _(E):
                for f in st["act"]:
                    f(E)

            @block.vector
            def _(E):
                for f in st["dve"]:
                    f(E)

            @block.gpsimd
            def _(E):
                for f in st["pool"]:
                    f(E)
        self.streams = {e: [] for e in self.ENG}


def build_program(debug=()):
    nc = bass.Bass("TRN2", target_bir_lowering=False)

    def din(name, shape, dt=F32):
        return nc.dram_tensor(name, list(shape), dt, kind="ExternalInput").ap()

    def dscr(name, shape, dt):
        return nc.dram_tensor(name, list(shape), dt, kind="Internal").ap()

    xb = din("xb", [8192, 1024])
    xq = din("xq", [2048, 1024])
    xw = din("xw", [2816, 1024])
    cT = din("cT", [128, 8])
    w_ada = din("w_ada", [1024, 6144])
    b_ada = din("b_ada", [1, 6144])
    g_pre_mix = din("g_pre_mix", [1, 1024])
    g_post_mix = din("g_post_mix", [1, 1024])
    g_pre_ffn = din("g_pre_ffn", [1, 1024])
    g_post_ffn = din("g_post_ffn", [1, 1024])
    w_in = din("w_in", [1024, 3072])
    w_out = din("w_out", [1024, 1024])
    lamv = din("lamv", [1, 256])
    g_subln = din("g_subln", [1, 128])
    natb = din("natb", [5, 128, 8 * 7 * 128], BF16)
    w_router = din("w_router", [1024, 32])
    b_router = din("b_router", [1, 32])
    if os.environ.get("KSTOP", "") not in ("1", "2"):
        W1p = din("W1p", [8192, 8192])
        W2p = din("W2p", [8192, 4096])
        b1 = din("b1", [32, 2048])
        b2 = din("b2", [32, 1024])
    qaug = din("qaug", [4, 3, 2, 2048], BF16)
    kaug = din("kaug", [2, 8192], BF16)
    ktab = din("ktab", [128, 4 * 2 * 64])
    bdiag = din("bdiag", [128, 4 * 128], BF16)
    identb_d = din("identb", [128, 128], BF16)
    identf_d = din("identf", [128, 128])
    ltri_d = din("ltri", [128, 128], BF16)
    iota32_d = din("iota32", [128, 32])
    thr16_d = din("thr16", [128, 512])
    iota96_d = din("iota96", [128, NB])
    pidx_d = din("pidx", [128, 1])
    out = nc.dram_tensor("out", [2048, 1024], F32, kind="ExternalOutput").ap()

    Qs = dscr("Qs", [4, 128, 2048], BF16)
    Ks = dscr("Ks", [4, 128, 8192], BF16)
    Vs = dscr("Vs", [4, 128, 64, 130], BF16)
    NQs = dscr("NQs", [4, 128, 2048], BF16)
    NKs = dscr("NKs", [4, 128, 2816], BF16)
    NVs = dscr("NVs", [128, 22, 8 * 66], BF16)
    x1s = dscr("x1s", [2048, 1024], F32)
    Xs = dscr("Xs", [NB * 128, 1024], BF16)
    Os = dscr("Os", [NB * 128, 32], BF16)
    Ys = dscr("Ys", [NB * 128, 1024], F32)

    dbg = {}

    def dbgout(name, shape, dt=F32):
        if name in debug:
            dbg[name] = nc.dram_tensor("dbg_" + name, list(shape), dt, kind="ExternalOutput").ap()
            return dbg[name]
        return None

    with contextlib.ExitStack() as top:
        em = Em(nc, top)
        YB = em.buf("yout")

        def sbt(st, name, shape, dt=F32):
            return st.enter_context(nc.sbuf_tensor(name, list(shape), dt))

        def pst(st, name, shape, dt=F32):
            return st.enter_context(nc.psum_tensor(name, list(shape), dt))

        def dma(out_, in_, reads, writes, q="sp", sembuf=None):
            em.dma(q, lambda E: [E.dma_start(out=out_, in_=in_)], reads=reads, writes=writes, sembuf=sembuf)

        def act(out_, in_, func, reads, writes, **kw):
            em.op("act", lambda E: E.activation(out=out_, in_=in_, func=func, **kw), reads, writes)

        def pe(mms, reads, writes):
            def fn(E):
                ins = None
                for (o, l, r, s0, s1) in mms:
                    ins = E.matmul(o, lhsT=l, rhs=r, start=s0, stop=s1)
                return ins
            em.op("pe", fn, reads, writes)

        def pet(trs, reads, writes):
            def fn(E):
                ins = None
                for (o, i, idn) in trs:
                    ins = E.transpose(o, i, idn)
                return ins
            em.op("pe", fn, reads, writes)

        def dve(f, reads, writes, eng="dve"):
            em.op(eng, f, reads, writes)

        def ts(out_, in0, s1, s2, op0, op1=None, reads=(), writes=(), eng="dve"):
            if op1 is None:
                dve(lambda E: E.tensor_scalar(out=out_, in0=in0, scalar1=s1, scalar2=None, op0=op0), reads, writes, eng)
            else:
                dve(lambda E: E.tensor_scalar(out=out_, in0=in0, scalar1=s1, scalar2=s2, op0=op0, op1=op1), reads, writes, eng)

        def tt(out_, in0, in1, op, reads, writes, eng="dve"):
            dve(lambda E: E.tensor_tensor(out=out_, in0=in0, in1=in1, op=op), reads, writes, eng)

        def stt(out_, in0, scalar, in1, op0, op1, reads, writes):
            dve(lambda E: E.scalar_tensor_tensor(out=out_, in0=in0, scalar=scalar, in1=in1, op0=op0, op1=op1), reads, writes)

        def cp(out_, in_, reads, writes, eng="dve"):
            dve(lambda E: E.tensor_copy(out=out_, in_=in_), reads, writes, eng)

        def mset(ap, v, writes, eng="dve"):
            dve(lambda E: E.memset(ap, v), (), writes, eng)

        def dump(name, dst_shape_src):
            pass

        identb = sbt(top, "identb_s", [128, 128], BF16)
        identf = sbt(top, "identf_s", [128, 128])
        onesb = sbt(top, "onesb", [128, 512], BF16)
        onesf = sbt(top, "onesf", [128, 128])
        rows = sbt(top, "rows", [128, 4, 1024])
        o_all = sbt(top, "o_all", [128, 16, 512], BF16)
        stat = sbt(top, "stat", [1, 16])
        negM = sbt(top, "negM", [128, 8])
        neglam = sbt(top, "neglam", [128, 1])
        gsub = sbt(top, "gsub", [128, 128])
        B_identb, B_identf, B_onesb, B_onesf, B_rows, B_stat, B_negM, B_neglam, B_gsub = em.bufs(9, "const")
        B_oall = em.bufs(16, "oall")
        oTd = sbt(top, "oTd", [128, 4, 2048], BF16)
        B_oTd = [em.bufs(4, "oTd%d" % h) for h in range(4)]
        g8col = sbt(top, "g8col", [128, 1])
        B_g8col = em.buf("g8col")
        dma(g8col[:], g_subln.rearrange("o e -> e o"), [], [B_g8col])
        ts(g8col[:], g8col[:], 0.8, None, ALU.mult, None, [B_g8col], [B_g8col])
        dma(identb[:], identb_d, [], [B_identb])
        dma(identf[:], identf_d, [], [B_identf])
        mset(onesb[:], 1.0, [B_onesb])
        mset(onesf[:], 1.0, [B_onesf])
        mset(stat[:], 0.0, [B_stat])
        dma(gsub[:], g_subln.to_broadcast([128, 128]), [], [B_gsub])

        with contextlib.ExitStack() as st01:
            Wall = sbt(st01, "Wall", [128, 8, 3072], BF16)
            biasrow = sbt(st01, "biasrow", [1, 3072], BF16)
            B_Wall = em.bufs(8, "Wall")
            B_biasrow = em.buf("biasrow")
            with contextlib.ExitStack() as st:
                sil = sbt(st, "sil", [128, 8])
                silrep = sbt(st, "silrep", [128, 8, 128], BF16)
                wada = [sbt(st, "wada%d" % i, [128, 8, 512], BF16) for i in range(2)]
                bada = sbt(st, "bada", [1, 6144], BF16)
                modrow = sbt(st, "modrow", [128, 6144])
                grow = sbt(st, "grow", [128, 4, 1024])
                s1row = sbt(st, "s1row", [128, 1024])
                tmpd = sbt(st, "tmpd", [128, 128])
                s1T = sbt(st, "s1T", [128, 8])
                sh1T = sbt(st, "sh1T", [128, 8])
                wst = [sbt(st, "wst%d" % i, [128, 3072]) for i in range(2)]
                lam_t = sbt(st, "lam_t", [1, 256])
                lam_s = sbt(st, "lam_s", [1, 8])
                pmod = [pst(st, "pmod%d" % i, [128, 512]) for i in range(2)]
                pbias = pst(st, "pbias", [1, 3072])
                B_sil, B_silrep, B_bada, B_grow, B_s1row, B_tmpd, B_s1T, B_sh1T, B_lamt, B_lams, B_pbias, B_plam = em.bufs(12, "p0")
                B_wada = em.bufs(2, "wada")
                B_modrow = em.bufs(12, "modrow")
                B_wst = em.bufs(2, "wst")
                B_pmod = em.bufs(2, "pmod")

                dma(sil[:], cT, [], [B_sil])
                act(sil[:], sil[:], AF.Silu, [B_sil], [B_sil])
                for c in range(8):
                    cp(silrep[:, c, :], sil[:, c:c + 1].to_broadcast([128, 128]), [B_sil], [B_silrep])
                dma(bada[:], b_ada, [], [B_bada], q="pool")
                for i, g in enumerate([g_pre_mix, g_post_mix, g_pre_ffn, g_post_ffn]):
                    dma(grow[:, i, :], g.to_broadcast([128, 1024]), [], [B_grow])
                for j in range(12):
                    wb = wada[j % 2]
                    dma(wb[:], w_ada[:, j * 512:(j + 1) * 512].rearrange("(c p) f -> p c f", p=128), [], [B_wada[j % 2]], q="pool")
                    mms = [(pmod[j % 2][:], silrep[:, c, :], wb[:, c, :], c == 0, False) for c in range(8)]
                    mms.append((pmod[j % 2][:], onesb[0:1, 0:128], bada[0:1, j * 512:(j + 1) * 512], False, True))
                    pe(mms, [B_silrep, B_wada[j % 2], B_bada, B_onesb], [B_pmod[j % 2]])
                    cp(modrow[:, j * 512:(j + 1) * 512], pmod[j % 2][:], [B_pmod[j % 2]], [B_modrow[j]])
                MR = lambda i: [B_modrow[2 * i], B_modrow[2 * i + 1]]
                m = lambda i: modrow[:, i * 1024:(i + 1) * 1024]
                stt(s1row[:], m(1), 1.0, grow[:, 0, :], ALU.add, ALU.mult, MR(1) + [B_grow], [B_s1row])
                stt(rows[:, 0, :], m(4), 1.0, grow[:, 2, :], ALU.add, ALU.mult, MR(4) + [B_grow], [B_rows])
                cp(rows[:, 1, :], m(3), MR(3) + [B_rows], [B_rows])
                tt(rows[:, 2, :], m(2), grow[:, 1, :], ALU.mult, MR(2) + [B_grow, B_rows], [B_rows])
                tt(rows[:, 3, :], m(5), grow[:, 3, :], ALU.mult, MR(5) + [B_grow, B_rows], [B_rows])
                for c in range(8):
                    tt(tmpd[:], s1row[:, c * 128:(c + 1) * 128], identf[:], ALU.mult, [B_s1row, B_identf], [B_tmpd])
                    dve(lambda E, c=c: E.reduce_sum(out=s1T[:, c:c + 1], in_=tmpd[:], axis=AX.X), [B_tmpd], [B_s1T])
                    tt(tmpd[:], modrow[:, c * 128:(c + 1) * 128], identf[:], ALU.mult, MR(0) + [B_identf], [B_tmpd])
                    dve(lambda E, c=c: E.reduce_sum(out=sh1T[:, c:c + 1], in_=tmpd[:], axis=AX.X), [B_tmpd], [B_sh1T])
                for c in range(8):
                    wsb = wst[c % 2]
                    dma(wsb[:], w_in[c * 128:(c + 1) * 128, :], [], [B_wst[c % 2]])
                    mms = [(pbias[0:1, n * 512:(n + 1) * 512], sh1T[:, c:c + 1], wsb[:, n * 512:(n + 1) * 512], c == 0, c == 7) for n in range(6)]
                    pe(mms, [B_sh1T, B_wst[c % 2]], [B_pbias])
                    act(Wall[:, c, :], wsb[:], AF.Copy, [B_wst[c % 2], B_s1T], [B_Wall[c]], scale=s1T[:, c:c + 1])
                cp(biasrow[:], pbias[:], [B_pbias], [B_biasrow])
                dma(lam_t[:], lamv, [], [B_lamt])
                tt(lam_t[0:1, 0:64], lam_t[0:1, 0:64], lam_t[0:1, 64:128], ALU.mult, [B_lamt], [B_lamt])
                tt(lam_t[0:1, 128:192], lam_t[0:1, 128:192], lam_t[0:1, 192:256], ALU.mult, [B_lamt], [B_lamt])
                dve(lambda E: E.reduce_sum(out=lam_s[0:1, 0:1], in_=lam_t[0:1, 0:64], axis=AX.X), [B_lamt], [B_lams])
                dve(lambda E: E.reduce_sum(out=lam_s[0:1, 1:2], in_=lam_t[0:1, 128:192], axis=AX.X), [B_lamt], [B_lams])
                act(lam_s[0:1, 2:4], lam_s[0:1, 0:2], AF.Exp, [B_lams], [B_lams])
                stt(lam_s[0:1, 4:5], lam_s[0:1, 3:4], -0.2, lam_s[0:1, 2:3], ALU.add, ALU.subtract, [B_lams], [B_lams])
                pe([(pmod[0][:, 0:1], onesf[0:1, 0:128], lam_s[0:1, 4:5], True, True)], [B_onesf, B_lams], [B_pmod[0]])
                cp(neglam[:], pmod[0][:, 0:1], [B_pmod[0]], [B_neglam])
                d = dbgout("rows", [128, 4096])
                if d is not None:
                    dma(d, rows[:].rearrange("p a f -> p (a f)"), [B_rows], [YB], sembuf=B_rows)
                d = dbgout("neglam", [128, 1])
                if d is not None:
                    dma(d, neglam[:], [B_neglam], [YB], sembuf=B_neglam)
                em.barrier()
                em.emit()

            with contextlib.ExitStack() as st:
                xt = [sbt(st, "xt%d" % i, [128, 1024]) for i in range(2)]
                xn = [sbt(st, "xn%d" % i, [128, 1024], BF16) for i in range(2)]
                xnT = [sbt(st, "xnT%d" % i, [128, 8, 512], BF16) for i in range(2)]
                ssq = sbt(st, "ssq", [128, 4])
                junk = sbt(st, "junk", [128, 1024], BF16)
                ev = [sbt(st, "ev%d" % i, [128, 512], BF16) for i in range(3)]
                sq = [sbt(st, "sq%d" % i, [128, 512], BF16) for i in range(2)]
                vt = [sbt(st, "vt%d" % i, [128, 4, 130], BF16) for i in range(2)]
                nvt = [sbt(st, "nvt%d" % i, [128, 8, 66], BF16) for i in range(2)]
                mx = sbt(st, "mx", [1, 2])
                ptr = [pst(st, "ptr%d" % i, [128, 8, 128], BF16) for i in range(2)]
                pp = [pst(st, "pp%d" % i, [128, 512]) for i in range(3)]
                pn = pst(st, "pn", [1, 512])
                B_xt = em.bufs(2, "xt"); B_xn = em.bufs(2, "xn"); B_xnT = em.bufs(2, "xnT")
                B_ssq = em.buf("ssq"); B_junk = em.buf("junk"); B_ev = em.bufs(3, "ev"); B_sq = em.bufs(2, "sq")
                B_vt = em.bufs(2, "vt"); B_nvt = em.bufs(2, "nvt"); B_mx = em.buf("mx")
                B_ptr = em.bufs(2, "ptr"); B_pp = em.bufs(3, "pp"); B_pn = em.buf("pn")
                B_scr = em.buf("scr1")
                for i in range(2):
                    mset(vt[i][:, :, 128:129], 1.0, [B_vt[i]])
                    mset(vt[i][:, :, 129:130], 0.0, [B_vt[i]])
                    mset(nvt[i][:, :, 64:65], 1.0, [B_nvt[i]])
                    mset(nvt[i][:, :, 65:66], 0.0, [B_nvt[i]])
                cnt = {"tile": 0, "grp": 0, "pp": 0, "ev": 0, "sq": 0, "vt": 0, "nvt": 0}

                def norm_group(src, g):
                    gi = cnt["grp"] % 2
                    cnt["grp"] += 1
                    for t in range(4):
                        i = cnt["tile"] % 2
                        cnt["tile"] += 1
                        r0 = g * 512 + t * 128
                        dma(xt[i][:], src[r0:r0 + 128, :], [], [B_xt[i]])
                        act(junk[:], xt[i][:], AF.Square, [B_xt[i]], [B_junk, B_ssq], accum_out=ssq[:, 0:1])
                        ts(ssq[:, 1:2], ssq[:, 0:1], 1.0 / 1024, 1e-6, ALU.mult, ALU.add, [B_ssq], [B_ssq])
                        act(ssq[:, 2:3], ssq[:, 1:2], AF.Sqrt, [B_ssq], [B_ssq])
                        dve(lambda E: E.reciprocal(out=ssq[:, 3:4], in_=ssq[:, 2:3]), [B_ssq], [B_ssq])
                        act(xn[i][:], xt[i][:], AF.Copy, [B_xt[i], B_ssq], [B_xn[i]], scale=ssq[:, 3:4])
                        pet([(ptr[i][:, c, :], xn[i][:, c * 128:(c + 1) * 128], identb[:]) for c in range(8)],
                            [B_xn[i], B_identb], [B_ptr[i]])
                        cp(xnT[gi][:, :, t * 128:(t + 1) * 128], ptr[i][:], [B_ptr[i]], [B_xnT[gi]])
                    return gi

                def proj_T(gi, col0, scale, dst, stat_idx):
                    k = cnt["pp"] % 3; cnt["pp"] += 1
                    mms = [(pp[k][:], Wall[:, c, col0:col0 + 128], xnT[gi][:, c, :], c == 0, False) for c in range(8)]
                    mms.append((pp[k][:], biasrow[0:1, col0:col0 + 128], onesb[0:1, 0:512], False, True))
                    pe(mms, B_Wall + [B_xnT[gi], B_biasrow, B_onesb], [B_pp[k]])
                    e = cnt["ev"] % 3; cnt["ev"] += 1
                    act(ev[e][:], pp[k][:], AF.Copy, [B_pp[k]], [B_ev[e]], scale=scale)
                    dma(dst, ev[e][:], [B_ev[e]], [B_scr], sembuf=B_ev[e])
                    s = cnt["sq"] % 2; cnt["sq"] += 1
                    tt(sq[s][:], ev[e][:], ev[e][:], ALU.mult, [B_ev[e]], [B_sq[s]])
                    pe([(pn[:], onesb[:, 0:1], sq[s][:], True, True)], [B_onesb, B_sq[s]], [B_pn])
                    dve(lambda E: E.reduce_max(out=mx[0:1, 0:1], in_=pn[0:1, :], axis=AX.X), [B_pn], [B_mx])
                    tt(stat[0:1, stat_idx:stat_idx + 1], stat[0:1, stat_idx:stat_idx + 1], mx[0:1, 0:1], ALU.max, [B_mx, B_stat], [B_stat])

                def proj_tok(gi, t, col0):
                    k = cnt["pp"] % 3; cnt["pp"] += 1
                    mms = [(pp[k][:], xnT[gi][:, c, t * 128:(t + 1) * 128], Wall[:, c, col0:col0 + 512], c == 0, False) for c in range(8)]
                    mms.append((pp[k][:], onesb[0:1, 0:128], biasrow[0:1, col0:col0 + 512], False, True))
                    pe(mms, B_Wall + [B_xnT[gi], B_biasrow, B_onesb], [B_pp[k]])
                    return k

                def norm_part(src, g, ntl):
                    gi = cnt["grp"] % 2
                    cnt["grp"] += 1
                    for t in range(ntl):
                        i = cnt["tile"] % 2
                        cnt["tile"] += 1
                        r0 = g * 512 + t * 128
                        dma(xt[i][:], src[r0:r0 + 128, :], [], [B_xt[i]])
                        act(junk[:], xt[i][:], AF.Square, [B_xt[i]], [B_junk, B_ssq], accum_out=ssq[:, 0:1])
                        ts(ssq[:, 1:2], ssq[:, 0:1], 1.0 / 1024, 1e-6, ALU.mult, ALU.add, [B_ssq], [B_ssq])
                        act(ssq[:, 2:3], ssq[:, 1:2], AF.Sqrt, [B_ssq], [B_ssq])
                        dve(lambda E: E.reciprocal(out=ssq[:, 3:4], in_=ssq[:, 2:3]), [B_ssq], [B_ssq])
                        act(xn[i][:], xt[i][:], AF.Copy, [B_xt[i], B_ssq], [B_xn[i]], scale=ssq[:, 3:4])
                        pet([(ptr[i][:, c, :], xn[i][:, c * 128:(c + 1) * 128], identb[:]) for c in range(8)],
                            [B_xn[i], B_identb], [B_ptr[i]])
                        cp(xnT[gi][:, :, t * 128:(t + 1) * 128], ptr[i][:], [B_ptr[i]], [B_xnT[gi]])
                    return gi

                def projB(kind, g, gi):
                    if kind == "own":
                        for h in range(4):
                            proj_T(gi, h * 128, 0.125, Qs[h, :, g * 512:(g + 1) * 512], h)
                        for c4 in range(4):
                            proj_T(gi, 1536 + c4 * 128, 0.125, NQs[c4, :, g * 512:(g + 1) * 512], 8 + c4)
                    elif kind == "seq":
                        for h in range(4):
                            proj_T(gi, 512 + h * 128, 1.0, Ks[h, :, g * 512:(g + 1) * 512], 4 + h)
                        for t in range(4):
                            k = proj_tok(gi, t, 1024)
                            v = cnt["vt"] % 2; cnt["vt"] += 1
                            cp(vt[v][:, :, 0:128], pp[k][:].rearrange("p (h e) -> p h e", h=4), [B_pp[k]], [B_vt[v]])
                            dma(Vs[:, :, g * 4 + t, :].rearrange("h p e -> p h e"), vt[v][:], [B_vt[v]], [B_scr], sembuf=B_vt[v])
                    else:
                        ntl = 4 if g < 5 else 2
                        ncol = ntl * 128
                        for c4 in range(4):
                            k = cnt["pp"] % 3; cnt["pp"] += 1
                            col0 = 2048 + c4 * 128
                            mms = [(pp[k][:, 0:ncol], Wall[:, c, col0:col0 + 128], xnT[gi][:, c, 0:ncol], c == 0, False) for c in range(8)]
                            mms.append((pp[k][:, 0:ncol], biasrow[0:1, col0:col0 + 128], onesb[0:1, 0:ncol], False, True))
                            pe(mms, B_Wall + [B_xnT[gi], B_biasrow, B_onesb], [B_pp[k]])
                            e = cnt["ev"] % 3; cnt["ev"] += 1
                            act(ev[e][:, 0:ncol], pp[k][:, 0:ncol], AF.Copy, [B_pp[k]], [B_ev[e]])
                            dma(NKs[c4, :, g * 512:g * 512 + ncol], ev[e][:, 0:ncol], [B_ev[e]], [B_scr], sembuf=B_ev[e])
                            s_ = cnt["sq"] % 2; cnt["sq"] += 1
                            tt(sq[s_][:, 0:ncol], ev[e][:, 0:ncol], ev[e][:, 0:ncol], ALU.mult, [B_ev[e]], [B_sq[s_]])
                            pe([(pn[:, 0:ncol], onesb[:, 0:1], sq[s_][:, 0:ncol], True, True)], [B_onesb, B_sq[s_]], [B_pn])
                            dve(lambda E, ncol=ncol: E.reduce_max(out=mx[0:1, 0:1], in_=pn[0:1, 0:ncol], axis=AX.X), [B_pn], [B_mx])
                            tt(stat[0:1, 12 + c4:13 + c4], stat[0:1, 12 + c4:13 + c4], mx[0:1, 0:1], ALU.max, [B_mx, B_stat], [B_stat])
                        for t in range(ntl):
                            k = proj_tok(gi, t, 2560)
                            v = cnt["nvt"] % 2; cnt["nvt"] += 1
                            cp(nvt[v][:, :, 0:64], pp[k][:].rearrange("p (h e) -> p h e", h=8), [B_pp[k]], [B_nvt[v]])
                            dma(NVs[:, g * 4 + t, :], nvt[v][:].rearrange("p h e -> p (h e)"), [B_nvt[v]], [B_scr], sembuf=B_nvt[v])

                groups = [("own", g, xq, 4) for g in range(4)] + [("seq", g, xb, 4) for g in range(16)] \
                    + [("win", g, xw, 4 if g < 5 else 2) for g in range(6)]
                gis = [None] * len(groups)
                gis[0] = norm_part(groups[0][2], groups[0][1], groups[0][3])
                for n_, (kind, g, src, ntl) in enumerate(groups):
                    if n_ + 1 < len(groups):
                        kn, gn, sn, tn = groups[n_ + 1]
                        gis[n_ + 1] = norm_part(sn, gn, tn)
                    projB(kind, g, gis[n_])
                mm_ = sbt(st, "mm_", [1, 8])
                pM = pst(st, "pM", [128, 8])
                B_mm, B_pM = em.bufs(2, "mm")
                tt(mm_[0:1, 0:4], stat[0:1, 0:4], stat[0:1, 4:8], ALU.mult, [B_stat], [B_mm])
                tt(mm_[0:1, 4:8], stat[0:1, 8:12], stat[0:1, 12:16], ALU.mult, [B_stat, B_mm], [B_mm])
                act(mm_[:], mm_[:], AF.Sqrt, [B_mm], [B_mm])
                ts(mm_[:], mm_[:], -1.05, None, ALU.mult, None, [B_mm], [B_mm])
                pe([(pM[:], onesf[0:1, 0:128], mm_[0:1, :], True, True)], [B_onesf, B_mm], [B_pM])
                cp(negM[:], pM[:], [B_pM], [B_negM])
                d = dbgout("negM", [128, 8])
                if d is not None:
                    dma(d, negM[:], [B_negM], [YB], sembuf=B_negM)
                em.barrier()
                em.emit()
        STOP = os.environ.get("KSTOP", "")
        if STOP != "1":
            with contextlib.ExitStack() as st:
                KA = [sbt(st, "KA%d" % m, [66, 8192], BF16) for m in range(2)]
                QA = [[sbt(st, "QA%d_%d" % (m, v), [66, 2048], BF16) for v in range(3)] for m in range(2)]
                Vh = sbt(st, "Vh", [128, 64, 130], BF16)
                ktab_t = sbt(st, "ktab_s", [128, 4, 2, 64])
                kb = sbt(st, "kb", [128, 2, 64])
                bdg = sbt(st, "bdg_s", [128, 4, 128], BF16)
                PT = [sbt(st, "PT%d" % i, [128, 512], BF16) for i in range(3)]
                rz = sbt(st, "rz", [128, 512])
                O1T = sbt(st, "O1T", [128, 512])
                dT = sbt(st, "dT", [128, 512])
                sqb = sbt(st, "sqb", [128, 512], BF16)
                rs = sbt(st, "rs", [128, 512])
                gs8 = sbt(st, "gs8", [128, 128])
                S = [pst(st, "S%d" % i, [128, 512]) for i in range(3)]
                OT = [pst(st, "OT%d" % i, [128, 512]) for i in range(2)]
                ZB = [pst(st, "ZB%d" % i, [128, 512]) for i in range(2)]
                B_OT = em.bufs(2, "OT"); B_ZB = em.bufs(2, "ZB")
                B_rz, B_O1T, B_dT, B_sqb, B_rs = em.bufs(5, "ep")
                B_KA = em.bufs(2, "KA"); B_QA = em.bufs(2, "QA"); B_Vh = em.buf("Vh"); B_ktab = em.buf("ktab")
                B_kb = em.buf("kb"); B_bdg = em.buf("bdg"); B_PT = em.bufs(3, "PT")
                B_gs8 = em.buf("gs8")
                B_S = em.bufs(3, "S")
                dma(ktab_t[:].rearrange("p a b c -> p (a b c)"), ktab, [], [B_ktab])
                dma(bdg[:].rearrange("p a b -> p (a b)"), bdiag, [], [B_bdg])
                ts(gs8[:], gsub[:], 0.8, None, ALU.mult, None, [B_gsub], [B_gs8])
                it = 0
                for h in range(4):
                    for m in range(2):
                        dma(KA[m][0:64, :], Ks[h, 64 * m:64 * m + 64, :], [], [B_KA[m]])
                        dma(KA[m][64:66, :], kaug, [], [B_KA[m]])
                        for v in range(3):
                            dma(QA[m][v][0:64, :], Qs[h, 64 * m:64 * m + 64, :], [], [B_QA[m]])
                            dma(QA[m][v][64:66, :], qaug[h, v], [], [B_QA[m]])
                    dma(Vh[:], Vs[h], [], [B_Vh])
                    ts(kb[:], ktab_t[:, h], negM[:, h:h + 1], None, ALU.add, None, [B_ktab, B_negM], [B_kb])
                    seq = [(qc, m, kt) for qc in range(4) for m in range(2) for kt in range(64)]

                    def segs_of(qc, kt):
                        if kt >= 16:
                            return [(0, 4, 0, kb[:, 0, kt:kt + 1])]
                        segs = []
                        for t in range(4):
                            qt = 4 * qc + t
                            if qt > kt:
                                cls = (0, kb[:, 0, kt:kt + 1])
                            elif qt == kt:
                                cls = (2, negM[:, h:h + 1])
                            else:
                                cls = (1, kb[:, 1, kt:kt + 1])
                            if segs and segs[-1][2] == cls[0]:
                                segs[-1] = (segs[-1][0], t + 1, cls[0], cls[1])
                            else:
                                segs.append((t, t + 1, cls[0], cls[1]))
                        return segs

                    def emit_S(n):
                        qc, m, kt = seq[n]
                        sb = (it + n) % 3
                        mms = []
                        for (t0, t1, v, col) in segs_of(qc, kt):
                            c0, c1 = t0 * 128, t1 * 128
                            q0 = qc * 512
                            mms.append((S[sb][:, c0:c1], KA[m][0:66, kt * 128:(kt + 1) * 128], QA[m][v][0:66, q0 + c0:q0 + c1], True, v != 2))
                            if v == 2:
                                mms.append((S[sb][:, c0:c1], identb[:], bdg[:, h, :], False, True))
                        pe(mms, [B_KA[m], B_QA[m], B_identb, B_bdg], [B_S[sb]])

                    def emit_rest(n):
                        qc, m, kt = seq[n]
                        sb = (it + n) % 3
                        pb = (it + n) % 3
                        for (t0, t1, v, col) in segs_of(qc, kt):
                            c0, c1 = t0 * 128, t1 * 128
                            act(PT[pb][:, c0:c1], S[sb][:, c0:c1], AF.Exp, [B_S[sb], B_kb, B_negM], [B_PT[pb]], bias=col, scale=1.0)
                        ob = ((it + n) // 64) % 2
                        pe([(OT[ob][:], Vh[:, kt, 0:128], PT[pb][:], kt == 0, kt == 63),
                            (ZB[ob][:], onesb[:, 0:128], PT[pb][:], kt == 0, kt == 63)],
                           [B_PT[pb], B_Vh, B_onesb], [B_OT[ob], B_ZB[ob]])
                        if kt != 63:
                            return
                        dve(lambda E, ob=ob: E.reciprocal(out=rz[:], in_=ZB[ob][:]), [B_ZB[ob]], [B_rz])
                        if m == 0:
                            tt(O1T[:], OT[ob][:], rz[:], ALU.mult, [B_OT[ob], B_rz], [B_O1T])
                        else:
                            tt(dT[:], OT[ob][:], rz[:], ALU.mult, [B_OT[ob], B_rz], [B_dT])
                            stt(dT[:], dT[:], neglam[:, 0:1], O1T[:], ALU.mult, ALU.add, [B_dT, B_neglam, B_O1T], [B_dT])
                            tt(sqb[:], dT[:], dT[:], ALU.mult, [B_dT], [B_sqb])
                            pe([(ZB[ob][:], onesb[:, 0:128], sqb[:], True, True)], [B_sqb, B_onesb], [B_ZB[ob]])
                            ts(rs[:], ZB[ob][:], 1.0 / 128, 1e-6, ALU.mult, ALU.add, [B_ZB[ob]], [B_rs])
                            act(rs[:], rs[:], AF.Sqrt, [B_rs], [B_rs])
                            dve(lambda E: E.reciprocal(out=rs[:], in_=rs[:]), [B_rs], [B_rs])
                            tt(dT[:], dT[:], rs[:], ALU.mult, [B_dT, B_rs], [B_dT])
                            ts(oTd[:, h, qc * 512:(qc + 1) * 512], dT[:], g8col[:, 0:1], None, ALU.mult, None,
                               [B_dT, B_g8col], [B_oTd[h][qc]])

                    emit_S(0)
                    emit_S(1)
                    for n in range(len(seq)):
                        if n + 2 < len(seq):
                            emit_S(n + 2)
                        emit_rest(n)
                    it += len(seq)
                em.barrier()
                em.emit()

            with contextlib.ExitStack() as st:
                NQT = sbt(st, "NQT", [128, 4, 2048], BF16)
                NKT = sbt(st, "NKT", [128, 4, 2816], BF16)
                NV = sbt(st, "NV", [128, 22, 528], BF16)
                nbt = [sbt(st, "nbt%d" % i, [128, 8 * 7 * 128], BF16) for i in range(2)]
                PN = [sbt(st, "PN%d" % i, [128, 896], BF16) for i in range(3)]
                sn = sbt(st, "sn", [128, 2])
                SN = [pst(st, "SN%d" % i, [128, 1024]) for i in range(3)]
                NO = [pst(st, "NO%d" % i, [128, 512]) for i in range(2)]
                B_NQT, B_NKT, B_NV, B_sn = em.bufs(4, "nat")
                B_nbt = em.bufs(2, "nbt"); B_PN = em.bufs(3, "PN"); B_SN = em.bufs(3, "SN"); B_NO = em.bufs(2, "NO")
                dma(NQT[:], NQs.rearrange("c p t -> p c t"), [], [B_NQT])
                dma(NKT[:], NKs.rearrange("c p t -> p c t"), [], [B_NKT])
                dma(NV[:], NVs, [], [B_NV])
                items = [(j, h) for j in range(16) for h in range(8)]

                def nat_S(n):
                    j, h = items[n]
                    nb_ = nbt[j % 2]
                    if h == 0:
                        slot = {0: 1, 1: 2, 14: 3, 15: 4}.get(j, 0)
                        dma(nb_[:], natb[slot], [], [B_nbt[j % 2]])
                    c4, hp = h // 2, (h % 2) * 64
                    sb = n % 3
                    mms = []
                    for o in range(7):
                        oc = slice(o * 128, (o + 1) * 128)
                        mms.append((SN[sb][:, oc], NKT[hp:hp + 64, c4, (j + o) * 128:(j + o + 1) * 128],
                                    NQT[hp:hp + 64, c4, j * 128:(j + 1) * 128], True, False))
                        mms.append((SN[sb][:, oc], identb[:], nb_[:, (h * 7 + o) * 128:(h * 7 + o + 1) * 128], False, True))
                    pe(mms, [B_NKT, B_NQT, B_identb, B_nbt[j % 2]], [B_SN[sb]])

                def nat_rest(n):
                    j, h = items[n]
                    c4 = h // 2
                    sb = n % 3
                    act(PN[sb][:], SN[sb][:, 0:896], AF.Exp, [B_SN[sb], B_negM], [B_PN[sb]], bias=negM[:, 4 + c4:5 + c4], scale=1.0)
                    nb2 = n % 2
                    mms = [(NO[nb2][:, 0:66], PN[sb][:, o * 128:(o + 1) * 128], NV[:, j + o, h * 66:h * 66 + 66], o == 0, o == 6) for o in range(7)]
                    pe(mms, [B_PN[sb], B_NV], [B_NO[nb2]])
                    dve(lambda E, nb2=nb2: E.reciprocal(out=sn[:, 0:1], in_=NO[nb2][:, 64:65]), [B_NO[nb2]], [B_sn])
                    ts(o_all[:, j, h * 64:(h + 1) * 64], NO[nb2][:, 0:64], sn[:, 0:1], None, ALU.mult, None,
                       [B_NO[nb2], B_sn], [B_oall[j]])

                nat_S(0)
                nat_S(1)
                for n in range(len(items)):
                    if n + 2 < len(items):
                        nat_S(n + 2)
                    nat_rest(n)
                d = dbgout("o_all", [128, 16 * 512], BF16)
                if d is not None:
                    dma(d, o_all[:].rearrange("p a f -> p (a f)"), B_oall, [YB], sembuf=B_oall[0])
                em.barrier()
                em.emit()

        if STOP not in ("1", "2"):
            with contextlib.ExitStack() as st:
                wr = sbt(st, "wr", [128, 8, 32])
                br = sbt(st, "br", [1, 32])
                mskb = sbt(st, "mskb", [128, 16, 32], BF16)
                gate4 = sbt(st, "gate4", [128, 16, 4])
                eidx = sbt(st, "eidx", [128, 16, 8])
                dsti = sbt(st, "dsti", [128, 64], I32)
                idxW = sbt(st, "idxW", [128, 4, NB], I32)
                ohTall = sbt(st, "ohTall", [32, NB])
                B_wr, B_br, B_mskb, B_gate4, B_eidx, B_dsti, B_idxW, B_ohTall = em.bufs(8, "p4")
                B_hrow = em.bufs(16, "hrow")
                B_x1s = em.bufs(16, "x1s")
                dma(wr[:], w_router.rearrange("(c p) f -> p c f", p=128), [], [B_wr])
                dma(br[:], b_router, [], [B_br])
                with contextlib.ExitStack() as s4:
                    Wout = sbt(s4, "Wout", [128, 8, 1024], BF16)
                    hrow = sbt(s4, "hrow", [128, 16, 1024], BF16)
                    zt = sbt(s4, "zt", [128, 2, 1024], BF16)
                    B_zt, B_Xz = em.bufs(2, "zx")
                    mset(zt[:], 0.0, [B_zt], eng="pool")
                    for cz in range(NB // 2):
                        dma(Xs[cz * 256:(cz + 1) * 256, :].rearrange("(r p) f -> p r f", p=128), zt[:], [B_zt], [B_Xz], sembuf=B_Xz)
                    B_Wout = em.buf("Wout")
                    dma(Wout[:], w_out.rearrange("(c p) f -> p c f", p=128), [], [B_Wout], q="pool")
                    ltri = sbt(s4, "ltri_s", [128, 128], BF16)
                    iota32 = sbt(s4, "iota32_s", [128, 32])
                    thr16 = sbt(s4, "thr16_s", [128, 32, 16])
                    iota96 = sbt(s4, "iota96_s", [128, NB])
                    pidx = sbt(s4, "pidx_s", [128, 1])
                    B_ltri, B_iota32, B_thr16, B_iota96, B_pidx = em.bufs(5, "cst")
                    dma(ltri[:], ltri_d, [], [B_ltri])
                    dma(iota32[:], iota32_d, [], [B_iota32])
                    dma(thr16[:].rearrange("p a b -> p (a b)"), thr16_d, [], [B_thr16])
                    dma(iota96[:], iota96_d, [], [B_iota96])
                    dma(pidx[:], pidx_d, [], [B_pidx])
                    def dbl(fn):
                        return [fn(0), fn(1)]
                    oT_ = dbl(lambda i: sbt(s4, "oT%d" % i, [128, 8, 128], BF16))
                    xt4_ = dbl(lambda i: sbt(s4, "xt4_%d" % i, [128, 1024]))
                    tmp4_ = dbl(lambda i: sbt(s4, "tmp4_%d" % i, [128, 1024]))
                    x1t_ = dbl(lambda i: sbt(s4, "x1t_%d" % i, [128, 1024]))
                    h2t_ = dbl(lambda i: sbt(s4, "h2t_%d" % i, [128, 1024]))
                    h2Tf_ = dbl(lambda i: sbt(s4, "h2Tf_%d" % i, [128, 8, 128]))
                    junk4_ = dbl(lambda i: sbt(s4, "junk4_%d" % i, [128, 1024], BF16))
                    s4s_ = dbl(lambda i: sbt(s4, "s4s_%d" % i, [128, 16]))
                    lg_ = dbl(lambda i: sbt(s4, "lg_%d" % i, [128, 32]))
                    v8_ = dbl(lambda i: sbt(s4, "v8_%d" % i, [128, 8]))
                    i8_ = dbl(lambda i: sbt(s4, "i8_%d" % i, [128, 8], U32))
                    msk_ = dbl(lambda i: sbt(s4, "msk_%d" % i, [128, 32]))
                    e4_ = dbl(lambda i: sbt(s4, "e4_%d" % i, [128, 4]))
                    pto_ = dbl(lambda i: pst(s4, "pto%d" % i, [128, 8, 128], BF16))
                    pmix = pst(s4, "pmix", [128, 1024])
                    ptf = pst(s4, "ptf", [128, 8, 128])
                    plg = pst(s4, "plg", [128, 32])
                    BB = {n: em.bufs(2, "s4" + n) for n in ["oT", "xt4", "tmp4", "x1t", "h2t", "h2Tf", "junk4", "s4s", "lg", "v8", "i8", "msk", "e4", "pto"]}
                    B_pmix, B_ptf, B_plg = em.bufs(3, "s4p")
                    for j in range(16):
                        q2 = j % 2
                        oT, xt4, tmp4, x1t, h2t, h2Tf, junk4, s4s, lg, v8, i8, msk, e4, pto = (
                            oT_[q2], xt4_[q2], tmp4_[q2], x1t_[q2], h2t_[q2], h2Tf_[q2], junk4_[q2], s4s_[q2], lg_[q2], v8_[q2], i8_[q2], msk_[q2], e4_[q2], pto_[q2])
                        (B_oT, B_xt4, B_tmp4, B_x1t, B_h2t, B_h2Tf, B_junk4, B_s4s, B_lg, B_v8, B_i8, B_msk, B_e4, B_pto) = (
                            BB[n][q2] for n in ["oT", "xt4", "tmp4", "x1t", "h2t", "h2Tf", "junk4", "s4s", "lg", "v8", "i8", "msk", "e4", "pto"])
                        pet([(pto[:, c, :], o_all[:, j, c * 128:(c + 1) * 128], identb[:]) for c in range(4)], [B_oall[j], B_identb], [B_pto])
                        cp(oT[:, 0:4, :], pto[:, 0:4, :], [B_pto], [B_oT])
                        mms = []
                        for n in range(2):
                            for c in range(4):
                                mms.append((pmix[:, n * 512:(n + 1) * 512], oTd[:, c, j * 128:(j + 1) * 128], Wout[:, c, n * 512:(n + 1) * 512], c == 0, False))
                            for c in range(4):
                                mms.append((pmix[:, n * 512:(n + 1) * 512], oT[:, c, :], Wout[:, 4 + c, n * 512:(n + 1) * 512], False, c == 3))
                        pe(mms, [B_oT, B_Wout] + [B_oTd[c][j // 4] for c in range(4)], [B_pmix])
                        dma(xt4[:], xq[j * 128:(j + 1) * 128, :], [], [B_xt4])
                        act(junk4[:], pmix[:], AF.Square, [B_pmix], [B_junk4, B_s4s], accum_out=s4s[:, 0:1])
                        ts(s4s[:, 1:2], s4s[:, 0:1], 1.0 / 1024, 1e-6, ALU.mult, ALU.add, [B_s4s], [B_s4s])
                        act(s4s[:, 2:3], s4s[:, 1:2], AF.Sqrt, [B_s4s], [B_s4s])
                        dve(lambda E, s4s=s4s, v8=v8, lg=lg, i8=i8, e4=e4: E.reciprocal(out=s4s[:, 3:4], in_=s4s[:, 2:3]), [B_s4s], [B_s4s])
                        stt(tmp4[:], pmix[:], s4s[:, 3:4], rows[:, 2, :], ALU.mult, ALU.mult, [B_pmix, B_s4s, B_rows], [B_tmp4])
                        tt(x1t[:], tmp4[:], xt4[:], ALU.add, [B_tmp4, B_xt4], [B_x1t])
                        dma(x1s[j * 128:(j + 1) * 128, :], x1t[:], [B_x1t], [B_x1s[j]], sembuf=B_x1s[j])
                        act(junk4[:], x1t[:], AF.Square, [B_x1t], [B_junk4, B_s4s], accum_out=s4s[:, 4:5])
                        ts(s4s[:, 5:6], s4s[:, 4:5], 1.0 / 1024, 1e-6, ALU.mult, ALU.add, [B_s4s], [B_s4s])
                        act(s4s[:, 6:7], s4s[:, 5:6], AF.Sqrt, [B_s4s], [B_s4s])
                        dve(lambda E, s4s=s4s, v8=v8, lg=lg, i8=i8, e4=e4: E.reciprocal(out=s4s[:, 7:8], in_=s4s[:, 6:7]), [B_s4s], [B_s4s])
                        stt(tmp4[:], x1t[:], s4s[:, 7:8], rows[:, 0, :], ALU.mult, ALU.mult, [B_x1t, B_s4s, B_rows], [B_tmp4])
                        tt(h2t[:], tmp4[:], rows[:, 1, :], ALU.add, [B_tmp4, B_rows], [B_h2t])
                        act(hrow[:, j, :], h2t[:], AF.Copy, [B_h2t], [B_hrow[j]])
                        pet([(ptf[:, c, :], h2t[:, c * 128:(c + 1) * 128], identf[:]) for c in range(8)], [B_h2t, B_identf], [B_ptf])
                        cp(h2Tf[:], ptf[:], [B_ptf], [B_h2Tf])
                        mms = [(plg[:], h2Tf[:, c, :], wr[:, c, :], c == 0, False) for c in range(8)]
                        mms.append((plg[:], onesf[0:1, 0:128], br[0:1, :], False, True))
                        pe(mms, [B_h2Tf, B_wr, B_br, B_onesf], [B_plg])
                        cp(lg[:], plg[:], [B_plg], [B_lg])
                        dve(lambda E, s4s=s4s, v8=v8, lg=lg, i8=i8, e4=e4: E.max(out=v8[:], in_=lg[:]), [B_lg], [B_v8])
                        dve(lambda E, s4s=s4s, v8=v8, lg=lg, i8=i8, e4=e4: E.max_index(out=i8[:], in_max=v8[:], in_values=lg[:]), [B_lg, B_v8], [B_i8])
                        cp(eidx[:, j, :], i8[:], [B_i8], [B_eidx])
                        ts(msk[:], lg[:], v8[:, 3:4], None, ALU.is_ge, None, [B_lg, B_v8], [B_msk])
                        cp(mskb[:, j, :], msk[:], [B_msk], [B_mskb])
                        ts(s4s[:, 8:9], v8[:, 0:1], -1.0, None, ALU.mult, None, [B_v8, B_s4s], [B_s4s])
                        act(e4[:], v8[:, 0:4], AF.Exp, [B_v8, B_s4s], [B_e4], bias=s4s[:, 8:9], scale=1.0)
                        dve(lambda E, s4s=s4s, v8=v8, lg=lg, i8=i8, e4=e4: E.reduce_sum(out=s4s[:, 9:10], in_=e4[:], axis=AX.X), [B_e4, B_s4s], [B_s4s])
                        dve(lambda E, s4s=s4s, v8=v8, lg=lg, i8=i8, e4=e4: E.reciprocal(out=s4s[:, 10:11], in_=s4s[:, 9:10]), [B_s4s], [B_s4s])
                        ts(gate4[:, j, :], e4[:], s4s[:, 10:11], None, ALU.mult, None, [B_e4, B_s4s], [B_gate4])
                    cnt_t = sbt(s4, "cnt_t", [128, 32])
                    cmp1 = sbt(s4, "cmp1", [128, 32, 16])
                    nbk = sbt(s4, "nbk", [128, 32])
                    ones32 = sbt(s4, "ones32", [128, 32])
                    cum = sbt(s4, "cum", [128, 32])
                    pstart = sbt(s4, "pstart", [128, 32])
                    cmp2 = sbt(s4, "cmp2", [128, NB, 32])
                    blk = sbt(s4, "blk", [128, NB])
                    chg = sbt(s4, "chg", [128, NB])
                    idxf = sbt(s4, "idxf", [128, 5, NB])
                    chg2 = sbt(s4, "chg2", [128, NB])
                    B_chg2 = em.buf("chg2")
                    destf = sbt(s4, "destf", [128, 16, 32])
                    ohf = sbt(s4, "ohf", [128, 32])
                    dstf = sbt(s4, "dstf", [128, 64])
                    (B_cnt, B_cmp1, B_nbk, B_ones32, B_cum, B_pstart, B_cmp2, B_blk, B_chg, B_idxf, B_destf, B_ohf, B_dstf) = em.bufs(13, "rt")
                    mms = [(plg[:], onesb[:, 0:128], mskb[:, j, :], j == 0, j == 15) for j in range(16)]
                    pe(mms, [B_onesb, B_mskb], [B_plg])
                    cp(cnt_t[:], plg[:], [B_plg], [B_cnt])
                    tt(cmp1[:], cnt_t[:].unsqueeze(2).to_broadcast([128, 32, 16]), thr16[:], ALU.is_gt, [B_cnt, B_thr16], [B_cmp1])
                    dve(lambda E: E.reduce_sum(out=nbk[:], in_=cmp1[:], axis=AX.X), [B_cmp1], [B_nbk])
                    mset(ones32[:], 1.0, [B_ones32])
                    dve(lambda E: E.tensor_tensor_scan(out=cum[:], data0=ones32[:], data1=nbk[:], initial=0.0, op0=ALU.mult, op1=ALU.add),
                        [B_ones32, B_nbk], [B_cum])
                    tt(pstart[:], cum[:], nbk[:], ALU.subtract, [B_cum, B_nbk], [B_pstart])
                    ts(pstart[:], pstart[:], 128.0, None, ALU.mult, None, [B_pstart], [B_pstart])
                    tt(cmp2[:], cum[:].unsqueeze(1).to_broadcast([128, NB, 32]), iota96[:].unsqueeze(2).to_broadcast([128, NB, 32]), ALU.is_le,
                       [B_cum, B_iota96], [B_cmp2])
                    dve(lambda E: E.reduce_sum(out=blk[:], in_=cmp2[:], axis=AX.X), [B_cmp2], [B_blk])
                    ts(blk[:], blk[:], 31.0, None, ALU.min, None, [B_blk], [B_blk])
                    ts(ohTall[:], blk[0:32, :], pidx[0:32, 0:1], None, ALU.is_equal, None, [B_blk, B_pidx], [B_ohTall])
                    mset(chg[:, 0:1], 1.0, [B_chg])
                    tt(chg[:, 1:NB], blk[:, 1:NB], blk[:, 0:NB - 1], ALU.not_equal, [B_blk, B_chg], [B_chg])
                    mset(chg2[:, 0:2], 1.0, [B_chg2])
                    tt(chg2[:, 2:NB], blk[:, 2:NB], blk[:, 0:NB - 2], ALU.not_equal, [B_blk, B_chg2], [B_chg2])
                    ts(chg[:], chg[:], -float(2 ** 27), float(2 ** 27), ALU.mult, ALU.add, [B_chg], [B_chg])
                    ts(chg2[:], chg2[:], -float(2 ** 27), float(2 ** 27), ALU.mult, ALU.add, [B_chg2], [B_chg2])
                    ts(blk[:], blk[:], 128.0, pidx[:, 0:1], ALU.mult, ALU.add, [B_blk, B_pidx], [B_blk])
                    tt(idxf[:, 4, :], blk[:], chg[:], ALU.add, [B_blk, B_chg], [B_idxf])
                    ts(idxf[:, 0, :], idxf[:, 4, :], 2.0, None, ALU.mult, None, [B_idxf], [B_idxf])
                    ts(idxf[:, 1, :], idxf[:, 4, :], 2.0, 1.0, ALU.mult, ALU.add, [B_idxf], [B_idxf])
                    tt(idxf[:, 4, :], blk[:], chg2[:], ALU.add, [B_blk, B_chg2, B_idxf], [B_idxf])
                    ts(idxf[:, 2, :], idxf[:, 4, :], 2.0, None, ALU.mult, None, [B_idxf], [B_idxf])
                    ts(idxf[:, 3, :], idxf[:, 4, :], 2.0, 1.0, ALU.mult, ALU.add, [B_idxf], [B_idxf])
                    cp(idxW[:], idxf[:, 0:4, :], [B_idxf], [B_idxW])
                    for j in range(16):
                        mms = [(plg[:], onesb[:, 0:128], mskb[:, jj, :], jj == 0, False) for jj in range(j)]
                        mms.append((plg[:], ltri[:], mskb[:, j, :], j == 0, True))
                        pe(mms, [B_onesb, B_ltri, B_mskb], [B_plg])
                        tt(destf[:, j, :], plg[:], pstart[:], ALU.add, [B_plg, B_pstart], [B_destf])
                    B_XO = em.buf("XO")
                    B_dsti = em.bufs(16, "dsti")
                    for j in range(16):
                        for k in range(4):
                            ts(ohf[:], iota32[:], eidx[:, j, k:k + 1], None, ALU.is_equal, None, [B_iota32, B_eidx], [B_ohf])
                            tt(ohf[:], ohf[:], destf[:, j, :], ALU.mult, [B_ohf, B_destf], [B_ohf])
                            dve(lambda E, j=j, k=k: E.reduce_sum(out=dstf[:, 4 * j + k:4 * j + k + 1], in_=ohf[:], axis=AX.X), [B_ohf], [B_dstf])
                        cp(dsti[:, 4 * j:4 * j + 4], dstf[:, 4 * j:4 * j + 4], [B_dstf], [B_dsti[j]])
                        for k in range(4):
                            col = dsti[:, 4 * j + k:4 * j + k + 1]
                            bsc = em.buf("sc")
                            em.dma("pool", lambda E, j=j, col=col: [E.indirect_dma_start(
                                out=Xs, out_offset=bass.IndirectOffsetOnAxis(ap=col, axis=0), in_=hrow[:, j, :], in_offset=None)],
                                reads=[B_hrow[j], B_dsti[j], B_Xz], writes=[bsc], sembuf=B_XO)
                    for nm, srct, bb in (("dsti", dsti, B_dsti[15]), ("idxW", idxW, B_idxW)):
                        d = dbgout(nm, [128, srct.shape[1] * (srct.shape[2] if len(srct.shape) > 2 else 1)], I32)
                        if d is not None:
                            dma(d, srct[:] if len(srct.shape) == 2 else srct[:].rearrange("p a b -> p (a b)"), [bb], [YB], sembuf=bb)
                    d = dbgout("gate4", [128, 64])
                    if d is not None:
                        dma(d, gate4[:].rearrange("p a b -> p (a b)"), [B_gate4], [YB], sembuf=B_gate4)
                    em.barrier()
                    em.emit()
                with contextlib.ExitStack() as s5:
                    wb1s = [sbt(s5, "wb1_%d" % i, [128, 8, 2048], BF16) for i in range(2)]
                    B_wb1 = [em.bufs(2, "wb1p%d" % i) for i in range(2)]
                    wb2 = sbt(s5, "wb2", [128, 9, 1024], BF16)
                    b1all = sbt(s5, "b1all", [32, 2048], BF16)
                    b2all = sbt(s5, "b2all", [32, 1024], BF16)
                    xbk = [sbt(s5, "xbk%d" % i, [128, 1024], BF16) for i in range(2)]
                    xT = [sbt(s5, "xT%d" % i, [128, 8, 128], BF16) for i in range(2)]
                    ohT = [sbt(s5, "ohT%d" % i, [32, 128], BF16) for i in range(2)]
                    glu = sbt(s5, "glu", [128, 1024])
                    lin = sbt(s5, "lin", [128, 1024])
                    sig = sbt(s5, "sig", [128, 1024], BF16)
                    ab = sbt(s5, "ab", [128, 1024], BF16)
                    aT = sbt(s5, "aT", [128, 8, 128], BF16)
                    yb = [sbt(s5, "yb%d" % i, [128, 1024]) for i in range(2)]
                    TX = pst(s5, "TX", [128, 8, 128], BF16)
                    TA = pst(s5, "TA", [128, 8, 128], BF16)
                    H = pst(s5, "H", [128, 2048])
                    Y = pst(s5, "Y", [128, 1024])
                    Ybf = Y.bitcast(BF16)
                    B_wb1a, B_wb1b, B_wb2, B_b1all, B_b2all, B_glu, B_lin, B_sig, B_ab, B_aT, B_TX, B_TA, B_H, B_Y, B_Ys = em.bufs(15, "s5")
                    B_xbk = em.bufs(2, "xbk"); B_obk = em.bufs(2, "obk"); B_xT = em.bufs(2, "xT"); B_ohT = em.bufs(2, "ohT"); B_yb = em.bufs(2, "yb")
                    bcreg = s5.enter_context(nc.gpsimd.register("bcreg"))
                    em.streams["pool"].append(lambda E: E.reg_mov(bcreg, 8191))
                    dma(b1all[:], b1, [], [B_b1all], q="pool")
                    dma(b2all[:], b2, [], [B_b2all], q="pool")
                    ab2 = [ab, sbt(s5, "ab_b", [128, 1024], BF16)]
                    B_ab2 = [B_ab, em.buf("ab_b")]

                    def loads(i):
                        p = i % 2
                        dma(xbk[p][:], Xs[i * 128:(i + 1) * 128, :], [], [B_xbk[p]])

                    def gathers(i):
                        for hh in range(2):
                            em.dma("pool", lambda E, i=i, hh=hh: [E.indirect_dma_start(
                                out=wb1s[i % 2][:, 4 * hh:4 * hh + 4, :].rearrange("p c f -> p (c f)"), out_offset=None, in_=W1p,
                                in_offset=bass.IndirectOffsetOnAxis(ap=idxW[:, 2 + hh, i:i + 1], axis=0), bounds_check=bcreg, oob_is_err=False)],
                                reads=[B_idxW], writes=[B_wb1[i % 2][hh]])

                    def gathers2(i):
                        for hh in range(2):
                            em.dma("pool", lambda E, i=i, hh=hh: [E.indirect_dma_start(
                                out=wb2[:, 4 * hh:4 * hh + 4, :].rearrange("p c f -> p (c f)"), out_offset=None, in_=W2p,
                                in_offset=bass.IndirectOffsetOnAxis(ap=idxW[:, hh, i:i + 1], axis=0), bounds_check=bcreg, oob_is_err=False)],
                                reads=[B_idxW], writes=[B_wb2])

                    def stageA(i):
                        p = i % 2
                        if i + 1 < NB:
                            loads(i + 1)
                        pet([(TX[:, c, :], xbk[p][:, c * 128:(c + 1) * 128], identb[:]) for c in range(8)], [B_xbk[p], B_identb], [B_TX])
                        cp(xT[p][:], TX[:], [B_TX], [B_xT[p]])
                        cp(ohT[p][:], ohTall[:, i:i + 1].to_broadcast([32, 128]), [B_ohTall], [B_ohT[p]])
                        mms = []
                        for n in range(4):
                            for c in range(8):
                                mms.append((H[:, n * 512:(n + 1) * 512], xT[p][:, c, :], wb1s[p][:, c, n * 512:(n + 1) * 512], c == 0, False))
                            mms.append((H[:, n * 512:(n + 1) * 512], ohT[p][:], b1all[:, n * 512:(n + 1) * 512], False, True))
                        pe(mms, [B_xT[p], B_ohT[p], B_wb1[p][0], B_wb1[p][1], B_b1all], [B_H])
                        if i + 2 < NB:
                            gathers(i + 2)
                        ts(glu[:], H[:, 0:2048:2], 7.0, None, ALU.min, None, [B_H], [B_glu])
                        ts(lin[:], H[:, 1:2048:2], -7.0, 7.0, ALU.max, ALU.min, [B_H], [B_lin])
                        act(sig[:], glu[:], AF.Sigmoid, [B_glu], [B_sig], scale=1.702)
                        stt(lin[:], lin[:], 1.0, glu[:], ALU.add, ALU.mult, [B_lin, B_glu], [B_lin])
                        tt(ab2[p][:], lin[:], sig[:], ALU.mult, [B_lin, B_sig], [B_ab2[p]])

                    def stageB(i):
                        p = i % 2
                        pet([(TA[:, c, :], ab2[p][:, c * 128:(c + 1) * 128], identb[:]) for c in range(8)], [B_ab2[p], B_identb], [B_TA])
                        act(aT[:], TA[:], AF.Copy, [B_TA], [B_aT])
                        mms = []
                        for n in range(2):
                            for c in range(8):
                                mms.append((Y[:, n * 512:(n + 1) * 512], aT[:, c, :], wb2[:, c, n * 512:(n + 1) * 512], c == 0, False))
                            mms.append((Y[:, n * 512:(n + 1) * 512], ohT[p][:], b2all[:, n * 512:(n + 1) * 512], False, True))
                        pe(mms, [B_aT, B_ohT[p], B_wb2, B_b2all], [B_Y])
                        if i + 1 < NB:
                            gathers2(i + 1)
                        act(yb[p][:], Y[:], AF.Copy, [B_Y], [B_yb[p]])
                        dma(Ys[i * 128:(i + 1) * 128, :], yb[p][:], [B_yb[p]], [B_Ys], sembuf=B_yb[p])

                    loads(0)
                    gathers(0)
                    gathers(1)
                    gathers2(0)
                    stageA(0)
                    for i in range(NB):
                        if i + 1 < NB:
                            stageA(i + 1)
                        stageB(i)
                    em.barrier()
                    em.emit()
                with contextlib.ExitStack() as s6:
                    x1r = [sbt(s6, "x1r%d" % i, [128, 1024]) for i in range(2)]
                    ot = [sbt(s6, "ot%d" % i, [128, 1024]) for i in range(2)]
                    yk = [sbt(s6, "yk%d" % i, [128, 4, 1024]) for i in range(2)]
                    ft = sbt(s6, "ft", [128, 1024])
                    junk6 = sbt(s6, "junk6", [128, 1024], BF16)
                    s6s = sbt(s6, "s6s", [128, 4])
                    B_x1r = em.bufs(2, "x1r"); B_ot = em.bufs(2, "ot"); B_yk = [em.bufs(4, "yk%d" % i) for i in range(2)]
                    B_ft, B_junk6, B_s6s = em.bufs(3, "s6")
                    for j in range(16):
                        i = j % 2
                        dma(x1r[i][:], x1s[j * 128:(j + 1) * 128, :], [B_x1s[j]], [B_x1r[i]])
                        for k in range(4):
                            em.dma("pool", lambda E, i=i, j=j, k=k: [E.indirect_dma_start(
                                out=yk[i][:, k, :], out_offset=None, in_=Ys,
                                in_offset=bass.IndirectOffsetOnAxis(ap=dsti[:, 4 * j + k:4 * j + k + 1], axis=0))],
                                reads=[B_dsti[j]], writes=[B_yk[i][k]])
                        ts(ft[:], yk[i][:, 0, :], gate4[:, j, 0:1], None, ALU.mult, None, [B_yk[i][0], B_gate4], [B_ft])
                        for k in range(1, 4):
                            stt(ft[:], yk[i][:, k, :], gate4[:, j, k:k + 1], ft[:], ALU.mult, ALU.add, [B_yk[i][k], B_gate4, B_ft], [B_ft])
                        act(junk6[:], ft[:], AF.Square, [B_ft], [B_junk6, B_s6s], accum_out=s6s[:, 0:1])
                        ts(s6s[:, 1:2], s6s[:, 0:1], 1.0 / 1024, 1e-6, ALU.mult, ALU.add, [B_s6s], [B_s6s])
                        act(s6s[:, 2:3], s6s[:, 1:2], AF.Sqrt, [B_s6s], [B_s6s])
                        dve(lambda E: E.reciprocal(out=s6s[:, 3:4], in_=s6s[:, 2:3]), [B_s6s], [B_s6s])
                        stt(ot[i][:], ft[:], s6s[:, 3:4], rows[:, 3, :], ALU.mult, ALU.mult, [B_ft, B_s6s, B_rows], [B_ot[i]])
                        tt(ot[i][:], ot[i][:], x1r[i][:], ALU.add, [B_ot[i], B_x1r[i]], [B_ot[i]])
                        dma(out[j * 128:(j + 1) * 128, :], ot[i][:], [B_ot[i]], [YB], sembuf=B_ot[i])
                    em.barrier()
                    em.emit()
        else:
            mz = sbt(top, "mz", [128, 1024])
            B_mz = em.buf("mz")
            mset(mz[:], 0.0, [B_mz])
            for j in range(16):
                dma(out[j * 128:(j + 1) * 128, :], mz[:], [B_mz], [YB], sembuf=B_mz)
            for nm, src in (("Qs", Qs), ("Ks", Ks), ("NQs", NQs), ("NKs", NKs), ("Vs", Vs), ("NVs", NVs)):
                d = dbgout(nm, src.shape, BF16)
                if d is not None:
                    dma(d, src, [], [YB], sembuf=YB)
            em.barrier()
            em.emit()
    return nc, dbg


SLOPES = [2.0 ** (-2 * (h + 1)) for h in range(4)]
_CACHE = {}


def _bf(a):
    return np.ascontiguousarray(a.astype(ml_dtypes.bfloat16))


def _nat_tables(rpb, qr):
    R0 = 32 * qr
    def table(r0):
        t = np.full((128, 8, 7, 128), -30000.0, np.float32)
        qrow = np.repeat(np.array([r0, r0 + 1]), 64)
        qcol = np.tile(np.arange(64), 2)
        qstart = np.clip(qrow - 4, 0, 120)
        qcs = np.clip(qcol - 8, 0, 48)
        for o in range(7):
            krow = np.repeat(np.array([r0 - 6 + 2 * o, r0 - 5 + 2 * o]), 64)
            kcol = np.tile(np.arange(64), 2)
            valid = ((krow[:, None] >= qstart[None, :]) & (krow[:, None] < qstart[None, :] + 8)
                     & (krow[:, None] >= 0) & (krow[:, None] < 128)
                     & (kcol[:, None] >= qcs[None, :]) & (kcol[:, None] < qcs[None, :] + 16))
            dr = np.clip(krow[:, None] - qrow[None, :] + 7, 0, 14)
            dc = np.clip(kcol[:, None] - qcol[None, :], -15, 15) + 15
            for h in range(8):
                vals = rpb[h][dr, dc]
                t[:, h, o, :] = np.where(valid, vals, -30000.0)
        return t
    slots = [table(R0 + 2 * 6)] + [table(R0 + 2 * j) for j in (0, 1, 14, 15)]
    return _bf(np.stack(slots).reshape(5, 128, 8 * 7 * 128))


def _prep(inputs):
    f32 = np.float32
    x = np.asarray(inputs["x"], f32)
    c = np.asarray(inputs["c"], f32)
    shared = dict(
        w_ada=np.ascontiguousarray(inputs["w_ada"][0], f32), b_ada=np.ascontiguousarray(inputs["b_ada"], f32).reshape(1, 6144),
        g_pre_mix=np.asarray(inputs["g_pre_mix"], f32).reshape(1, 1024), g_post_mix=np.asarray(inputs["g_post_mix"], f32).reshape(1, 1024),
        g_pre_ffn=np.asarray(inputs["g_pre_ffn"], f32).reshape(1, 1024), g_post_ffn=np.asarray(inputs["g_post_ffn"], f32).reshape(1, 1024),
        w_in=np.ascontiguousarray(inputs["w_in"][0], f32), w_out=np.ascontiguousarray(inputs["w_out"][0], f32),
        lamv=np.concatenate([np.asarray(inputs[k], f32).reshape(-1) for k in ("lam_q1", "lam_k1", "lam_q2", "lam_k2")]).reshape(1, 256),
        g_subln=np.asarray(inputs["g_subln"], f32).reshape(1, 128),
        w_router=np.ascontiguousarray(inputs["w_router"][0], f32), b_router=np.asarray(inputs["b_router"], f32).reshape(1, 32),
        identb=_bf(np.eye(128, dtype=f32)), identf=np.eye(128, dtype=f32),
        ltri=_bf(np.triu(np.ones((128, 128), f32), 1)),
        iota32=np.tile(np.arange(32, dtype=f32), (128, 1)),
        thr16=np.tile((128.0 * np.arange(16, dtype=f32))[None, None, :], (128, 32, 1)).reshape(128, 512),
        iota96=np.tile(np.arange(NB, dtype=f32), (128, 1)),
        pidx=np.arange(128, dtype=f32).reshape(128, 1),
    )
    if os.environ.get("KSTOP", "") not in ("1", "2"):
        shared.update(
            W1p=np.ascontiguousarray(np.asarray(inputs["w1"][0], f32).reshape(32, 8, 128, 2048).transpose(0, 2, 1, 3)).reshape(8192, 8192),
            W2p=np.ascontiguousarray(np.asarray(inputs["w2"][0], f32).reshape(32, 8, 128, 1024).transpose(0, 2, 1, 3)).reshape(8192, 4096),
            b1=np.ascontiguousarray(inputs["b1"][0], f32), b2=np.ascontiguousarray(inputs["b2"][0], f32))
    kl = np.arange(128)
    bd = np.stack([-SLOPES[h] * np.abs(kl[:, None] - kl[None, :]) for h in range(4)], axis=1)
    shared["bdiag"] = _bf(bd.reshape(128, 512).astype(f32))
    rpb = np.asarray(inputs["nat_rpb"], f32)[0]
    in_maps = []
    for core in range(8):
        b, qr = core // 4, core % 4
        own = np.arange(qr * 2048, (qr + 1) * 2048)
        rest = np.concatenate([np.arange(0, qr * 2048), np.arange((qr + 1) * 2048, 8192)])
        perm = np.concatenate([own, rest])
        R0 = 32 * qr
        tok0 = (R0 - 6) * 64
        xw = np.zeros((2816, 1024), f32)
        lo, hi = max(tok0, 0), min(tok0 + 2816, 8192)
        xw[lo - tok0:hi - tok0] = x[b, lo:hi]
        ql = np.arange(2048)
        q_lo = (ql % 128).astype(f32)
        qt_abs = (qr * 16 + ql // 128).astype(f32)
        qa = np.zeros((4, 3, 2, 2048), f32)
        for h in range(4):
            qa[h, 0, 0] = -SLOPES[h] * q_lo
            qa[h, 0, 1] = -SLOPES[h] * 128.0 * qt_abs
            qa[h, 1] = -qa[h, 0]
        kabs = perm.astype(f32)
        ktile_abs = perm[::128] // 128
        sig = np.ones(64, f32)
        sig[16:] = np.where(ktile_abs[16:] < qr * 16, 1.0, -1.0)
        ka = np.ones((2, 8192), f32) * np.repeat(sig, 128)[None, :]
        kt_tab = np.zeros((128, 4, 2, 64), f32)
        kpos = kabs.reshape(64, 128).T
        for h in range(4):
            kt_tab[:, h, 0, :] = SLOPES[h] * kpos * sig[None, :]
            kt_tab[:, h, 1, :] = -SLOPES[h] * kpos
        m = dict(shared)
        m.update(
            xb=np.ascontiguousarray(x[b][perm]), xq=np.ascontiguousarray(x[b, own]), xw=xw,
            cT=np.ascontiguousarray(c[b].reshape(8, 128).T),
            natb=_nat_tables(rpb, qr), qaug=_bf(qa), kaug=_bf(ka), ktab=kt_tab.reshape(128, 512),
        )
        in_maps.append(m)
    return in_maps


def kernel(**inputs):
    debug = tuple(os.environ.get("KDEBUG", "").split(",")) if os.environ.get("KDEBUG") else ()
    key = (debug, os.environ.get("KSTOP", ""))
    if key not in _CACHE:
        _CACHE[key] = build_program(debug)
    nc, dbg = _CACHE[key]
    in_maps = _prep(inputs)
    res = run_bass_kernel_spmd(nc, in_maps, core_ids=list(range(8)))
    outs = [np.asarray(r["out"], np.float32) for r in res.results]
    full = np.stack([np.concatenate(outs[0:4], axis=0), np.concatenate(outs[4:8], axis=0)], axis=0)
    if debug:
        kernel.last_debug = [{k: np.asarray(r["dbg_" + k]) for k in dbg} for r in res.results]
    return full
```

```python
import contextlib
import os
import numpy as np
import ml_dtypes
import concourse.bass as bass
import concourse.mybir as mybir
from concourse.bass_utils import run_bass_kernel_spmd

F32 = mybir.dt.float32
BF16 = mybir.dt.bfloat16
I32 = mybir.dt.int32
U32 = mybir.dt.uint32
AF = mybir.ActivationFunctionType
ALU = mybir.AluOpType
AX = mybir.AxisListType

NB = 96
BIG = float(2 ** 30)


class Buf:
    __slots__ = ("name", "w", "rs", "dsem", "dcnt")

    def __init__(self, name):
        self.name = name
        self.w = []
        self.rs = []
        self.dsem = None
        self.dcnt = 0


class Em:
    ENG = ("pe", "act", "dve", "pool", "sp")

    def __init__(self, nc, stack):
        self.nc = nc
        self.stack = stack
        self.streams = {e: [] for e in self.ENG}
        self.cnt = {e: 0 for e in self.ENG}
        self.esem = {e: stack.enter_context(nc.semaphore("sem_" + e)) for e in self.ENG}
        self.waited = {e: {} for e in self.ENG}
        self.nbuf = 0
        self.dbufs = []

    def buf(self, name=None):
        self.nbuf += 1
        return Buf("%s_%d" % (name or "b", self.nbuf))

    def bufs(self, n, name=None):
        return [self.buf(name) for _ in range(n)]

    def _dsem(self, b):
        if b.dsem is None:
            b.dsem = self.stack.enter_context(self.nc.semaphore("d_" + b.name))
            self.dbufs.append(b)
        return b.dsem

    def _deps(self, eng, reads, writes):
        toks = {}

        def add(t):
            key, val, h = t
            if eng == "pe" and key == "pe":
                return
            if key not in toks or toks[key][1] < val:
                toks[key] = t
        for b in reads:
            for t in b.w:
                add(t)
        for b in writes:
            for t in b.w:
                add(t)
            for t in b.rs:
                add(t)
        return self._filter(eng, toks.values())

    def _filter(self, eng, toks):
        out = []
        wd = self.waited[eng]
        for key, val, h in toks:
            if wd.get(key, 0) >= val:
                continue
            wd[key] = val
            out.append((h, val))
        return out

    def _update(self, tok, reads, writes):
        for b in reads:
            b.rs.append(tok)
        for b in writes:
            b.w = [tok]
            b.rs = []

    def op(self, eng, fn, reads=(), writes=()):
        waits = self._deps(eng, reads, writes)
        self.cnt[eng] += 1
        sem = self.esem[eng]
        tok = (eng, self.cnt[eng], sem)

        def run(E, fn=fn, waits=waits, sem=sem):
            for h, v in waits:
                E.wait_ge(h, v)
            fn(E).then_inc(sem, 1)
        self.streams[eng].append(run)
        self._update(tok, reads, writes)

    def dma(self, q, fn, reads=(), writes=(), n=1, sembuf=None):
        waits = self._deps(q, reads, writes)
        sb = sembuf if sembuf is not None else (writes[0] if writes else reads[0])
        sem = self._dsem(sb)
        sb.dcnt += 16 * n
        tok = ("d_" + sb.name, sb.dcnt, sem)

        def run(E, fn=fn, waits=waits, sem=sem, n=n):
            for h, v in waits:
                E.wait_ge(h, v)
            lst = fn(E)
            assert len(lst) == n
            for ins in lst:
                ins.then_inc(sem, 16)
        self.streams[q].append(run)
        self._update(tok, reads, writes)

    def barrier(self):
        toks = [(e, self.cnt[e], self.esem[e]) for e in self.ENG if self.cnt[e] > 0]
        toks += [("d_" + b.name, b.dcnt, b.dsem) for b in self.dbufs]
        for eng in self.ENG:
            waits = self._filter(eng, [t for t in toks if t[0] != eng])

            def run(E, waits=waits):
                for h, v in waits:
                    E.wait_ge(h, v)
            self.streams[eng].append(run)

    def emit(self):
        nc = self.nc
        st = self.streams
        with nc.Block() as block:
            @block.sync
            def _(E):
                for f in st["sp"]:
                    f(E)

            @block.tensor
            def _(E):
                for f in st["pe"]:
                    f(E)

            @block.scalar
            def _(E):
                for f in st["act"]:
                    f(E)

            @block.vector
            def _(E):
                for f in st["dve"]:
                    f(E)

            @block.gpsimd
            def _(E):
                for f in st["pool"]:
                    f(E)
        self.streams = {e: [] for e in self.ENG}


def build_program(debug=()):
    nc = bass.Bass("TRN2", target_bir_lowering=False)

    def din(name, shape, dt=F32):
        return nc.dram_tensor(name, list(shape), dt, kind="ExternalInput").ap()

    def dscr(name, shape, dt):
        return nc.dram_tensor(name, list(shape), dt, kind="Internal").ap()

    xb = din("xb", [8192, 1024])
    xq = din("xq", [2048, 1024])
    xw = din("xw", [2816, 1024])
    cT = din("cT", [128, 8])
    w_ada = din("w_ada", [1024, 6144])
    b_ada = din("b_ada", [1, 6144])
    g_pre_mix = din("g_pre_mix", [1, 1024])
    g_post_mix = din("g_post_mix", [1, 1024])
    g_pre_ffn = din("g_pre_ffn", [1, 1024])
    g_post_ffn = din("g_post_ffn", [1, 1024])
    w_in = din("w_in", [1024, 3072])
    w_out = din("w_out", [1024, 1024])
    lamv = din("lamv", [1, 256])
    g_subln = din("g_subln", [1, 128])
    natb = din("natb", [5, 128, 8 * 7 * 128], BF16)
    w_router = din("w_router", [1024, 32])
    b_router = din("b_router", [1, 32])
    if os.environ.get("KSTOP", "") not in ("1", "2"):
        W1p = din("W1p", [8192, 8192])
        W2p = din("W2p", [8192, 4096])
        b1 = din("b1", [32, 2048])
        b2 = din("b2", [32, 1024])
    qaug = din("qaug", [4, 3, 2, 2048], BF16)
    kaug = din("kaug", [2, 8192], BF16)
    ktab = din("ktab", [128, 4 * 2 * 64])
    bdiag = din("bdiag", [128, 4 * 128], BF16)
    identb_d = din("identb", [128, 128], BF16)
    identf_d = din("identf", [128, 128])
    ltri_d = din("ltri", [128, 128], BF16)
    iota32_d = din("iota32", [128, 32])
    thr16_d = din("thr16", [128, 512])
    iota96_d = din("iota96", [128, NB])
    pidx_d = din("pidx", [128, 1])
    out = nc.dram_tensor("out", [2048, 1024], F32, kind="ExternalOutput").ap()

    Qs = dscr("Qs", [4, 128, 2048], BF16)
    Ks = dscr("Ks", [4, 128, 8192], BF16)
    Vs = dscr("Vs", [4, 128, 64, 130], BF16)
    NQs = dscr("NQs", [4, 128, 2048], BF16)
    NKs = dscr("NKs", [4, 128, 2816], BF16)
    NVs = dscr("NVs", [128, 22, 8 * 66], BF16)
    x1s = dscr("x1s", [2048, 1024], F32)
    Xs = dscr("Xs", [NB * 128, 1024], BF16)
    Os = dscr("Os", [NB * 128, 32], BF16)
    Ys = dscr("Ys", [NB * 128, 1024], F32)

    dbg = {}

    def dbgout(name, shape, dt=F32):
        if name in debug:
            dbg[name] = nc.dram_tensor("dbg_" + name, list(shape), dt, kind="ExternalOutput").ap()
            return dbg[name]
        return None

    with contextlib.ExitStack() as top:
        em = Em(nc, top)
        YB = em.buf("yout")

        def sbt(st, name, shape, dt=F32):
            return st.enter_context(nc.sbuf_tensor(name, list(shape), dt))

        def pst(st, name, shape, dt=F32):
            return st.enter_context(nc.psum_tensor(name, list(shape), dt))

        def dma(out_, in_, reads, writes, q="sp", sembuf=None):
            em.dma(q, lambda E: [E.dma_start(out=out_, in_=in_)], reads=reads, writes=writes, sembuf=sembuf)

        def act(out_, in_, func, reads, writes, **kw):
            em.op("act", lambda E: E.activation(out=out_, in_=in_, func=func, **kw), reads, writes)

        def pe(mms, reads, writes):
            def fn(E):
                ins = None
                for (o, l, r, s0, s1) in mms:
                    ins = E.matmul(o, lhsT=l, rhs=r, start=s0, stop=s1)
                return ins
            em.op("pe", fn, reads, writes)

        def pet(trs, reads, writes):
            def fn(E):
                ins = None
                for (o, i, idn) in trs:
                    ins = E.transpose(o, i, idn)
                return ins
            em.op("pe", fn, reads, writes)

        def dve(f, reads, writes, eng="dve"):
            em.op(eng, f, reads, writes)

        def ts(out_, in0, s1, s2, op0, op1=None, reads=(), writes=(), eng="dve"):
            if op1 is None:
                dve(lambda E: E.tensor_scalar(out=out_, in0=in0, scalar1=s1, scalar2=None, op0=op0), reads, writes, eng)
            else:
                dve(lambda E: E.tensor_scalar(out=out_, in0=in0, scalar1=s1, scalar2=s2, op0=op0, op1=op1), reads, writes, eng)

        def tt(out_, in0, in1, op, reads, writes, eng="dve"):
            dve(lambda E: E.tensor_tensor(out=out_, in0=in0, in1=in1, op=op), reads, writes, eng)

        def stt(out_, in0, scalar, in1, op0, op1, reads, writes):
            dve(lambda E: E.scalar_tensor_tensor(out=out_, in0=in0, scalar=scalar, in1=in1, op0=op0, op1=op1), reads, writes)

        def cp(out_, in_, reads, writes, eng="dve"):
            dve(lambda E: E.tensor_copy(out=out_, in_=in_), reads, writes, eng)

        def mset(ap, v, writes, eng="dve"):
            dve(lambda E: E.memset(ap, v), (), writes, eng)

        def dump(name, dst_shape_src):
            pass

        identb = sbt(top, "identb_s", [128, 128], BF16)
        identf = sbt(top, "identf_s", [128, 128])
        onesb = sbt(top, "onesb", [128, 512], BF16)
        onesf = sbt(top, "onesf", [128, 128])
        rows = sbt(top, "rows", [128, 4, 1024])
        o_all = sbt(top, "o_all", [128, 16, 512], BF16)
        stat = sbt(top, "stat", [1, 16])
        negM = sbt(top, "negM", [128, 8])
        neglam = sbt(top, "neglam", [128, 1])
        gsub = sbt(top, "gsub", [128, 128])
        B_identb, B_identf, B_onesb, B_onesf, B_rows, B_stat, B_negM, B_neglam, B_gsub = em.bufs(9, "const")
        B_oall = em.bufs(16, "oall")
        oTd = sbt(top, "oTd", [128, 4, 2048], BF16)
        B_oTd = [em.bufs(4, "oTd%d" % h) for h in range(4)]
        g8col = sbt(top, "g8col", [128, 1])
        B_g8col = em.buf("g8col")
        dma(g8col[:], g_subln.rearrange("o e -> e o"), [], [B_g8col])
        ts(g8col[:], g8col[:], 0.8, None, ALU.mult, None, [B_g8col], [B_g8col])
        dma(identb[:], identb_d, [], [B_identb])
        dma(identf[:], identf_d, [], [B_identf])
        mset(onesb[:], 1.0, [B_onesb])
        mset(onesf[:], 1.0, [B_onesf])
        mset(stat[:], 0.0, [B_stat])
        dma(gsub[:], g_subln.to_broadcast([128, 128]), [], [B_gsub])

        with contextlib.ExitStack() as st01:
            Wall = sbt(st01, "Wall", [128, 8, 3072], BF16)
            biasrow = sbt(st01, "biasrow", [1, 3072], BF16)
            B_Wall = em.bufs(8, "Wall")
            B_biasrow = em.buf("biasrow")
            with contextlib.ExitStack() as st:
                sil = sbt(st, "sil", [128, 8])
                silrep = sbt(st, "silrep", [128, 8, 128], BF16)
                wada = [sbt(st, "wada%d" % i, [128, 8, 512], BF16) for i in range(2)]
                bada = sbt(st, "bada", [1, 6144], BF16)
                modrow = sbt(st, "modrow", [128, 6144])
                grow = sbt(st, "grow", [128, 4, 1024])
                s1row = sbt(st, "s1row", [128, 1024])
                tmpd = sbt(st, "tmpd", [128, 128])
                s1T = sbt(st, "s1T", [128, 8])
                sh1T = sbt(st, "sh1T", [128, 8])
                wst = [sbt(st, "wst%d" % i, [128, 3072]) for i in range(2)]
                lam_t = sbt(st, "lam_t", [1, 256])
                lam_s = sbt(st, "lam_s", [1, 8])
                pmod = [pst(st, "pmod%d" % i, [128, 512]) for i in range(2)]
                pbias = pst(st, "pbias", [1, 3072])
                B_sil, B_silrep, B_bada, B_grow, B_s1row, B_tmpd, B_s1T, B_sh1T, B_lamt, B_lams, B_pbias, B_plam = em.bufs(12, "p0")
                B_wada = em.bufs(2, "wada")
                B_modrow = em.bufs(12, "modrow")
                B_wst = em.bufs(2, "wst")
                B_pmod = em.bufs(2, "pmod")

                dma(sil[:], cT, [], [B_sil])
                act(sil[:], sil[:], AF.Silu, [B_sil], [B_sil])
                for c in range(8):
                    cp(silrep[:, c, :], sil[:, c:c + 1].to_broadcast([128, 128]), [B_sil], [B_silrep])
                dma(bada[:], b_ada, [], [B_bada], q="pool")
                for i, g in enumerate([g_pre_mix, g_post_mix, g_pre_ffn, g_post_ffn]):
                    dma(grow[:, i, :], g.to_broadcast([128, 1024]), [], [B_grow])
                for j in range(12):
                    wb = wada[j % 2]
                    dma(wb[:], w_ada[:, j * 512:(j + 1) * 512].rearrange("(c p) f -> p c f", p=128), [], [B_wada[j % 2]], q="pool")
                    mms = [(pmod[j % 2][:], silrep[:, c, :], wb[:, c, :], c == 0, False) for c in range(8)]
                    mms.append((pmod[j % 2][:], onesb[0:1, 0:128], bada[0:1, j * 512:(j + 1) * 512], False, True))
                    pe(mms, [B_silrep, B_wada[j % 2], B_bada, B_onesb], [B_pmod[j % 2]])
                    cp(modrow[:, j * 512:(j + 1) * 512], pmod[j % 2][:], [B_pmod[j % 2]], [B_modrow[j]])
                MR = lambda i: [B_modrow[2 * i], B_modrow[2 * i + 1]]
                m = lambda i: modrow[:, i * 1024:(i + 1) * 1024]
                stt(s1row[:], m(1), 1.0, grow[:, 0, :], ALU.add, ALU.mult, MR(1) + [B_grow], [B_s1row])
                stt(rows[:, 0, :], m(4), 1.0, grow[:, 2, :], ALU.add, ALU.mult, MR(4) + [B_grow], [B_rows])
                cp(rows[:, 1, :], m(3), MR(3) + [B_rows], [B_rows])
                tt(rows[:, 2, :], m(2), grow[:, 1, :], ALU.mult, MR(2) + [B_grow, B_rows], [B_rows])
                tt(rows[:, 3, :], m(5), grow[:, 3, :], ALU.mult, MR(5) + [B_grow, B_rows], [B_rows])
                for c in range(8):
                    tt(tmpd[:], s1row[:, c * 128:(c + 1) * 128], identf[:], ALU.mult, [B_s1row, B_identf], [B_tmpd])
                    dve(lambda E, c=c: E.reduce_sum(out=s1T[:, c:c + 1], in_=tmpd[:], axis=AX.X), [B_tmpd], [B_s1T])
                    tt(tmpd[:], modrow[:, c * 128:(c + 1) * 128], identf[:], ALU.mult, MR(0) + [B_identf], [B_tmpd])
                    dve(lambda E, c=c: E.reduce_sum(out=sh1T[:, c:c + 1], in_=tmpd[:], axis=AX.X), [B_tmpd], [B_sh1T])
                for c in range(8):
                    wsb = wst[c % 2]
                    dma(wsb[:], w_in[c * 128:(c + 1) * 128, :], [], [B_wst[c % 2]])
                    mms = [(pbias[0:1, n * 512:(n + 1) * 512], sh1T[:, c:c + 1], wsb[:, n * 512:(n + 1) * 512], c == 0, c == 7) for n in range(6)]
                    pe(mms, [B_sh1T, B_wst[c % 2]], [B_pbias])
                    act(Wall[:, c, :], wsb[:], AF.Copy, [B_wst[c % 2], B_s1T], [B_Wall[c]], scale=s1T[:, c:c + 1])
                cp(biasrow[:], pbias[:], [B_pbias], [B_biasrow])
                dma(lam_t[:], lamv, [], [B_lamt])
                tt(lam_t[0:1, 0:64], lam_t[0:1, 0:64], lam_t[0:1, 64:128], ALU.mult, [B_lamt], [B_lamt])
                tt(lam_t[0:1, 128:192], lam_t[0:1, 128:192], lam_t[0:1, 192:256], ALU.mult, [B_lamt], [B_lamt])
                dve(lambda E: E.reduce_sum(out=lam_s[0:1, 0:1], in_=lam_t[0:1, 0:64], axis=AX.X), [B_lamt], [B_lams])
                dve(lambda E: E.reduce_sum(out=lam_s[0:1, 1:2], in_=lam_t[0:1, 128:192], axis=AX.X), [B_lamt], [B_lams])
                act(lam_s[0:1, 2:4], lam_s[0:1, 0:2], AF.Exp, [B_lams], [B_lams])
                stt(lam_s[0:1, 4:5], lam_s[0:1, 3:4], -0.2, lam_s[0:1, 2:3], ALU.add, ALU.subtract, [B_lams], [B_lams])
                pe([(pmod[0][:, 0:1], onesf[0:1, 0:128], lam_s[0:1, 4:5], True, True)], [B_onesf, B_lams], [B_pmod[0]])
                cp(neglam[:], pmod[0][:, 0:1], [B_pmod[0]], [B_neglam])
                d = dbgout("rows", [128, 4096])
                if d is not None:
                    dma(d, rows[:].rearrange("p a f -> p (a f)"), [B_rows], [YB], sembuf=B_rows)
                d = dbgout("neglam", [128, 1])
                if d is not None:
                    dma(d, neglam[:], [B_neglam], [YB], sembuf=B_neglam)
                em.barrier()
                em.emit()

            with contextlib.ExitStack() as st:
                xt = [sbt(st, "xt%d" % i, [128, 1024]) for i in range(2)]
                xn = [sbt(st, "xn%d" % i, [128, 1024], BF16) for i in range(2)]
                xnT = [sbt(st, "xnT%d" % i, [128, 8, 512], BF16) for i in range(2)]
                ssq = sbt(st, "ssq", [128, 4])
                junk = sbt(st, "junk", [128, 1024], BF16)
                ev = [sbt(st, "ev%d" % i, [128, 512], BF16) for i in range(3)]
                sq = [sbt(st, "sq%d" % i, [128, 512], BF16) for i in range(2)]
                vt = [sbt(st, "vt%d" % i, [128, 4, 130], BF16) for i in range(2)]
                nvt = [sbt(st, "nvt%d" % i, [128, 8, 66], BF16) for i in range(2)]
                mx = sbt(st, "mx", [1, 2])
                ptr = [pst(st, "ptr%d" % i, [128, 8, 128], BF16) for i in range(2)]
                pp = [pst(st, "pp%d" % i, [128, 512]) for i in range(3)]
                pn = pst(st, "pn", [1, 512])
                B_xt = em.bufs(2, "xt"); B_xn = em.bufs(2, "xn"); B_xnT = em.bufs(2, "xnT")
                B_ssq = em.buf("ssq"); B_junk = em.buf("junk"); B_ev = em.bufs(3, "ev"); B_sq = em.bufs(2, "sq")
                B_vt = em.bufs(2, "vt"); B_nvt = em.bufs(2, "nvt"); B_mx = em.buf("mx")
                B_ptr = em.bufs(2, "ptr"); B_pp = em.bufs(3, "pp"); B_pn = em.buf("pn")
                B_scr = em.buf("scr1")
                for i in range(2):
                    mset(vt[i][:, :, 128:129], 1.0, [B_vt[i]])
                    mset(vt[i][:, :, 129:130], 0.0, [B_vt[i]])
                    mset(nvt[i][:, :, 64:65], 1.0, [B_nvt[i]])
                    mset(nvt[i][:, :, 65:66], 0.0, [B_nvt[i]])
                cnt = {"tile": 0, "grp": 0, "pp": 0, "ev": 0, "sq": 0, "vt": 0, "nvt": 0}

                def norm_group(src, g):
                    gi = cnt["grp"] % 2
                    cnt["grp"] += 1
                    for t in range(4):
                        i = cnt["tile"] % 2
                        cnt["tile"] += 1
                        r0 = g * 512 + t * 128
                        dma(xt[i][:], src[r0:r0 + 128, :], [], [B_xt[i]])
                        act(junk[:], xt[i][:], AF.Square, [B_xt[i]], [B_junk, B_ssq], accum_out=ssq[:, 0:1])
                        ts(ssq[:, 1:2], ssq[:, 0:1], 1.0 / 1024, 1e-6, ALU.mult, ALU.add, [B_ssq], [B_ssq])
                        act(ssq[:, 2:3], ssq[:, 1:2], AF.Sqrt, [B_ssq], [B_ssq])
                        dve(lambda E: E.reciprocal(out=ssq[:, 3:4], in_=ssq[:, 2:3]), [B_ssq], [B_ssq])
                        act(xn[i][:], xt[i][:], AF.Copy, [B_xt[i], B_ssq], [B_xn[i]], scale=ssq[:, 3:4])
                        pet([(ptr[i][:, c, :], xn[i][:, c * 128:(c + 1) * 128], identb[:]) for c in range(8)],
                            [B_xn[i], B_identb], [B_ptr[i]])
                        cp(xnT[gi][:, :, t * 128:(t + 1) * 128], ptr[i][:], [B_ptr[i]], [B_xnT[gi]])
                    return gi

                def proj_T(gi, col0, scale, dst, stat_idx):
                    k = cnt["pp"] % 3; cnt["pp"] += 1
                    mms = [(pp[k][:], Wall[:, c, col0:col0 + 128], xnT[gi][:, c, :], c == 0, False) for c in range(8)]
                    mms.append((pp[k][:], biasrow[0:1, col0:col0 + 128], onesb[0:1, 0:512], False, True))
                    pe(mms, B_Wall + [B_xnT[gi], B_biasrow, B_onesb], [B_pp[k]])
                    e = cnt["ev"] % 3; cnt["ev"] += 1
                    act(ev[e][:], pp[k][:], AF.Copy, [B_pp[k]], [B_ev[e]], scale=scale)
                    dma(dst, ev[e][:], [B_ev[e]], [B_scr], sembuf=B_ev[e])
                    s = cnt["sq"] % 2; cnt["sq"] += 1
                    tt(sq[s][:], ev[e][:], ev[e][:], ALU.mult, [B_ev[e]], [B_sq[s]])
                    pe([(pn[:], onesb[:, 0:1], sq[s][:], True, True)], [B_onesb, B_sq[s]], [B_pn])
                    dve(lambda E: E.reduce_max(out=mx[0:1, 0:1], in_=pn[0:1, :], axis=AX.X), [B_pn], [B_mx])
                    tt(stat[0:1, stat_idx:stat_idx + 1], stat[0:1, stat_idx:stat_idx + 1], mx[0:1, 0:1], ALU.max, [B_mx, B_stat], [B_stat])

                def proj_tok(gi, t, col0):
                    k = cnt["pp"] % 3; cnt["pp"] += 1
                    mms = [(pp[k][:], xnT[gi][:, c, t * 128:(t + 1) * 128], Wall[:, c, col0:col0 + 512], c == 0, False) for c in range(8)]
                    mms.append((pp[k][:], onesb[0:1, 0:128], biasrow[0:1, col0:col0 + 512], False, True))
                    pe(mms, B_Wall + [B_xnT[gi], B_biasrow, B_onesb], [B_pp[k]])
                    return k

                def norm_part(src, g, ntl):
                    gi = cnt["grp"] % 2
                    cnt["grp"] += 1
                    for t in range(ntl):
                        i = cnt["tile"] % 2
                        cnt["tile"] += 1
                        r0 = g * 512 + t * 128
                        dma(xt[i][:], src[r0:r0 + 128, :], [], [B_xt[i]])
                        act(junk[:], xt[i][:], AF.Square, [B_xt[i]], [B_junk, B_ssq], accum_out=ssq[:, 0:1])
                        ts(ssq[:, 1:2], ssq[:, 0:1], 1.0 / 1024, 1e-6, ALU.mult, ALU.add, [B_ssq], [B_ssq])
                        act(ssq[:, 2:3], ssq[:, 1:2], AF.Sqrt, [B_ssq], [B_ssq])
                        dve(lambda E: E.reciprocal(out=ssq[:, 3:4], in_=ssq[:, 2:3]), [B_ssq], [B_ssq])
                        act(xn[i][:], xt[i][:], AF.Copy, [B_xt[i], B_ssq], [B_xn[i]], scale=ssq[:, 3:4])
                        pet([(ptr[i][:, c, :], xn[i][:, c * 128:(c + 1) * 128], identb[:]) for c in range(8)],
                            [B_xn[i], B_identb], [B_ptr[i]])
                        cp(xnT[gi][:, :, t * 128:(t + 1) * 128], ptr[i][:], [B_ptr[i]], [B_xnT[gi]])
                    return gi

                def projB(kind, g, gi):
                    if kind == "own":
                        for h in range(4):
                            proj_T(gi, h * 128, 0.125, Qs[h, :, g * 512:(g + 1) * 512], h)
                        for c4 in range(4):
                            proj_T(gi, 1536 + c4 * 128, 0.125, NQs[c4, :, g * 512:(g + 1) * 512], 8 + c4)
                    elif kind == "seq":
                        for h in range(4):
                            proj_T(gi, 512 + h * 128, 1.0, Ks[h, :, g * 512:(g + 1) * 512], 4 + h)
                        for t in range(4):
                            k = proj_tok(gi, t, 1024)
                            v = cnt["vt"] % 2; cnt["vt"] += 1
                            cp(vt[v][:, :, 0:128], pp[k][:].rearrange("p (h e) -> p h e", h=4), [B_pp[k]], [B_vt[v]])
                            dma(Vs[:, :, g * 4 + t, :].rearrange("h p e -> p h e"), vt[v][:], [B_vt[v]], [B_scr], sembuf=B_vt[v])
                    else:
                        ntl = 4 if g < 5 else 2
                        ncol = ntl * 128
                        for c4 in range(4):
                            k = cnt["pp"] % 3; cnt["pp"] += 1
                            col0 = 2048 + c4 * 128
                            mms = [(pp[k][:, 0:ncol], Wall[:, c, col0:col0 + 128], xnT[gi][:, c, 0:ncol], c == 0, False) for c in range(8)]
                            mms.append((pp[k][:, 0:ncol], biasrow[0:1, col0:col0 + 128], onesb[0:1, 0:ncol], False, True))
                            pe(mms, B_Wall + [B_xnT[gi], B_biasrow, B_onesb], [B_pp[k]])
                            e = cnt["ev"] % 3; cnt["ev"] += 1
                            act(ev[e][:, 0:ncol], pp[k][:, 0:ncol], AF.Copy, [B_pp[k]], [B_ev[e]])
                            dma(NKs[c4, :, g * 512:g * 512 + ncol], ev[e][:, 0:ncol], [B_ev[e]], [B_scr], sembuf=B_ev[e])
                            s_ = cnt["sq"] % 2; cnt["sq"] += 1
                            tt(sq[s_][:, 0:ncol], ev[e][:, 0:ncol], ev[e][:, 0:ncol], ALU.mult, [B_ev[e]], [B_sq[s_]])
                            pe([(pn[:, 0:ncol], onesb[:, 0:1], sq[s_][:, 0:ncol], True, True)], [B_onesb, B_sq[s_]], [B_pn])
                            dve(lambda E, ncol=ncol: E.reduce_max(out=mx[0:1, 0:1], in_=pn[0:1, 0:ncol], axis=AX.X), [B_pn], [B_mx])
                            tt(stat[0:1, 12 + c4:13 + c4], stat[0:1, 12 + c4:13 + c4], mx[0:1, 0:1], ALU.max, [B_mx, B_stat], [B_stat])
                        for t in range(ntl):
                            k = proj_tok(gi, t, 2560)
                            v = cnt["nvt"] % 2; cnt["nvt"] += 1
                            cp(nvt[v][:, :, 0:64], pp[k][:].rearrange("p (h e) -> p h e", h=8), [B_pp[k]], [B_nvt[v]])
                            dma(NVs[:, g * 4 + t, :], nvt[v][:].rearrange("p h e -> p (h e)"), [B_nvt[v]], [B_scr], sembuf=B_nvt[v])

                groups = [("own", g, xq, 4) for g in range(4)] + [("seq", g, xb, 4) for g in range(16)] \
                    + [("win", g, xw, 4 if g < 5 else 2) for g in range(6)]
                gis = [None] * len(groups)
                gis[0] = norm_part(groups[0][2], groups[0][1], groups[0][3])
                for n_, (kind, g, src, ntl) in enumerate(groups):
                    if n_ + 1 < len(groups):
                        kn, gn, sn, tn = groups[n_ + 1]
                        gis[n_ + 1] = norm_part(sn, gn, tn)
                    projB(kind, g, gis[n_])
                mm_ = sbt(st, "mm_", [1, 8])
                pM = pst(st, "pM", [128, 8])
                B_mm, B_pM = em.bufs(2, "mm")
                tt(mm_[0:1, 0:4], stat[0:1, 0:4], stat[0:1, 4:8], ALU.mult, [B_stat], [B_mm])
                tt(mm_[0:1, 4:8], stat[0:1, 8:12], stat[0:1, 12:16], ALU.mult, [B_stat, B_mm], [B_mm])
                act(mm_[:], mm_[:], AF.Sqrt, [B_mm], [B_mm])
                ts(mm_[:], mm_[:], -1.05, None, ALU.mult, None, [B_mm], [B_mm])
                pe([(pM[:], onesf[0:1, 0:128], mm_[0:1, :], True, True)], [B_onesf, B_mm], [B_pM])
                cp(negM[:], pM[:], [B_pM], [B_negM])
                d = dbgout("negM", [128, 8])
                if d is not None:
                    dma(d, negM[:], [B_negM], [YB], sembuf=B_negM)
                em.barrier()
                em.emit()
        STOP = os.environ.get("KSTOP", "")
        if STOP != "1":
            with contextlib.ExitStack() as st:
                KA2 = [[sbt(st, "KA%d_%d" % (p_, m), [66, 8192], BF16) for m in range(2)] for p_ in range(2)]
                QA1 = [[sbt(st, "QA%d_%d" % (m, v), [66, 2048], BF16) for v in range(3)] for m in range(2)]
                QA2 = [QA1, QA1]
                Vh2 = [sbt(st, "Vh%d" % p_, [128, 64, 130], BF16) for p_ in range(2)]
                ktab_t = sbt(st, "ktab_s", [128, 4, 2, 64])
                kb = sbt(st, "kb", [128, 2, 64])
                bdg = sbt(st, "bdg_s", [128, 4, 128], BF16)
                PT = [sbt(st, "PT%d" % i, [128, 512], BF16) for i in range(3)]
                rz = sbt(st, "rz", [128, 512])
                O1T = sbt(st, "O1T", [128, 512])
                dT = sbt(st, "dT", [128, 512])
                sqb = sbt(st, "sqb", [128, 512], BF16)
                rs = sbt(st, "rs", [128, 512])
                gs8 = sbt(st, "gs8", [128, 128])
                S = [pst(st, "S%d" % i, [128, 512]) for i in range(3)]
                OT = [pst(st, "OT%d" % i, [128, 512]) for i in range(2)]
                ZB = [pst(st, "ZB%d" % i, [128, 512]) for i in range(2)]
                B_OT = em.bufs(2, "OT"); B_ZB = em.bufs(2, "ZB")
                B_rz, B_O1T, B_dT, B_sqb, B_rs = em.bufs(5, "ep")
                B_KA2 = [em.bufs(2, "KA%d" % p_) for p_ in range(2)]; B_QA1 = em.bufs(2, "QA"); B_QA2 = [B_QA1, B_QA1]; B_Vh2 = em.bufs(2, "Vh"); B_ktab = em.buf("ktab")
                B_kb = em.buf("kb"); B_bdg = em.buf("bdg"); B_PT = em.bufs(3, "PT")
                B_gs8 = em.buf("gs8")
                B_S = em.bufs(3, "S")
                dma(ktab_t[:].rearrange("p a b c -> p (a b c)"), ktab, [], [B_ktab])
                dma(bdg[:].rearrange("p a b -> p (a b)"), bdiag, [], [B_bdg])
                ts(gs8[:], gsub[:], 0.8, None, ALU.mult, None, [B_gsub], [B_gs8])
                it = 0

                def head_loads(hh):
                    p_ = hh % 2
                    for m in range(2):
                        dma(KA2[p_][m][0:64, :], Ks[hh, 64 * m:64 * m + 64, :], [], [B_KA2[p_][m]])
                        dma(KA2[p_][m][64:66, :], kaug, [], [B_KA2[p_][m]])
                    dma(Vh2[p_][:], Vs[hh], [], [B_Vh2[p_]])

                def q_loads(hh):
                    for m in range(2):
                        for v in range(3):
                            dma(QA1[m][v][0:64, :], Qs[hh, 64 * m:64 * m + 64, :], [], [B_QA1[m]])
                            dma(QA1[m][v][64:66, :], qaug[hh, v], [], [B_QA1[m]])

                for h in range(4):
                    q_loads(h)
                    if h == 0:
                        head_loads(0)
                    if h + 1 < 4:
                        head_loads(h + 1)
                    KA, QA, Vh = KA2[h % 2], QA2[h % 2], Vh2[h % 2]
                    B_KA, B_QA, B_Vh = B_KA2[h % 2], B_QA2[h % 2], B_Vh2[h % 2]
                    ts(kb[:], ktab_t[:, h], negM[:, h:h + 1], None, ALU.add, None, [B_ktab, B_negM], [B_kb])
                    seq = [(qc, m, kt) for qc in range(4) for m in range(2) for kt in range(64)]

                    def segs_of(qc, kt):
                        if kt >= 16:
                            return [(0, 4, 0, kb[:, 0, kt:kt + 1])]
                        segs = []
                        for t in range(4):
                            qt = 4 * qc + t
                            if qt > kt:
                                cls = (0, kb[:, 0, kt:kt + 1])
                            elif qt == kt:
                                cls = (2, negM[:, h:h + 1])
                            else:
                                cls = (1, kb[:, 1, kt:kt + 1])
                            if segs and segs[-1][2] == cls[0]:
                                segs[-1] = (segs[-1][0], t + 1, cls[0], cls[1])
                            else:
                                segs.append((t, t + 1, cls[0], cls[1]))
                        return segs

                    def emit_S(n):
                        qc, m, kt = seq[n]
                        sb = (it + n) % 3
                        mms = []
                        for (t0, t1, v, col) in segs_of(qc, kt):
                            c0, c1 = t0 * 128, t1 * 128
                            q0 = qc * 512
                            mms.append((S[sb][:, c0:c1], KA[m][0:66, kt * 128:(kt + 1) * 128], QA[m][v][0:66, q0 + c0:q0 + c1], True, v != 2))
                            if v == 2:
                                mms.append((S[sb][:, c0:c1], identb[:], bdg[:, h, :], False, True))
                        pe(mms, [B_KA[m], B_QA[m], B_identb, B_bdg], [B_S[sb]])

                    def emit_rest(n):
                        qc, m, kt = seq[n]
                        sb = (it + n) % 3
                        pb = (it + n) % 3
                        for (t0, t1, v, col) in segs_of(qc, kt):
                            c0, c1 = t0 * 128, t1 * 128
                            act(PT[pb][:, c0:c1], S[sb][:, c0:c1], AF.Exp, [B_S[sb], B_kb, B_negM], [B_PT[pb]], bias=col, scale=1.0)
                        ob = ((it + n) // 64) % 2
                        pe([(OT[ob][:], Vh[:, kt, 0:128], PT[pb][:], kt == 0, kt == 63),
                            (ZB[ob][:], onesb[:, 0:128], PT[pb][:], kt == 0, kt == 63)],
                           [B_PT[pb], B_Vh, B_onesb], [B_OT[ob], B_ZB[ob]])
                        if kt != 63:
                            return
                        dve(lambda E, ob=ob: E.reciprocal(out=rz[:], in_=ZB[ob][:]), [B_ZB[ob]], [B_rz])
                        if m == 0:
                            tt(O1T[:], OT[ob][:], rz[:], ALU.mult, [B_OT[ob], B_rz], [B_O1T])
                        else:
                            tt(dT[:], OT[ob][:], rz[:], ALU.mult, [B_OT[ob], B_rz], [B_dT])
                            stt(dT[:], dT[:], neglam[:, 0:1], O1T[:], ALU.mult, ALU.add, [B_dT, B_neglam, B_O1T], [B_dT])
                            tt(sqb[:], dT[:], dT[:], ALU.mult, [B_dT], [B_sqb])
                            pe([(ZB[ob][:], onesb[:, 0:128], sqb[:], True, True)], [B_sqb, B_onesb], [B_ZB[ob]])
                            ts(rs[:], ZB[ob][:], 1.0 / 128, 1e-6, ALU.mult, ALU.add, [B_ZB[ob]], [B_rs])
                            act(rs[:], rs[:], AF.Sqrt, [B_rs], [B_rs])
                            dve(lambda E: E.reciprocal(out=rs[:], in_=rs[:]), [B_rs], [B_rs])
                            tt(dT[:], dT[:], rs[:], ALU.mult, [B_dT, B_rs], [B_dT])
                            ts(oTd[:, h, qc * 512:(qc + 1) * 512], dT[:], g8col[:, 0:1], None, ALU.mult, None,
                               [B_dT, B_g8col], [B_oTd[h][qc]])

                    emit_S(0)
                    emit_S(1)
                    for n in range(len(seq)):
                        if n + 2 < len(seq):
                            emit_S(n + 2)
                        emit_rest(n)
                    it += len(seq)
                em.barrier()
                em.emit()

            with contextlib.ExitStack() as st:
                NQT = sbt(st, "NQT", [128, 4, 2048], BF16)
                NKT = sbt(st, "NKT", [128, 4, 2816], BF16)
                NV = sbt(st, "NV", [128, 22, 528], BF16)
                nbt = [sbt(st, "nbt%d" % i, [128, 8 * 7 * 128], BF16) for i in range(2)]
                PN = [sbt(st, "PN%d" % i, [128, 896], BF16) for i in range(3)]
                sn = sbt(st, "sn", [128, 2])
                SN = [pst(st, "SN%d" % i, [128, 1024]) for i in range(3)]
                NO = [pst(st, "NO%d" % i, [128, 512]) for i in range(2)]
                B_NQT, B_NKT, B_NV, B_sn = em.bufs(4, "nat")
                B_nbt = em.bufs(2, "nbt"); B_PN = em.bufs(3, "PN"); B_SN = em.bufs(3, "SN"); B_NO = em.bufs(2, "NO")
                dma(NQT[:], NQs.rearrange("c p t -> p c t"), [], [B_NQT])
                dma(NKT[:], NKs.rearrange("c p t -> p c t"), [], [B_NKT])
                dma(NV[:], NVs, [], [B_NV])
                items = [(j, h) for j in range(16) for h in range(8)]

                def nat_S(n):
                    j, h = items[n]
                    nb_ = nbt[j % 2]
                    if h == 0:
                        slot = {0: 1, 1: 2, 14: 3, 15: 4}.get(j, 0)
                        dma(nb_[:], natb[slot], [], [B_nbt[j % 2]])
                    c4, hp = h // 2, (h % 2) * 64
                    sb = n % 3
                    mms = []
                    for o in range(7):
                        oc = slice(o * 128, (o + 1) * 128)
                        mms.append((SN[sb][:, oc], NKT[hp:hp + 64, c4, (j + o) * 128:(j + o + 1) * 128],
                                    NQT[hp:hp + 64, c4, j * 128:(j + 1) * 128], True, False))
                        mms.append((SN[sb][:, oc], identb[:], nb_[:, (h * 7 + o) * 128:(h * 7 + o + 1) * 128], False, True))
                    pe(mms, [B_NKT, B_NQT, B_identb, B_nbt[j % 2]], [B_SN[sb]])

                def nat_rest(n):
                    j, h = items[n]
                    c4 = h // 2
                    sb = n % 3
                    act(PN[sb][:], SN[sb][:, 0:896], AF.Exp, [B_SN[sb], B_negM], [B_PN[sb]], bias=negM[:, 4 + c4:5 + c4], scale=1.0)
                    nb2 = n % 2
                    mms = [(NO[nb2][:, 0:66], PN[sb][:, o * 128:(o + 1) * 128], NV[:, j + o, h * 66:h * 66 + 66], o == 0, o == 6) for o in range(7)]
                    pe(mms, [B_PN[sb], B_NV], [B_NO[nb2]])
                    dve(lambda E, nb2=nb2: E.reciprocal(out=sn[:, 0:1], in_=NO[nb2][:, 64:65]), [B_NO[nb2]], [B_sn])
                    ts(o_all[:, j, h * 64:(h + 1) * 64], NO[nb2][:, 0:64], sn[:, 0:1], None, ALU.mult, None,
                       [B_NO[nb2], B_sn], [B_oall[j]])

                nat_S(0)
                nat_S(1)
                for n in range(len(items)):
                    if n + 2 < len(items):
                        nat_S(n + 2)
                    nat_rest(n)
                d = dbgout("o_all", [128, 16 * 512], BF16)
                if d is not None:
                    dma(d, o_all[:].rearrange("p a f -> p (a f)"), B_oall, [YB], sembuf=B_oall[0])
                em.barrier()
                em.emit()

        if STOP not in ("1", "2"):
            with contextlib.ExitStack() as st:
                wr = sbt(st, "wr", [128, 8, 32])
                br = sbt(st, "br", [1, 32])
                mskb = sbt(st, "mskb", [128, 16, 32], BF16)
                gate4 = sbt(st, "gate4", [128, 16, 4])
                eidx = sbt(st, "eidx", [128, 16, 8])
                dsti = sbt(st, "dsti", [128, 64], I32)
                idxW = sbt(st, "idxW", [128, 4, NB], I32)
                ohTall = sbt(st, "ohTall", [32, NB])
                B_wr, B_br, B_mskb, B_gate4, B_eidx, B_dsti, B_idxW, B_ohTall = em.bufs(8, "p4")
                B_hrow = em.bufs(16, "hrow")
                B_x1s = em.bufs(16, "x1s")
                dma(wr[:], w_router.rearrange("(c p) f -> p c f", p=128), [], [B_wr])
                dma(br[:], b_router, [], [B_br])
                with contextlib.ExitStack() as s4:
                    Wout = sbt(s4, "Wout", [128, 8, 1024], BF16)
                    hrow = sbt(s4, "hrow", [128, 16, 1024], BF16)
                    zt = sbt(s4, "zt", [128, 2, 1024], BF16)
                    B_zt, B_Xz = em.bufs(2, "zx")
                    mset(zt[:], 0.0, [B_zt], eng="pool")
                    for cz in range(NB // 2):
                        dma(Xs[cz * 256:(cz + 1) * 256, :].rearrange("(r p) f -> p r f", p=128), zt[:], [B_zt], [B_Xz], sembuf=B_Xz)
                    B_Wout = em.buf("Wout")
                    dma(Wout[:], w_out.rearrange("(c p) f -> p c f", p=128), [], [B_Wout], q="pool")
                    ltri = sbt(s4, "ltri_s", [128, 128], BF16)
                    iota32 = sbt(s4, "iota32_s", [128, 32])
                    thr16 = sbt(s4, "thr16_s", [128, 32, 16])
                    iota96 = sbt(s4, "iota96_s", [128, NB])
                    pidx = sbt(s4, "pidx_s", [128, 1])
                    B_ltri, B_iota32, B_thr16, B_iota96, B_pidx = em.bufs(5, "cst")
                    dma(ltri[:], ltri_d, [], [B_ltri])
                    dma(iota32[:], iota32_d, [], [B_iota32])
                    dma(thr16[:].rearrange("p a b -> p (a b)"), thr16_d, [], [B_thr16])
                    dma(iota96[:], iota96_d, [], [B_iota96])
                    dma(pidx[:], pidx_d, [], [B_pidx])
                    def dbl(fn):
                        return [fn(0), fn(1)]
                    oT_ = dbl(lambda i: sbt(s4, "oT%d" % i, [128, 8, 128], BF16))
                    xt4_ = dbl(lambda i: sbt(s4, "xt4_%d" % i, [128, 1024]))
                    tmp4_ = dbl(lambda i: sbt(s4, "tmp4_%d" % i, [128, 1024]))
                    x1t_ = dbl(lambda i: sbt(s4, "x1t_%d" % i, [128, 1024]))
                    h2t_ = dbl(lambda i: sbt(s4, "h2t_%d" % i, [128, 1024]))
                    h2Tf_ = dbl(lambda i: sbt(s4, "h2Tf_%d" % i, [128, 8, 128]))
                    junk4_ = dbl(lambda i: sbt(s4, "junk4_%d" % i, [128, 1024], BF16))
                    s4s_ = dbl(lambda i: sbt(s4, "s4s_%d" % i, [128, 16]))
                    lg_ = dbl(lambda i: sbt(s4, "lg_%d" % i, [128, 32]))
                    v8_ = dbl(lambda i: sbt(s4, "v8_%d" % i, [128, 8]))
                    i8_ = dbl(lambda i: sbt(s4, "i8_%d" % i, [128, 8], U32))
                    msk_ = dbl(lambda i: sbt(s4, "msk_%d" % i, [128, 32]))
                    e4_ = dbl(lambda i: sbt(s4, "e4_%d" % i, [128, 4]))
                    pto_ = dbl(lambda i: pst(s4, "pto%d" % i, [128, 8, 128], BF16))
                    pmix = pst(s4, "pmix", [128, 1024])
                    ptf = pst(s4, "ptf", [128, 8, 128])
                    plg = pst(s4, "plg", [128, 32])
                    BB = {n: em.bufs(2, "s4" + n) for n in ["oT", "xt4", "tmp4", "x1t", "h2t", "h2Tf", "junk4", "s4s", "lg", "v8", "i8", "msk", "e4", "pto"]}
                    B_pmix, B_ptf, B_plg = em.bufs(3, "s4p")
                    for j in range(16):
                        q2 = j % 2
                        oT, xt4, tmp4, x1t, h2t, h2Tf, junk4, s4s, lg, v8, i8, msk, e4, pto = (
                            oT_[q2], xt4_[q2], tmp4_[q2], x1t_[q2], h2t_[q2], h2Tf_[q2], junk4_[q2], s4s_[q2], lg_[q2], v8_[q2], i8_[q2], msk_[q2], e4_[q2], pto_[q2])
                        (B_oT, B_xt4, B_tmp4, B_x1t, B_h2t, B_h2Tf, B_junk4, B_s4s, B_lg, B_v8, B_i8, B_msk, B_e4, B_pto) = (
                            BB[n][q2] for n in ["oT", "xt4", "tmp4", "x1t", "h2t", "h2Tf", "junk4", "s4s", "lg", "v8", "i8", "msk", "e4", "pto"])
                        pet([(pto[:, c, :], o_all[:, j, c * 128:(c + 1) * 128], identb[:]) for c in range(4)], [B_oall[j], B_identb], [B_pto])
                        cp(oT[:, 0:4, :], pto[:, 0:4, :], [B_pto], [B_oT])
                        mms = []
                        for n in range(2):
                            for c in range(4):
                                mms.append((pmix[:, n * 512:(n + 1) * 512], oTd[:, c, j * 128:(j + 1) * 128], Wout[:, c, n * 512:(n + 1) * 512], c == 0, False))
                            for c in range(4):
                                mms.append((pmix[:, n * 512:(n + 1) * 512], oT[:, c, :], Wout[:, 4 + c, n * 512:(n + 1) * 512], False, c == 3))
                        pe(mms, [B_oT, B_Wout] + [B_oTd[c][j // 4] for c in range(4)], [B_pmix])
                        dma(xt4[:], xq[j * 128:(j + 1) * 128, :], [], [B_xt4])
                        act(junk4[:], pmix[:], AF.Square, [B_pmix], [B_junk4, B_s4s], accum_out=s4s[:, 0:1])
                        ts(s4s[:, 1:2], s4s[:, 0:1], 1.0 / 1024, 1e-6, ALU.mult, ALU.add, [B_s4s], [B_s4s])
                        act(s4s[:, 2:3], s4s[:, 1:2], AF.Sqrt, [B_s4s], [B_s4s])
                        dve(lambda E, s4s=s4s, v8=v8, lg=lg, i8=i8, e4=e4: E.reciprocal(out=s4s[:, 3:4], in_=s4s[:, 2:3]), [B_s4s], [B_s4s])
                        stt(tmp4[:], pmix[:], s4s[:, 3:4], rows[:, 2, :], ALU.mult, ALU.mult, [B_pmix, B_s4s, B_rows], [B_tmp4])
                        tt(x1t[:], tmp4[:], xt4[:], ALU.add, [B_tmp4, B_xt4], [B_x1t])
                        dma(x1s[j * 128:(j + 1) * 128, :], x1t[:], [B_x1t], [B_x1s[j]], sembuf=B_x1s[j])
                        act(junk4[:], x1t[:], AF.Square, [B_x1t], [B_junk4, B_s4s], accum_out=s4s[:, 4:5])
                        ts(s4s[:, 5:6], s4s[:, 4:5], 1.0 / 1024, 1e-6, ALU.mult, ALU.add, [B_s4s], [B_s4s])
                        act(s4s[:, 6:7], s4s[:, 5:6], AF.Sqrt, [B_s4s], [B_s4s])
                        dve(lambda E, s4s=s4s, v8=v8, lg=lg, i8=i8, e4=e4: E.reciprocal(out=s4s[:, 7:8], in_=s4s[:, 6:7]), [B_s4s], [B_s4s])
                        stt(tmp4[:], x1t[:], s4s[:, 7:8], rows[:, 0, :], ALU.mult, ALU.mult, [B_x1t, B_s4s, B_rows], [B_tmp4])
                        tt(h2t[:], tmp4[:], rows[:, 1, :], ALU.add, [B_tmp4, B_rows], [B_h2t])
                        act(hrow[:, j, :], h2t[:], AF.Copy, [B_h2t], [B_hrow[j]])
                        pet([(ptf[:, c, :], h2t[:, c * 128:(c + 1) * 128], identf[:]) for c in range(8)], [B_h2t, B_identf], [B_ptf])
                        cp(h2Tf[:], ptf[:], [B_ptf], [B_h2Tf])
                        mms = [(plg[:], h2Tf[:, c, :], wr[:, c, :], c == 0, False) for c in range(8)]
                        mms.append((plg[:], onesf[0:1, 0:128], br[0:1, :], False, True))
                        pe(mms, [B_h2Tf, B_wr, B_br, B_onesf], [B_plg])
                        cp(lg[:], plg[:], [B_plg], [B_lg])
                        dve(lambda E, s4s=s4s, v8=v8, lg=lg, i8=i8, e4=e4: E.max(out=v8[:], in_=lg[:]), [B_lg], [B_v8])
                        dve(lambda E, s4s=s4s, v8=v8, lg=lg, i8=i8, e4=e4: E.max_index(out=i8[:], in_max=v8[:], in_values=lg[:]), [B_lg, B_v8], [B_i8])
                        cp(eidx[:, j, :], i8[:], [B_i8], [B_eidx])
                        ts(msk[:], lg[:], v8[:, 3:4], None, ALU.is_ge, None, [B_lg, B_v8], [B_msk])
                        cp(mskb[:, j, :], msk[:], [B_msk], [B_mskb])
                        ts(s4s[:, 8:9], v8[:, 0:1], -1.0, None, ALU.mult, None, [B_v8, B_s4s], [B_s4s])
                        act(e4[:], v8[:, 0:4], AF.Exp, [B_v8, B_s4s], [B_e4], bias=s4s[:, 8:9], scale=1.0)
                        dve(lambda E, s4s=s4s, v8=v8, lg=lg, i8=i8, e4=e4: E.reduce_sum(out=s4s[:, 9:10], in_=e4[:], axis=AX.X), [B_e4, B_s4s], [B_s4s])
                        dve(lambda E, s4s=s4s, v8=v8, lg=lg, i8=i8, e4=e4: E.reciprocal(out=s4s[:, 10:11], in_=s4s[:, 9:10]), [B_s4s], [B_s4s])
                        ts(gate4[:, j, :], e4[:], s4s[:, 10:11], None, ALU.mult, None, [B_e4, B_s4s], [B_gate4])
                    cnt_t = sbt(s4, "cnt_t", [128, 32])
                    cmp1 = sbt(s4, "cmp1", [128, 32, 16])
                    nbk = sbt(s4, "nbk", [128, 32])
                    ones32 = sbt(s4, "ones32", [128, 32])
                    cum = sbt(s4, "cum", [128, 32])
                    pstart = sbt(s4, "pstart", [128, 32])
                    cmp2 = sbt(s4, "cmp2", [128, NB, 32])
                    blk = sbt(s4, "blk", [128, NB])
                    chg = sbt(s4, "chg", [128, NB])
                    idxf = sbt(s4, "idxf", [128, 5, NB])
                    chg2 = sbt(s4, "chg2", [128, NB])
                    B_chg2 = em.buf("chg2")
                    destf = sbt(s4, "destf", [128, 16, 32])
                    ohf = sbt(s4, "ohf", [128, 32])
                    dstf = sbt(s4, "dstf", [128, 64])
                    (B_cnt, B_cmp1, B_nbk, B_ones32, B_cum, B_pstart, B_cmp2, B_blk, B_chg, B_idxf, B_destf, B_ohf, B_dstf) = em.bufs(13, "rt")
                    mms = [(plg[:], onesb[:, 0:128], mskb[:, j, :], j == 0, j == 15) for j in range(16)]
                    pe(mms, [B_onesb, B_mskb], [B_plg])
                    cp(cnt_t[:], plg[:], [B_plg], [B_cnt])
                    tt(cmp1[:], cnt_t[:].unsqueeze(2).to_broadcast([128, 32, 16]), thr16[:], ALU.is_gt, [B_cnt, B_thr16], [B_cmp1])
                    dve(lambda E: E.reduce_sum(out=nbk[:], in_=cmp1[:], axis=AX.X), [B_cmp1], [B_nbk])
                    mset(ones32[:], 1.0, [B_ones32])
                    dve(lambda E: E.tensor_tensor_scan(out=cum[:], data0=ones32[:], data1=nbk[:], initial=0.0, op0=ALU.mult, op1=ALU.add),
                        [B_ones32, B_nbk], [B_cum])
                    tt(pstart[:], cum[:], nbk[:], ALU.subtract, [B_cum, B_nbk], [B_pstart])
                    ts(pstart[:], pstart[:], 128.0, None, ALU.mult, None, [B_pstart], [B_pstart])
                    tt(cmp2[:], cum[:].unsqueeze(1).to_broadcast([128, NB, 32]), iota96[:].unsqueeze(2).to_broadcast([128, NB, 32]), ALU.is_le,
                       [B_cum, B_iota96], [B_cmp2])
                    dve(lambda E: E.reduce_sum(out=blk[:], in_=cmp2[:], axis=AX.X), [B_cmp2], [B_blk])
                    ts(blk[:], blk[:], 31.0, None, ALU.min, None, [B_blk], [B_blk])
                    ts(ohTall[:], blk[0:32, :], pidx[0:32, 0:1], None, ALU.is_equal, None, [B_blk, B_pidx], [B_ohTall])
                    mset(chg[:, 0:1], 1.0, [B_chg])
                    tt(chg[:, 1:NB], blk[:, 1:NB], blk[:, 0:NB - 1], ALU.not_equal, [B_blk, B_chg], [B_chg])
                    mset(chg2[:, 0:2], 1.0, [B_chg2])
                    tt(chg2[:, 2:NB], blk[:, 2:NB], blk[:, 0:NB - 2], ALU.not_equal, [B_blk, B_chg2], [B_chg2])
                    ts(chg[:], chg[:], -float(2 ** 27), float(2 ** 27), ALU.mult, ALU.add, [B_chg], [B_chg])
                    ts(chg2[:], chg2[:], -float(2 ** 27), float(2 ** 27), ALU.mult, ALU.add, [B_chg2], [B_chg2])
                    ts(blk[:], blk[:], 128.0, pidx[:, 0:1], ALU.mult, ALU.add, [B_blk, B_pidx], [B_blk])
                    tt(idxf[:, 4, :], blk[:], chg[:], ALU.add, [B_blk, B_chg], [B_idxf])
                    ts(idxf[:, 0, :], idxf[:, 4, :], 2.0, None, ALU.mult, None, [B_idxf], [B_idxf])
                    ts(idxf[:, 1, :], idxf[:, 4, :], 2.0, 1.0, ALU.mult, ALU.add, [B_idxf], [B_idxf])
                    tt(idxf[:, 4, :], blk[:], chg2[:], ALU.add, [B_blk, B_chg2, B_idxf], [B_idxf])
                    ts(idxf[:, 2, :], idxf[:, 4, :], 2.0, None, ALU.mult, None, [B_idxf], [B_idxf])
                    ts(idxf[:, 3, :], idxf[:, 4, :], 2.0, 1.0, ALU.mult, ALU.add, [B_idxf], [B_idxf])
                    cp(idxW[:], idxf[:, 0:4, :], [B_idxf], [B_idxW])
                    for j in range(16):
                        mms = [(plg[:], onesb[:, 0:128], mskb[:, jj, :], jj == 0, False) for jj in range(j)]
                        mms.append((plg[:], ltri[:], mskb[:, j, :], j == 0, True))
                        pe(mms, [B_onesb, B_ltri, B_mskb], [B_plg])
                        tt(destf[:, j, :], plg[:], pstart[:], ALU.add, [B_plg, B_pstart], [B_destf])
                    B_XO = em.buf("XO")
                    B_dsti = em.bufs(16, "dsti")
                    for j in range(16):
                        for k in range(4):
                            ts(ohf[:], iota32[:], eidx[:, j, k:k + 1], None, ALU.is_equal, None, [B_iota32, B_eidx], [B_ohf])
                            tt(ohf[:], ohf[:], destf[:, j, :], ALU.mult, [B_ohf, B_destf], [B_ohf])
                            dve(lambda E, j=j, k=k: E.reduce_sum(out=dstf[:, 4 * j + k:4 * j + k + 1], in_=ohf[:], axis=AX.X), [B_ohf], [B_dstf])
                        cp(dsti[:, 4 * j:4 * j + 4], dstf[:, 4 * j:4 * j + 4], [B_dstf], [B_dsti[j]])
                        for k in range(4):
                            col = dsti[:, 4 * j + k:4 * j + k + 1]
                            bsc = em.buf("sc")
                            em.dma("pool", lambda E, j=j, col=col: [E.indirect_dma_start(
                                out=Xs, out_offset=bass.IndirectOffsetOnAxis(ap=col, axis=0), in_=hrow[:, j, :], in_offset=None)],
                                reads=[B_hrow[j], B_dsti[j], B_Xz], writes=[bsc], sembuf=B_XO)
                    for nm, srct, bb in (("dsti", dsti, B_dsti[15]), ("idxW", idxW, B_idxW)):
                        d = dbgout(nm, [128, srct.shape[1] * (srct.shape[2] if len(srct.shape) > 2 else 1)], I32)
                        if d is not None:
                            dma(d, srct[:] if len(srct.shape) == 2 else srct[:].rearrange("p a b -> p (a b)"), [bb], [YB], sembuf=bb)
                    d = dbgout("gate4", [128, 64])
                    if d is not None:
                        dma(d, gate4[:].rearrange("p a b -> p (a b)"), [B_gate4], [YB], sembuf=B_gate4)
                    em.barrier()
                    em.emit()
                with contextlib.ExitStack() as s5:
                    wb1s = [sbt(s5, "wb1_%d" % i, [128, 8, 2048], BF16) for i in range(2)]
                    B_wb1 = [em.bufs(2, "wb1p%d" % i) for i in range(2)]
                    wb2 = sbt(s5, "wb2", [128, 9, 1024], BF16)
                    b1all = sbt(s5, "b1all", [32, 2048], BF16)
                    b2all = sbt(s5, "b2all", [32, 1024], BF16)
                    xbk = [sbt(s5, "xbk%d" % i, [128, 1024], BF16) for i in range(2)]
                    xT = [sbt(s5, "xT%d" % i, [128, 8, 128], BF16) for i in range(2)]
                    ohT = [sbt(s5, "ohT%d" % i, [32, 128], BF16) for i in range(2)]
                    glu = sbt(s5, "glu", [128, 1024])
                    lin = sbt(s5, "lin", [128, 1024])
                    sig = sbt(s5, "sig", [128, 1024], BF16)
                    ab = sbt(s5, "ab", [128, 1024], BF16)
                    aT = sbt(s5, "aT", [128, 8, 128], BF16)
                    yb = [sbt(s5, "yb%d" % i, [128, 1024]) for i in range(2)]
                    TX = pst(s5, "TX", [128, 8, 128], BF16)
                    TA = pst(s5, "TA", [128, 8, 128], BF16)
                    H = pst(s5, "H", [128, 2048])
                    Y = pst(s5, "Y", [128, 1024])
                    Ybf = Y.bitcast(BF16)
                    B_wb1a, B_wb1b, B_wb2, B_b1all, B_b2all, B_glu, B_lin, B_sig, B_ab, B_aT, B_TX, B_TA, B_H, B_Y, B_Ys = em.bufs(15, "s5")
                    B_xbk = em.bufs(2, "xbk"); B_obk = em.bufs(2, "obk"); B_xT = em.bufs(2, "xT"); B_ohT = em.bufs(2, "ohT"); B_yb = em.bufs(2, "yb")
                    bcreg = s5.enter_context(nc.gpsimd.register("bcreg"))
                    em.streams["pool"].append(lambda E: E.reg_mov(bcreg, 8191))
                    dma(b1all[:], b1, [], [B_b1all], q="pool")
                    dma(b2all[:], b2, [], [B_b2all], q="pool")
                    ab2 = [ab, sbt(s5, "ab_b", [128, 1024], BF16)]
                    B_ab2 = [B_ab, em.buf("ab_b")]

                    def loads(i):
                        p = i % 2
                        dma(xbk[p][:], Xs[i * 128:(i + 1) * 128, :], [], [B_xbk[p]])

                    def gathers(i):
                        for hh in range(2):
                            em.dma("pool", lambda E, i=i, hh=hh: [E.indirect_dma_start(
                                out=wb1s[i % 2][:, 4 * hh:4 * hh + 4, :].rearrange("p c f -> p (c f)"), out_offset=None, in_=W1p,
                                in_offset=bass.IndirectOffsetOnAxis(ap=idxW[:, 2 + hh, i:i + 1], axis=0), bounds_check=bcreg, oob_is_err=False)],
                                reads=[B_idxW], writes=[B_wb1[i % 2][hh]])

                    def gathers2(i):
                        for hh in range(2):
                            em.dma("pool", lambda E, i=i, hh=hh: [E.indirect_dma_start(
                                out=wb2[:, 4 * hh:4 * hh + 4, :].rearrange("p c f -> p (c f)"), out_offset=None, in_=W2p,
                                in_offset=bass.IndirectOffsetOnAxis(ap=idxW[:, hh, i:i + 1], axis=0), bounds_check=bcreg, oob_is_err=False)],
                                reads=[B_idxW], writes=[B_wb2])

                    def stageA(i):
                        p = i % 2
                        if i + 1 < NB:
                            loads(i + 1)
                        pet([(TX[:, c, :], xbk[p][:, c * 128:(c + 1) * 128], identb[:]) for c in range(8)], [B_xbk[p], B_identb], [B_TX])
                        cp(xT[p][:], TX[:], [B_TX], [B_xT[p]])
                        cp(ohT[p][:], ohTall[:, i:i + 1].to_broadcast([32, 128]), [B_ohTall], [B_ohT[p]])
                        mms = []
                        for n in range(4):
                            for c in range(8):
                                mms.append((H[:, n * 512:(n + 1) * 512], xT[p][:, c, :], wb1s[p][:, c, n * 512:(n + 1) * 512], c == 0, False))
                            mms.append((H[:, n * 512:(n + 1) * 512], ohT[p][:], b1all[:, n * 512:(n + 1) * 512], False, True))
                        pe(mms, [B_xT[p], B_ohT[p], B_wb1[p][0], B_wb1[p][1], B_b1all], [B_H])
                        if i + 2 < NB:
                            gathers(i + 2)
                        ts(glu[:], H[:, 0:2048:2], 7.0, None, ALU.min, None, [B_H], [B_glu])
                        ts(lin[:], H[:, 1:2048:2], -7.0, 7.0, ALU.max, ALU.min, [B_H], [B_lin])
                        act(sig[:], glu[:], AF.Sigmoid, [B_glu], [B_sig], scale=1.702)
                        stt(lin[:], lin[:], 1.0, glu[:], ALU.add, ALU.mult, [B_lin, B_glu], [B_lin])
                        tt(ab2[p][:], lin[:], sig[:], ALU.mult, [B_lin, B_sig], [B_ab2[p]])

                    def stageB(i):
                        p = i % 2
                        pet([(TA[:, c, :], ab2[p][:, c * 128:(c + 1) * 128], identb[:]) for c in range(8)], [B_ab2[p], B_identb], [B_TA])
                        act(aT[:], TA[:], AF.Copy, [B_TA], [B_aT])
                        mms = []
                        for n in range(2):
                            for c in range(8):
                                mms.append((Y[:, n * 512:(n + 1) * 512], aT[:, c, :], wb2[:, c, n * 512:(n + 1) * 512], c == 0, False))
                            mms.append((Y[:, n * 512:(n + 1) * 512], ohT[p][:], b2all[:, n * 512:(n + 1) * 512], False, True))
                        pe(mms, [B_aT, B_ohT[p], B_wb2, B_b2all], [B_Y])
                        if i + 1 < NB:
                            gathers2(i + 1)
                        act(yb[p][:], Y[:], AF.Copy, [B_Y], [B_yb[p]])
                        dma(Ys[i * 128:(i + 1) * 128, :], yb[p][:], [B_yb[p]], [B_Ys], sembuf=B_yb[p])

                    loads(0)
                    gathers(0)
                    gathers(1)
                    gathers2(0)
                    stageA(0)
                    for i in range(NB):
                        if i + 1 < NB:
                            stageA(i + 1)
                        stageB(i)
                    em.barrier()
                    em.emit()
                with contextlib.ExitStack() as s6:
                    x1r = [sbt(s6, "x1r%d" % i, [128, 1024]) for i in range(2)]
                    ot = [sbt(s6, "ot%d" % i, [128, 1024]) for i in range(2)]
                    yk = [sbt(s6, "yk%d" % i, [128, 4, 1024]) for i in range(2)]
                    ft = sbt(s6, "ft", [128, 1024])
                    junk6 = sbt(s6, "junk6", [128, 1024], BF16)
                    s6s = sbt(s6, "s6s", [128, 4])
                    B_x1r = em.bufs(2, "x1r"); B_ot = em.bufs(2, "ot"); B_yk = [em.bufs(4, "yk%d" % i) for i in range(2)]
                    B_ft, B_junk6, B_s6s = em.bufs(3, "s6")
                    for j in range(16):
                        i = j % 2
                        dma(x1r[i][:], x1s[j * 128:(j + 1) * 128, :], [B_x1s[j]], [B_x1r[i]])
                        for k in range(4):
                            em.dma("pool", lambda E, i=i, j=j, k=k: [E.indirect_dma_start(
                                out=yk[i][:, k, :], out_offset=None, in_=Ys,
                                in_offset=bass.IndirectOffsetOnAxis(ap=dsti[:, 4 * j + k:4 * j + k + 1], axis=0))],
                                reads=[B_dsti[j]], writes=[B_yk[i][k]])
                        ts(ft[:], yk[i][:, 0, :], gate4[:, j, 0:1], None, ALU.mult, None, [B_yk[i][0], B_gate4], [B_ft])
                        for k in range(1, 4):
                            stt(ft[:], yk[i][:, k, :], gate4[:, j, k:k + 1], ft[:], ALU.mult, ALU.add, [B_yk[i][k], B_gate4, B_ft], [B_ft])
                        act(junk6[:], ft[:], AF.Square, [B_ft], [B_junk6, B_s6s], accum_out=s6s[:, 0:1])
                        ts(s6s[:, 1:2], s6s[:, 0:1], 1.0 / 1024, 1e-6, ALU.mult, ALU.add, [B_s6s], [B_s6s])
                        act(s6s[:, 2:3], s6s[:, 1:2], AF.Sqrt, [B_s6s], [B_s6s])
                        dve(lambda E: E.reciprocal(out=s6s[:, 3:4], in_=s6s[:, 2:3]), [B_s6s], [B_s6s])
                        stt(ot[i][:], ft[:], s6s[:, 3:4], rows[:, 3, :], ALU.mult, ALU.mult, [B_ft, B_s6s, B_rows], [B_ot[i]])
                        tt(ot[i][:], ot[i][:], x1r[i][:], ALU.add, [B_ot[i], B_x1r[i]], [B_ot[i]])
                        dma(out[j * 128:(j + 1) * 128, :], ot[i][:], [B_ot[i]], [YB], sembuf=B_ot[i])
                    em.barrier()
                    em.emit()
        else:
            mz = sbt(top, "mz", [128, 1024])
            B_mz = em.buf("mz")
            mset(mz[:], 0.0, [B_mz])
            for j in range(16):
                dma(out[j * 128:(j + 1) * 128, :], mz[:], [B_mz], [YB], sembuf=B_mz)
            for nm, src in (("Qs", Qs), ("Ks", Ks), ("NQs", NQs), ("NKs", NKs), ("Vs", Vs), ("NVs", NVs)):
                d = dbgout(nm, src.shape, BF16)
                if d is not None:
                    dma(d, src, [], [YB], sembuf=YB)
            em.barrier()
            em.emit()
    return nc, dbg


SLOPES = [2.0 ** (-2 * (h + 1)) for h in range(4)]
_CACHE = {}


def _bf(a):
    return np.ascontiguousarray(a.astype(ml_dtypes.bfloat16))


def _nat_tables(rpb, qr):
    R0 = 32 * qr
    def table(r0):
        t = np.full((128, 8, 7, 128), -30000.0, np.float32)
        qrow = np.repeat(np.array([r0, r0 + 1]), 64)
        qcol = np.tile(np.arange(64), 2)
        qstart = np.clip(qrow - 4, 0, 120)
        qcs = np.clip(qcol - 8, 0, 48)
        for o in range(7):
            krow = np.repeat(np.array([r0 - 6 + 2 * o, r0 - 5 + 2 * o]), 64)
            kcol = np.tile(np.arange(64), 2)
            valid = ((krow[:, None] >= qstart[None, :]) & (krow[:, None] < qstart[None, :] + 8)
                     & (krow[:, None] >= 0) & (krow[:, None] < 128)
                     & (kcol[:, None] >= qcs[None, :]) & (kcol[:, None] < qcs[None, :] + 16))
            dr = np.clip(krow[:, None] - qrow[None, :] + 7, 0, 14)
            dc = np.clip(kcol[:, None] - qcol[None, :], -15, 15) + 15
            for h in range(8):
                vals = rpb[h][dr, dc]
                t[:, h, o, :] = np.where(valid, vals, -30000.0)
        return t
    slots = [table(R0 + 2 * 6)] + [table(R0 + 2 * j) for j in (0, 1, 14, 15)]
    return _bf(np.stack(slots).reshape(5, 128, 8 * 7 * 128))


def _prep(inputs):
    f32 = np.float32
    x = np.asarray(inputs["x"], f32)
    c = np.asarray(inputs["c"], f32)
    shared = dict(
        w_ada=np.ascontiguousarray(inputs["w_ada"][0], f32), b_ada=np.ascontiguousarray(inputs["b_ada"], f32).reshape(1, 6144),
        g_pre_mix=np.asarray(inputs["g_pre_mix"], f32).reshape(1, 1024), g_post_mix=np.asarray(inputs["g_post_mix"], f32).reshape(1, 1024),
        g_pre_ffn=np.asarray(inputs["g_pre_ffn"], f32).reshape(1, 1024), g_post_ffn=np.asarray(inputs["g_post_ffn"], f32).reshape(1, 1024),
        w_in=np.ascontiguousarray(inputs["w_in"][0], f32), w_out=np.ascontiguousarray(inputs["w_out"][0], f32),
        lamv=np.concatenate([np.asarray(inputs[k], f32).reshape(-1) for k in ("lam_q1", "lam_k1", "lam_q2", "lam_k2")]).reshape(1, 256),
        g_subln=np.asarray(inputs["g_subln"], f32).reshape(1, 128),
        w_router=np.ascontiguousarray(inputs["w_router"][0], f32), b_router=np.asarray(inputs["b_router"], f32).reshape(1, 32),
        identb=_bf(np.eye(128, dtype=f32)), identf=np.eye(128, dtype=f32),
        ltri=_bf(np.triu(np.ones((128, 128), f32), 1)),
        iota32=np.tile(np.arange(32, dtype=f32), (128, 1)),
        thr16=np.tile((128.0 * np.arange(16, dtype=f32))[None, None, :], (128, 32, 1)).reshape(128, 512),
        iota96=np.tile(np.arange(NB, dtype=f32), (128, 1)),
        pidx=np.arange(128, dtype=f32).reshape(128, 1),
    )
    if os.environ.get("KSTOP", "") not in ("1", "2"):
        shared.update(
            W1p=np.ascontiguousarray(np.asarray(inputs["w1"][0], f32).reshape(32, 8, 128, 2048).transpose(0, 2, 1, 3)).reshape(8192, 8192),
            W2p=np.ascontiguousarray(np.asarray(inputs["w2"][0], f32).reshape(32, 8, 128, 1024).transpose(0, 2, 1, 3)).reshape(8192, 4096),
            b1=np.ascontiguousarray(inputs["b1"][0], f32), b2=np.ascontiguousarray(inputs["b2"][0], f32))
    kl = np.arange(128)
    bd = np.stack([-SLOPES[h] * np.abs(kl[:, None] - kl[None, :]) for h in range(4)], axis=1)
    shared["bdiag"] = _bf(bd.reshape(128, 512).astype(f32))
    rpb = np.asarray(inputs["nat_rpb"], f32)[0]
    in_maps = []
    for core in range(8):
        b, qr = core // 4, core % 4
        own = np.arange(qr * 2048, (qr + 1) * 2048)
        rest = np.concatenate([np.arange(0, qr * 2048), np.arange((qr + 1) * 2048, 8192)])
        perm = np.concatenate([own, rest])
        R0 = 32 * qr
        tok0 = (R0 - 6) * 64
        xw = np.zeros((2816, 1024), f32)
        lo, hi = max(tok0, 0), min(tok0 + 2816, 8192)
        xw[lo - tok0:hi - tok0] = x[b, lo:hi]
        ql = np.arange(2048)
        q_lo = (ql % 128).astype(f32)
        qt_abs = (qr * 16 + ql // 128).astype(f32)
        qa = np.zeros((4, 3, 2, 2048), f32)
        for h in range(4):
            qa[h, 0, 0] = -SLOPES[h] * q_lo
            qa[h, 0, 1] = -SLOPES[h] * 128.0 * qt_abs
            qa[h, 1] = -qa[h, 0]
        kabs = perm.astype(f32)
        ktile_abs = perm[::128] // 128
        sig = np.ones(64, f32)
        sig[16:] = np.where(ktile_abs[16:] < qr * 16, 1.0, -1.0)
        ka = np.ones((2, 8192), f32) * np.repeat(sig, 128)[None, :]
        kt_tab = np.zeros((128, 4, 2, 64), f32)
        kpos = kabs.reshape(64, 128).T
        for h in range(4):
            kt_tab[:, h, 0, :] = SLOPES[h] * kpos * sig[None, :]
            kt_tab[:, h, 1, :] = -SLOPES[h] * kpos
        m = dict(shared)
        m.update(
            xb=np.ascontiguousarray(x[b][perm]), xq=np.ascontiguousarray(x[b, own]), xw=xw,
            cT=np.ascontiguousarray(c[b].reshape(8, 128).T),
            natb=_nat_tables(rpb, qr), qaug=_bf(qa), kaug=_bf(ka), ktab=kt_tab.reshape(128, 512),
        )
        in_maps.append(m)
    return in_maps


def kernel(**inputs):
    debug = tuple(os.environ.get("KDEBUG", "").split(",")) if os.environ.get("KDEBUG") else ()
    key = (debug, os.environ.get("KSTOP", ""))
    if key not in _CACHE:
        _CACHE[key] = build_program(debug)
    nc, dbg = _CACHE[key]
    in_maps = _prep(inputs)
    res = run_bass_kernel_spmd(nc, in_maps, core_ids=list(range(8)))
    outs = [np.asarray(r["out"], np.float32) for r in res.results]
    full = np.stack([np.concatenate(outs[0:4], axis=0), np.concatenate(outs[4:8], axis=0)], axis=0)
    if debug:
        kernel.last_debug = [{k: np.asarray(r["dbg_" + k]) for k in dbg} for r in res.results]
    return full
```

```python
import contextlib
import os
import numpy as np
import ml_dtypes
import concourse.bass as bass
import concourse.mybir as mybir
from concourse.bass_utils import run_bass_kernel_spmd

F32 = mybir.dt.float32
BF16 = mybir.dt.bfloat16
I32 = mybir.dt.int32
U32 = mybir.dt.uint32
AF = mybir.ActivationFunctionType
ALU = mybir.AluOpType
AX = mybir.AxisListType

NB = 96
BIG = float(2 ** 30)


class Buf:
    __slots__ = ("name", "w", "rs", "dsem", "dcnt")

    def __init__(self, name):
        self.name = name
        self.w = []
        self.rs = []
        self.dsem = None
        self.dcnt = 0


class Em:
    ENG = ("pe", "act", "dve", "pool", "sp")

    def __init__(self, nc, stack):
        self.nc = nc
        self.stack = stack
        self.streams = {e: [] for e in self.ENG}
        self.cnt = {e: 0 for e in self.ENG}
        self.esem = {e: stack.enter_context(nc.semaphore("sem_" + e)) for e in self.ENG}
        self.waited = {e: {} for e in self.ENG}
        self.nbuf = 0
        self.dbufs = []

    def buf(self, name=None):
        self.nbuf += 1
        return Buf("%s_%d" % (name or "b", self.nbuf))

    def bufs(self, n, name=None):
        return [self.buf(name) for _ in range(n)]

    def _dsem(self, b):
        if b.dsem is None:
            b.dsem = self.stack.enter_context(self.nc.semaphore("d_" + b.name))
            self.dbufs.append(b)
        return b.dsem

    def _deps(self, eng, reads, writes):
        toks = {}

        def add(t):
            key, val, h = t
            if eng == "pe" and key == "pe":
                return
            if key not in toks or toks[key][1] < val:
                toks[key] = t
        for b in reads:
            for t in b.w:
                add(t)
        for b in writes:
            for t in b.w:
                add(t)
            for t in b.rs:
                add(t)
        return self._filter(eng, toks.values())

    def _filter(self, eng, toks):
        out = []
        wd = self.waited[eng]
        for key, val, h in toks:
            if wd.get(key, 0) >= val:
                continue
            wd[key] = val
            out.append((h, val))
        return out

    def _update(self, tok, reads, writes):
        for b in reads:
            b.rs.append(tok)
        for b in writes:
            b.w = [tok]
            b.rs = []

    def op(self, eng, fn, reads=(), writes=()):
        waits = self._deps(eng, reads, writes)
        self.cnt[eng] += 1
        sem = self.esem[eng]
        tok = (eng, self.cnt[eng], sem)

        def run(E, fn=fn, waits=waits, sem=sem):
            for h, v in waits:
                E.wait_ge(h, v)
            fn(E).then_inc(sem, 1)
        self.streams[eng].append(run)
        self._update(tok, reads, writes)

    def dma(self, q, fn, reads=(), writes=(), n=1, sembuf=None):
        waits = self._deps(q, reads, writes)
        sb = sembuf if sembuf is not None else (writes[0] if writes else reads[0])
        sem = self._dsem(sb)
        sb.dcnt += 16 * n
        tok = ("d_" + sb.name, sb.dcnt, sem)

        def run(E, fn=fn, waits=waits, sem=sem, n=n):
            for h, v in waits:
                E.wait_ge(h, v)
            lst = fn(E)
            assert len(lst) == n
            for ins in lst:
                ins.then_inc(sem, 16)
        self.streams[q].append(run)
        self._update(tok, reads, writes)

    def barrier(self):
        toks = [(e, self.cnt[e], self.esem[e]) for e in self.ENG if self.cnt[e] > 0]
        toks += [("d_" + b.name, b.dcnt, b.dsem) for b in self.dbufs]
        for eng in self.ENG:
            waits = self._filter(eng, [t for t in toks if t[0] != eng])

            def run(E, waits=waits):
                for h, v in waits:
                    E.wait_ge(h, v)
            self.streams[eng].append(run)

    def emit(self):
        nc = self.nc
        st = self.streams
        with nc.Block() as block:
            @block.sync
            def _(E):
                for f in st["sp"]:
                    f(E)

            @block.tensor
            def _(E):
                for f in st["pe"]:
                    f(E)

            @block.scalar
            def _(E):
                for f in st["act"]:
                    f(E)

            @block.vector
            def _(E):
                for f in st["dve"]:
                    f(E)

            @block.gpsimd
            def _(E):
                for f in st["pool"]:
                    f(E)
        self.streams = {e: [] for e in self.ENG}


def build_program(debug=()):
    nc = bass.Bass("TRN2", target_bir_lowering=False)

    def din(name, shape, dt=F32):
        return nc.dram_tensor(name, list(shape), dt, kind="ExternalInput").ap()

    def dscr(name, shape, dt):
        return nc.dram_tensor(name, list(shape), dt, kind="Internal").ap()

    xb = din("xb", [8192, 1024])
    xq = din("xq", [2048, 1024])
    xw = din("xw", [2816, 1024])
    cT = din("cT", [128, 8])
    w_ada = din("w_ada", [1024, 6144])
    b_ada = din("b_ada", [1, 6144])
    g_pre_mix = din("g_pre_mix", [1, 1024])
    g_post_mix = din("g_post_mix", [1, 1024])
    g_pre_ffn = din("g_pre_ffn", [1, 1024])
    g_post_ffn = din("g_post_ffn", [1, 1024])
    w_in = din("w_in", [1024, 3072])
    w_out = din("w_out", [1024, 1024])
    lamv = din("lamv", [1, 256])
    g_subln = din("g_subln", [1, 128])
    natb = din("natb", [5, 128, 8 * 7 * 128], BF16)
    w_router = din("w_router", [1024, 32])
    b_router = din("b_router", [1, 32])
    if os.environ.get("KSTOP", "") not in ("1", "2"):
        W1p = din("W1p", [8192, 8192])
        W2p = din("W2p", [8192, 4096])
        b1 = din("b1", [32, 2048])
        b2 = din("b2", [32, 1024])
    qaug = din("qaug", [4, 3, 2, 2048], BF16)
    kaug = din("kaug", [2, 8192], BF16)
    ktab = din("ktab", [128, 4 * 2 * 64])
    bdiag = din("bdiag", [128, 4 * 128], BF16)
    identb_d = din("identb", [128, 128], BF16)
    identf_d = din("identf", [128, 128])
    ltri_d = din("ltri", [128, 128], BF16)
    iota32_d = din("iota32", [128, 32])
    thr16_d = din("thr16", [128, 512])
    iota96_d = din("iota96", [128, NB])
    pidx_d = din("pidx", [128, 1])
    out = nc.dram_tensor("out", [2048, 1024], F32, kind="ExternalOutput").ap()

    Qs = dscr("Qs", [4, 128, 2048], BF16)
    Ks = dscr("Ks", [4, 128, 8192], BF16)
    Vs = dscr("Vs", [4, 128, 64, 130], BF16)
    NQs = dscr("NQs", [4, 128, 2048], BF16)
    NKs = dscr("NKs", [4, 128, 2816], BF16)
    NVs = dscr("NVs", [128, 22, 8 * 66], BF16)
    x1s = dscr("x1s", [2048, 1024], F32)
    Xs = dscr("Xs", [NB * 128, 1024], BF16)
    Os = dscr("Os", [NB * 128, 32], BF16)
    Ys = dscr("Ys", [NB * 128, 1024], F32)

    dbg = {}

    def dbgout(name, shape, dt=F32):
        if name in debug:
            dbg[name] = nc.dram_tensor("dbg_" + name, list(shape), dt, kind="ExternalOutput").ap()
            return dbg[name]
        return None

    with contextlib.ExitStack() as top:
        em = Em(nc, top)
        YB = em.buf("yout")

        def sbt(st, name, shape, dt=F32):
            return st.enter_context(nc.sbuf_tensor(name, list(shape), dt))

        def pst(st, name, shape, dt=F32):
            return st.enter_context(nc.psum_tensor(name, list(shape), dt))

        def dma(out_, in_, reads, writes, q="sp", sembuf=None):
            em.dma(q, lambda E: [E.dma_start(out=out_, in_=in_)], reads=reads, writes=writes, sembuf=sembuf)

        def act(out_, in_, func, reads, writes, **kw):
            em.op("act", lambda E: E.activation(out=out_, in_=in_, func=func, **kw), reads, writes)

        def pe(mms, reads, writes):
            def fn(E):
                ins = None
                for (o, l, r, s0, s1) in mms:
                    ins = E.matmul(o, lhsT=l, rhs=r, start=s0, stop=s1)
                return ins
            em.op("pe", fn, reads, writes)

        def pet(trs, reads, writes):
            def fn(E):
                ins = None
                for (o, i, idn) in trs:
                    ins = E.transpose(o, i, idn)
                return ins
            em.op("pe", fn, reads, writes)

        def dve(f, reads, writes, eng="dve"):
            em.op(eng, f, reads, writes)

        def ts(out_, in0, s1, s2, op0, op1=None, reads=(), writes=(), eng="dve"):
            if op1 is None:
                dve(lambda E: E.tensor_scalar(out=out_, in0=in0, scalar1=s1, scalar2=None, op0=op0), reads, writes, eng)
            else:
                dve(lambda E: E.tensor_scalar(out=out_, in0=in0, scalar1=s1, scalar2=s2, op0=op0, op1=op1), reads, writes, eng)

        def tt(out_, in0, in1, op, reads, writes, eng="dve"):
            dve(lambda E: E.tensor_tensor(out=out_, in0=in0, in1=in1, op=op), reads, writes, eng)

        def stt(out_, in0, scalar, in1, op0, op1, reads, writes):
            dve(lambda E: E.scalar_tensor_tensor(out=out_, in0=in0, scalar=scalar, in1=in1, op0=op0, op1=op1), reads, writes)

        def cp(out_, in_, reads, writes, eng="dve"):
            dve(lambda E: E.tensor_copy(out=out_, in_=in_), reads, writes, eng)

        def mset(ap, v, writes, eng="dve"):
            dve(lambda E: E.memset(ap, v), (), writes, eng)

        def dump(name, dst_shape_src):
            pass

        identb = sbt(top, "identb_s", [128, 128], BF16)
        identf = sbt(top, "identf_s", [128, 128])
        onesb = sbt(top, "onesb", [128, 512], BF16)
        onesf = sbt(top, "onesf", [128, 128])
        rows = sbt(top, "rows", [128, 4, 1024])
        o_all = sbt(top, "o_all", [128, 16, 512], BF16)
        stat = sbt(top, "stat", [1, 16])
        negM = sbt(top, "negM", [128, 8])
        neglam = sbt(top, "neglam", [128, 1])
        gsub = sbt(top, "gsub", [128, 128])
        B_identb, B_identf, B_onesb, B_onesf, B_rows, B_stat, B_negM, B_neglam, B_gsub = em.bufs(9, "const")
        B_oall = em.bufs(16, "oall")
        oTd = sbt(top, "oTd", [128, 4, 2048], BF16)
        B_oTd = [em.bufs(4, "oTd%d" % h) for h in range(4)]
        g8col = sbt(top, "g8col", [128, 1])
        B_g8col = em.buf("g8col")
        dma(g8col[:], g_subln.rearrange("o e -> e o"), [], [B_g8col])
        ts(g8col[:], g8col[:], 0.8, None, ALU.mult, None, [B_g8col], [B_g8col])
        dma(identb[:], identb_d, [], [B_identb])
        dma(identf[:], identf_d, [], [B_identf])
        mset(onesb[:], 1.0, [B_onesb])
        mset(onesf[:], 1.0, [B_onesf])
        mset(stat[:], 0.0, [B_stat])
        dma(gsub[:], g_subln.to_broadcast([128, 128]), [], [B_gsub])

        with contextlib.ExitStack() as st01:
            Wall = sbt(st01, "Wall", [128, 8, 3072], BF16)
            biasrow = sbt(st01, "biasrow", [1, 3072], BF16)
            B_Wall = em.bufs(8, "Wall")
            B_biasrow = em.buf("biasrow")
            with contextlib.ExitStack() as st:
                sil = sbt(st, "sil", [128, 8])
                silrep = sbt(st, "silrep", [128, 8, 128], BF16)
                wada = [sbt(st, "wada%d" % i, [128, 8, 512], BF16) for i in range(2)]
                bada = sbt(st, "bada", [1, 6144], BF16)
                modrow = sbt(st, "modrow", [128, 6144])
                grow = sbt(st, "grow", [128, 4, 1024])
                s1row = sbt(st, "s1row", [128, 1024])
                tmpd = sbt(st, "tmpd", [128, 128])
                s1T = sbt(st, "s1T", [128, 8])
                sh1T = sbt(st, "sh1T", [128, 8])
                wst = [sbt(st, "wst%d" % i, [128, 3072]) for i in range(2)]
                lam_t = sbt(st, "lam_t", [1, 256])
                lam_s = sbt(st, "lam_s", [1, 8])
                pmod = [pst(st, "pmod%d" % i, [128, 512]) for i in range(2)]
                pbias = pst(st, "pbias", [1, 3072])
                B_sil, B_silrep, B_bada, B_grow, B_s1row, B_tmpd, B_s1T, B_sh1T, B_lamt, B_lams, B_pbias, B_plam = em.bufs(12, "p0")
                B_wada = em.bufs(2, "wada")
                B_modrow = em.bufs(12, "modrow")
                B_wst = em.bufs(2, "wst")
                B_pmod = em.bufs(2, "pmod")

                dma(sil[:], cT, [], [B_sil])
                act(sil[:], sil[:], AF.Silu, [B_sil], [B_sil])
                for c in range(8):
                    cp(silrep[:, c, :], sil[:, c:c + 1].to_broadcast([128, 128]), [B_sil], [B_silrep])
                dma(bada[:], b_ada, [], [B_bada], q="pool")
                for i, g in enumerate([g_pre_mix, g_post_mix, g_pre_ffn, g_post_ffn]):
                    dma(grow[:, i, :], g.to_broadcast([128, 1024]), [], [B_grow])
                for j in range(12):
                    wb = wada[j % 2]
                    dma(wb[:], w_ada[:, j * 512:(j + 1) * 512].rearrange("(c p) f -> p c f", p=128), [], [B_wada[j % 2]], q="pool")
                    mms = [(pmod[j % 2][:], silrep[:, c, :], wb[:, c, :], c == 0, False) for c in range(8)]
                    mms.append((pmod[j % 2][:], onesb[0:1, 0:128], bada[0:1, j * 512:(j + 1) * 512], False, True))
                    pe(mms, [B_silrep, B_wada[j % 2], B_bada, B_onesb], [B_pmod[j % 2]])
                    cp(modrow[:, j * 512:(j + 1) * 512], pmod[j % 2][:], [B_pmod[j % 2]], [B_modrow[j]])
                MR = lambda i: [B_modrow[2 * i], B_modrow[2 * i + 1]]
                m = lambda i: modrow[:, i * 1024:(i + 1) * 1024]
                stt(s1row[:], m(1), 1.0, grow[:, 0, :], ALU.add, ALU.mult, MR(1) + [B_grow], [B_s1row])
                stt(rows[:, 0, :], m(4), 1.0, grow[:, 2, :], ALU.add, ALU.mult, MR(4) + [B_grow], [B_rows])
                cp(rows[:, 1, :], m(3), MR(3) + [B_rows], [B_rows])
                tt(rows[:, 2, :], m(2), grow[:, 1, :], ALU.mult, MR(2) + [B_grow, B_rows], [B_rows])
                tt(rows[:, 3, :], m(5), grow[:, 3, :], ALU.mult, MR(5) + [B_grow, B_rows], [B_rows])
                for c in range(8):
                    tt(tmpd[:], s1row[:, c * 128:(c + 1) * 128], identf[:], ALU.mult, [B_s1row, B_identf], [B_tmpd])
                    dve(lambda E, c=c: E.reduce_sum(out=s1T[:, c:c + 1], in_=tmpd[:], axis=AX.X), [B_tmpd], [B_s1T])
                    tt(tmpd[:], modrow[:, c * 128:(c + 1) * 128], identf[:], ALU.mult, MR(0) + [B_identf], [B_tmpd])
                    dve(lambda E, c=c: E.reduce_sum(out=sh1T[:, c:c + 1], in_=tmpd[:], axis=AX.X), [B_tmpd], [B_sh1T])
                for c in range(8):
                    wsb = wst[c % 2]
                    dma(wsb[:], w_in[c * 128:(c + 1) * 128, :], [], [B_wst[c % 2]])
                    mms = [(pbias[0:1, n * 512:(n + 1) * 512], sh1T[:, c:c + 1], wsb[:, n * 512:(n + 1) * 512], c == 0, c == 7) for n in range(6)]
                    pe(mms, [B_sh1T, B_wst[c % 2]], [B_pbias])
                    act(Wall[:, c, :], wsb[:], AF.Copy, [B_wst[c % 2], B_s1T], [B_Wall[c]], scale=s1T[:, c:c + 1])
                cp(biasrow[:], pbias[:], [B_pbias], [B_biasrow])
                dma(lam_t[:], lamv, [], [B_lamt])
                tt(lam_t[0:1, 0:64], lam_t[0:1, 0:64], lam_t[0:1, 64:128], ALU.mult, [B_lamt], [B_lamt])
                tt(lam_t[0:1, 128:192], lam_t[0:1, 128:192], lam_t[0:1, 192:256], ALU.mult, [B_lamt], [B_lamt])
                dve(lambda E: E.reduce_sum(out=lam_s[0:1, 0:1], in_=lam_t[0:1, 0:64], axis=AX.X), [B_lamt], [B_lams])
                dve(lambda E: E.reduce_sum(out=lam_s[0:1, 1:2], in_=lam_t[0:1, 128:192], axis=AX.X), [B_lamt], [B_lams])
                act(lam_s[0:1, 2:4], lam_s[0:1, 0:2], AF.Exp, [B_lams], [B_lams])
                stt(lam_s[0:1, 4:5], lam_s[0:1, 3:4], -0.2, lam_s[0:1, 2:3], ALU.add, ALU.subtract, [B_lams], [B_lams])
                pe([(pmod[0][:, 0:1], onesf[0:1, 0:128], lam_s[0:1, 4:5], True, True)], [B_onesf, B_lams], [B_pmod[0]])
                cp(neglam[:], pmod[0][:, 0:1], [B_pmod[0]], [B_neglam])
                d = dbgout("rows", [128, 4096])
                if d is not None:
                    dma(d, rows[:].rearrange("p a f -> p (a f)"), [B_rows], [YB], sembuf=B_rows)
                d = dbgout("neglam", [128, 1])
                if d is not None:
                    dma(d, neglam[:], [B_neglam], [YB], sembuf=B_neglam)
                em.barrier()
                em.emit()

            with contextlib.ExitStack() as st:
                xt = [sbt(st, "xt%d" % i, [128, 1024]) for i in range(2)]
                xn = [sbt(st, "xn%d" % i, [128, 1024], BF16) for i in range(2)]
                xnT = [sbt(st, "xnT%d" % i, [128, 8, 512], BF16) for i in range(2)]
                ssq = sbt(st, "ssq", [128, 4])
                junk = sbt(st, "junk", [128, 1024], BF16)
                ev = [sbt(st, "ev%d" % i, [128, 512], BF16) for i in range(3)]
                sq = [sbt(st, "sq%d" % i, [128, 512], BF16) for i in range(2)]
                vt = [sbt(st, "vt%d" % i, [128, 4, 130], BF16) for i in range(2)]
                nvt = [sbt(st, "nvt%d" % i, [128, 8, 66], BF16) for i in range(2)]
                mx = sbt(st, "mx", [1, 2])
                ptr = [pst(st, "ptr%d" % i, [128, 8, 128], BF16) for i in range(2)]
                pp = [pst(st, "pp%d" % i, [128, 512]) for i in range(3)]
                pn = pst(st, "pn", [1, 512])
                B_xt = em.bufs(2, "xt"); B_xn = em.bufs(2, "xn"); B_xnT = em.bufs(2, "xnT")
                B_ssq = em.buf("ssq"); B_junk = em.buf("junk"); B_ev = em.bufs(3, "ev"); B_sq = em.bufs(2, "sq")
                B_vt = em.bufs(2, "vt"); B_nvt = em.bufs(2, "nvt"); B_mx = em.buf("mx")
                B_ptr = em.bufs(2, "ptr"); B_pp = em.bufs(3, "pp"); B_pn = em.buf("pn")
                B_scr = em.buf("scr1")
                for i in range(2):
                    mset(vt[i][:, :, 128:129], 1.0, [B_vt[i]])
                    mset(vt[i][:, :, 129:130], 0.0, [B_vt[i]])
                    mset(nvt[i][:, :, 64:65], 1.0, [B_nvt[i]])
                    mset(nvt[i][:, :, 65:66], 0.0, [B_nvt[i]])
                cnt = {"tile": 0, "grp": 0, "pp": 0, "ev": 0, "sq": 0, "vt": 0, "nvt": 0}

                def norm_group(src, g):
                    gi = cnt["grp"] % 2
                    cnt["grp"] += 1
                    for t in range(4):
                        i = cnt["tile"] % 2
                        cnt["tile"] += 1
                        r0 = g * 512 + t * 128
                        dma(xt[i][:], src[r0:r0 + 128, :], [], [B_xt[i]])
                        act(junk[:], xt[i][:], AF.Square, [B_xt[i]], [B_junk, B_ssq], accum_out=ssq[:, 0:1])
                        ts(ssq[:, 1:2], ssq[:, 0:1], 1.0 / 1024, 1e-6, ALU.mult, ALU.add, [B_ssq], [B_ssq])
                        act(ssq[:, 2:3], ssq[:, 1:2], AF.Sqrt, [B_ssq], [B_ssq])
                        dve(lambda E: E.reciprocal(out=ssq[:, 3:4], in_=ssq[:, 2:3]), [B_ssq], [B_ssq])
                        act(xn[i][:], xt[i][:], AF.Copy, [B_xt[i], B_ssq], [B_xn[i]], scale=ssq[:, 3:4])
                        pet([(ptr[i][:, c, :], xn[i][:, c * 128:(c + 1) * 128], identb[:]) for c in range(8)],
                            [B_xn[i], B_identb], [B_ptr[i]])
                        cp(xnT[gi][:, :, t * 128:(t + 1) * 128], ptr[i][:], [B_ptr[i]], [B_xnT[gi]])
                    return gi

                def proj_T(gi, col0, scale, dst, stat_idx):
                    k = cnt["pp"] % 3; cnt["pp"] += 1
                    mms = [(pp[k][:], Wall[:, c, col0:col0 + 128], xnT[gi][:, c, :], c == 0, False) for c in range(8)]
                    mms.append((pp[k][:], biasrow[0:1, col0:col0 + 128], onesb[0:1, 0:512], False, True))
                    pe(mms, B_Wall + [B_xnT[gi], B_biasrow, B_onesb], [B_pp[k]])
                    e = cnt["ev"] % 3; cnt["ev"] += 1
                    act(ev[e][:], pp[k][:], AF.Copy, [B_pp[k]], [B_ev[e]], scale=scale)
                    dma(dst, ev[e][:], [B_ev[e]], [B_scr], sembuf=B_ev[e])
                    s = cnt["sq"] % 2; cnt["sq"] += 1
                    tt(sq[s][:], ev[e][:], ev[e][:], ALU.mult, [B_ev[e]], [B_sq[s]])
                    pe([(pn[:], onesb[:, 0:1], sq[s][:], True, True)], [B_onesb, B_sq[s]], [B_pn])
                    dve(lambda E: E.reduce_max(out=mx[0:1, 0:1], in_=pn[0:1, :], axis=AX.X), [B_pn], [B_mx])
                    tt(stat[0:1, stat_idx:stat_idx + 1], stat[0:1, stat_idx:stat_idx + 1], mx[0:1, 0:1], ALU.max, [B_mx, B_stat], [B_stat])

                def proj_tok(gi, t, col0):
                    k = cnt["pp"] % 3; cnt["pp"] += 1
                    mms = [(pp[k][:], xnT[gi][:, c, t * 128:(t + 1) * 128], Wall[:, c, col0:col0 + 512], c == 0, False) for c in range(8)]
                    mms.append((pp[k][:], onesb[0:1, 0:128], biasrow[0:1, col0:col0 + 512], False, True))
                    pe(mms, B_Wall + [B_xnT[gi], B_biasrow, B_onesb], [B_pp[k]])
                    return k

                def norm_part(src, g, ntl):
                    gi = cnt["grp"] % 2
                    cnt["grp"] += 1
                    for t in range(ntl):
                        i = cnt["tile"] % 2
                        cnt["tile"] += 1
                        r0 = g * 512 + t * 128
                        dma(xt[i][:], src[r0:r0 + 128, :], [], [B_xt[i]])
                        act(junk[:], xt[i][:], AF.Square, [B_xt[i]], [B_junk, B_ssq], accum_out=ssq[:, 0:1])
                        ts(ssq[:, 1:2], ssq[:, 0:1], 1.0 / 1024, 1e-6, ALU.mult, ALU.add, [B_ssq], [B_ssq])
                        act(ssq[:, 2:3], ssq[:, 1:2], AF.Sqrt, [B_ssq], [B_ssq])
                        dve(lambda E: E.reciprocal(out=ssq[:, 3:4], in_=ssq[:, 2:3]), [B_ssq], [B_ssq])
                        act(xn[i][:], xt[i][:], AF.Copy, [B_xt[i], B_ssq], [B_xn[i]], scale=ssq[:, 3:4])
                        pet([(ptr[i][:, c, :], xn[i][:, c * 128:(c + 1) * 128], identb[:]) for c in range(8)],
                            [B_xn[i], B_identb], [B_ptr[i]])
                        cp(xnT[gi][:, :, t * 128:(t + 1) * 128], ptr[i][:], [B_ptr[i]], [B_xnT[gi]])
                    return gi

                def projB(kind, g, gi):
                    if kind == "own":
                        for h in range(4):
                            proj_T(gi, h * 128, 0.125, Qs[h, :, g * 512:(g + 1) * 512], h)
                        for c4 in range(4):
                            proj_T(gi, 1536 + c4 * 128, 0.125, NQs[c4, :, g * 512:(g + 1) * 512], 8 + c4)
                    elif kind == "seq":
                        for h in range(4):
                            proj_T(gi, 512 + h * 128, 1.0, Ks[h, :, g * 512:(g + 1) * 512], 4 + h)
                        for t in range(4):
                            k = proj_tok(gi, t, 1024)
                            v = cnt["vt"] % 2; cnt["vt"] += 1
                            cp(vt[v][:, :, 0:128], pp[k][:].rearrange("p (h e) -> p h e", h=4), [B_pp[k]], [B_vt[v]])
                            dma(Vs[:, :, g * 4 + t, :].rearrange("h p e -> p h e"), vt[v][:], [B_vt[v]], [B_scr], sembuf=B_vt[v])
                    else:
                        ntl = 4 if g < 5 else 2
                        ncol = ntl * 128
                        for c4 in range(4):
                            k = cnt["pp"] % 3; cnt["pp"] += 1
                            col0 = 2048 + c4 * 128
                            mms = [(pp[k][:, 0:ncol], Wall[:, c, col0:col0 + 128], xnT[gi][:, c, 0:ncol], c == 0, False) for c in range(8)]
                            mms.append((pp[k][:, 0:ncol], biasrow[0:1, col0:col0 + 128], onesb[0:1, 0:ncol], False, True))
                            pe(mms, B_Wall + [B_xnT[gi], B_biasrow, B_onesb], [B_pp[k]])
                            e = cnt["ev"] % 3; cnt["ev"] += 1
                            act(ev[e][:, 0:ncol], pp[k][:, 0:ncol], AF.Copy, [B_pp[k]], [B_ev[e]])
                            dma(NKs[c4, :, g * 512:g * 512 + ncol], ev[e][:, 0:ncol], [B_ev[e]], [B_scr], sembuf=B_ev[e])
                            s_ = cnt["sq"] % 2; cnt["sq"] += 1
                            tt(sq[s_][:, 0:ncol], ev[e][:, 0:ncol], ev[e][:, 0:ncol], ALU.mult, [B_ev[e]], [B_sq[s_]])
                            pe([(pn[:, 0:ncol], onesb[:, 0:1], sq[s_][:, 0:ncol], True, True)], [B_onesb, B_sq[s_]], [B_pn])
                            dve(lambda E, ncol=ncol: E.reduce_max(out=mx[0:1, 0:1], in_=pn[0:1, 0:ncol], axis=AX.X), [B_pn], [B_mx])
                            tt(stat[0:1, 12 + c4:13 + c4], stat[0:1, 12 + c4:13 + c4], mx[0:1, 0:1], ALU.max, [B_mx, B_stat], [B_stat])
                        for t in range(ntl):
                            k = proj_tok(gi, t, 2560)
                            v = cnt["nvt"] % 2; cnt["nvt"] += 1
                            cp(nvt[v][:, :, 0:64], pp[k][:].rearrange("p (h e) -> p h e", h=8), [B_pp[k]], [B_nvt[v]])
                            dma(NVs[:, g * 4 + t, :], nvt[v][:].rearrange("p h e -> p (h e)"), [B_nvt[v]], [B_scr], sembuf=B_nvt[v])

                groups = [("own", g, xq, 4) for g in range(4)] + [("seq", g, xb, 4) for g in range(16)] \
                    + [("win", g, xw, 4 if g < 5 else 2) for g in range(6)]
                gis = [None] * len(groups)
                gis[0] = norm_part(groups[0][2], groups[0][1], groups[0][3])
                for n_, (kind, g, src, ntl) in enumerate(groups):
                    if n_ + 1 < len(groups):
                        kn, gn, sn, tn = groups[n_ + 1]
                        gis[n_ + 1] = norm_part(sn, gn, tn)
                    projB(kind, g, gis[n_])
                mm_ = sbt(st, "mm_", [1, 8])
                pM = pst(st, "pM", [128, 8])
                B_mm, B_pM = em.bufs(2, "mm")
                tt(mm_[0:1, 0:4], stat[0:1, 0:4], stat[0:1, 4:8], ALU.mult, [B_stat], [B_mm])
                tt(mm_[0:1, 4:8], stat[0:1, 8:12], stat[0:1, 12:16], ALU.mult, [B_stat, B_mm], [B_mm])
                act(mm_[:], mm_[:], AF.Sqrt, [B_mm], [B_mm])
                ts(mm_[:], mm_[:], -1.05, None, ALU.mult, None, [B_mm], [B_mm])
                pe([(pM[:], onesf[0:1, 0:128], mm_[0:1, :], True, True)], [B_onesf, B_mm], [B_pM])
                cp(negM[:], pM[:], [B_pM], [B_negM])
                d = dbgout("negM", [128, 8])
                if d is not None:
                    dma(d, negM[:], [B_negM], [YB], sembuf=B_negM)
                em.barrier()
                em.emit()
        STOP = os.environ.get("KSTOP", "")
        if STOP != "1":
            with contextlib.ExitStack() as st:
                KA2 = [[sbt(st, "KA%d_%d" % (p_, m), [66, 8192], BF16) for m in range(2)] for p_ in range(2)]
                QA1 = [[sbt(st, "QA%d_%d" % (m, v), [66, 2048], BF16) for v in range(3)] for m in range(2)]
                QA2 = [QA1, QA1]
                Vh2 = [sbt(st, "Vh%d" % p_, [128, 64, 130], BF16) for p_ in range(2)]
                ktab_t = sbt(st, "ktab_s", [128, 4, 2, 64])
                kb = sbt(st, "kb", [128, 2, 64])
                bdg = sbt(st, "bdg_s", [128, 4, 128], BF16)
                PT = [sbt(st, "PT%d" % i, [128, 512], BF16) for i in range(3)]
                rz = sbt(st, "rz", [128, 512])
                O1T = sbt(st, "O1T", [128, 512])
                dT = sbt(st, "dT", [128, 512])
                sqb = sbt(st, "sqb", [128, 512], BF16)
                rs = sbt(st, "rs", [128, 512])
                gs8 = sbt(st, "gs8", [128, 128])
                S = [pst(st, "S%d" % i, [128, 512]) for i in range(3)]
                OT = [pst(st, "OT%d" % i, [128, 512]) for i in range(2)]
                ZB = [pst(st, "ZB%d" % i, [128, 512]) for i in range(2)]
                B_OT = em.bufs(2, "OT"); B_ZB = em.bufs(2, "ZB")
                B_rz, B_O1T, B_dT, B_sqb, B_rs = em.bufs(5, "ep")
                B_KA2 = [em.bufs(2, "KA%d" % p_) for p_ in range(2)]; B_QA1 = em.bufs(2, "QA"); B_QA2 = [B_QA1, B_QA1]; B_Vh2 = em.bufs(2, "Vh"); B_ktab = em.buf("ktab")
                B_kb = em.buf("kb"); B_bdg = em.buf("bdg"); B_PT = em.bufs(3, "PT")
                B_gs8 = em.buf("gs8")
                B_S = em.bufs(3, "S")
                dma(ktab_t[:].rearrange("p a b c -> p (a b c)"), ktab, [], [B_ktab])
                dma(bdg[:].rearrange("p a b -> p (a b)"), bdiag, [], [B_bdg])
                ts(gs8[:], gsub[:], 0.8, None, ALU.mult, None, [B_gsub], [B_gs8])
                it = 0

                def head_loads(hh):
                    p_ = hh % 2
                    for m in range(2):
                        dma(KA2[p_][m][0:64, :], Ks[hh, 64 * m:64 * m + 64, :], [], [B_KA2[p_][m]])
                        dma(KA2[p_][m][64:66, :], kaug, [], [B_KA2[p_][m]])
                    dma(Vh2[p_][:], Vs[hh], [], [B_Vh2[p_]])

                def q_loads(hh):
                    for m in range(2):
                        for v in range(3):
                            dma(QA1[m][v][0:64, :], Qs[hh, 64 * m:64 * m + 64, :], [], [B_QA1[m]])
                            dma(QA1[m][v][64:66, :], qaug[hh, v], [], [B_QA1[m]])

                for h in range(4):
                    q_loads(h)
                    if h == 0:
                        head_loads(0)
                    if h + 1 < 4:
                        head_loads(h + 1)
                    KA, QA, Vh = KA2[h % 2], QA2[h % 2], Vh2[h % 2]
                    B_KA, B_QA, B_Vh = B_KA2[h % 2], B_QA2[h % 2], B_Vh2[h % 2]
                    ts(kb[:], ktab_t[:, h], negM[:, h:h + 1], None, ALU.add, None, [B_ktab, B_negM], [B_kb])
                    seq = [(qc, m, kt) for qc in range(4) for m in range(2) for kt in range(64)]

                    def segs_of(qc, kt):
                        if kt >= 16:
                            return [(0, 4, 0, kb[:, 0, kt:kt + 1])]
                        segs = []
                        for t in range(4):
                            qt = 4 * qc + t
                            if qt > kt:
                                cls = (0, kb[:, 0, kt:kt + 1])
                            elif qt == kt:
                                cls = (2, negM[:, h:h + 1])
                            else:
                                cls = (1, kb[:, 1, kt:kt + 1])
                            if segs and segs[-1][2] == cls[0]:
                                segs[-1] = (segs[-1][0], t + 1, cls[0], cls[1])
                            else:
                                segs.append((t, t + 1, cls[0], cls[1]))
                        return segs

                    def emit_S(n):
                        qc, m, kt = seq[n]
                        sb = (it + n) % 3
                        mms = []
                        for (t0, t1, v, col) in segs_of(qc, kt):
                            c0, c1 = t0 * 128, t1 * 128
                            q0 = qc * 512
                            mms.append((S[sb][:, c0:c1], KA[m][0:66, kt * 128:(kt + 1) * 128], QA[m][v][0:66, q0 + c0:q0 + c1], True, v != 2))
                            if v == 2:
                                mms.append((S[sb][:, c0:c1], identb[:], bdg[:, h, :], False, True))
                        pe(mms, [B_KA[m], B_QA[m], B_identb, B_bdg], [B_S[sb]])

                    def emit_rest(n):
                        qc, m, kt = seq[n]
                        sb = (it + n) % 3
                        pb = (it + n) % 3
                        for (t0, t1, v, col) in segs_of(qc, kt):
                            c0, c1 = t0 * 128, t1 * 128
                            act(PT[pb][:, c0:c1], S[sb][:, c0:c1], AF.Exp, [B_S[sb], B_kb, B_negM], [B_PT[pb]], bias=col, scale=1.0)
                        ob = ((it + n) // 64) % 2
                        pe([(OT[ob][:], Vh[:, kt, 0:128], PT[pb][:], kt == 0, kt == 63),
                            (ZB[ob][:], onesb[:, 0:128], PT[pb][:], kt == 0, kt == 63)],
                           [B_PT[pb], B_Vh, B_onesb], [B_OT[ob], B_ZB[ob]])
                        if kt != 63:
                            return
                        dve(lambda E, ob=ob: E.reciprocal(out=rz[:], in_=ZB[ob][:]), [B_ZB[ob]], [B_rz])
                        if m == 0:
                            tt(O1T[:], OT[ob][:], rz[:], ALU.mult, [B_OT[ob], B_rz], [B_O1T])
                        else:
                            tt(dT[:], OT[ob][:], rz[:], ALU.mult, [B_OT[ob], B_rz], [B_dT])
                            stt(dT[:], dT[:], neglam[:, 0:1], O1T[:], ALU.mult, ALU.add, [B_dT, B_neglam, B_O1T], [B_dT])
                            tt(sqb[:], dT[:], dT[:], ALU.mult, [B_dT], [B_sqb])

                            def tail(ob=ob, qc=qc):
                                pe([(ZB[ob][:], onesb[:, 0:128], sqb[:], True, True)], [B_sqb, B_onesb], [B_ZB[ob]])
                                ts(rs[:], ZB[ob][:], 1.0 / 128, 1e-6, ALU.mult, ALU.add, [B_ZB[ob]], [B_rs])
                                act(rs[:], rs[:], AF.Sqrt, [B_rs], [B_rs])
                                dve(lambda E: E.reciprocal(out=rs[:], in_=rs[:]), [B_rs], [B_rs])
                                tt(dT[:], dT[:], rs[:], ALU.mult, [B_dT, B_rs], [B_dT])
                                ts(oTd[:, h, qc * 512:(qc + 1) * 512], dT[:], g8col[:, 0:1], None, ALU.mult, None,
                                   [B_dT, B_g8col], [B_oTd[h][qc]])
                            pending.append((n + 4, tail))

                    pending = []
                    emit_S(0)
                    emit_S(1)
                    for n in range(len(seq)):
                        if n + 2 < len(seq):
                            emit_S(n + 2)
                        emit_rest(n)
                        while pending and pending[0][0] <= n:
                            pending.pop(0)[1]()
                    while pending:
                        pending.pop(0)[1]()
                    it += len(seq)
                em.barrier()
                em.emit()

            with contextlib.ExitStack() as st:
                NQT = sbt(st, "NQT", [128, 4, 2048], BF16)
                NKT = sbt(st, "NKT", [128, 4, 2816], BF16)
                NV = sbt(st, "NV", [128, 22, 528], BF16)
                nbt = [sbt(st, "nbt%d" % i, [128, 8 * 7 * 128], BF16) for i in range(2)]
                PN = [sbt(st, "PN%d" % i, [128, 896], BF16) for i in range(3)]
                sn = sbt(st, "sn", [128, 2])
                SN = [pst(st, "SN%d" % i, [128, 1024]) for i in range(3)]
                NO = [pst(st, "NO%d" % i, [128, 512]) for i in range(2)]
                B_NQT, B_NKT, B_NV, B_sn = em.bufs(4, "nat")
                B_nbt = em.bufs(2, "nbt"); B_PN = em.bufs(3, "PN"); B_SN = em.bufs(3, "SN"); B_NO = em.bufs(2, "NO")
                dma(NQT[:], NQs.rearrange("c p t -> p c t"), [], [B_NQT])
                dma(NKT[:], NKs.rearrange("c p t -> p c t"), [], [B_NKT])
                dma(NV[:], NVs, [], [B_NV])
                items = [(j, h) for j in range(16) for h in range(8)]

                def nat_S(n):
                    j, h = items[n]
                    nb_ = nbt[j % 2]
                    if h == 0:
                        slot = {0: 1, 1: 2, 14: 3, 15: 4}.get(j, 0)
                        dma(nb_[:], natb[slot], [], [B_nbt[j % 2]])
                    c4, hp = h // 2, (h % 2) * 64
                    sb = n % 3
                    mms = []
                    for o in range(7):
                        oc = slice(o * 128, (o + 1) * 128)
                        mms.append((SN[sb][:, oc], NKT[hp:hp + 64, c4, (j + o) * 128:(j + o + 1) * 128],
                                    NQT[hp:hp + 64, c4, j * 128:(j + 1) * 128], True, False))
                        mms.append((SN[sb][:, oc], identb[:], nb_[:, (h * 7 + o) * 128:(h * 7 + o + 1) * 128], False, True))
                    pe(mms, [B_NKT, B_NQT, B_identb, B_nbt[j % 2]], [B_SN[sb]])

                def nat_rest(n):
                    j, h = items[n]
                    c4 = h // 2
                    sb = n % 3
                    act(PN[sb][:], SN[sb][:, 0:896], AF.Exp, [B_SN[sb], B_negM], [B_PN[sb]], bias=negM[:, 4 + c4:5 + c4], scale=1.0)
                    nb2 = n % 2
                    mms = [(NO[nb2][:, 0:66], PN[sb][:, o * 128:(o + 1) * 128], NV[:, j + o, h * 66:h * 66 + 66], o == 0, o == 6) for o in range(7)]
                    pe(mms, [B_PN[sb], B_NV], [B_NO[nb2]])
                    dve(lambda E, nb2=nb2: E.reciprocal(out=sn[:, 0:1], in_=NO[nb2][:, 64:65]), [B_NO[nb2]], [B_sn])
                    ts(o_all[:, j, h * 64:(h + 1) * 64], NO[nb2][:, 0:64], sn[:, 0:1], None, ALU.mult, None,
                       [B_NO[nb2], B_sn], [B_oall[j]])

                nat_S(0)
                nat_S(1)
                for n in range(len(items)):
                    if n + 2 < len(items):
                        nat_S(n + 2)
                    nat_rest(n)
                d = dbgout("o_all", [128, 16 * 512], BF16)
                if d is not None:
                    dma(d, o_all[:].rearrange("p a f -> p (a f)"), B_oall, [YB], sembuf=B_oall[0])
                em.barrier()
                em.emit()

        if STOP not in ("1", "2"):
            with contextlib.ExitStack() as st:
                wr = sbt(st, "wr", [128, 8, 32])
                br = sbt(st, "br", [1, 32])
                mskb = sbt(st, "mskb", [128, 16, 32], BF16)
                gate4 = sbt(st, "gate4", [128, 16, 4])
                eidx = sbt(st, "eidx", [128, 16, 8])
                dsti = sbt(st, "dsti", [128, 64], I32)
                idxW = sbt(st, "idxW", [128, 4, NB], I32)
                ohTall = sbt(st, "ohTall", [32, NB])
                B_wr, B_br, B_mskb, B_gate4, B_eidx, B_dsti, B_idxW, B_ohTall = em.bufs(8, "p4")
                B_hrow = em.bufs(16, "hrow")
                B_x1s = em.bufs(16, "x1s")
                dma(wr[:], w_router.rearrange("(c p) f -> p c f", p=128), [], [B_wr])
                dma(br[:], b_router, [], [B_br])
                with contextlib.ExitStack() as s4:
                    Wout = sbt(s4, "Wout", [128, 8, 1024], BF16)
                    hrow = sbt(s4, "hrow", [128, 16, 1024], BF16)
                    zt = sbt(s4, "zt", [128, 2, 1024], BF16)
                    B_zt, B_Xz = em.bufs(2, "zx")
                    mset(zt[:], 0.0, [B_zt], eng="pool")
                    for cz in range(NB // 2):
                        dma(Xs[cz * 256:(cz + 1) * 256, :].rearrange("(r p) f -> p r f", p=128), zt[:], [B_zt], [B_Xz], sembuf=B_Xz)
                    B_Wout = em.buf("Wout")
                    dma(Wout[:], w_out.rearrange("(c p) f -> p c f", p=128), [], [B_Wout], q="pool")
                    ltri = sbt(s4, "ltri_s", [128, 128], BF16)
                    iota32 = sbt(s4, "iota32_s", [128, 32])
                    thr16 = sbt(s4, "thr16_s", [128, 32, 16])
                    iota96 = sbt(s4, "iota96_s", [128, NB])
                    pidx = sbt(s4, "pidx_s", [128, 1])
                    B_ltri, B_iota32, B_thr16, B_iota96, B_pidx = em.bufs(5, "cst")
                    dma(ltri[:], ltri_d, [], [B_ltri])
                    dma(iota32[:], iota32_d, [], [B_iota32])
                    dma(thr16[:].rearrange("p a b -> p (a b)"), thr16_d, [], [B_thr16])
                    dma(iota96[:], iota96_d, [], [B_iota96])
                    dma(pidx[:], pidx_d, [], [B_pidx])
                    def dbl(fn):
                        return [fn(0), fn(1)]
                    oT_ = dbl(lambda i: sbt(s4, "oT%d" % i, [128, 8, 128], BF16))
                    xt4_ = dbl(lambda i: sbt(s4, "xt4_%d" % i, [128, 1024]))
                    tmp4_ = dbl(lambda i: sbt(s4, "tmp4_%d" % i, [128, 1024]))
                    x1t_ = dbl(lambda i: sbt(s4, "x1t_%d" % i, [128, 1024]))
                    h2t_ = dbl(lambda i: sbt(s4, "h2t_%d" % i, [128, 1024]))
                    h2Tf_ = dbl(lambda i: sbt(s4, "h2Tf_%d" % i, [128, 8, 128]))
                    junk4_ = dbl(lambda i: sbt(s4, "junk4_%d" % i, [128, 1024], BF16))
                    s4s_ = dbl(lambda i: sbt(s4, "s4s_%d" % i, [128, 16]))
                    lg_ = dbl(lambda i: sbt(s4, "lg_%d" % i, [128, 32]))
                    v8_ = dbl(lambda i: sbt(s4, "v8_%d" % i, [128, 8]))
                    i8_ = dbl(lambda i: sbt(s4, "i8_%d" % i, [128, 8], U32))
                    msk_ = dbl(lambda i: sbt(s4, "msk_%d" % i, [128, 32]))
                    e4_ = dbl(lambda i: sbt(s4, "e4_%d" % i, [128, 4]))
                    pto_ = dbl(lambda i: pst(s4, "pto%d" % i, [128, 8, 128], BF16))
                    pmix = pst(s4, "pmix", [128, 1024])
                    ptf = pst(s4, "ptf", [128, 8, 128])
                    plg = pst(s4, "plg", [128, 32])
                    BB = {n: em.bufs(2, "s4" + n) for n in ["oT", "xt4", "tmp4", "x1t", "h2t", "h2Tf", "junk4", "s4s", "lg", "v8", "i8", "msk", "e4", "pto"]}
                    B_pmix, B_ptf, B_plg = em.bufs(3, "s4p")
                    for j in range(16):
                        q2 = j % 2
                        oT, xt4, tmp4, x1t, h2t, h2Tf, junk4, s4s, lg, v8, i8, msk, e4, pto = (
                            oT_[q2], xt4_[q2], tmp4_[q2], x1t_[q2], h2t_[q2], h2Tf_[q2], junk4_[q2], s4s_[q2], lg_[q2], v8_[q2], i8_[q2], msk_[q2], e4_[q2], pto_[q2])
                        (B_oT, B_xt4, B_tmp4, B_x1t, B_h2t, B_h2Tf, B_junk4, B_s4s, B_lg, B_v8, B_i8, B_msk, B_e4, B_pto) = (
                            BB[n][q2] for n in ["oT", "xt4", "tmp4", "x1t", "h2t", "h2Tf", "junk4", "s4s", "lg", "v8", "i8", "msk", "e4", "pto"])
                        pet([(pto[:, c, :], o_all[:, j, c * 128:(c + 1) * 128], identb[:]) for c in range(4)], [B_oall[j], B_identb], [B_pto])
                        cp(oT[:, 0:4, :], pto[:, 0:4, :], [B_pto], [B_oT])
                        mms = []
                        for n in range(2):
                            for c in range(4):
                                mms.append((pmix[:, n * 512:(n + 1) * 512], oTd[:, c, j * 128:(j + 1) * 128], Wout[:, c, n * 512:(n + 1) * 512], c == 0, False))
                            for c in range(4):
                                mms.append((pmix[:, n * 512:(n + 1) * 512], oT[:, c, :], Wout[:, 4 + c, n * 512:(n + 1) * 512], False, c == 3))
                        pe(mms, [B_oT, B_Wout] + [B_oTd[c][j // 4] for c in range(4)], [B_pmix])
                        dma(xt4[:], xq[j * 128:(j + 1) * 128, :], [], [B_xt4])
                        act(junk4[:], pmix[:], AF.Square, [B_pmix], [B_junk4, B_s4s], accum_out=s4s[:, 0:1])
                        ts(s4s[:, 1:2], s4s[:, 0:1], 1.0 / 1024, 1e-6, ALU.mult, ALU.add, [B_s4s], [B_s4s])
                        act(s4s[:, 2:3], s4s[:, 1:2], AF.Sqrt, [B_s4s], [B_s4s])
                        dve(lambda E, s4s=s4s, v8=v8, lg=lg, i8=i8, e4=e4: E.reciprocal(out=s4s[:, 3:4], in_=s4s[:, 2:3]), [B_s4s], [B_s4s])
                        stt(tmp4[:], pmix[:], s4s[:, 3:4], rows[:, 2, :], ALU.mult, ALU.mult, [B_pmix, B_s4s, B_rows], [B_tmp4])
                        tt(x1t[:], tmp4[:], xt4[:], ALU.add, [B_tmp4, B_xt4], [B_x1t])
                        dma(x1s[j * 128:(j + 1) * 128, :], x1t[:], [B_x1t], [B_x1s[j]], sembuf=B_x1s[j])
                        act(junk4[:], x1t[:], AF.Square, [B_x1t], [B_junk4, B_s4s], accum_out=s4s[:, 4:5])
                        ts(s4s[:, 5:6], s4s[:, 4:5], 1.0 / 1024, 1e-6, ALU.mult, ALU.add, [B_s4s], [B_s4s])
                        act(s4s[:, 6:7], s4s[:, 5:6], AF.Sqrt, [B_s4s], [B_s4s])
                        dve(lambda E, s4s=s4s, v8=v8, lg=lg, i8=i8, e4=e4: E.reciprocal(out=s4s[:, 7:8], in_=s4s[:, 6:7]), [B_s4s], [B_s4s])
                        stt(tmp4[:], x1t[:], s4s[:, 7:8], rows[:, 0, :], ALU.mult, ALU.mult, [B_x1t, B_s4s, B_rows], [B_tmp4])
                        tt(h2t[:], tmp4[:], rows[:, 1, :], ALU.add, [B_tmp4, B_rows], [B_h2t])
                        act(hrow[:, j, :], h2t[:], AF.Copy, [B_h2t], [B_hrow[j]])
                        pet([(ptf[:, c, :], h2t[:, c * 128:(c + 1) * 128], identf[:]) for c in range(8)], [B_h2t, B_identf], [B_ptf])
                        cp(h2Tf[:], ptf[:], [B_ptf], [B_h2Tf])
                        mms = [(plg[:], h2Tf[:, c, :], wr[:, c, :], c == 0, False) for c in range(8)]
                        mms.append((plg[:], onesf[0:1, 0:128], br[0:1, :], False, True))
                        pe(mms, [B_h2Tf, B_wr, B_br, B_onesf], [B_plg])
                        cp(lg[:], plg[:], [B_plg], [B_lg])
                        dve(lambda E, s4s=s4s, v8=v8, lg=lg, i8=i8, e4=e4: E.max(out=v8[:], in_=lg[:]), [B_lg], [B_v8])
                        dve(lambda E, s4s=s4s, v8=v8, lg=lg, i8=i8, e4=e4: E.max_index(out=i8[:], in_max=v8[:], in_values=lg[:]), [B_lg, B_v8], [B_i8])
                        cp(eidx[:, j, :], i8[:], [B_i8], [B_eidx])
                        ts(msk[:], lg[:], v8[:, 3:4], None, ALU.is_ge, None, [B_lg, B_v8], [B_msk])
                        cp(mskb[:, j, :], msk[:], [B_msk], [B_mskb])
                        ts(s4s[:, 8:9], v8[:, 0:1], -1.0, None, ALU.mult, None, [B_v8, B_s4s], [B_s4s])
                        act(e4[:], v8[:, 0:4], AF.Exp, [B_v8, B_s4s], [B_e4], bias=s4s[:, 8:9], scale=1.0)
                        dve(lambda E, s4s=s4s, v8=v8, lg=lg, i8=i8, e4=e4: E.reduce_sum(out=s4s[:, 9:10], in_=e4[:], axis=AX.X), [B_e4, B_s4s], [B_s4s])
                        dve(lambda E, s4s=s4s, v8=v8, lg=lg, i8=i8, e4=e4: E.reciprocal(out=s4s[:, 10:11], in_=s4s[:, 9:10]), [B_s4s], [B_s4s])
                        ts(gate4[:, j, :], e4[:], s4s[:, 10:11], None, ALU.mult, None, [B_e4, B_s4s], [B_gate4])
                    cnt_t = sbt(s4, "cnt_t", [128, 32])
                    cmp1 = sbt(s4, "cmp1", [128, 32, 16])
                    nbk = sbt(s4, "nbk", [128, 32])
                    ones32 = sbt(s4, "ones32", [128, 32])
                    cum = sbt(s4, "cum", [128, 32])
                    pstart = sbt(s4, "pstart", [128, 32])
                    cmp2 = sbt(s4, "cmp2", [128, NB, 32])
                    blk = sbt(s4, "blk", [128, NB])
                    chg = sbt(s4, "chg", [128, NB])
                    idxf = sbt(s4, "idxf", [128, 5, NB])
                    chg2 = sbt(s4, "chg2", [128, NB])
                    B_chg2 = em.buf("chg2")
                    destf = sbt(s4, "destf", [128, 16, 32])
                    ohf = sbt(s4, "ohf", [128, 32])
                    dstf = sbt(s4, "dstf", [128, 64])
                    (B_cnt, B_cmp1, B_nbk, B_ones32, B_cum, B_pstart, B_cmp2, B_blk, B_chg, B_idxf, B_destf, B_ohf, B_dstf) = em.bufs(13, "rt")
                    mms = [(plg[:], onesb[:, 0:128], mskb[:, j, :], j == 0, j == 15) for j in range(16)]
                    pe(mms, [B_onesb, B_mskb], [B_plg])
                    cp(cnt_t[:], plg[:], [B_plg], [B_cnt])
                    tt(cmp1[:], cnt_t[:].unsqueeze(2).to_broadcast([128, 32, 16]), thr16[:], ALU.is_gt, [B_cnt, B_thr16], [B_cmp1])
                    dve(lambda E: E.reduce_sum(out=nbk[:], in_=cmp1[:], axis=AX.X), [B_cmp1], [B_nbk])
                    mset(ones32[:], 1.0, [B_ones32])
                    dve(lambda E: E.tensor_tensor_scan(out=cum[:], data0=ones32[:], data1=nbk[:], initial=0.0, op0=ALU.mult, op1=ALU.add),
                        [B_ones32, B_nbk], [B_cum])
                    tt(pstart[:], cum[:], nbk[:], ALU.subtract, [B_cum, B_nbk], [B_pstart])
                    ts(pstart[:], pstart[:], 128.0, None, ALU.mult, None, [B_pstart], [B_pstart])
                    tt(cmp2[:], cum[:].unsqueeze(1).to_broadcast([128, NB, 32]), iota96[:].unsqueeze(2).to_broadcast([128, NB, 32]), ALU.is_le,
                       [B_cum, B_iota96], [B_cmp2])
                    dve(lambda E: E.reduce_sum(out=blk[:], in_=cmp2[:], axis=AX.X), [B_cmp2], [B_blk])
                    ts(blk[:], blk[:], 31.0, None, ALU.min, None, [B_blk], [B_blk])
                    ts(ohTall[:], blk[0:32, :], pidx[0:32, 0:1], None, ALU.is_equal, None, [B_blk, B_pidx], [B_ohTall])
                    mset(chg[:, 0:1], 1.0, [B_chg])
                    tt(chg[:, 1:NB], blk[:, 1:NB], blk[:, 0:NB - 1], ALU.not_equal, [B_blk, B_chg], [B_chg])
                    mset(chg2[:, 0:2], 1.0, [B_chg2])
                    tt(chg2[:, 2:NB], blk[:, 2:NB], blk[:, 0:NB - 2], ALU.not_equal, [B_blk, B_chg2], [B_chg2])
                    ts(chg[:], chg[:], -float(2 ** 27), float(2 ** 27), ALU.mult, ALU.add, [B_chg], [B_chg])
                    ts(chg2[:], chg2[:], -float(2 ** 27), float(2 ** 27), ALU.mult, ALU.add, [B_chg2], [B_chg2])
                    ts(blk[:], blk[:], 128.0, pidx[:, 0:1], ALU.mult, ALU.add, [B_blk, B_pidx], [B_blk])
                    tt(idxf[:, 4, :], blk[:], chg[:], ALU.add, [B_blk, B_chg], [B_idxf])
                    ts(idxf[:, 0, :], idxf[:, 4, :], 2.0, None, ALU.mult, None, [B_idxf], [B_idxf])
                    ts(idxf[:, 1, :], idxf[:, 4, :], 2.0, 1.0, ALU.mult, ALU.add, [B_idxf], [B_idxf])
                    tt(idxf[:, 4, :], blk[:], chg2[:], ALU.add, [B_blk, B_chg2, B_idxf], [B_idxf])
                    ts(idxf[:, 2, :], idxf[:, 4, :], 2.0, None, ALU.mult, None, [B_idxf], [B_idxf])
                    ts(idxf[:, 3, :], idxf[:, 4, :], 2.0, 1.0, ALU.mult, ALU.add, [B_idxf], [B_idxf])
                    cp(idxW[:], idxf[:, 0:4, :], [B_idxf], [B_idxW])
                    for j in range(16):
                        mms = [(plg[:], onesb[:, 0:128], mskb[:, jj, :], jj == 0, False) for jj in range(j)]
                        mms.append((plg[:], ltri[:], mskb[:, j, :], j == 0, True))
                        pe(mms, [B_onesb, B_ltri, B_mskb], [B_plg])
                        tt(destf[:, j, :], plg[:], pstart[:], ALU.add, [B_plg, B_pstart], [B_destf])
                    B_XO = em.buf("XO")
                    B_dsti = em.bufs(16, "dsti")
                    for j in range(16):
                        for k in range(4):
                            ts(ohf[:], iota32[:], eidx[:, j, k:k + 1], None, ALU.is_equal, None, [B_iota32, B_eidx], [B_ohf])
                            tt(ohf[:], ohf[:], destf[:, j, :], ALU.mult, [B_ohf, B_destf], [B_ohf])
                            dve(lambda E, j=j, k=k: E.reduce_sum(out=dstf[:, 4 * j + k:4 * j + k + 1], in_=ohf[:], axis=AX.X), [B_ohf], [B_dstf])
                        cp(dsti[:, 4 * j:4 * j + 4], dstf[:, 4 * j:4 * j + 4], [B_dstf], [B_dsti[j]])
                        for k in range(4):
                            col = dsti[:, 4 * j + k:4 * j + k + 1]
                            bsc = em.buf("sc")
                            em.dma("pool", lambda E, j=j, col=col: [E.indirect_dma_start(
                                out=Xs, out_offset=bass.IndirectOffsetOnAxis(ap=col, axis=0), in_=hrow[:, j, :], in_offset=None)],
                                reads=[B_hrow[j], B_dsti[j], B_Xz], writes=[bsc], sembuf=B_XO)
                    for nm, srct, bb in (("dsti", dsti, B_dsti[15]), ("idxW", idxW, B_idxW)):
                        d = dbgout(nm, [128, srct.shape[1] * (srct.shape[2] if len(srct.shape) > 2 else 1)], I32)
                        if d is not None:
                            dma(d, srct[:] if len(srct.shape) == 2 else srct[:].rearrange("p a b -> p (a b)"), [bb], [YB], sembuf=bb)
                    d = dbgout("gate4", [128, 64])
                    if d is not None:
                        dma(d, gate4[:].rearrange("p a b -> p (a b)"), [B_gate4], [YB], sembuf=B_gate4)
                    em.barrier()
                    em.emit()
                with contextlib.ExitStack() as s5:
                    wb1s = [sbt(s5, "wb1_%d" % i, [128, 8, 2048], BF16) for i in range(2)]
                    B_wb1 = [em.bufs(2, "wb1p%d" % i) for i in range(2)]
                    wb2 = sbt(s5, "wb2", [128, 9, 1024], BF16)
                    b1all = sbt(s5, "b1all", [32, 2048], BF16)
                    b2all = sbt(s5, "b2all", [32, 1024], BF16)
                    xbk = [sbt(s5, "xbk%d" % i, [128, 1024], BF16) for i in range(2)]
                    xT = [sbt(s5, "xT%d" % i, [128, 8, 128], BF16) for i in range(2)]
                    ohT = [sbt(s5, "ohT%d" % i, [32, 128], BF16) for i in range(2)]
                    glu = sbt(s5, "glu", [128, 1024])
                    lin = sbt(s5, "lin", [128, 1024])
                    sig = sbt(s5, "sig", [128, 1024], BF16)
                    ab = sbt(s5, "ab", [128, 1024], BF16)
                    aT = sbt(s5, "aT", [128, 8, 128], BF16)
                    yb = [sbt(s5, "yb%d" % i, [128, 1024]) for i in range(2)]
                    TX = pst(s5, "TX", [128, 8, 128], BF16)
                    TA = pst(s5, "TA", [128, 8, 128], BF16)
                    H = pst(s5, "H", [128, 2048])
                    Y = pst(s5, "Y", [128, 1024])
                    Ybf = Y.bitcast(BF16)
                    B_wb1a, B_wb1b, B_wb2, B_b1all, B_b2all, B_glu, B_lin, B_sig, B_ab, B_aT, B_TX, B_TA, B_H, B_Y, B_Ys = em.bufs(15, "s5")
                    B_xbk = em.bufs(2, "xbk"); B_obk = em.bufs(2, "obk"); B_xT = em.bufs(2, "xT"); B_ohT = em.bufs(2, "ohT"); B_yb = em.bufs(2, "yb")
                    bcreg = s5.enter_context(nc.gpsimd.register("bcreg"))
                    em.streams["pool"].append(lambda E: E.reg_mov(bcreg, 8191))
                    dma(b1all[:], b1, [], [B_b1all], q="pool")
                    dma(b2all[:], b2, [], [B_b2all], q="pool")
                    ab2 = [ab, sbt(s5, "ab_b", [128, 1024], BF16)]
                    B_ab2 = [B_ab, em.buf("ab_b")]

                    def loads(i):
                        p = i % 2
                        dma(xbk[p][:], Xs[i * 128:(i + 1) * 128, :], [], [B_xbk[p]])

                    def gathers(i):
                        for hh in range(2):
                            em.dma("pool", lambda E, i=i, hh=hh: [E.indirect_dma_start(
                                out=wb1s[i % 2][:, 4 * hh:4 * hh + 4, :].rearrange("p c f -> p (c f)"), out_offset=None, in_=W1p,
                                in_offset=bass.IndirectOffsetOnAxis(ap=idxW[:, 2 + hh, i:i + 1], axis=0), bounds_check=bcreg, oob_is_err=False)],
                                reads=[B_idxW], writes=[B_wb1[i % 2][hh]])

                    def gathers2(i):
                        for hh in range(2):
                            em.dma("pool", lambda E, i=i, hh=hh: [E.indirect_dma_start(
                                out=wb2[:, 4 * hh:4 * hh + 4, :].rearrange("p c f -> p (c f)"), out_offset=None, in_=W2p,
                                in_offset=bass.IndirectOffsetOnAxis(ap=idxW[:, hh, i:i + 1], axis=0), bounds_check=bcreg, oob_is_err=False)],
                                reads=[B_idxW], writes=[B_wb2])

                    def stageA(i):
                        p = i % 2
                        if i + 1 < NB:
                            loads(i + 1)
                        pet([(TX[:, c, :], xbk[p][:, c * 128:(c + 1) * 128], identb[:]) for c in range(8)], [B_xbk[p], B_identb], [B_TX])
                        cp(xT[p][:], TX[:], [B_TX], [B_xT[p]])
                        cp(ohT[p][:], ohTall[:, i:i + 1].to_broadcast([32, 128]), [B_ohTall], [B_ohT[p]])
                        mms = []
                        for n in range(4):
                            for c in range(8):
                                mms.append((H[:, n * 512:(n + 1) * 512], xT[p][:, c, :], wb1s[p][:, c, n * 512:(n + 1) * 512], c == 0, False))
                            mms.append((H[:, n * 512:(n + 1) * 512], ohT[p][:], b1all[:, n * 512:(n + 1) * 512], False, True))
                        pe(mms, [B_xT[p], B_ohT[p], B_wb1[p][0], B_wb1[p][1], B_b1all], [B_H])
                        if i + 2 < NB:
                            gathers(i + 2)
                        ts(glu[:], H[:, 0:2048:2], 7.0, None, ALU.min, None, [B_H], [B_glu])
                        ts(lin[:], H[:, 1:2048:2], -7.0, 7.0, ALU.max, ALU.min, [B_H], [B_lin])
                        act(sig[:], glu[:], AF.Sigmoid, [B_glu], [B_sig], scale=1.702)
                        stt(lin[:], lin[:], 1.0, glu[:], ALU.add, ALU.mult, [B_lin, B_glu], [B_lin])
                        tt(ab2[p][:], lin[:], sig[:], ALU.mult, [B_lin, B_sig], [B_ab2[p]])

                    def stageB(i):
                        p = i % 2
                        pet([(TA[:, c, :], ab2[p][:, c * 128:(c + 1) * 128], identb[:]) for c in range(8)], [B_ab2[p], B_identb], [B_TA])
                        act(aT[:], TA[:], AF.Copy, [B_TA], [B_aT])
                        mms = []
                        for n in range(2):
                            for c in range(8):
                                mms.append((Y[:, n * 512:(n + 1) * 512], aT[:, c, :], wb2[:, c, n * 512:(n + 1) * 512], c == 0, False))
                            mms.append((Y[:, n * 512:(n + 1) * 512], ohT[p][:], b2all[:, n * 512:(n + 1) * 512], False, True))
                        pe(mms, [B_aT, B_ohT[p], B_wb2, B_b2all], [B_Y])
                        if i + 1 < NB:
                            gathers2(i + 1)
                        act(yb[p][:], Y[:], AF.Copy, [B_Y], [B_yb[p]])
                        dma(Ys[i * 128:(i + 1) * 128, :], yb[p][:], [B_yb[p]], [B_Ys], sembuf=B_yb[p])

                    loads(0)
                    gathers(0)
                    gathers(1)
                    gathers2(0)
                    stageA(0)
                    for i in range(NB):
                        if i + 1 < NB:
                            stageA(i + 1)
                        stageB(i)
                    em.barrier()
                    em.emit()
                with contextlib.ExitStack() as s6:
                    x1r = [sbt(s6, "x1r%d" % i, [128, 1024]) for i in range(2)]
                    ot = [sbt(s6, "ot%d" % i, [128, 1024]) for i in range(2)]
                    yk = [sbt(s6, "yk%d" % i, [128, 4, 1024]) for i in range(2)]
                    ft = sbt(s6, "ft", [128, 1024])
                    junk6 = sbt(s6, "junk6", [128, 1024], BF16)
                    s6s = sbt(s6, "s6s", [128, 4])
                    B_x1r = em.bufs(2, "x1r"); B_ot = em.bufs(2, "ot"); B_yk = [em.bufs(4, "yk%d" % i) for i in range(2)]
                    B_ft, B_junk6, B_s6s = em.bufs(3, "s6")
                    for j in range(16):
                        i = j % 2
                        dma(x1r[i][:], x1s[j * 128:(j + 1) * 128, :], [B_x1s[j]], [B_x1r[i]])
                        for k in range(4):
                            em.dma("pool", lambda E, i=i, j=j, k=k: [E.indirect_dma_start(
                                out=yk[i][:, k, :], out_offset=None, in_=Ys,
                                in_offset=bass.IndirectOffsetOnAxis(ap=dsti[:, 4 * j + k:4 * j + k + 1], axis=0))],
                                reads=[B_dsti[j]], writes=[B_yk[i][k]])
                        ts(ft[:], yk[i][:, 0, :], gate4[:, j, 0:1], None, ALU.mult, None, [B_yk[i][0], B_gate4], [B_ft])
                        for k in range(1, 4):
                            stt(ft[:], yk[i][:, k, :], gate4[:, j, k:k + 1], ft[:], ALU.mult, ALU.add, [B_yk[i][k], B_gate4, B_ft], [B_ft])
                        act(junk6[:], ft[:], AF.Square, [B_ft], [B_junk6, B_s6s], accum_out=s6s[:, 0:1])
                        ts(s6s[:, 1:2], s6s[:, 0:1], 1.0 / 1024, 1e-6, ALU.mult, ALU.add, [B_s6s], [B_s6s])
                        act(s6s[:, 2:3], s6s[:, 1:2], AF.Sqrt, [B_s6s], [B_s6s])
                        dve(lambda E: E.reciprocal(out=s6s[:, 3:4], in_=s6s[:, 2:3]), [B_s6s], [B_s6s])
                        stt(ot[i][:], ft[:], s6s[:, 3:4], rows[:, 3, :], ALU.mult, ALU.mult, [B_ft, B_s6s, B_rows], [B_ot[i]])
                        tt(ot[i][:], ot[i][:], x1r[i][:], ALU.add, [B_ot[i], B_x1r[i]], [B_ot[i]])
                        dma(out[j * 128:(j + 1) * 128, :], ot[i][:], [B_ot[i]], [YB], sembuf=B_ot[i])
                    em.barrier()
                    em.emit()
        else:
            mz = sbt(top, "mz", [128, 1024])
            B_mz = em.buf("mz")
            mset(mz[:], 0.0, [B_mz])
            for j in range(16):
                dma(out[j * 128:(j + 1) * 128, :], mz[:], [B_mz], [YB], sembuf=B_mz)
            for nm, src in (("Qs", Qs), ("Ks", Ks), ("NQs", NQs), ("NKs", NKs), ("Vs", Vs), ("NVs", NVs)):
                d = dbgout(nm, src.shape, BF16)
                if d is not None:
                    dma(d, src, [], [YB], sembuf=YB)
            em.barrier()
            em.emit()
    return nc, dbg


SLOPES = [2.0 ** (-2 * (h + 1)) for h in range(4)]
_CACHE = {}


def _bf(a):
    return np.ascontiguousarray(a.astype(ml_dtypes.bfloat16))


def _nat_tables(rpb, qr):
    R0 = 32 * qr
    def table(r0):
        t = np.full((128, 8, 7, 128), -30000.0, np.float32)
        qrow = np.repeat(np.array([r0, r0 + 1]), 64)
        qcol = np.tile(np.arange(64), 2)
        qstart = np.clip(qrow - 4, 0, 120)
        qcs = np.clip(qcol - 8, 0, 48)
        for o in range(7):
            krow = np.repeat(np.array([r0 - 6 + 2 * o, r0 - 5 + 2 * o]), 64)
            kcol = np.tile(np.arange(64), 2)
            valid = ((krow[:, None] >= qstart[None, :]) & (krow[:, None] < qstart[None, :] + 8)
                     & (krow[:, None] >= 0) & (krow[:, None] < 128)
                     & (kcol[:, None] >= qcs[None, :]) & (kcol[:, None] < qcs[None, :] + 16))
            dr = np.clip(krow[:, None] - qrow[None, :] + 7, 0, 14)
            dc = np.clip(kcol[:, None] - qcol[None, :], -15, 15) + 15
            for h in range(8):
                vals = rpb[h][dr, dc]
                t[:, h, o, :] = np.where(valid, vals, -30000.0)
        return t
    slots = [table(R0 + 2 * 6)] + [table(R0 + 2 * j) for j in (0, 1, 14, 15)]
    return _bf(np.stack(slots).reshape(5, 128, 8 * 7 * 128))


def _prep(inputs):
    f32 = np.float32
    x = np.asarray(inputs["x"], f32)
    c = np.asarray(inputs["c"], f32)
    shared = dict(
        w_ada=np.ascontiguousarray(inputs["w_ada"][0], f32), b_ada=np.ascontiguousarray(inputs["b_ada"], f32).reshape(1, 6144),
        g_pre_mix=np.asarray(inputs["g_pre_mix"], f32).reshape(1, 1024), g_post_mix=np.asarray(inputs["g_post_mix"], f32).reshape(1, 1024),
        g_pre_ffn=np.asarray(inputs["g_pre_ffn"], f32).reshape(1, 1024), g_post_ffn=np.asarray(inputs["g_post_ffn"], f32).reshape(1, 1024),
        w_in=np.ascontiguousarray(inputs["w_in"][0], f32), w_out=np.ascontiguousarray(inputs["w_out"][0], f32),
        lamv=np.concatenate([np.asarray(inputs[k], f32).reshape(-1) for k in ("lam_q1", "lam_k1", "lam_q2", "lam_k2")]).reshape(1, 256),
        g_subln=np.asarray(inputs["g_subln"], f32).reshape(1, 128),
        w_router=np.ascontiguousarray(inputs["w_router"][0], f32), b_router=np.asarray(inputs["b_router"], f32).reshape(1, 32),
        identb=_bf(np.eye(128, dtype=f32)), identf=np.eye(128, dtype=f32),
        ltri=_bf(np.triu(np.ones((128, 128), f32), 1)),
        iota32=np.tile(np.arange(32, dtype=f32), (128, 1)),
        thr16=np.tile((128.0 * np.arange(16, dtype=f32))[None, None, :], (128, 32, 1)).reshape(128, 512),
        iota96=np.tile(np.arange(NB, dtype=f32), (128, 1)),
        pidx=np.arange(128, dtype=f32).reshape(128, 1),
    )
    if os.environ.get("KSTOP", "") not in ("1", "2"):
        shared.update(
            W1p=np.ascontiguousarray(np.asarray(inputs["w1"][0], f32).reshape(32, 8, 128, 2048).transpose(0, 2, 1, 3)).reshape(8192, 8192),
            W2p=np.ascontiguousarray(np.asarray(inputs["w2"][0], f32).reshape(32, 8, 128, 1024).transpose(0, 2, 1, 3)).reshape(8192, 4096),
            b1=np.ascontiguousarray(inputs["b1"][0], f32), b2=np.ascontiguousarray(inputs["b2"][0], f32))
    kl = np.arange(128)
    bd = np.stack([-SLOPES[h] * np.abs(kl[:, None] - kl[None, :]) for h in range(4)], axis=1)
    shared["bdiag"] = _bf(bd.reshape(128, 512).astype(f32))
    rpb = np.asarray(inputs["nat_rpb"], f32)[0]
    in_maps = []
    for core in range(8):
        b, qr = core // 4, core % 4
        own = np.arange(qr * 2048, (qr + 1) * 2048)
        rest = np.concatenate([np.arange(0, qr * 2048), np.arange((qr + 1) * 2048, 8192)])
        perm = np.concatenate([own, rest])
        R0 = 32 * qr
        tok0 = (R0 - 6) * 64
        xw = np.zeros((2816, 1024), f32)
        lo, hi = max(tok0, 0), min(tok0 + 2816, 8192)
        xw[lo - tok0:hi - tok0] = x[b, lo:hi]
        ql = np.arange(2048)
        q_lo = (ql % 128).astype(f32)
        qt_abs = (qr * 16 + ql // 128).astype(f32)
        qa = np.zeros((4, 3, 2, 2048), f32)
        for h in range(4):
            qa[h, 0, 0] = -SLOPES[h] * q_lo
            qa[h, 0, 1] = -SLOPES[h] * 128.0 * qt_abs
            qa[h, 1] = -qa[h, 0]
        kabs = perm.astype(f32)
        ktile_abs = perm[::128] // 128
        sig = np.ones(64, f32)
        sig[16:] = np.where(ktile_abs[16:] < qr * 16, 1.0, -1.0)
        ka = np.ones((2, 8192), f32) * np.repeat(sig, 128)[None, :]
        kt_tab = np.zeros((128, 4, 2, 64), f32)
        kpos = kabs.reshape(64, 128).T
        for h in range(4):
            kt_tab[:, h, 0, :] = SLOPES[h] * kpos * sig[None, :]
            kt_tab[:, h, 1, :] = -SLOPES[h] * kpos
        m = dict(shared)
        m.update(
            xb=np.ascontiguousarray(x[b][perm]), xq=np.ascontiguousarray(x[b, own]), xw=xw,
            cT=np.ascontiguousarray(c[b].reshape(8, 128).T),
            natb=_nat_tables(rpb, qr), qaug=_bf(qa), kaug=_bf(ka), ktab=kt_tab.reshape(128, 512),
        )
        in_maps.append(m)
    return in_maps


def kernel(**inputs):
    debug = tuple(os.environ.get("KDEBUG", "").split(",")) if os.environ.get("KDEBUG") else ()
    key = (debug, os.environ.get("KSTOP", ""))
    if key not in _CACHE:
        _CACHE[key] = build_program(debug)
    nc, dbg = _CACHE[key]
    in_maps = _prep(inputs)
    res = run_bass_kernel_spmd(nc, in_maps, core_ids=list(range(8)))
    outs = [np.asarray(r["out"], np.float32) for r in res.results]
    full = np.stack([np.concatenate(outs[0:4], axis=0), np.concatenate(outs[4:8], axis=0)], axis=0)
    if debug:
        kernel.last_debug = [{k: np.asarray(r["dbg_" + k]) for k in dbg} for r in res.results]
    return full
```
